# Optimizing a Trainium2 kernel written in Bass

```python
import math
import jax, jax.numpy as jnp
from jax import lax
import numpy as np

D_MODEL = 1024
BATCH = 16
SEQ = 2048
DEPTH = 1

EPS = 1e-6
NEG_INF = -1e30

A_HEADS = 8
A_HEAD_DIM = 64
A_WIDTH = A_HEADS * A_HEAD_DIM
DILATED_PATTERNS = ((128, 1), (512, 4), (2048, 16))
A_BLOCK = 64

REL_BUCKETS = 32
REL_MAX_DISTANCE = 1024

B_HEADS = 8
B_Q_LORA = 256
B_KV_LORA = 128
B_QK_NOPE = 64
B_QK_ROPE = 32
B_V_DIM = 64
B_WIDTH = B_HEADS * B_V_DIM
ROPE_THETA = 10000.0
B_Q_BLOCK = 128

MIX_WIDTH = A_WIDTH + B_WIDTH
IN_PROJ_WIDTH = 3 * A_WIDTH + B_Q_LORA + B_KV_LORA + B_QK_ROPE

N_GROUPS = 4
EXPERTS_PER_GROUP = 8
N_EXPERTS = N_GROUPS * EXPERTS_PER_GROUP
TOP_K_IN_GROUP = 2
EXPERT_FF = 256
MOE_BLOCK = 128

kernel_name = "hymba_dilated_mla_hmoe_encoder"


def rmsnorm(x, g):
    xf = x.astype(jnp.float32)
    y = xf * lax.rsqrt(jnp.mean(xf * xf, axis=-1, keepdims=True) + EPS)
    return (y * g.astype(jnp.float32)).astype(x.dtype)


def t5_relative_bucket(rel):
    half = REL_BUCKETS // 2
    max_exact = half // 2
    n = np.abs(rel)
    large = max_exact + (np.log(np.maximum(n, 1) / max_exact)
                         / math.log(REL_MAX_DISTANCE / max_exact) * (half - max_exact)).astype(np.int32)
    large = np.minimum(large, half - 1)
    return (np.where(rel > 0, half, 0) + np.where(n < max_exact, n, large)).astype(np.int32)


def dilated_window_attention(q, k, v, rel_bias, dilation, half_steps):
    b, h, s, dh = q.shape
    L = s // dilation
    qb = math.gcd(L, A_BLOCK)
    nb = L // qb
    span = qb + 2 * half_steps

    def to_residue(t):
        return t.reshape(b, h, L, dilation, dh).transpose(0, 1, 3, 2, 4)

    qr, kr, vr = to_residue(q), to_residue(k), to_residue(v)
    pad = ((0, 0), (0, 0), (0, 0), (half_steps, half_steps), (0, 0))
    kp, vp = jnp.pad(kr, pad), jnp.pad(vr, pad)
    key_idx = np.arange(nb)[:, None] * qb + np.arange(span)[None, :]
    kb = jnp.take(kp, key_idx, axis=3)
    vb = jnp.take(vp, key_idx, axis=3)
    qblk = qr.reshape(b, h, dilation, nb, qb, dh)
    scores = jnp.einsum('bhrnqd,bhrnkd->bhrnqk', qblk, kb,
                        preferred_element_type=jnp.float32) * (dh ** -0.5)
    rel_steps = np.arange(span)[None, :] - half_steps - np.arange(qb)[:, None]
    buckets = t5_relative_bucket(rel_steps * dilation)
    bias = rel_bias[buckets].astype(jnp.float32).transpose(2, 0, 1)
    key_pos = key_idx - half_steps
    valid = ((np.abs(rel_steps) <= half_steps)[None]
             & (key_pos >= 0)[:, None, :] & (key_pos < L)[:, None, :])
    scores = jnp.where(valid[None, None, None], scores + bias[None, :, None, None], NEG_INF)
    m = jnp.max(scores, axis=-1, keepdims=True)
    p = jnp.exp(scores - m)
    l = jnp.sum(p, axis=-1, keepdims=True)
    o = jnp.einsum('bhrnqk,bhrnkd->bhrnqd', p, vb, preferred_element_type=jnp.float32) / l
    lse = (m + jnp.log(l))[..., 0]
    o = o.reshape(b, h, dilation, L, dh).transpose(0, 1, 3, 2, 4).reshape(b, h, s, dh)
    lse = lse.reshape(b, h, dilation, L).transpose(0, 1, 3, 2).reshape(b, h, s)
    return o, lse


def mixer_dilated(q, k, v, rel_bias):
    outs, lses = [], []
    for window, dilation in DILATED_PATTERNS:
        o, lse = dilated_window_attention(q, k, v, rel_bias, dilation, window // (2 * dilation))
        outs.append(o)
        lses.append(lse)
    w = jax.nn.softmax(jnp.stack(lses, 0), axis=0)
    return jnp.einsum('pbhs,pbhsd->bhsd', w, jnp.stack(outs, 0))


def apply_rope(x):
    s, d = x.shape[1], x.shape[-1]
    half = d // 2
    inv_freq = ROPE_THETA ** (-(jnp.arange(half, dtype=jnp.float32) / half))
    ang = jnp.arange(s, dtype=jnp.float32)[:, None] * inv_freq[None, :]
    cos, sin = jnp.cos(ang)[None, :, None, :], jnp.sin(ang)[None, :, None, :]
    x1, x2 = x[..., :half].astype(jnp.float32), x[..., half:].astype(jnp.float32)
    return jnp.concatenate([x1 * cos - x2 * sin, x1 * sin + x2 * cos], axis=-1).astype(x.dtype)


def mixer_mla(c_q, c_kv, k_rope, g_q_latent, w_q_up, g_kv_latent, w_kv_up):
    b, s, _ = c_q.shape
    q = (rmsnorm(c_q, g_q_latent) @ w_q_up).reshape(b, s, B_HEADS, B_QK_NOPE + B_QK_ROPE)
    q_nope, q_rope = q[..., :B_QK_NOPE], apply_rope(q[..., B_QK_NOPE:])
    kv = (rmsnorm(c_kv, g_kv_latent) @ w_kv_up).reshape(b, s, B_HEADS, B_QK_NOPE + B_V_DIM)
    k_nope, v = kv[..., :B_QK_NOPE], kv[..., B_QK_NOPE:].astype(jnp.float32)
    k_r = apply_rope(k_rope[:, :, None, :])[:, :, 0]
    scale = (B_QK_NOPE + B_QK_ROPE) ** -0.5
    nb = s // B_Q_BLOCK

    def block(args):
        qn, qr = args
        sc = (jnp.einsum('bqhd,bkhd->bhqk', qn, k_nope, preferred_element_type=jnp.float32)
              + jnp.einsum('bqhd,bkd->bhqk', qr, k_r, preferred_element_type=jnp.float32)) * scale
        p = jax.nn.softmax(sc, axis=-1)
        return jnp.einsum('bhqk,bkhd->bqhd', p, v)

    qn_b = q_nope.reshape(b, nb, B_Q_BLOCK, B_HEADS, B_QK_NOPE).transpose(1, 0, 2, 3, 4)
    qr_b = q_rope.reshape(b, nb, B_Q_BLOCK, B_HEADS, B_QK_ROPE).transpose(1, 0, 2, 3, 4)
    o = lax.map(block, (qn_b, qr_b))
    return o.transpose(1, 0, 2, 3, 4).reshape(b, s, B_WIDTH)


def hierarchical_moe(t, w_router_group, b_router_group, w_router_expert, b_router_expert,
                     w_gate, w_up, w_down):
    n_tok, d = t.shape
    group_logits = jnp.dot(t, w_router_group, preferred_element_type=jnp.float32) + b_router_group.astype(jnp.float32)
    group_probs = jax.nn.softmax(group_logits, axis=-1)
    group_idx = jnp.argmax(group_probs, axis=-1)
    group_gate = jnp.take_along_axis(group_probs, group_idx[:, None], axis=-1)
    expert_logits = (jnp.dot(t, w_router_expert, preferred_element_type=jnp.float32)
                     + b_router_expert.astype(jnp.float32)).reshape(n_tok, N_GROUPS, EXPERTS_PER_GROUP)
    in_group = jnp.take_along_axis(expert_logits, group_idx[:, None, None], axis=1)[:, 0]
    top_p, top_i = lax.top_k(jax.nn.softmax(in_group, axis=-1), TOP_K_IN_GROUP)
    gates = group_gate * top_p / jnp.sum(top_p, axis=-1, keepdims=True)
    expert_ids = group_idx[:, None] * EXPERTS_PER_GROUP + top_i

    n_assign = n_tok * TOP_K_IN_GROUP
    flat_expert = expert_ids.reshape(-1)
    flat_token = jnp.repeat(jnp.arange(n_tok), TOP_K_IN_GROUP)
    flat_gate = gates.reshape(-1)
    order = jnp.argsort(flat_expert)
    s_expert, s_token, s_gate = flat_expert[order], flat_token[order], flat_gate[order]
    counts = jnp.bincount(flat_expert, length=N_EXPERTS)
    starts = jnp.cumsum(counts) - counts
    padded = (counts + MOE_BLOCK - 1) // MOE_BLOCK * MOE_BLOCK
    padded_ends = jnp.cumsum(padded)
    padded_starts = padded_ends - padded
    dest = padded_starts[s_expert] + jnp.arange(n_assign) - starts[s_expert]
    n_blocks = -(-n_assign // MOE_BLOCK) + N_EXPERTS
    buf = jnp.zeros((n_blocks * MOE_BLOCK, d), t.dtype).at[dest].set(t[s_token])
    block_expert = jnp.minimum(
        jnp.searchsorted(padded_ends, jnp.arange(n_blocks) * MOE_BLOCK, side='right'), N_EXPERTS - 1)

    def expert_block(args):
        xb, e = args
        hb = jax.nn.silu(xb @ w_gate[e]) * (xb @ w_up[e])
        return hb @ w_down[e]

    out = lax.map(expert_block, (buf.reshape(n_blocks, MOE_BLOCK, d), block_expert))
    contrib = out.reshape(n_blocks * MOE_BLOCK, d)[dest].astype(jnp.float32) * s_gate[:, None]
    return jax.ops.segment_sum(contrib, s_token, num_segments=n_tok)


def setup_inputs(seed: int = 0) -> dict:
    key = jax.random.key(seed)
    ks = jax.random.split(key, 24)
    f32 = jnp.float32
    nrm = lambda k, shape, fan_in: jax.random.normal(k, shape, f32) * (fan_in ** -0.5)
    gain = lambda k, shape: 1.0 + 0.02 * jax.random.normal(k, shape, f32)
    return {
        "x": jax.random.normal(ks[0], (BATCH, SEQ, D_MODEL), f32),
        "g_attn_norm": gain(ks[1], (DEPTH, D_MODEL)),
        "w_in": nrm(ks[2], (DEPTH, D_MODEL, IN_PROJ_WIDTH), D_MODEL),
        "rel_bias": 0.5 * jax.random.normal(ks[3], (REL_BUCKETS, A_HEADS), f32),
        "g_q_latent": gain(ks[4], (DEPTH, B_Q_LORA)),
        "w_q_up": nrm(ks[5], (DEPTH, B_Q_LORA, B_HEADS * (B_QK_NOPE + B_QK_ROPE)), B_Q_LORA),
        "g_kv_latent": gain(ks[6], (DEPTH, B_KV_LORA)),
        "w_kv_up": nrm(ks[7], (DEPTH, B_KV_LORA, B_HEADS * (B_QK_NOPE + B_V_DIM)), B_KV_LORA),
        "g_out_a": gain(ks[8], (DEPTH, A_WIDTH)),
        "g_out_b": gain(ks[9], (DEPTH, B_WIDTH)),
        "w_out": nrm(ks[10], (DEPTH, MIX_WIDTH, D_MODEL), MIX_WIDTH),
        "g_ffn_norm": gain(ks[11], (DEPTH, D_MODEL)),
        "w_router_group": nrm(ks[12], (DEPTH, D_MODEL, N_GROUPS), D_MODEL),
        "b_router_group": 0.01 * jax.random.normal(ks[13], (DEPTH, N_GROUPS), f32),
        "w_router_expert": nrm(ks[14], (DEPTH, D_MODEL, N_EXPERTS), D_MODEL),
        "b_router_expert": 0.01 * jax.random.normal(ks[15], (DEPTH, N_EXPERTS), f32),
        "w_gate": nrm(ks[16], (DEPTH, N_EXPERTS, D_MODEL, EXPERT_FF), D_MODEL),
        "w_up": nrm(ks[17], (DEPTH, N_EXPERTS, D_MODEL, EXPERT_FF), D_MODEL),
        "w_down": nrm(ks[18], (DEPTH, N_EXPERTS, EXPERT_FF, D_MODEL), EXPERT_FF),
        "g_final": gain(ks[19], (D_MODEL,)),
    }


def reference(x, g_attn_norm, w_in, rel_bias, g_q_latent, w_q_up, g_kv_latent, w_kv_up,
              g_out_a, g_out_b, w_out, g_ffn_norm, w_router_group, b_router_group,
              w_router_expert, b_router_expert, w_gate, w_up, w_down, g_final):
    b, s, d = x.shape
    splits = list(np.cumsum([A_WIDTH, A_WIDTH, A_WIDTH, B_Q_LORA, B_KV_LORA]))
    for l in range(DEPTH):
        h = rmsnorm(x, g_attn_norm[l])
        proj = h @ w_in[l]
        q_a, k_a, v_a, c_q, c_kv, k_rope = jnp.split(proj, splits, axis=-1)
        heads = lambda t: t.reshape(b, s, A_HEADS, A_HEAD_DIM).transpose(0, 2, 1, 3)
        out_a = mixer_dilated(heads(q_a), heads(k_a), heads(v_a), rel_bias)
        out_a = out_a.transpose(0, 2, 1, 3).reshape(b, s, A_WIDTH)
        out_b = mixer_mla(c_q, c_kv, k_rope, g_q_latent[l], w_q_up[l], g_kv_latent[l], w_kv_up[l])
        mixed = jnp.concatenate([rmsnorm(out_a, g_out_a[l]), rmsnorm(out_b, g_out_b[l])], axis=-1)
        x = x + (mixed.astype(x.dtype) @ w_out[l])
        h2 = rmsnorm(x, g_ffn_norm[l]).reshape(b * s, d)
        y = hierarchical_moe(h2, w_router_group[l], b_router_group[l], w_router_expert[l],
                             b_router_expert[l], w_gate[l], w_up[l], w_down[l])
        x = x + y.reshape(b, s, d).astype(x.dtype)
    return rmsnorm(x, g_final)
```

```python
import contextlib
import math
import numpy as np
import concourse.bass as bass
import concourse.mybir as mybir
from concourse.bass_utils import run_bass_kernel_spmd

F32 = mybir.dt.float32
BF16 = mybir.dt.bfloat16
I32 = mybir.dt.int32
AF = mybir.ActivationFunctionType
ALU = mybir.AluOpType
AX = mybir.AxisListType

N_CORES = 8
SEQ = 2048
D = 1024
NSEQ = 2
NTOK = NSEQ * SEQ
NT = SEQ // 128
EPS = 1e-6
NEG = -30000.0
PATTERNS = (16, 4, 1)
N_EXP = 32
CAPB = 4
NROWS = N_EXP * CAPB * 128
K2C = 99


class Sync:
    COMPUTE = ("pe", "act", "dve", "pool")

    def __init__(self, nc, stack):
        self.nc = nc
        self.stack = stack
        self.ops = {e: [] for e in ("pe", "act", "dve", "pool", "sp")}
        self.sem = {}
        for e in self.COMPUTE:
            self.sem[e] = stack.enter_context(nc.semaphore("s_" + e))
        self.cnt = {e: 0 for e in self.COMPUTE}
        self.seen = {e: {} for e in self.ops}
        self.keys = {}
        self.pending = {e: ([], []) for e in self.COMPUTE}
        self.slots = {}

    def _key(self, k):
        st = self.keys.get(k)
        if st is None:
            st = {"w": None, "r": []}
            self.keys[k] = st
        return st

    def _need(self, eng, reads, writes, is_dma=False):
        need = {}

        def add(p):
            if p is None:
                return
            s, v, src = p
            if eng == "pe" and src == "pe":
                return
            if need.get(id(s), (None, -1))[1] < v:
                need[id(s)] = (s, v)

        for k in reads:
            st = self._key(k)
            add(st["w"])
            if isinstance(k, tuple) and k and k[0] == "ps":
                for p in st["r"]:
                    if p[2] != eng:
                        add(p)
        for k in writes:
            st = self._key(k)
            if st["w"] is not None and (is_dma or st["w"][2] != eng):
                add(st["w"])
            for p in st["r"]:
                if is_dma or p[2] != eng:
                    add(p)
        out = []
        seen = self.seen[eng]
        for sid, (s, v) in need.items():
            if seen.get(sid, -1) >= v:
                continue
            seen[sid] = v
            out.append((s, v))
        return out

    def _commit(self, reads, writes, prod):
        for k in writes:
            st = self._key(k)
            st["w"] = prod
            st["r"] = []
        for k in reads:
            if k in writes:
                continue
            st = self._key(k)
            st["r"] = [p for p in st["r"] if p[0] is not prod[0]] + [prod]

    def op(self, eng, fn, reads=(), writes=(), inc=True):
        reads, writes = list(reads), list(writes)
        waits = self._need(eng, reads, writes)
        if inc:
            self.cnt[eng] += 1
            prod = (self.sem[eng], self.cnt[eng], eng)
            pr, pw = self.pending[eng]
            self._commit(reads + pr, writes + pw, prod)
            self.pending[eng] = ([], [])
            self.ops[eng].append((waits, fn, (self.sem[eng], 1)))
        else:
            pr, pw = self.pending[eng]
            pr.extend(reads)
            pw.extend(writes)
            self.ops[eng].append((waits, fn, None))

    def dma(self, q, slot, fn, reads=(), writes=()):
        reads, writes = list(reads), list(writes)
        if slot not in self.slots:
            s = self.stack.enter_context(self.nc.semaphore("d_" + slot))
            self.slots[slot] = [s, 0]
        sl = self.slots[slot]
        waits = self._need(q, reads, writes, is_dma=True)
        sl[1] += 16
        self._commit(reads, writes, (sl[0], sl[1], "dma"))
        self.ops[q].append((waits, fn, (sl[0], 16)))

    def barrier(self):
        targets = [(self.sem[e], self.cnt[e]) for e in self.COMPUTE if self.cnt[e] > 0]
        targets += [(s, v) for (s, v) in self.slots.values() if v > 0]
        for e in self.ops:
            waits = []
            seen = self.seen[e]
            for s, v in targets:
                if seen.get(id(s), -1) >= v:
                    continue
                seen[id(s)] = v
                waits.append((s, v))
            if waits:
                self.ops[e].append((waits, None, None))
        self.keys = {}
        self.pending = {e: ([], []) for e in self.COMPUTE}

    def emit(self):
        nc = self.nc
        ops = self.ops

        def run(e, lst):
            for waits, fn, inc in lst:
                for s, v in waits:
                    e.wait_ge(s, v)
                if fn is not None:
                    ins = fn(e)
                    if inc is not None:
                        ins.then_inc(inc[0], inc[1])

        with nc.Block() as block:
            @block.sync
            def _(e):
                run(e, ops["sp"])

            @block.tensor
            def _(e):
                run(e, ops["pe"])

            @block.scalar
            def _(e):
                run(e, ops["act"])

            @block.vector
            def _(e):
                run(e, ops["dve"])

            @block.gpsimd
            def _(e):
                run(e, ops["pool"])


def _t5_bucket(rel):
    half = 16
    max_exact = 8
    n = np.abs(rel)
    large = max_exact + (np.log(np.maximum(n, 1) / max_exact)
                         / math.log(1024 / max_exact) * (half - max_exact)).astype(np.int32)
    large = np.minimum(large, half - 1)
    return (np.where(rel > 0, half, 0) + np.where(n < max_exact, n, large)).astype(np.int32)


def _consts():
    half = 16
    inv_freq = (np.float32(10000.0) ** (-(np.arange(half, dtype=np.float32) / np.float32(half)))).astype(np.float32)
    ang = (np.arange(SEQ, dtype=np.float32)[:, None] * inv_freq[None, :]).astype(np.float32)
    cos, sin = np.cos(ang).astype(np.float32), np.sin(ang).astype(np.float32)
    rope_cs = np.concatenate([cos, cos, sin], axis=1).astype(np.float32)
    oh = np.zeros((33, 3, 512), np.float32)
    for di, d in enumerate(PATTERNS):
        m = np.arange(512)
        delta = m - 255
        valid = np.abs(delta) <= 64
        b = _t5_bucket(delta * d)
        for mm in range(512):
            if valid[mm]:
                oh[b[mm], di, mm] = 1.0
            else:
                oh[32, di, mm] = 1.0
    return rope_cs, oh.reshape(33, 1536)


def build_nc(dbg=False, stop_after=None):
    nc = bass.Bass("TRN2", target_bir_lowering=False)

    def din(name, shape, dt=F32):
        return nc.dram_tensor(name, list(shape), dt, kind="ExternalInput").ap()

    def dscr(name, shape, dt, expose=False):
        kind = "ExternalOutput" if (dbg and expose) else "Internal"
        return nc.dram_tensor(name, list(shape), dt, kind=kind).ap()

    x = din("x", [NTOK, D])
    w_in = din("w_in", [D, 1952])
    rel_bias = din("rel_bias", [32, 8])
    w_q_up = din("w_q_up", [256, 768])
    w_kv_up = din("w_kv_up", [128, 1024])
    w_out = din("w_out", [D, D])
    w_rg = din("w_rg", [D, 4])
    w_re = din("w_re", [D, 32])
    w_gate = din("w_gate", [N_EXP, D, 256])
    w_up = din("w_up", [N_EXP, D, 256])
    w_down = din("w_down", [N_EXP, 256, D])
    g_attn = din("g_attn", [1, D])
    g_q = din("g_q", [1, 256])
    g_kv = din("g_kv", [1, 128])
    g_out = din("g_out", [1, D])
    g_ffn = din("g_ffn", [1, D])
    g_fin = din("g_fin", [1, D])
    b_r = din("b_r", [1, 36])
    rope_cs = din("rope_cs", [SEQ, 48])
    oh_bias = din("oh_bias", [33, 1536])
    out = nc.dram_tensor("out", [NTOK, D], F32, kind="ExternalOutput").ap()

    va_d = dscr("va_d", [NTOK, 520], BF16)
    oa_d = dscr("oa_d", [2, NTOK, 520], F32)
    mixb_d = dscr("mixb_d", [NTOK, 512], BF16, expose=True)
    mixa_d = dscr("mixa_d", [NTOK, 512], BF16, expose=True) if dbg else None
    x1_d = dscr("x1_d", [NTOK, D], F32, expose=True)
    fvec_d = dscr("fvec_d", [8, 1536], F32)
    xs_d = dscr("xs_d", [NROWS + 128, D], BF16)
    ys_d = dscr("ys_d", [NROWS + 128, D], BF16)

    def bcast_rows(ap, n):
        return bass.AP(tensor=ap.tensor, offset=0, ap=[[0, 128], [1, n]])

    with contextlib.ExitStack() as st:
        S = Sync(nc, st)

        uniq = [0]

        def sbuf(stk, name, shape, dt):
            uniq[0] += 1
            return stk.enter_context(nc.sbuf_tensor("%s_%d" % (name, uniq[0]), list(shape), dt))

        psbig = [st.enter_context(nc.psum_tensor("psb%d" % i, [128, 1024], F32)) for i in range(4)]
        bank = [psbig[i // 2][:, (i % 2) * 512:(i % 2 + 1) * 512] for i in range(8)]

        def PS(i):
            return ("ps", i)

        def bank_bf(i):
            return bank[i][:].bitcast(BF16)

        ident_f = sbuf(st, "ident_f", [128, 128], F32)
        ident_b = sbuf(st, "ident_b", [128, 128], BF16)
        jrev_f = sbuf(st, "jrev_f", [128, 128], F32)
        jrev_b = sbuf(st, "jrev_b", [128, 128], BF16)
        ones_f = sbuf(st, "ones_f", [128, 128], F32)
        ltri_f = sbuf(st, "ltri_f", [128, 128], F32)
        eps_t = sbuf(st, "eps_t", [128, 1], F32)
        rope_t = sbuf(st, "rope_t", [128, NT, 48], F32)
        wq_b = sbuf(st, "wq_b", [128, 2, 768], BF16)
        wkv_b = sbuf(st, "wkv_b", [128, 1024], BF16)
        wr_f = sbuf(st, "wr_f", [128, 8, 36], F32)
        g_q_t = sbuf(st, "g_q_t", [128, 256], F32)
        g_kv_t = sbuf(st, "g_kv_t", [128, 128], F32)
        g_out_t = sbuf(st, "g_out_t", [128, D], F32)

        S.op("pool", lambda e: e.memset(ident_f[:], 1.0), writes=["ident_f"])
        S.op("pool", lambda e: e.affine_select(out=ident_f[:], in_=ident_f[:], pattern=[[-1, 128]],
                                               compare_op=ALU.is_equal, fill=0.0, base=0,
                                               channel_multiplier=1), reads=["ident_f"], writes=["ident_f"])
        S.op("pool", lambda e: e.memset(jrev_f[:], 1.0), writes=["jrev_f"])
        S.op("pool", lambda e: e.affine_select(out=jrev_f[:], in_=jrev_f[:], pattern=[[1, 128]],
                                               compare_op=ALU.is_equal, fill=0.0, base=-127,
                                               channel_multiplier=1), reads=["jrev_f"], writes=["jrev_f"])
        S.op("pool", lambda e: e.memset(ones_f[:], 1.0), writes=["ones_f"])
        S.op("pool", lambda e: e.memset(ltri_f[:], 1.0), writes=["ltri_f"])
        S.op("pool", lambda e: e.affine_select(out=ltri_f[:], in_=ltri_f[:], pattern=[[1, 128]],
                                               compare_op=ALU.is_ge, fill=0.0, base=-1,
                                               channel_multiplier=-1), reads=["ltri_f"], writes=["ltri_f"])
        S.op("pool", lambda e: e.memset(eps_t[:], EPS), writes=["eps_t"])
        S.op("dve", lambda e: e.tensor_copy(out=ident_b[:], in_=ident_f[:]), reads=["ident_f"], writes=["ident_b"])
        S.op("dve", lambda e: e.tensor_copy(out=jrev_b[:], in_=jrev_f[:]), reads=["jrev_f"], writes=["jrev_b"])

        S.dma("sp", "c_rope", lambda e: e.dma_start(out=rope_t[:], in_=rope_cs.rearrange("(t p) c -> p t c", p=128)),
              writes=["rope_t"])
        S.dma("pool", "c_wq", lambda e: e.dma_start(out=wq_b[:], in_=w_q_up.rearrange("(c p) n -> p c n", p=128)),
              writes=["wq_b"])
        S.dma("pool", "c_wkv", lambda e: e.dma_start(out=wkv_b[:], in_=w_kv_up), writes=["wkv_b"])
        with nc.allow_non_contiguous_dma(reason="tiny router weights"):
            S.dma("sp", "c_wr", lambda e: e.dma_start(out=wr_f[:, :, 0:4], in_=w_rg.rearrange("(k p) n -> p k n", p=128)),
                  writes=["wr_f"])
            S.dma("sp", "c_wr", lambda e: e.dma_start(out=wr_f[:, :, 4:36], in_=w_re.rearrange("(k p) n -> p k n", p=128)),
                  writes=["wr_f"])
        S.dma("sp", "c_gq", lambda e: e.dma_start(out=g_q_t[:], in_=bcast_rows(g_q, 256)), writes=["g_q_t"])
        S.dma("sp", "c_gkv", lambda e: e.dma_start(out=g_kv_t[:], in_=bcast_rows(g_kv, 128)), writes=["g_kv_t"])
        S.dma("sp", "c_gout", lambda e: e.dma_start(out=g_out_t[:], in_=bcast_rows(g_out, D)), writes=["g_out_t"])

        with contextlib.ExitStack() as s0:
            relb = sbuf(s0, "relb", [33, 8], F32)
            oh_t = sbuf(s0, "oh_t", [33, 1536], F32)
            fvec_sb = sbuf(s0, "fvec_sb", [8, 1536], F32)
            S.op("pool", lambda e: e.memset(relb[32:33, :], NEG), writes=["relb32"])
            S.dma("sp", "c_relb", lambda e: e.dma_start(out=relb[0:32, :], in_=rel_bias), writes=["relb"])
            S.dma("sp", "c_oh", lambda e: e.dma_start(out=oh_t[:], in_=oh_bias), writes=["oh_t"])
            for di in range(3):
                S.op("pe", lambda e, di=di: e.matmul(bank[di][0:8, :], lhsT=relb[:, :], rhs=oh_t[:, di * 512:(di + 1) * 512],
                                                      start=True, stop=True),
                     reads=["relb", "relb32", "oh_t"], writes=[PS(di)])
                S.op("dve", lambda e, di=di: e.tensor_copy(out=fvec_sb[:, di * 512:(di + 1) * 512], in_=bank[di][0:8, :]),
                     reads=[PS(di)], writes=["fvec_sb"])
            S.dma("sp", "c_fv", lambda e: e.dma_start(out=fvec_d, in_=fvec_sb[:]), reads=["fvec_sb"], writes=["fvec_d"])
            S.barrier()

        def rstd_from_ss(ss_ap, n, lnv_ap, rstd_ap, rk, wk):
            S.op("act", lambda e: e.activation(out=lnv_ap, in_=ss_ap, func=AF.Ln, scale=1.0 / n, bias=eps_t[:, 0:1]),
                 reads=[rk, "eps_t"], writes=[wk + "_ln"])
            S.op("act", lambda e: e.activation(out=rstd_ap, in_=lnv_ap, func=AF.Exp, scale=-0.5),
                 reads=[wk + "_ln"], writes=[wk])

        wst = contextlib.ExitStack()
        w_in_b = sbuf(wst, "w_in_b", [128, 8, 1952], BF16)
        for k in range(8):
            S.dma("pool", "w_in", lambda e, k=k: e.dma_start(out=w_in_b[:, k, :], in_=w_in[k * 128:(k + 1) * 128, :]), writes=["w_in_b"])

        def seq_body(s):
            r0 = s * SEQ
            with contextlib.ExitStack() as sq:
                qaT = sbuf(sq, "qaT", [128, 4, SEQ], BF16)
                kaT = sbuf(sq, "kaT", [128, 4, SEQ], BF16)
                sqB = contextlib.ExitStack()
                cqnT = sbuf(sqB, "cqnT", [128, 2, SEQ], BF16)
                ckvnT = sbuf(sqB, "ckvnT", [128, SEQ], BF16)
                kbT = sbuf(sqB, "kbT", [96, 8, SEQ], BF16)
                Vb = sbuf(sqB, "Vb", [128, NT, 8, 65], BF16)
                S.op("pool", lambda e: e.memset(Vb[:, :, :, 64:65], 1.0), writes=["Vb_ones"])

                with contextlib.ExitStack() as p1:
                    g_attn_t = sbuf(p1, "g_attn_t", [128, D], F32)
                    hT = sbuf(p1, "hT", [128, 8, SEQ], BF16)
                    xt = [sbuf(p1, "xt%d" % i, [128, D], F32) for i in range(3)]
                    hb = [sbuf(p1, "hb%d" % i, [128, D], BF16) for i in range(3)]
                    junk = sbuf(p1, "junk", [128, 384], BF16)
                    st1 = sbuf(p1, "st1", [128, 3, 8], F32)
                    vt = [sbuf(p1, "vt%d" % i, [128, 8, 65], BF16) for i in range(2)]
                    cqn = [sbuf(p1, "cqn%d" % i, [128, 256], BF16) for i in range(3)]
                    ckvn = [sbuf(p1, "ckvn%d" % i, [128, 128], BF16) for i in range(3)]
                    ks = [sbuf(p1, "ks%d" % i, [128, 8, 96], BF16) for i in range(3)]
                    krs = [sbuf(p1, "krs%d" % i, [128, 32], F32) for i in range(3)]
                    rtmp = [sbuf(p1, "rtmp%d" % i, [128, 64], F32) for i in range(3)]

                    S.dma("sp", "gattn", lambda e: e.dma_start(out=g_attn_t[:], in_=bcast_rows(g_attn, D)), writes=["g_attn_t"])
                    for i in range(2):
                        S.op("pool", lambda e, i=i: e.memset(vt[i][:, :, 64:65], 1.0), writes=[("vt1", i)])

                    for t in range(NT):
                        b = t % 3
                        S.dma("sp", "xt%d" % b, lambda e, b=b, t=t: e.dma_start(out=xt[b][:], in_=x[r0 + t * 128:r0 + (t + 1) * 128, :]),
                              writes=[("xt", b)])
                        S.op("act", lambda e, b=b: e.activation(out=hb[b][:], in_=xt[b][:], func=AF.Square, accum_out=st1[:, b, 0:1]),
                             reads=[("xt", b)], writes=[("ss", b), ("hb", b)])
                        rstd_from_ss(st1[:, b, 0:1], D, st1[:, b, 1:2], st1[:, b, 2:3], ("ss", b), "rstd%d" % b)
                        S.op("dve", lambda e, b=b: e.scalar_tensor_tensor(out=hb[b][:], in0=xt[b][:], scalar=st1[:, b, 2:3], in1=g_attn_t[:],
                                                                        op0=ALU.mult, op1=ALU.mult),
                             reads=[("xt", b), "rstd%d" % b, "g_attn_t"], writes=[("hb", b)])
                        pb = t % 2
                        pv = bank_bf(pb).rearrange("p (k t) -> p k t", k=8)
                        for k in range(8):
                            S.op("pe", lambda e, k=k, b=b, pv=pv: e.transpose(out=pv[:, k, :], in_=hb[b][:, k * 128:(k + 1) * 128], identity=ident_b[:]),
                                 reads=[("hb", b), "ident_b"], writes=[PS(pb)], inc=(k == 7))
                        eng = "act" if t % 2 == 0 else "dve"
                        if eng == "act":
                            S.op("act", lambda e, t=t, pv=pv: e.activation(out=hT[:, :, t * 128:(t + 1) * 128], in_=pv, func=AF.Copy),
                                 reads=[PS(pb)], writes=[("hT", t)])
                        else:
                            S.op("dve", lambda e, t=t, pv=pv: e.tensor_copy(out=hT[:, :, t * 128:(t + 1) * 128], in_=pv),
                                 reads=[PS(pb)], writes=[("hT", t)])

                    rot = [2, 3, 4, 5]
                    rc = [0]
                    sub = {"P1a": 0, "P1b": 1, "P1c": 2, "P1d": 3}.get(stop_after, 9)

                    def nextbank():
                        bk = rot[rc[0] % len(rot)]
                        rc[0] += 1
                        return bk

                    evc = [0]

                    def evac(out_ap, in_ap, reads, writes, scale=1.0):
                        evc[0] += 1
                        if evc[0] % 2 == 0:
                            S.op("act", lambda e: e.activation(out=out_ap, in_=in_ap, func=AF.Copy, scale=scale), reads=reads, writes=writes)
                        else:
                            S.op("dve", lambda e: e.tensor_scalar(out=out_ap, in0=in_ap, scalar1=scale, scalar2=None, op0=ALU.mult),
                                 reads=reads, writes=writes)

                    for c in range(8 if sub >= 1 else 0):
                        for j in range(4):
                            bk = nextbank()
                            for k in range(8):
                                S.op("pe", lambda e, c=c, j=j, k=k, bk=bk: e.matmul(bank[bk][:], lhsT=w_in_b[:, k, c * 128:(c + 1) * 128],
                                                                                    rhs=hT[:, k, j * 512:(j + 1) * 512], start=(k == 0), stop=(k == 7)),
                                     reads=["w_in_b"] + [("hT", 4 * j + i) for i in range(4)], writes=[PS(bk)], inc=(k == 7))
                            if c < 4:
                                evac(qaT[:, c, j * 512:(j + 1) * 512], bank[bk][:], [PS(bk)], [("qaT", c, j)], scale=0.125)
                            else:
                                evac(kaT[:, c - 4, j * 512:(j + 1) * 512], bank[bk][:], [PS(bk)], [("kaT", c - 4, j)])
                    for t in range(NT if sub >= 2 else 0):
                        b = t % 2
                        bk = nextbank()
                        for k in range(8):
                            S.op("pe", lambda e, t=t, k=k, bk=bk: e.matmul(bank[bk][:], lhsT=hT[:, k, t * 128:(t + 1) * 128],
                                                                            rhs=w_in_b[:, k, 1024:1536], start=(k == 0), stop=(k == 7)),
                                 reads=["w_in_b", ("hT", t)], writes=[PS(bk)], inc=(k == 7))
                        evac(vt[b][:, :, 0:64], bank[bk][:].rearrange("p (h c) -> p h c", h=8), [PS(bk), ("vt1", b)], [("vt", b)])
                        S.dma("sp", "vt%d" % b, lambda e, b=b, t=t: e.dma_start(out=va_d[r0 + t * 128:r0 + (t + 1) * 128, :],
                                                                                  in_=vt[b][:].rearrange("p h c -> p (h c)")),
                              reads=[("vt", b)], writes=[("va_d", t)])
                    for t in range((NT if K2C >= 99 else 1) if sub >= 3 else 0):
                        b = t % 3
                        bk = nextbank()
                        if K2C >= 1:
                            for k in range(8):
                                S.op("pe", lambda e, t=t, k=k, bk=bk: e.matmul(bank[bk][:, 0:416], lhsT=hT[:, k, t * 128:(t + 1) * 128],
                                                                                rhs=w_in_b[:, k, 1536:1952], start=(k == 0), stop=(k == 7)),
                                     reads=["w_in_b", ("hT", t)], writes=[PS(bk)], inc=(k == 7))
                        if K2C >= 2:
                            S.op("act", lambda e, b=b, bk=bk: e.activation(out=junk[:, 0:256], in_=bank[bk][:, 0:256], func=AF.Square,
                                                                            accum_out=st1[:, b, 3:4]), reads=[PS(bk)], writes=[("ssq", b)])
                        if K2C >= 2:
                            S.op("act", lambda e, b=b, bk=bk: e.activation(out=junk[:, 256:384], in_=bank[bk][:, 256:384], func=AF.Square,
                                                                            accum_out=st1[:, b, 5:6]), reads=[PS(bk)], writes=[("sskv", b)])
                        if K2C >= 3:
                            rstd_from_ss(st1[:, b, 3:4], 256, st1[:, b, 4:5], st1[:, b, 4:5], ("ssq", b), "rq%d" % b)
                        if K2C >= 3:
                            rstd_from_ss(st1[:, b, 5:6], 128, st1[:, b, 6:7], st1[:, b, 6:7], ("sskv", b), "rkv%d" % b)
                        if K2C >= 4:
                            S.op("dve", lambda e, b=b, bk=bk: e.scalar_tensor_tensor(out=cqn[b][:], in0=bank[bk][:, 0:256], scalar=st1[:, b, 4:5],
                                                                                      in1=g_q_t[:], op0=ALU.mult, op1=ALU.mult),
                                 reads=[PS(bk), "rq%d" % b, "g_q_t"], writes=[("cqn", b)])
                        if K2C >= 4:
                            S.op("dve", lambda e, b=b, bk=bk: e.scalar_tensor_tensor(out=ckvn[b][:], in0=bank[bk][:, 256:384], scalar=st1[:, b, 6:7],
                                                                                      in1=g_kv_t[:], op0=ALU.mult, op1=ALU.mult),
                                 reads=[PS(bk), "rkv%d" % b, "g_kv_t"], writes=[("ckvn", b)])
                        if K2C >= 5:
                            S.op("dve", lambda e, b=b, bk=bk: e.tensor_copy(out=krs[b][:], in_=bank[bk][:, 384:416]),
                                 reads=[PS(bk)], writes=[("krs", b)])
                        if K2C >= 5:
                            S.op("dve", lambda e, b=b, t=t: e.tensor_tensor(out=rtmp[b][:, 0:32], in0=krs[b][:], in1=rope_t[:, t, 0:32], op=ALU.mult),
                                 reads=[("krs", b), "rope_t"], writes=[("rtA", b)])
                        if K2C >= 5:
                            S.op("dve", lambda e, b=b, t=t: e.tensor_tensor(out=rtmp[b][:, 32:48], in0=krs[b][:, 16:32], in1=rope_t[:, t, 32:48], op=ALU.mult),
                                 reads=[("krs", b), "rope_t"], writes=[("rtB", b)])
                        if K2C >= 5:
                            S.op("dve", lambda e, b=b, t=t: e.tensor_tensor(out=rtmp[b][:, 48:64], in0=krs[b][:, 0:16], in1=rope_t[:, t, 32:48], op=ALU.mult),
                                 reads=[("krs", b), "rope_t"], writes=[("rtC", b)])
                        S.op("dve", lambda e, b=b: e.tensor_tensor(out=ks[b][:, :, 64:80], in0=rtmp[b][:, 0:16].unsqueeze(1).broadcast_to([128, 8, 16]),
                                                                    in1=rtmp[b][:, 32:48].unsqueeze(1).broadcast_to([128, 8, 16]), op=ALU.subtract),
                             reads=[("rtA", b), ("rtB", b)], writes=[("ks_r", b, 0)])
                        S.op("dve", lambda e, b=b: e.tensor_tensor(out=ks[b][:, :, 80:96], in0=rtmp[b][:, 16:32].unsqueeze(1).broadcast_to([128, 8, 16]),
                                                                    in1=rtmp[b][:, 48:64].unsqueeze(1).broadcast_to([128, 8, 16]), op=ALU.add),
                             reads=[("rtA", b), ("rtC", b)], writes=[("ks_r", b, 1)])
                        pb = t % 2
                        pv = bank_bf(pb).rearrange("p (k t) -> p k t", k=8)
                        S.op("pe", lambda e, b=b, pv=pv: e.transpose(out=pv[:, 0, :], in_=cqn[b][:, 0:128], identity=ident_b[:]),
                             reads=[("cqn", b), "ident_b"], writes=[PS(pb)], inc=False)
                        S.op("pe", lambda e, b=b, pv=pv: e.transpose(out=pv[:, 1, :], in_=cqn[b][:, 128:256], identity=ident_b[:]),
                             reads=[("cqn", b)], writes=[PS(pb)], inc=False)
                        S.op("pe", lambda e, b=b, pv=pv: e.transpose(out=pv[:, 2, :], in_=ckvn[b][:], identity=ident_b[:]),
                             reads=[("ckvn", b)], writes=[PS(pb)], inc=True)
                        S.op("act", lambda e, t=t, pv=pv: e.activation(out=cqnT[:, :, t * 128:(t + 1) * 128], in_=pv[:, 0:2, :], func=AF.Copy),
                             reads=[PS(pb)], writes=[("cqnT", t)])
                        S.op("dve", lambda e, t=t, pv=pv: e.tensor_copy(out=ckvnT[:, t * 128:(t + 1) * 128], in_=pv[:, 2, :]),
                             reads=[PS(pb)], writes=[("ckvnT", t)])
                        for half in range(2):
                            bk2 = nextbank()
                            S.op("pe", lambda e, t=t, half=half, bk2=bk2: e.matmul(bank[bk2][:], lhsT=ckvnT[:, t * 128:(t + 1) * 128],
                                                                                    rhs=wkv_b[:, half * 512:(half + 1) * 512], start=True, stop=True),
                                 reads=[("ckvnT", t), "wkv_b"], writes=[PS(bk2)])
                            kvv = bank[bk2][:].rearrange("p (h c) -> p h c", h=4)
                            S.op("act", lambda e, b=b, half=half, kvv=kvv: e.activation(out=ks[b][:, half * 4:(half + 1) * 4, 0:64], in_=kvv[:, :, 0:64], func=AF.Copy),
                                 reads=[PS(bk2)], writes=[("ks_n", b, half)])
                            S.op("dve", lambda e, t=t, half=half, kvv=kvv: e.tensor_copy(out=Vb[:, t, half * 4:(half + 1) * 4, 0:64], in_=kvv[:, :, 64:128]),
                                 reads=[PS(bk2), "Vb_ones"], writes=[("Vb", t, half)])
                        pb2 = 6 + t % 2
                        pv2 = bank_bf(pb2).rearrange("p (k t) -> p k t", k=8)
                        for h in range(8):
                            S.op("pe", lambda e, h=h, b=b, pv2=pv2: e.transpose(out=pv2[0:96, h, :], in_=ks[b][:, h, :], identity=ident_b[:]),
                                 reads=[("ks_n", b, 0), ("ks_n", b, 1), ("ks_r", b, 0), ("ks_r", b, 1), "ident_b"], writes=[PS(pb2)], inc=(h == 7))
                        S.op("act", lambda e, t=t, pv2=pv2: e.activation(out=kbT[:, :, t * 128:(t + 1) * 128], in_=pv2[0:96, :, :], func=AF.Copy),
                             reads=[PS(pb2)], writes=[("kbT", t)])
                    S.barrier()
                if stop_after in ("P1", "P1a", "P1b", "P1c", "P1d"):
                    sqB.close()
                    return

                with contextlib.ExitStack() as p3:
                    qbT = [sbuf(p3, "qbT%d" % i, [96, 8, 512], BF16) for i in range(2)]
                    qs = [sbuf(p3, "qs%d" % i, [128, 8, 96], BF16) for i in range(2)]
                    qr = [sbuf(p3, "qr%d" % i, [128, 8, 32], F32) for i in range(2)]
                    qtm = [sbuf(p3, "qtm%d" % i, [128, 8, 64], F32) for i in range(2)]
                    PT = [sbuf(p3, "PT%d" % i, [128, 1024], BF16) for i in range(3)]
                    ob = [sbuf(p3, "ob%d" % i, [128, 4, 8, 65], F32) for i in range(2)]
                    rl = sbuf(p3, "rl", [128, 8], F32)
                    onb = sbuf(p3, "onb", [128, 8, 64], F32)
                    junk3 = sbuf(p3, "junk3", [128, 512], BF16)
                    st3 = sbuf(p3, "st3", [128, 4], F32)
                    mb = [sbuf(p3, "mb%d" % i, [128, 512], BF16) for i in range(2)]

                    def mla_qproj(jq):
                        qb = jq % 2
                        for tt in range(4):
                            t = jq * 4 + tt
                            b2 = tt % 2
                            for half in range(2):
                                for c in range(2):
                                    S.op("pe", lambda e, half=half, c=c, t=t: e.matmul(bank[6 + half][:, 0:384], lhsT=cqnT[:, c, t * 128:(t + 1) * 128],
                                                                                       rhs=wq_b[:, c, half * 384:(half + 1) * 384], start=(c == 0), stop=(c == 1)),
                                         reads=["wq_b"], writes=[PS(6 + half)], inc=(c == 1))
                                pvh = bank[6 + half][:, 0:384].rearrange("p (h c) -> p h c", h=4)
                                S.op("dve", lambda e, half=half, b2=b2, pvh=pvh: e.tensor_copy(out=qs[b2][:, half * 4:(half + 1) * 4, 0:64], in_=pvh[:, :, 0:64]),
                                     reads=[PS(6 + half)], writes=[("qs_n", b2, half)])
                                S.op("dve", lambda e, half=half, b2=b2, pvh=pvh: e.tensor_copy(out=qr[b2][:, half * 4:(half + 1) * 4, :], in_=pvh[:, :, 64:96]),
                                     reads=[PS(6 + half)], writes=[("qr", b2, half)])
                            cos2 = rope_t[:, t, 0:32].unsqueeze(1).broadcast_to([128, 8, 32])
                            sin1 = rope_t[:, t, 32:48].unsqueeze(1).broadcast_to([128, 8, 16])
                            qrk = [("qr", b2, 0), ("qr", b2, 1)]
                            S.op("dve", lambda e, b2=b2, cos2=cos2: e.tensor_tensor(out=qtm[b2][:, :, 0:32], in0=qr[b2][:], in1=cos2, op=ALU.mult),
                                 reads=qrk, writes=[("qtA", b2)])
                            S.op("dve", lambda e, b2=b2, sin1=sin1: e.tensor_tensor(out=qtm[b2][:, :, 32:48], in0=qr[b2][:, :, 16:32], in1=sin1, op=ALU.mult),
                                 reads=qrk, writes=[("qtB", b2)])
                            S.op("dve", lambda e, b2=b2, sin1=sin1: e.tensor_tensor(out=qtm[b2][:, :, 48:64], in0=qr[b2][:, :, 0:16], in1=sin1, op=ALU.mult),
                                 reads=qrk, writes=[("qtC", b2)])
                            S.op("dve", lambda e, b2=b2: e.tensor_tensor(out=qs[b2][:, :, 64:80], in0=qtm[b2][:, :, 0:16], in1=qtm[b2][:, :, 32:48], op=ALU.subtract),
                                 reads=[("qtA", b2), ("qtB", b2)], writes=[("qs_r", b2, 0)])
                            S.op("dve", lambda e, b2=b2: e.tensor_tensor(out=qs[b2][:, :, 80:96], in0=qtm[b2][:, :, 16:32], in1=qtm[b2][:, :, 48:64], op=ALU.add),
                                 reads=[("qtA", b2), ("qtC", b2)], writes=[("qs_r", b2, 1)])
                            pv = bank_bf(6).rearrange("p (k t) -> p k t", k=8)
                            for h in range(8):
                                S.op("pe", lambda e, h=h, b2=b2, pv=pv: e.transpose(out=pv[0:96, h, :], in_=qs[b2][:, h, :], identity=ident_b[:]),
                                     reads=[("qs_n", b2, 0), ("qs_n", b2, 1), ("qs_r", b2, 0), ("qs_r", b2, 1)], writes=[PS(6)], inc=(h == 7))
                            S.op("dve", lambda e, qb=qb, tt=tt, pv=pv: e.tensor_copy(out=qbT[qb][:, :, tt * 128:(tt + 1) * 128], in_=pv[0:96, :, :]),
                                 reads=[PS(6)], writes=[("qbT", qb, tt)])

                    def mla_epilogue(jq):
                        obj = ob[jq % 2]
                        for qt in range(4):
                            t = jq * 4 + qt
                            m = qt % 2
                            S.op("dve", lambda e, qt=qt: e.reciprocal(out=rl[:], in_=obj[:, qt, :, 64]), reads=[("ob", jq % 2, h) for h in range(8)], writes=["rl"])
                            S.op("dve", lambda e, qt=qt: e.tensor_tensor(out=onb[:], in0=obj[:, qt, :, 0:64], in1=rl[:].unsqueeze(2).broadcast_to([128, 8, 64]), op=ALU.mult),
                                 reads=["rl"] + [("ob", jq % 2, h) for h in range(8)], writes=["onb"])
                            S.op("act", lambda e: e.activation(out=junk3[:], in_=onb[:].rearrange("p h c -> p (h c)"), func=AF.Square, accum_out=st3[:, 0:1]),
                                 reads=["onb"], writes=["ss3"])
                            rstd_from_ss(st3[:, 0:1], 512, st3[:, 1:2], st3[:, 2:3], "ss3", "rstd3")
                            S.op("dve", lambda e, m=m: e.scalar_tensor_tensor(out=mb[m][:], in0=onb[:].rearrange("p h c -> p (h c)"), scalar=st3[:, 2:3],
                                                                               in1=g_out_t[:, 512:1024], op0=ALU.mult, op1=ALU.mult),
                                 reads=["onb", "rstd3", "g_out_t"], writes=[("mb", m)])
                            S.dma("sp", "mb%d" % m, lambda e, m=m, t=t: e.dma_start(out=mixb_d[r0 + t * 128:r0 + (t + 1) * 128, :], in_=mb[m][:]),
                                  reads=[("mb", m)], writes=[("mixb_d", t)])

                    scale_b = 96.0 ** -0.5
                    NU = 8 * (NT // 2)
                    units = [(jq, h, ktp) for jq in range(4) for h in range(8) for ktp in range(NT // 2)]

                    def mla_S(g):
                        jq, h, ktp = units[g]
                        qb = jq % 2
                        pb = g % 2
                        for j in range(2):
                            kt = 2 * ktp + j
                            S.op("pe", lambda e, j=j, kt=kt: e.matmul(bank[2 * pb + j][:], lhsT=kbT[:, h, kt * 128:(kt + 1) * 128], rhs=qbT[qb][:, h, :],
                                                                      start=True, stop=True),
                                 reads=[("qbT", qb, i) for i in range(4)], writes=[PS(2 * pb + j)], inc=(j == 1))

                    def mla_PV(g):
                        jq, h, ktp = units[g]
                        pb = g % 2
                        pti = g % 3
                        pt = PT[pti]
                        accb = 4 + (h % 2)
                        accv = bank[accb][:, 0:260].rearrange("p (q c) -> p q c", q=4)
                        S.op("act", lambda e: e.activation(out=pt[:], in_=psbig[pb][:], func=AF.Exp, scale=scale_b),
                             reads=[PS(2 * pb), PS(2 * pb + 1)], writes=[("PT", pti)])
                        for j in range(2):
                            kt = 2 * ktp + j
                            for qt in range(4):
                                S.op("pe", lambda e, j=j, kt=kt, qt=qt: e.matmul(accv[:, qt, :], lhsT=pt[:, j * 512 + qt * 128:j * 512 + (qt + 1) * 128],
                                                                                  rhs=Vb[:, kt, h, :], start=(kt == 0 and qt == 0), stop=(kt == NT - 1),
                                                                                  skip_group_check=True),
                                     reads=[("PT", pti)], writes=[PS(accb)], inc=(j == 1 and qt == 3))
                        if ktp == NT // 2 - 1:
                            S.op("dve", lambda e: e.tensor_copy(out=ob[jq % 2][:, :, h, :], in_=accv), reads=[PS(accb)], writes=[("ob", jq % 2, h)])

                    mla_qproj(0)
                    mla_S(0)
                    for g in range(len(units)):
                        jq, m = g // NU, g % NU
                        if g + 1 < len(units):
                            mla_S(g + 1)
                        mla_PV(g)
                        if m == 10 and jq > 0:
                            mla_epilogue(jq - 1)
                        if m == 30 and jq < 3:
                            mla_qproj(jq + 1)
                    mla_epilogue(3)
                    S.barrier()
                if stop_after == "B":
                    sqB.close()
                    return

                sqB.close()
                with contextlib.ExitStack() as p4:
                    trev = sbuf(p4, "trev", [128, 3, 8, 384], BF16)
                    for di in range(3):
                        src = bass.AP(tensor=fvec_d.tensor, offset=di * 512, ap=[[1, 128], [1536, 8], [1, 384]])
                        S.dma("pool", "trev", lambda e, di=di, src=src: e.dma_start(out=trev[:, di, :, :], in_=src), writes=["trev"])
                    Vw = [sbuf(p4, "Vw%d" % i, [128, 4, 2, 520], BF16) for i in range(2)]
                    PTa = [sbuf(p4, "PTa%d" % i, [128, 512], BF16) for i in range(4)]
                    oa = [sbuf(p4, "oa%d" % i, [128, 4, 8, 65], F32) for i in range(2)]
                    o16 = [sbuf(p4, "o16_%d" % i, [128, 520], F32) for i in range(2)]
                    o4 = [sbuf(p4, "o4_%d" % i, [128, 520], F32) for i in range(2)]
                    ma_sb = sbuf(p4, "ma_sb", [128, NT, 512], BF16)
                    rl4 = sbuf(p4, "rl4", [128, 8], F32)
                    onb4 = sbuf(p4, "onb4", [128, 8, 64], F32)
                    junk4 = sbuf(p4, "junk4", [128, 512], BF16)
                    st4 = sbuf(p4, "st4", [128, 4], F32)
                    w_out_b = sbuf(p4, "w_out_b", [128, 8, D], BF16)
                    mbt = [sbuf(p4, "mbt%d" % i, [128, 512], BF16) for i in range(2)]
                    xt5 = [sbuf(p4, "xt5_%d" % i, [128, D], F32) for i in range(2)]
                    mixT = [sbuf(p4, "mixT%d" % i, [128, 8, 128], BF16) for i in range(2)]
                    x1t = [sbuf(p4, "x1t%d" % i, [128, D], F32) for i in range(2)]

                    for k in range(8):
                        S.dma("pool", "w_out", lambda e, k=k: e.dma_start(out=w_out_b[:, k, :], in_=w_out[k * 128:(k + 1) * 128, :]), writes=["w_out_b"])

                    unitsA = [(di, d, g, h) for di, d in enumerate(PATTERNS) for g in range(4) for h in range(8)]

                    def tiles_of(d, g):
                        L = SEQ // d
                        tps = L // 128
                        npc = 2 if L >= 256 else 1
                        tl = []
                        for qt in range(4):
                            qidx = g * 4 + qt
                            r, ti = qidx // tps, qidx % tps
                            wb = min(max(ti * 128 - 64, 0), L - 128 * npc)
                            tl.append((r, ti, wb))
                        return tl, npc

                    def A_S(n):
                        di, d, g, h = unitsA[n]
                        tiles, npc = tiles_of(d, g)
                        vb = (n // 8) % 2
                        if h == 0:
                            for qt in range(4):
                                r, ti, wb = tiles[qt]
                                src = bass.AP(tensor=va_d.tensor, offset=(r0 + r + d * wb) * 520,
                                              ap=[[d * 520, 128], [128 * d * 520, npc], [1, 520]])
                                S.dma("sp", "Vw%d_%d" % (vb, qt), lambda e, qt=qt, src=src: e.dma_start(out=Vw[vb][:, qt, 0:npc, :], in_=src),
                                      writes=[("Vw", vb, qt)])
                        pair, hb_ = h // 2, (h % 2) * 64
                        set_ = n % 2
                        for qt in range(4):
                            r, ti, wb = tiles[qt]
                            qs_ = r + d * ti * 128
                            qap = qaT[hb_:hb_ + 64, pair, qs_:qs_ + d * 127 + 1:d]
                            for pc in range(npc):
                                bk = 2 * set_ + pc
                                ks_ = r + d * (wb + pc * 128)
                                kap = kaT[hb_:hb_ + 64, pair, ks_:ks_ + d * 127 + 1:d]
                                S.op("pe", lambda e, bk=bk, qt=qt, kap=kap, qap=qap: e.matmul(bank[bk][:, qt * 128:(qt + 1) * 128], lhsT=kap, rhs=qap,
                                                                                             start=(qt == 0), stop=False, skip_group_check=True),
                                     writes=[PS(bk)], inc=False)
                        for qt in range(4):
                            r, ti, wb = tiles[qt]
                            for pc in range(npc):
                                bk = 2 * set_ + pc
                                j0 = wb + pc * 128 - ti * 128 + 128
                                S.op("pe", lambda e, bk=bk, qt=qt, j0=j0: e.matmul(bank[bk][:, qt * 128:(qt + 1) * 128],
                                                                                  lhsT=trev[:, di, h, j0:j0 + 128], rhs=jrev_b[:],
                                                                                  start=False, stop=True, skip_group_check=True),
                                     reads=["trev", "jrev_b"], writes=[PS(bk)], inc=(qt == 3))

                    def A_PV(n):
                        di, d, g, h = unitsA[n]
                        tiles, npc = tiles_of(d, g)
                        vb = (n // 8) % 2
                        set_ = n % 2
                        accb = 4 + (n % 2)
                        def merge_loads(qt):
                            t = g * 4 + qt
                            m = qt % 2
                            S.dma("pool", "o16_%d" % m, lambda e: e.dma_start(out=o16[m][:], in_=oa_d[0, r0 + t * 128:r0 + (t + 1) * 128, :]),
                                  reads=[("oa_dw", 0), ("oa_dw", 1)], writes=[("o16", m)])
                            S.dma("pool", "o4_%d" % m, lambda e: e.dma_start(out=o4[m][:], in_=oa_d[1, r0 + t * 128:r0 + (t + 1) * 128, :]),
                                  reads=[("oa_dw", 0), ("oa_dw", 1)], writes=[("o4", m)])

                        if h == 0 and d == 1:
                            merge_loads(0)
                            merge_loads(1)
                        if False:
                            for qt in range(4):
                                t = g * 4 + qt
                                m = qt % 2
                                S.dma("pool", "o16_%d" % m, lambda e, m=m, t=t: e.dma_start(out=o16[m][:], in_=oa_d[0, r0 + t * 128:r0 + (t + 1) * 128, :]),
                                      reads=[("oa_dw", 0), ("oa_dw", 1)], writes=[("o16", m)])
                                S.dma("pool", "o4_%d" % m, lambda e, m=m, t=t: e.dma_start(out=o4[m][:], in_=oa_d[1, r0 + t * 128:r0 + (t + 1) * 128, :]),
                                      reads=[("oa_dw", 0), ("oa_dw", 1)], writes=[("o4", m)])
                        for pc in range(npc):
                            bk = 2 * set_ + pc
                            S.op("act", lambda e, bk=bk: e.activation(out=PTa[bk][:], in_=bank[bk][:], func=AF.Exp),
                                 reads=[PS(bk)], writes=[("PTa", bk)])
                        accv = bank[accb][:, 0:260].rearrange("p (q c) -> p q c", q=4)
                        for qt in range(4):
                            for pc in range(npc):
                                bk = 2 * set_ + pc
                                S.op("pe", lambda e, bk=bk, qt=qt, pc=pc: e.matmul(
                                    accv[:, qt, :], lhsT=PTa[bk][:, qt * 128:(qt + 1) * 128], rhs=Vw[vb][:, qt, pc, h * 65:(h + 1) * 65],
                                    start=(pc == 0), stop=(pc == npc - 1), skip_group_check=True),
                                     reads=[("PTa", bk), ("Vw", vb, qt)], writes=[PS(accb)], inc=(qt == 3 and pc == npc - 1))
                        S.op("dve", lambda e: e.tensor_copy(out=oa[vb][:, :, h, :], in_=accv), reads=[PS(accb)], writes=[("oa", vb, h)])
                        if h != 7:
                            return
                        oak = [("oa", vb, hh) for hh in range(8)]
                        if d != 1:
                            for qt in range(4):
                                r, ti, wb = tiles[qt]
                                dst = bass.AP(tensor=oa_d.tensor, offset=(di * NTOK + r0 + r + d * ti * 128) * 520, ap=[[d * 520, 128], [1, 520]])
                                S.dma("sp", "oaw%d" % vb, lambda e, qt=qt, dst=dst: e.dma_start(out=dst, in_=oa[vb][:, qt].rearrange("p h c -> p (h c)")),
                                      reads=oak, writes=[("oa_dw", vb)])
                            return
                        for qt in range(4):
                            t = g * 4 + qt
                            m = qt % 2
                            S.op("dve", lambda e, m=m: e.tensor_tensor(out=o16[m][:], in0=o16[m][:], in1=o4[m][:], op=ALU.add),
                                 reads=[("o16", m), ("o4", m)], writes=[("o16", m)])
                            S.op("dve", lambda e, m=m, qt=qt: e.tensor_tensor(out=o16[m][:], in0=o16[m][:], in1=oa[vb][:, qt].rearrange("p h c -> p (h c)"), op=ALU.add),
                                 reads=[("o16", m)] + oak, writes=[("o16", m)])
                            ov = o16[m][:].rearrange("p (h c) -> p h c", h=8)
                            S.op("dve", lambda e, ov=ov: e.reciprocal(out=rl4[:], in_=ov[:, :, 64]), reads=[("o16", m)], writes=["rl4"])
                            S.op("dve", lambda e, ov=ov: e.tensor_tensor(out=onb4[:], in0=ov[:, :, 0:64], in1=rl4[:].unsqueeze(2).broadcast_to([128, 8, 64]), op=ALU.mult),
                                 reads=["rl4", ("o16", m)], writes=["onb4"])
                            if qt + 2 < 4:
                                merge_loads(qt + 2)
                            S.op("act", lambda e: e.activation(out=junk4[:], in_=onb4[:].rearrange("p h c -> p (h c)"), func=AF.Square, accum_out=st4[:, 0:1]),
                                 reads=["onb4"], writes=["ss4"])
                            rstd_from_ss(st4[:, 0:1], 512, st4[:, 1:2], st4[:, 2:3], "ss4", "rstd4")
                            S.op("dve", lambda e, t=t: e.scalar_tensor_tensor(out=ma_sb[:, t, :], in0=onb4[:].rearrange("p h c -> p (h c)"), scalar=st4[:, 2:3],
                                                                               in1=g_out_t[:, 0:512], op0=ALU.mult, op1=ALU.mult),
                                 reads=["onb4", "rstd4", "g_out_t"], writes=[("ma", t)])
                            if dbg:
                                S.dma("sp", "dbgma", lambda e, t=t: e.dma_start(out=mixa_d[r0 + t * 128:r0 + (t + 1) * 128, :], in_=ma_sb[:, t, :]),
                                      reads=[("ma", t)])

                    A_S(0)
                    for n in range(len(unitsA)):
                        if n + 1 < len(unitsA):
                            A_S(n + 1)
                        A_PV(n)
                    for t in range(NT):
                        b = t % 2
                        S.dma("sp", "mbt%d" % b, lambda e, b=b, t=t: e.dma_start(out=mbt[b][:], in_=mixb_d[r0 + t * 128:r0 + (t + 1) * 128, :]), writes=[("mbt", b)])
                        S.dma("sp", "xt5_%d" % b, lambda e, b=b, t=t: e.dma_start(out=xt5[b][:], in_=x[r0 + t * 128:r0 + (t + 1) * 128, :]), writes=[("xt5", b)])
                        pv = bank_bf(6).rearrange("p (k t) -> p k t", k=8)
                        for k in range(4):
                            S.op("pe", lambda e, k=k, t=t, pv=pv: e.transpose(out=pv[:, k, :], in_=ma_sb[:, t, k * 128:(k + 1) * 128], identity=ident_b[:]),
                                 reads=[("ma", t), "ident_b"], writes=[PS(6)], inc=False)
                        for k in range(4):
                            S.op("pe", lambda e, k=k, b=b, pv=pv: e.transpose(out=pv[:, 4 + k, :], in_=mbt[b][:, k * 128:(k + 1) * 128], identity=ident_b[:]),
                                 reads=[("mbt", b)], writes=[PS(6)], inc=(k == 3))
                        S.op("act", lambda e, b=b, pv=pv: e.activation(out=mixT[b][:], in_=pv, func=AF.Copy), reads=[PS(6)], writes=[("mixT", b)])
                        for half in range(2):
                            for k in range(8):
                                S.op("pe", lambda e, half=half, k=k, b=b: e.matmul(bank[half][:], lhsT=mixT[b][:, k, :], rhs=w_out_b[:, k, half * 512:(half + 1) * 512],
                                                                                  start=(k == 0), stop=(k == 7)),
                                     reads=[("mixT", b), "w_out_b"], writes=[PS(half)], inc=(k == 7))
                            S.op("dve", lambda e, half=half, b=b: e.tensor_tensor(out=x1t[b][:, half * 512:(half + 1) * 512], in0=bank[half][:],
                                                                                 in1=xt5[b][:, half * 512:(half + 1) * 512], op=ALU.add),
                                 reads=[PS(half), ("xt5", b)], writes=[("x1t", b, half)])
                        S.dma("pool", "x1t%d" % b, lambda e, b=b, t=t: e.dma_start(out=x1_d[r0 + t * 128:r0 + (t + 1) * 128, :], in_=x1t[b][:]),
                              reads=[("x1t", b, 0), ("x1t", b, 1)], writes=[("x1_d", s, t)])
                    S.barrier()


        for s_ in range(NSEQ if stop_after is None else (0 if stop_after == "P0" else 1)):
            seq_body(s_)
        wst.close()

        if stop_after is None:
          with contextlib.ExitStack() as pm:
            NTT = NTOK // 128
            g_ffn_t = sbuf(pm, "g_ffn_t", [128, D], F32)
            g_fin_t = sbuf(pm, "g_fin_t", [128, D], F32)
            b_r_t = sbuf(pm, "b_r_t", [128, 36], F32)
            h2b = sbuf(pm, "h2b", [128, NTT, D], BF16)
            gate1 = sbuf(pm, "gate1", [128, NTT], F32)
            gate2 = sbuf(pm, "gate2", [128, NTT], F32)
            d1i = sbuf(pm, "d1i", [128, NTT], I32)
            d2i = sbuf(pm, "d2i", [128, NTT], I32)
            S.dma("sp", "gffn", lambda e: e.dma_start(out=g_ffn_t[:], in_=bcast_rows(g_ffn, D)), writes=["g_ffn_t"])
            S.dma("sp", "gfin", lambda e: e.dma_start(out=g_fin_t[:], in_=bcast_rows(g_fin, D)), writes=["g_fin_t"])
            S.dma("sp", "brt", lambda e: e.dma_start(out=b_r_t[:], in_=bcast_rows(b_r, 36)), writes=["b_r_t"])
            zt = sbuf(pm, "zt", [128, D], BF16)
            S.op("pool", lambda e: e.memset(zt[:], 0.0), writes=["zt"])
            S.dma("sp", "zfilly", lambda e: e.dma_start(out=ys_d[NROWS:NROWS + 128, :], in_=zt[:]), reads=["zt"], writes=["ys_d"])
            xs_v = xs_d.rearrange("(n p) d -> p n d", p=128)
            NCH = (NROWS + 128) // 128
            for c0 in range(0, NCH, 16):
                c1 = min(c0 + 16, NCH)
                S.dma("sp", "zfill", lambda e, c0=c0, c1=c1: e.dma_start(out=xs_v[:, c0:c1, :], in_=zt[:].unsqueeze(1).broadcast_to([128, c1 - c0, D])),
                      reads=["zt"], writes=["xs_zf"])
            NW = 3
            wgu = [sbuf(pm, "wgu%d" % i, [128, 8, 512], BF16) for i in range(NW)]
            wd = [sbuf(pm, "wd%d" % i, [128, 2, D], BF16) for i in range(NW)]

            def load_w(ex):
                wi = ex % NW
                S.dma("pool", "wg%d" % wi, lambda e: e.dma_start(out=wgu[wi][:, :, 0:256], in_=w_gate[ex].rearrange("(k p) f -> p k f", p=128)),
                      writes=[("wgu", wi)])
                S.dma("pool", "wg%d" % wi, lambda e: e.dma_start(out=wgu[wi][:, :, 256:512], in_=w_up[ex].rearrange("(k p) f -> p k f", p=128)),
                      writes=[("wgu", wi)])
                S.dma("pool", "wd%d" % wi, lambda e: e.dma_start(out=wd[wi][:], in_=w_down[ex].rearrange("(c p) n -> p c n", p=128)),
                      writes=[("wd", wi)])

            for ex in range(NW):
                load_w(ex)
            with contextlib.ExitStack() as m1:
                mxt = [sbuf(m1, "mxt%d" % i, [128, D], F32) for i in range(2)]
                h2f = [sbuf(m1, "h2f%d" % i, [128, D], F32) for i in range(2)]
                h2lo = [sbuf(m1, "h2lo%d" % i, [128, D], BF16) for i in range(2)]
                hiT = [sbuf(m1, "hiT%d" % i, [128, 8, 128], BF16) for i in range(2)]
                loT = [sbuf(m1, "loT%d" % i, [128, 8, 128], BF16) for i in range(2)]
                lgt = [sbuf(m1, "lgt%d" % i, [128, 36], F32) for i in range(2)]
                wr2 = sbuf(m1, "wr2", [128, 8, 72], BF16)
                wrd = sbuf(m1, "wrd", [128, 8, 36], F32)
                S.op("dve", lambda e: e.tensor_copy(out=wr2[:, :, 0:36], in_=wr_f[:]), reads=["wr_f"], writes=["wr2a"])
                S.op("dve", lambda e: e.tensor_tensor(out=wrd[:], in0=wr_f[:], in1=wr2[:, :, 0:36], op=ALU.subtract), reads=["wr_f", "wr2a"], writes=["wrd"])
                S.op("dve", lambda e: e.tensor_copy(out=wr2[:, :, 36:72], in_=wrd[:]), reads=["wrd"], writes=["wr2"])
                junkm = sbuf(m1, "junkm", [128, D], BF16)
                stm = sbuf(m1, "stm", [128, 2, 4], F32)
                lg = sbuf(m1, "lg", [128, NTT, 36], F32)
                gmax = sbuf(m1, "gmax", [128, NTT], F32)
                gm = sbuf(m1, "gm", [128, NTT, 4], F32)
                gsh = sbuf(m1, "gsh", [128, NTT, 4], F32)
                gsum = sbuf(m1, "gsum", [128, NTT], F32)
                ggate = sbuf(m1, "ggate", [128, NTT], F32)
                t48 = sbuf(m1, "t48", [128, NTT, 4, 8], F32)
                ig = sbuf(m1, "ig", [128, NTT, 8], F32)
                ig2 = sbuf(m1, "ig2", [128, NTT, 8], F32)
                m1v = sbuf(m1, "m1v", [128, NTT], F32)
                m2v = sbuf(m1, "m2v", [128, NTT], F32)
                mask1 = sbuf(m1, "mask1", [128, NTT, 8], F32)
                mask2 = sbuf(m1, "mask2", [128, NTT, 8], F32)
                e2 = sbuf(m1, "e2", [128, NTT], F32)
                den = sbuf(m1, "den", [128, NTT], F32)
                OH1 = sbuf(m1, "OH1", [128, NTT, 4, 8], F32)
                OH2 = sbuf(m1, "OH2", [128, NTT, 4, 8], F32)
                OHs = sbuf(m1, "OHs", [128, NTT, 32], F32)
                cumT = sbuf(m1, "cumT", [128, NTT + 1, 32], F32)
                rank_all = sbuf(m1, "rank_all", [128, NTT, 32], F32)
                ebi = sbuf(m1, "ebi", [128, 32], I32)
                ebf = sbuf(m1, "ebf", [128, 32], F32)
                tsel = sbuf(m1, "tsel", [128, NTT, 32], F32)
                rsel = sbuf(m1, "rsel", [128, NTT], F32)
                esel = sbuf(m1, "esel", [128, NTT], F32)
                ovf = sbuf(m1, "ovf", [128, NTT], F32)

                def m1_tiles(t0, t1):
                  for i in range(t0, t1):
                    b = i % 2
                    S.dma("sp", "mxt%d" % b, lambda e, b=b, i=i: e.dma_start(out=mxt[b][:], in_=x1_d[i * 128:(i + 1) * 128, :]), writes=[("mxt", b)])
                    S.op("act", lambda e, b=b: e.activation(out=junkm[:], in_=mxt[b][:], func=AF.Square, accum_out=stm[:, b, 0:1]),
                         reads=[("mxt", b)], writes=[("mss", b)])
                    rstd_from_ss(stm[:, b, 0:1], D, stm[:, b, 1:2], stm[:, b, 2:3], ("mss", b), "mrstd%d" % b)
                    S.op("dve", lambda e, b=b: e.scalar_tensor_tensor(out=h2f[b][:], in0=mxt[b][:], scalar=stm[:, b, 2:3], in1=g_ffn_t[:], op0=ALU.mult, op1=ALU.mult),
                         reads=[("mxt", b), "mrstd%d" % b, "g_ffn_t"], writes=[("h2f", b)])
                    S.op("act", lambda e, b=b, i=i: e.activation(out=h2b[:, i, :], in_=h2f[b][:], func=AF.Copy), reads=[("h2f", b)], writes=[("h2b", i)])
                    S.op("dve", lambda e, b=b, i=i: e.tensor_tensor(out=h2lo[b][:], in0=h2f[b][:], in1=h2b[:, i, :], op=ALU.subtract),
                         reads=[("h2f", b), ("h2b", i)], writes=[("h2lo", b)])
                    pvh = bank_bf(0).rearrange("p (k t) -> p k t", k=8)
                    pvl = bank_bf(1).rearrange("p (k t) -> p k t", k=8)
                    for k in range(8):
                        S.op("pe", lambda e, k=k, i=i, pvh=pvh: e.transpose(out=pvh[:, k, :], in_=h2b[:, i, k * 128:(k + 1) * 128], identity=ident_b[:]),
                             reads=[("h2b", i), "ident_b"], writes=[PS(0)], inc=(k == 7))
                    S.op("act", lambda e, b=b, pvh=pvh: e.activation(out=hiT[b][:], in_=pvh, func=AF.Copy), reads=[PS(0)], writes=[("hiT", b)])
                    for k in range(8):
                        S.op("pe", lambda e, k=k, b=b, pvl=pvl: e.transpose(out=pvl[:, k, :], in_=h2lo[b][:, k * 128:(k + 1) * 128], identity=ident_b[:]),
                             reads=[("h2lo", b)], writes=[PS(1)], inc=(k == 7))
                    S.op("dve", lambda e, b=b, pvl=pvl: e.tensor_copy(out=loT[b][:], in_=pvl), reads=[PS(1)], writes=[("loT", b)])
                    lb = 2 + b
                    for k in range(8):
                        S.op("pe", lambda e, k=k, b=b, lb=lb: e.matmul(bank[lb][:, 0:72], lhsT=hiT[b][:, k, :], rhs=wr2[:, k, :], start=(k == 0), stop=False,
                                                                      skip_group_check=True),
                             reads=[("hiT", b), "wr2"], writes=[PS(lb)], inc=False)
                    for k in range(8):
                        S.op("pe", lambda e, k=k, b=b, lb=lb: e.matmul(bank[lb][:, 0:36], lhsT=loT[b][:, k, :], rhs=wr2[:, k, 0:36], start=False, stop=(k == 7),
                                                                      skip_group_check=True),
                             reads=[("loT", b), "wr2"], writes=[PS(lb)], inc=(k == 7))
                    S.op("dve", lambda e, b=b, lb=lb: e.tensor_tensor(out=lgt[b][:], in0=bank[lb][:, 36:72], in1=b_r_t[:], op=ALU.add),
                         reads=[PS(lb), "b_r_t"], writes=[("lgt", b)])
                    S.op("dve", lambda e, i=i, b=b, lb=lb: e.tensor_tensor(out=lg[:, i, :], in0=bank[lb][:, 0:36], in1=lgt[b][:], op=ALU.add),
                         reads=[PS(lb), ("lgt", b)], writes=["lg"])

                def m1_route(t0, t1):
                  nt = t1 - t0
                  gl = lg[:, t0:t1, 0:4]
                  el = lg[:, t0:t1, 4:36].rearrange("p t (g e) -> p t g e", g=4)

                  def bc(ap, shape, axis):
                    return ap.unsqueeze(axis).broadcast_to(shape)

                  V = lambda fn, reads, writes: S.op("dve", fn, reads=reads, writes=writes)
                  V(lambda e: e.tensor_reduce(out=gmax[:, t0:t1], in_=gl, op=ALU.max, axis=AX.X), ["lg"], ["gmax"])
                  V(lambda e: e.tensor_tensor(out=gm[:, t0:t1], in0=gl, in1=bc(gmax[:, t0:t1], [128, nt, 4], 2), op=ALU.is_equal), ["lg", "gmax"], ["gm"])
                  V(lambda e: e.tensor_tensor(out=gsh[:, t0:t1], in0=gl, in1=bc(gmax[:, t0:t1], [128, nt, 4], 2), op=ALU.subtract), ["lg", "gmax"], ["gsh"])
                  S.op("act", lambda e: e.activation(out=gsh[:, t0:t1], in_=gsh[:, t0:t1], func=AF.Exp), reads=["gsh"], writes=["gsh"])
                  V(lambda e: e.tensor_reduce(out=gsum[:, t0:t1], in_=gsh[:, t0:t1], op=ALU.add, axis=AX.X), ["gsh"], ["gsum"])
                  V(lambda e: e.reciprocal(out=ggate[:, t0:t1], in_=gsum[:, t0:t1]), ["gsum"], ["ggate"])
                  V(lambda e: e.tensor_tensor(out=t48[:, t0:t1], in0=el, in1=bc(gm[:, t0:t1], [128, nt, 4, 8], 3), op=ALU.mult), ["lg", "gm"], ["t48"])
                  V(lambda e: e.tensor_reduce(out=ig[:, t0:t1], in_=t48[:, t0:t1].rearrange("p t g e -> p t e g"), op=ALU.add, axis=AX.X), ["t48"], ["ig"])
                  V(lambda e: e.tensor_reduce(out=m1v[:, t0:t1], in_=ig[:, t0:t1], op=ALU.max, axis=AX.X), ["ig"], ["m1v"])
                  V(lambda e: e.tensor_tensor(out=mask1[:, t0:t1], in0=ig[:, t0:t1], in1=bc(m1v[:, t0:t1], [128, nt, 8], 2), op=ALU.is_equal), ["ig", "m1v"], ["mask1"])
                  V(lambda e: e.scalar_tensor_tensor(out=ig2[:, t0:t1].rearrange("p t e -> p (t e)"), in0=mask1[:, t0:t1].rearrange("p t e -> p (t e)"), scalar=-1e30,
                                                   in1=ig[:, t0:t1].rearrange("p t e -> p (t e)"), op0=ALU.mult, op1=ALU.add), ["mask1", "ig"], ["ig2"])
                  V(lambda e: e.tensor_reduce(out=m2v[:, t0:t1], in_=ig2[:, t0:t1], op=ALU.max, axis=AX.X), ["ig2"], ["m2v"])
                  V(lambda e: e.tensor_tensor(out=mask2[:, t0:t1], in0=ig2[:, t0:t1], in1=bc(m2v[:, t0:t1], [128, nt, 8], 2), op=ALU.is_equal), ["ig2", "m2v"], ["mask2"])
                  V(lambda e: e.tensor_tensor(out=e2[:, t0:t1], in0=m2v[:, t0:t1], in1=m1v[:, t0:t1], op=ALU.subtract), ["m1v", "m2v"], ["e2"])
                  S.op("act", lambda e: e.activation(out=e2[:, t0:t1], in_=e2[:, t0:t1], func=AF.Exp), reads=["e2"], writes=["e2"])
                  V(lambda e: e.tensor_scalar(out=den[:, t0:t1], in0=e2[:, t0:t1], scalar1=1.0, scalar2=None, op0=ALU.add), ["e2"], ["den"])
                  V(lambda e: e.reciprocal(out=den[:, t0:t1], in_=den[:, t0:t1]), ["den"], ["den"])
                  V(lambda e: e.tensor_tensor(out=gate1[:, t0:t1], in0=ggate[:, t0:t1], in1=den[:, t0:t1], op=ALU.mult), ["ggate", "den"], ["gate1"])
                  V(lambda e: e.tensor_tensor(out=gate2[:, t0:t1], in0=gate1[:, t0:t1], in1=e2[:, t0:t1], op=ALU.mult), ["gate1", "e2"], ["gate2"])
                  V(lambda e: e.tensor_tensor(out=OH1[:, t0:t1], in0=bc(gm[:, t0:t1], [128, nt, 4, 8], 3), in1=bc(mask1[:, t0:t1], [128, nt, 4, 8], 2), op=ALU.mult), ["gm", "mask1"], ["OH1"])
                  V(lambda e: e.tensor_tensor(out=OH2[:, t0:t1], in0=bc(gm[:, t0:t1], [128, nt, 4, 8], 3), in1=bc(mask2[:, t0:t1], [128, nt, 4, 8], 2), op=ALU.mult), ["gm", "mask2"], ["OH2"])
                  V(lambda e: e.tensor_tensor(out=OHs[:, t0:t1].rearrange("p t e -> p (t e)"), in0=OH1[:, t0:t1].rearrange("p t g e -> p (t g e)"),
                                            in1=OH2[:, t0:t1].rearrange("p t g e -> p (t g e)"), op=ALU.add), ["OH1", "OH2"], ["OHs"])
                  for i in range(t0, t1):
                    V(lambda e, i=i: e.tensor_tensor(out=cumT[:, i + 1, :], in0=cumT[:, i, :], in1=OHs[:, i, :], op=ALU.add), [("cumT", i), "OHs"], [("cumT", i + 1)])
                  for i in range(t0, t1):
                    rb = 4 + (i % 2)
                    S.op("pe", lambda e, i=i, rb=rb: e.matmul(bank[rb][:, 0:32], lhsT=ltri_f[:], rhs=OHs[:, i, :], start=True, stop=False),
                         reads=["ltri_f", "OHs"], writes=[PS(rb)], inc=False)
                    S.op("pe", lambda e, i=i, rb=rb: e.matmul(bank[rb][:, 0:32], lhsT=ones_f[:], rhs=cumT[:, i, :], start=False, stop=True),
                         reads=["ones_f", ("cumT", i)], writes=[PS(rb)], inc=True)
                    V(lambda e, i=i, rb=rb: e.tensor_copy(out=rank_all[:, i, :], in_=bank[rb][:, 0:32]), [PS(rb)], ["rank_all"])
                  for (OH, dst_i, nm) in ((OH1, d1i, "1"), (OH2, d2i, "2")):
                    ohf = OH[:, t0:t1].rearrange("p t g e -> p t (g e)")
                    V(lambda e, ohf=ohf: e.tensor_tensor(out=tsel[:, t0:t1], in0=rank_all[:, t0:t1], in1=ohf, op=ALU.mult), ["rank_all", "OH" + nm], ["tsel"])
                    V(lambda e: e.tensor_reduce(out=rsel[:, t0:t1], in_=tsel[:, t0:t1], op=ALU.add, axis=AX.X), ["tsel"], ["rsel"])
                    V(lambda e, ohf=ohf: e.tensor_tensor(out=tsel[:, t0:t1], in0=ohf, in1=bc(ebf[:], [128, nt, 32], 1), op=ALU.mult), ["ebf", "OH" + nm, "rsel"], ["tsel"])
                    V(lambda e: e.tensor_reduce(out=esel[:, t0:t1], in_=tsel[:, t0:t1], op=ALU.add, axis=AX.X), ["tsel"], ["esel"])
                    V(lambda e: e.tensor_scalar(out=ovf[:, t0:t1], in0=rsel[:, t0:t1], scalar1=float(CAPB * 128), scalar2=None, op0=ALU.is_lt), ["rsel"], ["ovf"])
                    V(lambda e: e.tensor_tensor(out=rsel[:, t0:t1], in0=rsel[:, t0:t1], in1=esel[:, t0:t1], op=ALU.add), ["rsel", "esel"], ["rsel"])
                    V(lambda e: e.scalar_tensor_tensor(out=rsel[:, t0:t1], in0=rsel[:, t0:t1], scalar=float(-NROWS), in1=ovf[:, t0:t1], op0=ALU.add, op1=ALU.mult), ["rsel", "ovf"], ["rsel"])
                    V(lambda e: e.tensor_scalar(out=rsel[:, t0:t1], in0=rsel[:, t0:t1], scalar1=float(NROWS), scalar2=None, op0=ALU.add), ["rsel"], ["rsel"])
                    V(lambda e, dst_i=dst_i: e.tensor_copy(out=dst_i[:, t0:t1], in_=rsel[:, t0:t1]), ["rsel"], ["dst" + nm])
                  for i in range(t0, t1):
                      for (dst_i, nm) in ((d1i, "1"), (d2i, "2")):
                          S.dma("pool", "scat" + nm, lambda e, i=i, dst_i=dst_i: e.indirect_dma_start(
                              out=xs_d, out_offset=bass.IndirectOffsetOnAxis(ap=dst_i[:, i:i + 1], axis=0), in_=h2b[:, i, :], in_offset=None),
                                reads=[("h2b", i), "dst" + nm, "xs_zf"], writes=[("xs_s", i, nm)])

                S.op("pool", lambda e: e.memset(cumT[:, 0, :], 0.0), writes=[("cumT", 0)])
                S.op("pool", lambda e: e.iota(ebi[:], pattern=[[CAPB * 128, 32]], base=0, channel_multiplier=0), writes=["ebi"])
                S.op("dve", lambda e: e.tensor_copy(out=ebf[:], in_=ebi[:]), reads=["ebi"], writes=["ebf"])
                NB1 = 4
                for jb in range(NB1):
                    m1_tiles(jb * (NTT // NB1), (jb + 1) * (NTT // NB1))
                    m1_route(jb * (NTT // NB1), (jb + 1) * (NTT // NB1))
                S.barrier()
            with contextlib.ExitStack() as m2:
                xblk = [sbuf(m2, "xblk%d" % i, [128, D], BF16) for i in range(8)]
                xT = [sbuf(m2, "xT%d" % i, [128, 8, 128], BF16) for i in range(3)]
                sg = [sbuf(m2, "sg%d" % i, [128, 256], F32) for i in range(2)]
                hblk = [sbuf(m2, "hblk%d" % i, [128, 256], BF16) for i in range(3)]
                hT2 = [sbuf(m2, "hT2_%d" % i, [128, 2, 128], BF16) for i in range(3)]
                yblk = [sbuf(m2, "yblk%d" % i, [128, D], BF16) for i in range(2)]
                NBLK = N_EXP * CAPB

                def P0(n):
                    xb = n % 8
                    row = n * 128
                    S.dma("sp", "xblk%d" % xb, lambda e: e.dma_start(out=xblk[xb][:], in_=xs_d[row:row + 128, :]), reads=["xs_d"], writes=[("xblk", xb)])

                def P1(n):
                    xb, pb, tb = n % 8, n % 2, n % 3
                    pv = bank_bf(pb).rearrange("p (k t) -> p k t", k=8)
                    for k in range(8):
                        S.op("pe", lambda e, k=k: e.transpose(out=pv[:, k, :], in_=xblk[xb][:, k * 128:(k + 1) * 128], identity=ident_b[:]),
                             reads=[("xblk", xb), "ident_b"], writes=[PS(pb)], inc=(k == 7))
                    S.op("act", lambda e: e.activation(out=xT[tb][:], in_=pv, func=AF.Copy), reads=[PS(pb)], writes=[("xT", tb)])

                def P2(n):
                    tb, gb, hb3, sb2 = n % 3, 2 + n % 2, n % 3, n % 2
                    wi = (n // CAPB) % NW
                    for k in range(8):
                        S.op("pe", lambda e, k=k: e.matmul(bank[gb][:], lhsT=xT[tb][:, k, :], rhs=wgu[wi][:, k, :], start=(k == 0), stop=(k == 7)),
                             reads=[("xT", tb), ("wgu", wi)], writes=[PS(gb)], inc=(k == 7))
                    S.op("act", lambda e: e.activation(out=sg[sb2][:], in_=bank[gb][:, 0:256], func=AF.Silu), reads=[PS(gb)], writes=[("sg", sb2)])
                    S.op("dve", lambda e: e.tensor_tensor(out=hblk[hb3][:], in0=sg[sb2][:], in1=bank[gb][:, 256:512], op=ALU.mult),
                         reads=[("sg", sb2), PS(gb)], writes=[("hblk", hb3)])

                def P3(n):
                    hb3, tb2 = n % 3, 4 + n % 2
                    pv2 = bank_bf(tb2).rearrange("p (k t) -> p k t", k=8)
                    for c in range(2):
                        S.op("pe", lambda e, c=c: e.transpose(out=pv2[:, c, :], in_=hblk[hb3][:, c * 128:(c + 1) * 128], identity=ident_b[:]),
                             reads=[("hblk", hb3)], writes=[PS(tb2)], inc=(c == 1))
                    S.op("dve", lambda e: e.tensor_copy(out=hT2[hb3][:], in_=pv2[:, 0:2, :]), reads=[PS(tb2)], writes=[("hT2", hb3)])

                def P4(n):
                    hb3, yb2 = n % 3, n % 2
                    wi = (n // CAPB) % NW
                    row = n * 128
                    for half in range(2):
                        yb = 6 + half
                        for c in range(2):
                            S.op("pe", lambda e, c=c, half=half, yb=yb: e.matmul(bank[yb][:], lhsT=hT2[hb3][:, c, :], rhs=wd[wi][:, c, half * 512:(half + 1) * 512],
                                                                                start=(c == 0), stop=(c == 1)),
                                 reads=[("hT2", hb3), ("wd", wi)], writes=[PS(yb)], inc=(c == 1))
                        if half == 0:
                            S.op("act", lambda e, yb=yb: e.activation(out=yblk[yb2][:, 0:512], in_=bank[yb][:], func=AF.Copy), reads=[PS(yb)], writes=[("yblk", yb2, 0)])
                        else:
                            S.op("dve", lambda e, yb=yb: e.tensor_copy(out=yblk[yb2][:, 512:1024], in_=bank[yb][:]), reads=[PS(yb)], writes=[("yblk", yb2, 1)])
                    S.dma("sp", "yblk%d" % yb2, lambda e: e.dma_start(out=ys_d[row:row + 128, :], in_=yblk[yb2][:]),
                          reads=[("yblk", yb2, 0), ("yblk", yb2, 1)], writes=[("ys_dw", yb2)])
                    if n % CAPB == CAPB - 1 and n // CAPB + NW < N_EXP:
                        load_w(n // CAPB + NW)

                for step in range(-4, NBLK + 3):
                    for stage, skew in ((P0, -4), (P1, 0), (P2, 1), (P3, 2), (P4, 3)):
                        n = step - skew
                        if 0 <= n < NBLK:
                            stage(n)
                S.barrier()
            with contextlib.ExitStack() as m3:
                y1t = [sbuf(m3, "y1t%d" % i, [128, D], BF16) for i in range(6)]
                y2t = [sbuf(m3, "y2t%d" % i, [128, D], BF16) for i in range(6)]
                fxt = [sbuf(m3, "fxt%d" % i, [128, D], F32) for i in range(6)]
                acc = [sbuf(m3, "facc%d" % i, [128, D], F32) for i in range(2)]
                ot = [sbuf(m3, "fot%d" % i, [128, D], F32) for i in range(2)]
                junkf = sbuf(m3, "junkf", [128, D], BF16)
                stf = sbuf(m3, "stf", [128, 3, 4], F32)
                for i in range(6):
                    S.op("pool", lambda e, i=i: e.memset(y1t[i][:], 0.0), writes=[("y1t", i)])
                    S.op("pool", lambda e, i=i: e.memset(y2t[i][:], 0.0), writes=[("y2t", i)])
                def m3_load(i):
                    b = i % 6
                    S.dma("pool", "y1t%d" % b, lambda e, b=b, i=i: e.indirect_dma_start(
                        out=y1t[b][:], out_offset=None, in_=ys_d, in_offset=bass.IndirectOffsetOnAxis(ap=d1i[:, i:i + 1], axis=0)),
                          reads=["ys_d"], writes=[("y1t", b)])
                    S.dma("pool", "y2t%d" % b, lambda e, b=b, i=i: e.indirect_dma_start(
                        out=y2t[b][:], out_offset=None, in_=ys_d, in_offset=bass.IndirectOffsetOnAxis(ap=d2i[:, i:i + 1], axis=0)),
                          reads=["ys_d"], writes=[("y2t", b)])
                    S.dma("sp", "fxt%d" % b, lambda e, b=b, i=i: e.dma_start(out=fxt[b][:], in_=x1_d[i * 128:(i + 1) * 128, :]), writes=[("fxt", b)])

                def m3_comp(i):
                    b = i % 6
                    c = i % 2
                    S.op("dve", lambda e, b=b, c=c, i=i: e.scalar_tensor_tensor(out=acc[c][:], in0=y1t[b][:], scalar=gate1[:, i:i + 1], in1=fxt[b][:], op0=ALU.mult, op1=ALU.add),
                         reads=[("y1t", b), ("fxt", b)], writes=[("facc", c)])
                    S.op("dve", lambda e, b=b, c=c, i=i: e.scalar_tensor_tensor(out=acc[c][:], in0=y2t[b][:], scalar=gate2[:, i:i + 1], in1=acc[c][:], op0=ALU.mult, op1=ALU.add),
                         reads=[("y2t", b), ("facc", c)], writes=[("facc", c)])
                    S.op("act", lambda e, b=b, c=c: e.activation(out=junkf[:], in_=acc[c][:], func=AF.Square, accum_out=stf[:, c, 0:1]), reads=[("facc", c)], writes=[("fss", c)])
                    rstd_from_ss(stf[:, c, 0:1], D, stf[:, c, 1:2], stf[:, c, 2:3], ("fss", c), "frstd%d" % c)
                    S.op("dve", lambda e, b=b, c=c: e.scalar_tensor_tensor(out=ot[c][:], in0=acc[c][:], scalar=stf[:, c, 2:3], in1=g_fin_t[:], op0=ALU.mult, op1=ALU.mult),
                         reads=[("facc", c), "frstd%d" % c, "g_fin_t"], writes=[("fot", c)])
                    S.dma("sp", "fot%d" % c, lambda e, b=b, c=c, i=i: e.dma_start(out=out[i * 128:(i + 1) * 128, :], in_=ot[c][:]), reads=[("fot", c)])

                PF = 5
                for i in range(PF):
                    m3_load(i)
                for i in range(NTT):
                    if i + PF < NTT:
                        m3_load(i + PF)
                    m3_comp(i)
                S.barrier()

        if stop_after is not None:
            with contextlib.ExitStack() as pz:
                z = sbuf(pz, "z", [128, D], F32)
                S.op("pool", lambda e: e.memset(z[:], 0.0), writes=["z"])
                S.dma("sp", "zout", lambda e: e.dma_start(out=out[0:128, :], in_=z[:]), reads=["z"])
                S.barrier()
        S.barrier()
        S.emit()
    return nc


def _prep_inputs(inputs):
    f = lambda a: np.ascontiguousarray(np.asarray(a, dtype=np.float32))
    rope_cs, oh = _consts()
    shared = {
        "w_in": f(inputs["w_in"][0]),
        "rel_bias": f(inputs["rel_bias"]),
        "w_q_up": f(inputs["w_q_up"][0]),
        "w_kv_up": f(inputs["w_kv_up"][0]),
        "w_out": f(inputs["w_out"][0]),
        "w_rg": f(inputs["w_router_group"][0]),
        "w_re": f(inputs["w_router_expert"][0]),
        "w_gate": f(inputs["w_gate"][0]),
        "w_up": f(inputs["w_up"][0]),
        "w_down": f(inputs["w_down"][0]),
        "g_attn": f(inputs["g_attn_norm"][0]).reshape(1, D),
        "g_q": f(inputs["g_q_latent"][0]).reshape(1, 256),
        "g_kv": f(inputs["g_kv_latent"][0]).reshape(1, 128),
        "g_out": np.concatenate([f(inputs["g_out_a"][0]), f(inputs["g_out_b"][0])]).reshape(1, D),
        "g_ffn": f(inputs["g_ffn_norm"][0]).reshape(1, D),
        "g_fin": f(inputs["g_final"]).reshape(1, D),
        "b_r": np.concatenate([f(inputs["b_router_group"][0]), f(inputs["b_router_expert"][0])]).reshape(1, 36),
        "rope_cs": rope_cs,
        "oh_bias": oh,
    }
    xs = f(inputs["x"]).reshape(N_CORES, NTOK, D)
    return [dict(shared, x=xs[c]) for c in range(N_CORES)]


def kernel(**inputs):
    in_maps = _prep_inputs(inputs)
    nc = build_nc()
    res = run_bass_kernel_spmd(nc, in_maps, core_ids=list(range(N_CORES)))
    outs = [np.asarray(r["out"], dtype=np.float32).reshape(NSEQ, SEQ, D) for r in res.results]
    return np.concatenate(outs, axis=0)
```

```python
import contextlib
import math
import numpy as np
import concourse.bass as bass
import concourse.mybir as mybir
from concourse.bass_utils import run_bass_kernel_spmd

F32 = mybir.dt.float32
BF16 = mybir.dt.bfloat16
I32 = mybir.dt.int32
AF = mybir.ActivationFunctionType
ALU = mybir.AluOpType
AX = mybir.AxisListType

N_CORES = 8
SEQ = 2048
D = 1024
NSEQ = 2
NTOK = NSEQ * SEQ
NT = SEQ // 128
EPS = 1e-6
NEG = -30000.0
PATTERNS = (16, 4, 1)
N_EXP = 32
CAPB = 4
NROWS = N_EXP * CAPB * 128
K2C = 99


class Sync:
    COMPUTE = ("pe", "act", "dve", "pool")

    def __init__(self, nc, stack):
        self.nc = nc
        self.stack = stack
        self.ops = {e: [] for e in ("pe", "act", "dve", "pool", "sp")}
        self.sem = {}
        for e in self.COMPUTE:
            self.sem[e] = stack.enter_context(nc.semaphore("s_" + e))
        self.cnt = {e: 0 for e in self.COMPUTE}
        self.seen = {e: {} for e in self.ops}
        self.keys = {}
        self.pending = {e: ([], []) for e in self.COMPUTE}
        self.slots = {}

    def _key(self, k):
        st = self.keys.get(k)
        if st is None:
            st = {"w": None, "r": []}
            self.keys[k] = st
        return st

    def _need(self, eng, reads, writes, is_dma=False):
        need = {}

        def add(p):
            if p is None:
                return
            s, v, src = p
            if eng == "pe" and src == "pe":
                return
            if need.get(id(s), (None, -1))[1] < v:
                need[id(s)] = (s, v)

        for k in reads:
            st = self._key(k)
            add(st["w"])
            if isinstance(k, tuple) and k and k[0] == "ps":
                for p in st["r"]:
                    if p[2] != eng:
                        add(p)
        for k in writes:
            st = self._key(k)
            if st["w"] is not None and (is_dma or st["w"][2] != eng):
                add(st["w"])
            for p in st["r"]:
                if is_dma or p[2] != eng:
                    add(p)
        out = []
        seen = self.seen[eng]
        for sid, (s, v) in need.items():
            if seen.get(sid, -1) >= v:
                continue
            seen[sid] = v
            out.append((s, v))
        return out

    def _commit(self, reads, writes, prod):
        for k in writes:
            st = self._key(k)
            st["w"] = prod
            st["r"] = []
        for k in reads:
            if k in writes:
                continue
            st = self._key(k)
            st["r"] = [p for p in st["r"] if p[0] is not prod[0]] + [prod]

    def op(self, eng, fn, reads=(), writes=(), inc=True):
        reads, writes = list(reads), list(writes)
        waits = self._need(eng, reads, writes)
        if inc:
            self.cnt[eng] += 1
            prod = (self.sem[eng], self.cnt[eng], eng)
            pr, pw = self.pending[eng]
            self._commit(reads + pr, writes + pw, prod)
            self.pending[eng] = ([], [])
            self.ops[eng].append((waits, fn, (self.sem[eng], 1)))
        else:
            pr, pw = self.pending[eng]
            pr.extend(reads)
            pw.extend(writes)
            self.ops[eng].append((waits, fn, None))

    def dma(self, q, slot, fn, reads=(), writes=()):
        reads, writes = list(reads), list(writes)
        if slot not in self.slots:
            s = self.stack.enter_context(self.nc.semaphore("d_" + slot))
            self.slots[slot] = [s, 0]
        sl = self.slots[slot]
        waits = self._need(q, reads, writes, is_dma=True)
        sl[1] += 16
        self._commit(reads, writes, (sl[0], sl[1], "dma"))
        self.ops[q].append((waits, fn, (sl[0], 16)))

    def barrier(self):
        targets = [(self.sem[e], self.cnt[e]) for e in self.COMPUTE if self.cnt[e] > 0]
        targets += [(s, v) for (s, v) in self.slots.values() if v > 0]
        for e in self.ops:
            waits = []
            seen = self.seen[e]
            for s, v in targets:
                if seen.get(id(s), -1) >= v:
                    continue
                seen[id(s)] = v
                waits.append((s, v))
            if waits:
                self.ops[e].append((waits, None, None))
        self.keys = {}
        self.pending = {e: ([], []) for e in self.COMPUTE}

    def emit(self):
        nc = self.nc
        ops = self.ops

        def run(e, lst):
            for waits, fn, inc in lst:
                for s, v in waits:
                    e.wait_ge(s, v)
                if fn is not None:
                    ins = fn(e)
                    if inc is not None:
                        ins.then_inc(inc[0], inc[1])

        with nc.Block() as block:
            @block.sync
            def _(e):
                run(e, ops["sp"])

            @block.tensor
            def _(e):
                run(e, ops["pe"])

            @block.scalar
            def _(e):
                run(e, ops["act"])

            @block.vector
            def _(e):
                run(e, ops["dve"])

            @block.gpsimd
            def _(e):
                run(e, ops["pool"])


def _t5_bucket(rel):
    half = 16
    max_exact = 8
    n = np.abs(rel)
    large = max_exact + (np.log(np.maximum(n, 1) / max_exact)
                         / math.log(1024 / max_exact) * (half - max_exact)).astype(np.int32)
    large = np.minimum(large, half - 1)
    return (np.where(rel > 0, half, 0) + np.where(n < max_exact, n, large)).astype(np.int32)


def _consts():
    half = 16
    inv_freq = (np.float32(10000.0) ** (-(np.arange(half, dtype=np.float32) / np.float32(half)))).astype(np.float32)
    ang = (np.arange(SEQ, dtype=np.float32)[:, None] * inv_freq[None, :]).astype(np.float32)
    cos, sin = np.cos(ang).astype(np.float32), np.sin(ang).astype(np.float32)
    rope_cs = np.concatenate([cos, cos, sin], axis=1).astype(np.float32)
    oh = np.zeros((33, 3, 512), np.float32)
    for di, d in enumerate(PATTERNS):
        m = np.arange(512)
        delta = m - 255
        valid = np.abs(delta) <= 64
        b = _t5_bucket(delta * d)
        for mm in range(512):
            if valid[mm]:
                oh[b[mm], di, mm] = 1.0
            else:
                oh[32, di, mm] = 1.0
    return rope_cs, oh.reshape(33, 1536)


def build_nc(dbg=False, stop_after=None):
    nc = bass.Bass("TRN2", target_bir_lowering=False)

    def din(name, shape, dt=F32):
        return nc.dram_tensor(name, list(shape), dt, kind="ExternalInput").ap()

    def dscr(name, shape, dt, expose=False):
        kind = "ExternalOutput" if (dbg and expose) else "Internal"
        return nc.dram_tensor(name, list(shape), dt, kind=kind).ap()

    x = din("x", [NTOK, D])
    w_in = din("w_in", [D, 1952])
    rel_bias = din("rel_bias", [32, 8])
    w_q_up = din("w_q_up", [256, 768])
    w_kv_up = din("w_kv_up", [128, 1024])
    w_out = din("w_out", [D, D])
    w_rg = din("w_rg", [D, 4])
    w_re = din("w_re", [D, 32])
    w_gate = din("w_gate", [N_EXP, D, 256])
    w_up = din("w_up", [N_EXP, D, 256])
    w_down = din("w_down", [N_EXP, 256, D])
    g_attn = din("g_attn", [1, D])
    g_q = din("g_q", [1, 256])
    g_kv = din("g_kv", [1, 128])
    g_out = din("g_out", [1, D])
    g_ffn = din("g_ffn", [1, D])
    g_fin = din("g_fin", [1, D])
    b_r = din("b_r", [1, 36])
    rope_cs = din("rope_cs", [SEQ, 48])
    oh_bias = din("oh_bias", [33, 1536])
    out = nc.dram_tensor("out", [NTOK, D], F32, kind="ExternalOutput").ap()

    va_d = dscr("va_d", [NTOK, 520], BF16)
    oa_d = dscr("oa_d", [2, NTOK, 520], F32)
    mixb_d = dscr("mixb_d", [NTOK, 512], BF16, expose=True)
    mixa_d = dscr("mixa_d", [NTOK, 512], BF16, expose=True) if dbg else None
    x1_d = dscr("x1_d", [NTOK, D], F32, expose=True)
    fvec_d = dscr("fvec_d", [8, 1536], F32)
    xs_d = dscr("xs_d", [NROWS + 128, D], BF16)
    ys_d = dscr("ys_d", [NROWS + 128, D], BF16)

    def bcast_rows(ap, n):
        return bass.AP(tensor=ap.tensor, offset=0, ap=[[0, 128], [1, n]])

    with contextlib.ExitStack() as st:
        S = Sync(nc, st)

        uniq = [0]

        def sbuf(stk, name, shape, dt):
            uniq[0] += 1
            return stk.enter_context(nc.sbuf_tensor("%s_%d" % (name, uniq[0]), list(shape), dt))

        psbig = [st.enter_context(nc.psum_tensor("psb%d" % i, [128, 1024], F32)) for i in range(4)]
        bank = [psbig[i // 2][:, (i % 2) * 512:(i % 2 + 1) * 512] for i in range(8)]

        def PS(i):
            return ("ps", i)

        def bank_bf(i):
            return bank[i][:].bitcast(BF16)

        ident_f = sbuf(st, "ident_f", [128, 128], F32)
        ident_b = sbuf(st, "ident_b", [128, 128], BF16)
        jrev_f = sbuf(st, "jrev_f", [128, 128], F32)
        jrev_b = sbuf(st, "jrev_b", [128, 128], BF16)
        ones_f = sbuf(st, "ones_f", [128, 128], F32)
        ltri_f = sbuf(st, "ltri_f", [128, 128], F32)
        eps_t = sbuf(st, "eps_t", [128, 1], F32)
        rope_t = sbuf(st, "rope_t", [128, NT, 48], F32)
        wq_b = sbuf(st, "wq_b", [128, 2, 768], BF16)
        wkv_b = sbuf(st, "wkv_b", [128, 1024], BF16)
        wr_f = sbuf(st, "wr_f", [128, 8, 36], F32)
        g_q_t = sbuf(st, "g_q_t", [128, 256], F32)
        g_kv_t = sbuf(st, "g_kv_t", [128, 128], F32)
        g_out_t = sbuf(st, "g_out_t", [128, D], F32)

        S.op("pool", lambda e: e.memset(ident_f[:], 1.0), writes=["ident_f"])
        S.op("pool", lambda e: e.affine_select(out=ident_f[:], in_=ident_f[:], pattern=[[-1, 128]],
                                               compare_op=ALU.is_equal, fill=0.0, base=0,
                                               channel_multiplier=1), reads=["ident_f"], writes=["ident_f"])
        S.op("pool", lambda e: e.memset(jrev_f[:], 1.0), writes=["jrev_f"])
        S.op("pool", lambda e: e.affine_select(out=jrev_f[:], in_=jrev_f[:], pattern=[[1, 128]],
                                               compare_op=ALU.is_equal, fill=0.0, base=-127,
                                               channel_multiplier=1), reads=["jrev_f"], writes=["jrev_f"])
        S.op("pool", lambda e: e.memset(ones_f[:], 1.0), writes=["ones_f"])
        S.op("pool", lambda e: e.memset(ltri_f[:], 1.0), writes=["ltri_f"])
        S.op("pool", lambda e: e.affine_select(out=ltri_f[:], in_=ltri_f[:], pattern=[[1, 128]],
                                               compare_op=ALU.is_ge, fill=0.0, base=-1,
                                               channel_multiplier=-1), reads=["ltri_f"], writes=["ltri_f"])
        S.op("pool", lambda e: e.memset(eps_t[:], EPS), writes=["eps_t"])
        S.op("dve", lambda e: e.tensor_copy(out=ident_b[:], in_=ident_f[:]), reads=["ident_f"], writes=["ident_b"])
        S.op("dve", lambda e: e.tensor_copy(out=jrev_b[:], in_=jrev_f[:]), reads=["jrev_f"], writes=["jrev_b"])

        S.dma("sp", "c_rope", lambda e: e.dma_start(out=rope_t[:], in_=rope_cs.rearrange("(t p) c -> p t c", p=128)),
              writes=["rope_t"])
        S.dma("pool", "c_wq", lambda e: e.dma_start(out=wq_b[:], in_=w_q_up.rearrange("(c p) n -> p c n", p=128)),
              writes=["wq_b"])
        S.dma("pool", "c_wkv", lambda e: e.dma_start(out=wkv_b[:], in_=w_kv_up), writes=["wkv_b"])
        with nc.allow_non_contiguous_dma(reason="tiny router weights"):
            S.dma("sp", "c_wr", lambda e: e.dma_start(out=wr_f[:, :, 0:4], in_=w_rg.rearrange("(k p) n -> p k n", p=128)),
                  writes=["wr_f"])
            S.dma("sp", "c_wr", lambda e: e.dma_start(out=wr_f[:, :, 4:36], in_=w_re.rearrange("(k p) n -> p k n", p=128)),
                  writes=["wr_f"])
        S.dma("sp", "c_gq", lambda e: e.dma_start(out=g_q_t[:], in_=bcast_rows(g_q, 256)), writes=["g_q_t"])
        S.dma("sp", "c_gkv", lambda e: e.dma_start(out=g_kv_t[:], in_=bcast_rows(g_kv, 128)), writes=["g_kv_t"])
        S.dma("sp", "c_gout", lambda e: e.dma_start(out=g_out_t[:], in_=bcast_rows(g_out, D)), writes=["g_out_t"])

        with contextlib.ExitStack() as s0:
            relb = sbuf(s0, "relb", [33, 8], F32)
            oh_t = sbuf(s0, "oh_t", [33, 1536], F32)
            fvec_sb = sbuf(s0, "fvec_sb", [8, 1536], F32)
            S.op("pool", lambda e: e.memset(relb[32:33, :], NEG), writes=["relb32"])
            S.dma("sp", "c_relb", lambda e: e.dma_start(out=relb[0:32, :], in_=rel_bias), writes=["relb"])
            S.dma("sp", "c_oh", lambda e: e.dma_start(out=oh_t[:], in_=oh_bias), writes=["oh_t"])
            for di in range(3):
                S.op("pe", lambda e, di=di: e.matmul(bank[di][0:8, :], lhsT=relb[:, :], rhs=oh_t[:, di * 512:(di + 1) * 512],
                                                      start=True, stop=True),
                     reads=["relb", "relb32", "oh_t"], writes=[PS(di)])
                S.op("dve", lambda e, di=di: e.tensor_copy(out=fvec_sb[:, di * 512:(di + 1) * 512], in_=bank[di][0:8, :]),
                     reads=[PS(di)], writes=["fvec_sb"])
            S.dma("sp", "c_fv", lambda e: e.dma_start(out=fvec_d, in_=fvec_sb[:]), reads=["fvec_sb"], writes=["fvec_d"])
            S.barrier()

        def rstd_from_ss(ss_ap, n, lnv_ap, rstd_ap, rk, wk):
            S.op("act", lambda e: e.activation(out=lnv_ap, in_=ss_ap, func=AF.Ln, scale=1.0 / n, bias=eps_t[:, 0:1]),
                 reads=[rk, "eps_t"], writes=[wk + "_ln"])
            S.op("act", lambda e: e.activation(out=rstd_ap, in_=lnv_ap, func=AF.Exp, scale=-0.5),
                 reads=[wk + "_ln"], writes=[wk])

        wst = contextlib.ExitStack()
        w_in_b = sbuf(wst, "w_in_b", [128, 8, 1952], BF16)
        for k in range(8):
            S.dma("pool", "w_in", lambda e, k=k: e.dma_start(out=w_in_b[:, k, :], in_=w_in[k * 128:(k + 1) * 128, :]), writes=["w_in_b"])

        def seq_body(s):
            r0 = s * SEQ
            with contextlib.ExitStack() as sq:
                qaT = sbuf(sq, "qaT", [128, 4, SEQ], BF16)
                kaT = sbuf(sq, "kaT", [128, 4, SEQ], BF16)
                sqB = contextlib.ExitStack()
                cqnT = sbuf(sqB, "cqnT", [128, 2, SEQ], BF16)
                ckvnT = sbuf(sqB, "ckvnT", [128, SEQ], BF16)
                kbT = sbuf(sqB, "kbT", [96, 8, SEQ], BF16)
                Vb = sbuf(sqB, "Vb", [128, NT, 8, 65], BF16)
                S.op("pool", lambda e: e.memset(Vb[:, :, :, 64:65], 1.0), writes=["Vb_ones"])

                with contextlib.ExitStack() as p1:
                    g_attn_t = sbuf(p1, "g_attn_t", [128, D], F32)
                    hT = sbuf(p1, "hT", [128, 8, SEQ], BF16)
                    xt = [sbuf(p1, "xt%d" % i, [128, D], F32) for i in range(3)]
                    hb = [sbuf(p1, "hb%d" % i, [128, D], BF16) for i in range(3)]
                    junk = sbuf(p1, "junk", [128, 384], BF16)
                    st1 = sbuf(p1, "st1", [128, 3, 8], F32)
                    vt = [sbuf(p1, "vt%d" % i, [128, 8, 65], BF16) for i in range(2)]
                    cqn = [sbuf(p1, "cqn%d" % i, [128, 256], BF16) for i in range(3)]
                    ckvn = [sbuf(p1, "ckvn%d" % i, [128, 128], BF16) for i in range(3)]
                    ks = [sbuf(p1, "ks%d" % i, [128, 8, 96], BF16) for i in range(3)]
                    krs = [sbuf(p1, "krs%d" % i, [128, 32], F32) for i in range(3)]
                    rtmp = [sbuf(p1, "rtmp%d" % i, [128, 64], F32) for i in range(3)]

                    S.dma("sp", "gattn", lambda e: e.dma_start(out=g_attn_t[:], in_=bcast_rows(g_attn, D)), writes=["g_attn_t"])
                    for i in range(2):
                        S.op("pool", lambda e, i=i: e.memset(vt[i][:, :, 64:65], 1.0), writes=[("vt1", i)])

                    for t in range(NT):
                        b = t % 3
                        S.dma("sp", "xt%d" % b, lambda e, b=b, t=t: e.dma_start(out=xt[b][:], in_=x[r0 + t * 128:r0 + (t + 1) * 128, :]),
                              writes=[("xt", b)])
                        S.op("act", lambda e, b=b: e.activation(out=hb[b][:], in_=xt[b][:], func=AF.Square, accum_out=st1[:, b, 0:1]),
                             reads=[("xt", b)], writes=[("ss", b), ("hb", b)])
                        rstd_from_ss(st1[:, b, 0:1], D, st1[:, b, 1:2], st1[:, b, 2:3], ("ss", b), "rstd%d" % b)
                        S.op("dve", lambda e, b=b: e.scalar_tensor_tensor(out=hb[b][:], in0=xt[b][:], scalar=st1[:, b, 2:3], in1=g_attn_t[:],
                                                                        op0=ALU.mult, op1=ALU.mult),
                             reads=[("xt", b), "rstd%d" % b, "g_attn_t"], writes=[("hb", b)])
                        pb = t % 2
                        pv = bank_bf(pb).rearrange("p (k t) -> p k t", k=8)
                        for k in range(8):
                            S.op("pe", lambda e, k=k, b=b, pv=pv: e.transpose(out=pv[:, k, :], in_=hb[b][:, k * 128:(k + 1) * 128], identity=ident_b[:]),
                                 reads=[("hb", b), "ident_b"], writes=[PS(pb)], inc=(k == 7))
                        eng = "act" if t % 2 == 0 else "dve"
                        if eng == "act":
                            S.op("act", lambda e, t=t, pv=pv: e.activation(out=hT[:, :, t * 128:(t + 1) * 128], in_=pv, func=AF.Copy),
                                 reads=[PS(pb)], writes=[("hT", t)])
                        else:
                            S.op("dve", lambda e, t=t, pv=pv: e.tensor_copy(out=hT[:, :, t * 128:(t + 1) * 128], in_=pv),
                                 reads=[PS(pb)], writes=[("hT", t)])

                    rot = [2, 3, 4, 5]
                    rc = [0]
                    sub = {"P1a": 0, "P1b": 1, "P1c": 2, "P1d": 3}.get(stop_after, 9)

                    def nextbank():
                        bk = rot[rc[0] % len(rot)]
                        rc[0] += 1
                        return bk

                    evc = [0]

                    def evac(out_ap, in_ap, reads, writes, scale=1.0):
                        evc[0] += 1
                        if evc[0] % 2 == 0:
                            S.op("act", lambda e: e.activation(out=out_ap, in_=in_ap, func=AF.Copy, scale=scale), reads=reads, writes=writes)
                        else:
                            S.op("dve", lambda e: e.tensor_scalar(out=out_ap, in0=in_ap, scalar1=scale, scalar2=None, op0=ALU.mult),
                                 reads=reads, writes=writes)

                    for c in range(8 if sub >= 1 else 0):
                        for j in range(4):
                            bk = nextbank()
                            for k in range(8):
                                S.op("pe", lambda e, c=c, j=j, k=k, bk=bk: e.matmul(bank[bk][:], lhsT=w_in_b[:, k, c * 128:(c + 1) * 128],
                                                                                    rhs=hT[:, k, j * 512:(j + 1) * 512], start=(k == 0), stop=(k == 7)),
                                     reads=["w_in_b"] + [("hT", 4 * j + i) for i in range(4)], writes=[PS(bk)], inc=(k == 7))
                            if c < 4:
                                evac(qaT[:, c, j * 512:(j + 1) * 512], bank[bk][:], [PS(bk)], [("qaT", c, j)], scale=0.125)
                            else:
                                evac(kaT[:, c - 4, j * 512:(j + 1) * 512], bank[bk][:], [PS(bk)], [("kaT", c - 4, j)])
                    for t in range(NT if sub >= 2 else 0):
                        b = t % 2
                        bk = nextbank()
                        for k in range(8):
                            S.op("pe", lambda e, t=t, k=k, bk=bk: e.matmul(bank[bk][:], lhsT=hT[:, k, t * 128:(t + 1) * 128],
                                                                            rhs=w_in_b[:, k, 1024:1536], start=(k == 0), stop=(k == 7)),
                                 reads=["w_in_b", ("hT", t)], writes=[PS(bk)], inc=(k == 7))
                        evac(vt[b][:, :, 0:64], bank[bk][:].rearrange("p (h c) -> p h c", h=8), [PS(bk), ("vt1", b)], [("vt", b)])
                        S.dma("sp", "vt%d" % b, lambda e, b=b, t=t: e.dma_start(out=va_d[r0 + t * 128:r0 + (t + 1) * 128, :],
                                                                                  in_=vt[b][:].rearrange("p h c -> p (h c)")),
                              reads=[("vt", b)], writes=[("va_d", t)])
                    for t in range((NT if K2C >= 99 else 1) if sub >= 3 else 0):
                        b = t % 3
                        bk = nextbank()
                        if K2C >= 1:
                            for k in range(8):
                                S.op("pe", lambda e, t=t, k=k, bk=bk: e.matmul(bank[bk][:, 0:416], lhsT=hT[:, k, t * 128:(t + 1) * 128],
                                                                                rhs=w_in_b[:, k, 1536:1952], start=(k == 0), stop=(k == 7)),
                                     reads=["w_in_b", ("hT", t)], writes=[PS(bk)], inc=(k == 7))
                        if K2C >= 2:
                            S.op("act", lambda e, b=b, bk=bk: e.activation(out=junk[:, 0:256], in_=bank[bk][:, 0:256], func=AF.Square,
                                                                            accum_out=st1[:, b, 3:4]), reads=[PS(bk)], writes=[("ssq", b)])
                        if K2C >= 2:
                            S.op("act", lambda e, b=b, bk=bk: e.activation(out=junk[:, 256:384], in_=bank[bk][:, 256:384], func=AF.Square,
                                                                            accum_out=st1[:, b, 5:6]), reads=[PS(bk)], writes=[("sskv", b)])
                        if K2C >= 3:
                            rstd_from_ss(st1[:, b, 3:4], 256, st1[:, b, 4:5], st1[:, b, 4:5], ("ssq", b), "rq%d" % b)
                        if K2C >= 3:
                            rstd_from_ss(st1[:, b, 5:6], 128, st1[:, b, 6:7], st1[:, b, 6:7], ("sskv", b), "rkv%d" % b)
                        if K2C >= 4:
                            S.op("dve", lambda e, b=b, bk=bk: e.scalar_tensor_tensor(out=cqn[b][:], in0=bank[bk][:, 0:256], scalar=st1[:, b, 4:5],
                                                                                      in1=g_q_t[:], op0=ALU.mult, op1=ALU.mult),
                                 reads=[PS(bk), "rq%d" % b, "g_q_t"], writes=[("cqn", b)])
                        if K2C >= 4:
                            S.op("dve", lambda e, b=b, bk=bk: e.scalar_tensor_tensor(out=ckvn[b][:], in0=bank[bk][:, 256:384], scalar=st1[:, b, 6:7],
                                                                                      in1=g_kv_t[:], op0=ALU.mult, op1=ALU.mult),
                                 reads=[PS(bk), "rkv%d" % b, "g_kv_t"], writes=[("ckvn", b)])
                        if K2C >= 5:
                            S.op("dve", lambda e, b=b, bk=bk: e.tensor_copy(out=krs[b][:], in_=bank[bk][:, 384:416]),
                                 reads=[PS(bk)], writes=[("krs", b)])
                        if K2C >= 5:
                            S.op("dve", lambda e, b=b, t=t: e.tensor_tensor(out=rtmp[b][:, 0:32], in0=krs[b][:], in1=rope_t[:, t, 0:32], op=ALU.mult),
                                 reads=[("krs", b), "rope_t"], writes=[("rtA", b)])
                        if K2C >= 5:
                            S.op("dve", lambda e, b=b, t=t: e.tensor_tensor(out=rtmp[b][:, 32:48], in0=krs[b][:, 16:32], in1=rope_t[:, t, 32:48], op=ALU.mult),
                                 reads=[("krs", b), "rope_t"], writes=[("rtB", b)])
                        if K2C >= 5:
                            S.op("dve", lambda e, b=b, t=t: e.tensor_tensor(out=rtmp[b][:, 48:64], in0=krs[b][:, 0:16], in1=rope_t[:, t, 32:48], op=ALU.mult),
                                 reads=[("krs", b), "rope_t"], writes=[("rtC", b)])
                        S.op("dve", lambda e, b=b: e.tensor_tensor(out=ks[b][:, :, 64:80], in0=rtmp[b][:, 0:16].unsqueeze(1).broadcast_to([128, 8, 16]),
                                                                    in1=rtmp[b][:, 32:48].unsqueeze(1).broadcast_to([128, 8, 16]), op=ALU.subtract),
                             reads=[("rtA", b), ("rtB", b)], writes=[("ks_r", b, 0)])
                        S.op("dve", lambda e, b=b: e.tensor_tensor(out=ks[b][:, :, 80:96], in0=rtmp[b][:, 16:32].unsqueeze(1).broadcast_to([128, 8, 16]),
                                                                    in1=rtmp[b][:, 48:64].unsqueeze(1).broadcast_to([128, 8, 16]), op=ALU.add),
                             reads=[("rtA", b), ("rtC", b)], writes=[("ks_r", b, 1)])
                        pb = t % 2
                        pv = bank_bf(pb).rearrange("p (k t) -> p k t", k=8)
                        S.op("pe", lambda e, b=b, pv=pv: e.transpose(out=pv[:, 0, :], in_=cqn[b][:, 0:128], identity=ident_b[:]),
                             reads=[("cqn", b), "ident_b"], writes=[PS(pb)], inc=False)
                        S.op("pe", lambda e, b=b, pv=pv: e.transpose(out=pv[:, 1, :], in_=cqn[b][:, 128:256], identity=ident_b[:]),
                             reads=[("cqn", b)], writes=[PS(pb)], inc=False)
                        S.op("pe", lambda e, b=b, pv=pv: e.transpose(out=pv[:, 2, :], in_=ckvn[b][:], identity=ident_b[:]),
                             reads=[("ckvn", b)], writes=[PS(pb)], inc=True)
                        S.op("act", lambda e, t=t, pv=pv: e.activation(out=cqnT[:, :, t * 128:(t + 1) * 128], in_=pv[:, 0:2, :], func=AF.Copy),
                             reads=[PS(pb)], writes=[("cqnT", t)])
                        S.op("dve", lambda e, t=t, pv=pv: e.tensor_copy(out=ckvnT[:, t * 128:(t + 1) * 128], in_=pv[:, 2, :]),
                             reads=[PS(pb)], writes=[("ckvnT", t)])
                        for half in range(2):
                            bk2 = nextbank()
                            S.op("pe", lambda e, t=t, half=half, bk2=bk2: e.matmul(bank[bk2][:], lhsT=ckvnT[:, t * 128:(t + 1) * 128],
                                                                                    rhs=wkv_b[:, half * 512:(half + 1) * 512], start=True, stop=True),
                                 reads=[("ckvnT", t), "wkv_b"], writes=[PS(bk2)])
                            kvv = bank[bk2][:].rearrange("p (h c) -> p h c", h=4)
                            S.op("act", lambda e, b=b, half=half, kvv=kvv: e.activation(out=ks[b][:, half * 4:(half + 1) * 4, 0:64], in_=kvv[:, :, 0:64], func=AF.Copy),
                                 reads=[PS(bk2)], writes=[("ks_n", b, half)])
                            S.op("dve", lambda e, t=t, half=half, kvv=kvv: e.tensor_copy(out=Vb[:, t, half * 4:(half + 1) * 4, 0:64], in_=kvv[:, :, 64:128]),
                                 reads=[PS(bk2), "Vb_ones"], writes=[("Vb", t, half)])
                        pb2 = 6 + t % 2
                        pv2 = bank_bf(pb2).rearrange("p (k t) -> p k t", k=8)
                        for h in range(8):
                            S.op("pe", lambda e, h=h, b=b, pv2=pv2: e.transpose(out=pv2[0:96, h, :], in_=ks[b][:, h, :], identity=ident_b[:]),
                                 reads=[("ks_n", b, 0), ("ks_n", b, 1), ("ks_r", b, 0), ("ks_r", b, 1), "ident_b"], writes=[PS(pb2)], inc=(h == 7))
                        S.op("act", lambda e, t=t, pv2=pv2: e.activation(out=kbT[:, :, t * 128:(t + 1) * 128], in_=pv2[0:96, :, :], func=AF.Copy),
                             reads=[PS(pb2)], writes=[("kbT", t)])
                    S.barrier()
                if stop_after in ("P1", "P1a", "P1b", "P1c", "P1d"):
                    sqB.close()
                    return

                with contextlib.ExitStack() as p3:
                    qbT = [sbuf(p3, "qbT%d" % i, [96, 8, 512], BF16) for i in range(2)]
                    qs = [sbuf(p3, "qs%d" % i, [128, 8, 96], BF16) for i in range(2)]
                    qr = [sbuf(p3, "qr%d" % i, [128, 8, 32], F32) for i in range(2)]
                    qtm = [sbuf(p3, "qtm%d" % i, [128, 8, 64], F32) for i in range(2)]
                    PT = [sbuf(p3, "PT%d" % i, [128, 1024], BF16) for i in range(3)]
                    ob = [sbuf(p3, "ob%d" % i, [128, 4, 8, 65], F32) for i in range(2)]
                    rl = sbuf(p3, "rl", [128, 8], F32)
                    onb = sbuf(p3, "onb", [128, 8, 64], F32)
                    junk3 = sbuf(p3, "junk3", [128, 512], BF16)
                    st3 = sbuf(p3, "st3", [128, 4], F32)
                    mb = [sbuf(p3, "mb%d" % i, [128, 512], BF16) for i in range(2)]

                    def mla_qproj(jq):
                        qb = jq % 2
                        for tt in range(4):
                            t = jq * 4 + tt
                            b2 = tt % 2
                            for half in range(2):
                                for c in range(2):
                                    S.op("pe", lambda e, half=half, c=c, t=t: e.matmul(bank[6 + half][:, 0:384], lhsT=cqnT[:, c, t * 128:(t + 1) * 128],
                                                                                       rhs=wq_b[:, c, half * 384:(half + 1) * 384], start=(c == 0), stop=(c == 1)),
                                         reads=["wq_b"], writes=[PS(6 + half)], inc=(c == 1))
                                pvh = bank[6 + half][:, 0:384].rearrange("p (h c) -> p h c", h=4)
                                S.op("dve", lambda e, half=half, b2=b2, pvh=pvh: e.tensor_copy(out=qs[b2][:, half * 4:(half + 1) * 4, 0:64], in_=pvh[:, :, 0:64]),
                                     reads=[PS(6 + half)], writes=[("qs_n", b2, half)])
                                S.op("dve", lambda e, half=half, b2=b2, pvh=pvh: e.tensor_copy(out=qr[b2][:, half * 4:(half + 1) * 4, :], in_=pvh[:, :, 64:96]),
                                     reads=[PS(6 + half)], writes=[("qr", b2, half)])
                            cos2 = rope_t[:, t, 0:32].unsqueeze(1).broadcast_to([128, 8, 32])
                            sin1 = rope_t[:, t, 32:48].unsqueeze(1).broadcast_to([128, 8, 16])
                            qrk = [("qr", b2, 0), ("qr", b2, 1)]
                            S.op("dve", lambda e, b2=b2, cos2=cos2: e.tensor_tensor(out=qtm[b2][:, :, 0:32], in0=qr[b2][:], in1=cos2, op=ALU.mult),
                                 reads=qrk, writes=[("qtA", b2)])
                            S.op("dve", lambda e, b2=b2, sin1=sin1: e.tensor_tensor(out=qtm[b2][:, :, 32:48], in0=qr[b2][:, :, 16:32], in1=sin1, op=ALU.mult),
                                 reads=qrk, writes=[("qtB", b2)])
                            S.op("dve", lambda e, b2=b2, sin1=sin1: e.tensor_tensor(out=qtm[b2][:, :, 48:64], in0=qr[b2][:, :, 0:16], in1=sin1, op=ALU.mult),
                                 reads=qrk, writes=[("qtC", b2)])
                            S.op("dve", lambda e, b2=b2: e.tensor_tensor(out=qs[b2][:, :, 64:80], in0=qtm[b2][:, :, 0:16], in1=qtm[b2][:, :, 32:48], op=ALU.subtract),
                                 reads=[("qtA", b2), ("qtB", b2)], writes=[("qs_r", b2, 0)])
                            S.op("dve", lambda e, b2=b2: e.tensor_tensor(out=qs[b2][:, :, 80:96], in0=qtm[b2][:, :, 16:32], in1=qtm[b2][:, :, 48:64], op=ALU.add),
                                 reads=[("qtA", b2), ("qtC", b2)], writes=[("qs_r", b2, 1)])
                            pv = bank_bf(6).rearrange("p (k t) -> p k t", k=8)
                            for h in range(8):
                                S.op("pe", lambda e, h=h, b2=b2, pv=pv: e.transpose(out=pv[0:96, h, :], in_=qs[b2][:, h, :], identity=ident_b[:]),
                                     reads=[("qs_n", b2, 0), ("qs_n", b2, 1), ("qs_r", b2, 0), ("qs_r", b2, 1)], writes=[PS(6)], inc=(h == 7))
                            S.op("dve", lambda e, qb=qb, tt=tt, pv=pv: e.tensor_copy(out=qbT[qb][:, :, tt * 128:(tt + 1) * 128], in_=pv[0:96, :, :]),
                                 reads=[PS(6)], writes=[("qbT", qb, tt)])

                    def mla_epilogue(jq):
                        obj = ob[jq % 2]
                        for qt in range(4):
                            t = jq * 4 + qt
                            m = qt % 2
                            S.op("dve", lambda e, qt=qt: e.reciprocal(out=rl[:], in_=obj[:, qt, :, 64]), reads=[("ob", jq % 2, h) for h in range(8)], writes=["rl"])
                            S.op("dve", lambda e, qt=qt: e.tensor_tensor(out=onb[:], in0=obj[:, qt, :, 0:64], in1=rl[:].unsqueeze(2).broadcast_to([128, 8, 64]), op=ALU.mult),
                                 reads=["rl"] + [("ob", jq % 2, h) for h in range(8)], writes=["onb"])
                            S.op("act", lambda e: e.activation(out=junk3[:], in_=onb[:].rearrange("p h c -> p (h c)"), func=AF.Square, accum_out=st3[:, 0:1]),
                                 reads=["onb"], writes=["ss3"])
                            rstd_from_ss(st3[:, 0:1], 512, st3[:, 1:2], st3[:, 2:3], "ss3", "rstd3")
                            S.op("dve", lambda e, m=m: e.scalar_tensor_tensor(out=mb[m][:], in0=onb[:].rearrange("p h c -> p (h c)"), scalar=st3[:, 2:3],
                                                                               in1=g_out_t[:, 512:1024], op0=ALU.mult, op1=ALU.mult),
                                 reads=["onb", "rstd3", "g_out_t"], writes=[("mb", m)])
                            S.dma("sp", "mb%d" % m, lambda e, m=m, t=t: e.dma_start(out=mixb_d[r0 + t * 128:r0 + (t + 1) * 128, :], in_=mb[m][:]),
                                  reads=[("mb", m)], writes=[("mixb_d", t)])

                    scale_b = 96.0 ** -0.5
                    NU = 8 * (NT // 2)
                    units = [(jq, h, ktp) for jq in range(4) for h in range(8) for ktp in range(NT // 2)]

                    def mla_S(g):
                        jq, h, ktp = units[g]
                        qb = jq % 2
                        pb = g % 2
                        for j in range(2):
                            kt = 2 * ktp + j
                            S.op("pe", lambda e, j=j, kt=kt: e.matmul(bank[2 * pb + j][:], lhsT=kbT[:, h, kt * 128:(kt + 1) * 128], rhs=qbT[qb][:, h, :],
                                                                      start=True, stop=True),
                                 reads=[("qbT", qb, i) for i in range(4)], writes=[PS(2 * pb + j)], inc=(j == 1))

                    def mla_PV(g):
                        jq, h, ktp = units[g]
                        pb = g % 2
                        pti = g % 3
                        pt = PT[pti]
                        accb = 4 + (h % 2)
                        accv = bank[accb][:, 0:260].rearrange("p (q c) -> p q c", q=4)
                        S.op("act", lambda e: e.activation(out=pt[:], in_=psbig[pb][:], func=AF.Exp, scale=scale_b),
                             reads=[PS(2 * pb), PS(2 * pb + 1)], writes=[("PT", pti)])
                        for j in range(2):
                            kt = 2 * ktp + j
                            for qt in range(4):
                                S.op("pe", lambda e, j=j, kt=kt, qt=qt: e.matmul(accv[:, qt, :], lhsT=pt[:, j * 512 + qt * 128:j * 512 + (qt + 1) * 128],
                                                                                  rhs=Vb[:, kt, h, :], start=(kt == 0 and qt == 0), stop=(kt == NT - 1),
                                                                                  skip_group_check=True),
                                     reads=[("PT", pti)], writes=[PS(accb)], inc=(j == 1 and qt == 3))
                        if ktp == NT // 2 - 1:
                            S.op("dve", lambda e: e.tensor_copy(out=ob[jq % 2][:, :, h, :], in_=accv), reads=[PS(accb)], writes=[("ob", jq % 2, h)])

                    mla_qproj(0)
                    mla_S(0)
                    for g in range(len(units)):
                        jq, m = g // NU, g % NU
                        if g + 1 < len(units):
                            mla_S(g + 1)
                        mla_PV(g)
                        if m == 10 and jq > 0:
                            mla_epilogue(jq - 1)
                        if m == 30 and jq < 3:
                            mla_qproj(jq + 1)
                    mla_epilogue(3)
                    S.barrier()
                if stop_after == "B":
                    sqB.close()
                    return

                sqB.close()
                with contextlib.ExitStack() as p4:
                    trev = sbuf(p4, "trev", [128, 3, 8, 384], BF16)
                    for di in range(3):
                        src = bass.AP(tensor=fvec_d.tensor, offset=di * 512, ap=[[1, 128], [1536, 8], [1, 384]])
                        S.dma("pool", "trev", lambda e, di=di, src=src: e.dma_start(out=trev[:, di, :, :], in_=src), writes=["trev"])
                    Vw = [sbuf(p4, "Vw%d" % i, [128, 4, 2, 520], BF16) for i in range(2)]
                    PTa = [sbuf(p4, "PTa%d" % i, [128, 512], BF16) for i in range(4)]
                    oa = [sbuf(p4, "oa%d" % i, [128, 4, 8, 65], F32) for i in range(2)]
                    o16 = [sbuf(p4, "o16_%d" % i, [128, 520], F32) for i in range(2)]
                    o4 = [sbuf(p4, "o4_%d" % i, [128, 520], F32) for i in range(2)]
                    ma_sb = sbuf(p4, "ma_sb", [128, NT, 512], BF16)
                    rl4 = sbuf(p4, "rl4", [128, 8], F32)
                    onb4 = sbuf(p4, "onb4", [128, 8, 64], F32)
                    junk4 = sbuf(p4, "junk4", [128, 512], BF16)
                    st4 = sbuf(p4, "st4", [128, 4], F32)
                    w_out_b = sbuf(p4, "w_out_b", [128, 8, D], BF16)
                    mbt = [sbuf(p4, "mbt%d" % i, [128, 512], BF16) for i in range(2)]
                    xt5 = [sbuf(p4, "xt5_%d" % i, [128, D], F32) for i in range(2)]
                    mixT = [sbuf(p4, "mixT%d" % i, [128, 8, 128], BF16) for i in range(2)]
                    x1t = [sbuf(p4, "x1t%d" % i, [128, D], F32) for i in range(2)]

                    for k in range(8):
                        S.dma("pool", "w_out", lambda e, k=k: e.dma_start(out=w_out_b[:, k, :], in_=w_out[k * 128:(k + 1) * 128, :]), writes=["w_out_b"])

                    unitsA = [(di, d, g, h) for di, d in enumerate(PATTERNS) for g in range(4) for h in range(8)]

                    def tiles_of(d, g):
                        L = SEQ // d
                        tps = L // 128
                        npc = 2 if L >= 256 else 1
                        tl = []
                        for qt in range(4):
                            qidx = g * 4 + qt
                            r, ti = qidx // tps, qidx % tps
                            wb = min(max(ti * 128 - 64, 0), L - 128 * npc)
                            tl.append((r, ti, wb))
                        return tl, npc

                    def A_S(n):
                        di, d, g, h = unitsA[n]
                        tiles, npc = tiles_of(d, g)
                        vb = (n // 8) % 2
                        if h == 0:
                            for qt in range(4):
                                r, ti, wb = tiles[qt]
                                src = bass.AP(tensor=va_d.tensor, offset=(r0 + r + d * wb) * 520,
                                              ap=[[d * 520, 128], [128 * d * 520, npc], [1, 520]])
                                S.dma("sp", "Vw%d_%d" % (vb, qt), lambda e, qt=qt, src=src: e.dma_start(out=Vw[vb][:, qt, 0:npc, :], in_=src),
                                      writes=[("Vw", vb, qt)])
                        pair, hb_ = h // 2, (h % 2) * 64
                        set_ = n % 2
                        for qt in range(4):
                            r, ti, wb = tiles[qt]
                            qs_ = r + d * ti * 128
                            qap = qaT[hb_:hb_ + 64, pair, qs_:qs_ + d * 127 + 1:d]
                            for pc in range(npc):
                                bk = 2 * set_ + pc
                                ks_ = r + d * (wb + pc * 128)
                                kap = kaT[hb_:hb_ + 64, pair, ks_:ks_ + d * 127 + 1:d]
                                S.op("pe", lambda e, bk=bk, qt=qt, kap=kap, qap=qap: e.matmul(bank[bk][:, qt * 128:(qt + 1) * 128], lhsT=kap, rhs=qap,
                                                                                             start=(qt == 0), stop=False, skip_group_check=True),
                                     writes=[PS(bk)], inc=False)
                        for qt in range(4):
                            r, ti, wb = tiles[qt]
                            for pc in range(npc):
                                bk = 2 * set_ + pc
                                j0 = wb + pc * 128 - ti * 128 + 128
                                S.op("pe", lambda e, bk=bk, qt=qt, j0=j0: e.matmul(bank[bk][:, qt * 128:(qt + 1) * 128],
                                                                                  lhsT=trev[:, di, h, j0:j0 + 128], rhs=jrev_b[:],
                                                                                  start=False, stop=True, skip_group_check=True),
                                     reads=["trev", "jrev_b"], writes=[PS(bk)], inc=(qt == 3))

                    def A_PV(n):
                        di, d, g, h = unitsA[n]
                        tiles, npc = tiles_of(d, g)
                        vb = (n // 8) % 2
                        set_ = n % 2
                        accb = 4 + (n % 2)
                        def merge_loads(qt):
                            t = g * 4 + qt
                            m = qt % 2
                            S.dma("pool", "o16_%d" % m, lambda e: e.dma_start(out=o16[m][:], in_=oa_d[0, r0 + t * 128:r0 + (t + 1) * 128, :]),
                                  reads=[("oa_dw", 0), ("oa_dw", 1)], writes=[("o16", m)])
                            S.dma("pool", "o4_%d" % m, lambda e: e.dma_start(out=o4[m][:], in_=oa_d[1, r0 + t * 128:r0 + (t + 1) * 128, :]),
                                  reads=[("oa_dw", 0), ("oa_dw", 1)], writes=[("o4", m)])

                        if h == 0 and d == 1:
                            merge_loads(0)
                            merge_loads(1)
                        if False:
                            for qt in range(4):
                                t = g * 4 + qt
                                m = qt % 2
                                S.dma("pool", "o16_%d" % m, lambda e, m=m, t=t: e.dma_start(out=o16[m][:], in_=oa_d[0, r0 + t * 128:r0 + (t + 1) * 128, :]),
                                      reads=[("oa_dw", 0), ("oa_dw", 1)], writes=[("o16", m)])
                                S.dma("pool", "o4_%d" % m, lambda e, m=m, t=t: e.dma_start(out=o4[m][:], in_=oa_d[1, r0 + t * 128:r0 + (t + 1) * 128, :]),
                                      reads=[("oa_dw", 0), ("oa_dw", 1)], writes=[("o4", m)])
                        for pc in range(npc):
                            bk = 2 * set_ + pc
                            S.op("act", lambda e, bk=bk: e.activation(out=PTa[bk][:], in_=bank[bk][:], func=AF.Exp),
                                 reads=[PS(bk)], writes=[("PTa", bk)])
                        accv = bank[accb][:, 0:260].rearrange("p (q c) -> p q c", q=4)
                        for qt in range(4):
                            for pc in range(npc):
                                bk = 2 * set_ + pc
                                S.op("pe", lambda e, bk=bk, qt=qt, pc=pc: e.matmul(
                                    accv[:, qt, :], lhsT=PTa[bk][:, qt * 128:(qt + 1) * 128], rhs=Vw[vb][:, qt, pc, h * 65:(h + 1) * 65],
                                    start=(pc == 0), stop=(pc == npc - 1), skip_group_check=True),
                                     reads=[("PTa", bk), ("Vw", vb, qt)], writes=[PS(accb)], inc=(qt == 3 and pc == npc - 1))
                        S.op("dve", lambda e: e.tensor_copy(out=oa[vb][:, :, h, :], in_=accv), reads=[PS(accb)], writes=[("oa", vb, h)])
                        if h != 7:
                            return
                        oak = [("oa", vb, hh) for hh in range(8)]
                        if d != 1:
                            for qt in range(4):
                                r, ti, wb = tiles[qt]
                                dst = bass.AP(tensor=oa_d.tensor, offset=(di * NTOK + r0 + r + d * ti * 128) * 520, ap=[[d * 520, 128], [1, 520]])
                                S.dma("sp", "oaw%d" % vb, lambda e, qt=qt, dst=dst: e.dma_start(out=dst, in_=oa[vb][:, qt].rearrange("p h c -> p (h c)")),
                                      reads=oak, writes=[("oa_dw", vb)])
                            return
                        for qt in range(4):
                            t = g * 4 + qt
                            m = qt % 2
                            S.op("dve", lambda e, m=m: e.tensor_tensor(out=o16[m][:], in0=o16[m][:], in1=o4[m][:], op=ALU.add),
                                 reads=[("o16", m), ("o4", m)], writes=[("o16", m)])
                            S.op("dve", lambda e, m=m, qt=qt: e.tensor_tensor(out=o16[m][:], in0=o16[m][:], in1=oa[vb][:, qt].rearrange("p h c -> p (h c)"), op=ALU.add),
                                 reads=[("o16", m)] + oak, writes=[("o16", m)])
                            ov = o16[m][:].rearrange("p (h c) -> p h c", h=8)
                            S.op("dve", lambda e, ov=ov: e.reciprocal(out=rl4[:], in_=ov[:, :, 64]), reads=[("o16", m)], writes=["rl4"])
                            S.op("dve", lambda e, ov=ov: e.tensor_tensor(out=onb4[:], in0=ov[:, :, 0:64], in1=rl4[:].unsqueeze(2).broadcast_to([128, 8, 64]), op=ALU.mult),
                                 reads=["rl4", ("o16", m)], writes=["onb4"])
                            if qt + 2 < 4:
                                merge_loads(qt + 2)
                            S.op("act", lambda e: e.activation(out=junk4[:], in_=onb4[:].rearrange("p h c -> p (h c)"), func=AF.Square, accum_out=st4[:, 0:1]),
                                 reads=["onb4"], writes=["ss4"])
                            rstd_from_ss(st4[:, 0:1], 512, st4[:, 1:2], st4[:, 2:3], "ss4", "rstd4")
                            S.op("dve", lambda e, t=t: e.scalar_tensor_tensor(out=ma_sb[:, t, :], in0=onb4[:].rearrange("p h c -> p (h c)"), scalar=st4[:, 2:3],
                                                                               in1=g_out_t[:, 0:512], op0=ALU.mult, op1=ALU.mult),
                                 reads=["onb4", "rstd4", "g_out_t"], writes=[("ma", t)])
                            if dbg:
                                S.dma("sp", "dbgma", lambda e, t=t: e.dma_start(out=mixa_d[r0 + t * 128:r0 + (t + 1) * 128, :], in_=ma_sb[:, t, :]),
                                      reads=[("ma", t)])

                    A_S(0)
                    for n in range(len(unitsA)):
                        if n + 1 < len(unitsA):
                            A_S(n + 1)
                        A_PV(n)
                    for t in range(NT):
                        b = t % 2
                        S.dma("sp", "mbt%d" % b, lambda e, b=b, t=t: e.dma_start(out=mbt[b][:], in_=mixb_d[r0 + t * 128:r0 + (t + 1) * 128, :]), writes=[("mbt", b)])
                        S.dma("sp", "xt5_%d" % b, lambda e, b=b, t=t: e.dma_start(out=xt5[b][:], in_=x[r0 + t * 128:r0 + (t + 1) * 128, :]), writes=[("xt5", b)])
                        pv = bank_bf(6).rearrange("p (k t) -> p k t", k=8)
                        for k in range(4):
                            S.op("pe", lambda e, k=k, t=t, pv=pv: e.transpose(out=pv[:, k, :], in_=ma_sb[:, t, k * 128:(k + 1) * 128], identity=ident_b[:]),
                                 reads=[("ma", t), "ident_b"], writes=[PS(6)], inc=False)
                        for k in range(4):
                            S.op("pe", lambda e, k=k, b=b, pv=pv: e.transpose(out=pv[:, 4 + k, :], in_=mbt[b][:, k * 128:(k + 1) * 128], identity=ident_b[:]),
                                 reads=[("mbt", b)], writes=[PS(6)], inc=(k == 3))
                        S.op("act", lambda e, b=b, pv=pv: e.activation(out=mixT[b][:], in_=pv, func=AF.Copy), reads=[PS(6)], writes=[("mixT", b)])
                        for half in range(2):
                            for k in range(8):
                                S.op("pe", lambda e, half=half, k=k, b=b: e.matmul(bank[half][:], lhsT=mixT[b][:, k, :], rhs=w_out_b[:, k, half * 512:(half + 1) * 512],
                                                                                  start=(k == 0), stop=(k == 7)),
                                     reads=[("mixT", b), "w_out_b"], writes=[PS(half)], inc=(k == 7))
                            S.op("dve", lambda e, half=half, b=b: e.tensor_tensor(out=x1t[b][:, half * 512:(half + 1) * 512], in0=bank[half][:],
                                                                                 in1=xt5[b][:, half * 512:(half + 1) * 512], op=ALU.add),
                                 reads=[PS(half), ("xt5", b)], writes=[("x1t", b, half)])
                        S.dma("pool", "x1t%d" % b, lambda e, b=b, t=t: e.dma_start(out=x1_d[r0 + t * 128:r0 + (t + 1) * 128, :], in_=x1t[b][:]),
                              reads=[("x1t", b, 0), ("x1t", b, 1)], writes=[("x1_d", s, t)])
                    S.barrier()


        for s_ in range(NSEQ if stop_after is None else (0 if stop_after == "P0" else 1)):
            seq_body(s_)
        wst.close()

        if stop_after is None:
          with contextlib.ExitStack() as pm:
            NTT = NTOK // 128
            g_ffn_t = sbuf(pm, "g_ffn_t", [128, D], F32)
            g_fin_t = sbuf(pm, "g_fin_t", [128, D], F32)
            b_r_t = sbuf(pm, "b_r_t", [128, 36], F32)
            h2b = sbuf(pm, "h2b", [128, NTT, D], BF16)
            gate1 = sbuf(pm, "gate1", [128, NTT], F32)
            gate2 = sbuf(pm, "gate2", [128, NTT], F32)
            d1i = sbuf(pm, "d1i", [128, NTT], I32)
            d2i = sbuf(pm, "d2i", [128, NTT], I32)
            S.dma("sp", "gffn", lambda e: e.dma_start(out=g_ffn_t[:], in_=bcast_rows(g_ffn, D)), writes=["g_ffn_t"])
            S.dma("sp", "gfin", lambda e: e.dma_start(out=g_fin_t[:], in_=bcast_rows(g_fin, D)), writes=["g_fin_t"])
            S.dma("sp", "brt", lambda e: e.dma_start(out=b_r_t[:], in_=bcast_rows(b_r, 36)), writes=["b_r_t"])
            zt = sbuf(pm, "zt", [128, D], BF16)
            S.op("pool", lambda e: e.memset(zt[:], 0.0), writes=["zt"])
            S.dma("sp", "zfilly", lambda e: e.dma_start(out=ys_d[NROWS:NROWS + 128, :], in_=zt[:]), reads=["zt"], writes=["ys_d"])
            xs_v = xs_d.rearrange("(n p) d -> p n d", p=128)
            NCH = (NROWS + 128) // 128
            for c0 in range(0, NCH, 16):
                c1 = min(c0 + 16, NCH)
                S.dma("sp", "zfill", lambda e, c0=c0, c1=c1: e.dma_start(out=xs_v[:, c0:c1, :], in_=zt[:].unsqueeze(1).broadcast_to([128, c1 - c0, D])),
                      reads=["zt"], writes=["xs_zf"])
            NW = 3
            wgu = [sbuf(pm, "wgu%d" % i, [128, 8, 512], BF16) for i in range(NW)]
            wd = [sbuf(pm, "wd%d" % i, [128, 2, D], BF16) for i in range(NW)]

            def load_w(ex):
                wi = ex % NW
                S.dma("pool", "wg%d" % wi, lambda e: e.dma_start(out=wgu[wi][:, :, 0:256], in_=w_gate[ex].rearrange("(k p) f -> p k f", p=128)),
                      writes=[("wgu", wi)])
                S.dma("pool", "wg%d" % wi, lambda e: e.dma_start(out=wgu[wi][:, :, 256:512], in_=w_up[ex].rearrange("(k p) f -> p k f", p=128)),
                      writes=[("wgu", wi)])
                S.dma("pool", "wd%d" % wi, lambda e: e.dma_start(out=wd[wi][:], in_=w_down[ex].rearrange("(c p) n -> p c n", p=128)),
                      writes=[("wd", wi)])

            for ex in range(NW):
                load_w(ex)
            with contextlib.ExitStack() as m1:
                mxt = [sbuf(m1, "mxt%d" % i, [128, D], F32) for i in range(2)]
                h2f = [sbuf(m1, "h2f%d" % i, [128, D], F32) for i in range(2)]
                h2lo = [sbuf(m1, "h2lo%d" % i, [128, D], BF16) for i in range(2)]
                hiT = [sbuf(m1, "hiT%d" % i, [128, 8, 128], BF16) for i in range(2)]
                loT = [sbuf(m1, "loT%d" % i, [128, 8, 128], BF16) for i in range(2)]
                lgt = [sbuf(m1, "lgt%d" % i, [128, 36], F32) for i in range(2)]
                wr2 = sbuf(m1, "wr2", [128, 8, 72], BF16)
                wrd = sbuf(m1, "wrd", [128, 8, 36], F32)
                S.op("dve", lambda e: e.tensor_copy(out=wr2[:, :, 0:36], in_=wr_f[:]), reads=["wr_f"], writes=["wr2a"])
                S.op("dve", lambda e: e.tensor_tensor(out=wrd[:], in0=wr_f[:], in1=wr2[:, :, 0:36], op=ALU.subtract), reads=["wr_f", "wr2a"], writes=["wrd"])
                S.op("dve", lambda e: e.tensor_copy(out=wr2[:, :, 36:72], in_=wrd[:]), reads=["wrd"], writes=["wr2"])
                junkm = sbuf(m1, "junkm", [128, D], BF16)
                stm = sbuf(m1, "stm", [128, 2, 4], F32)
                lg = sbuf(m1, "lg", [128, NTT, 36], F32)
                gmax = sbuf(m1, "gmax", [128, NTT], F32)
                gm = sbuf(m1, "gm", [128, NTT, 4], F32)
                gsh = sbuf(m1, "gsh", [128, NTT, 4], F32)
                gsum = sbuf(m1, "gsum", [128, NTT], F32)
                ggate = sbuf(m1, "ggate", [128, NTT], F32)
                t48 = sbuf(m1, "t48", [128, NTT, 4, 8], F32)
                ig = sbuf(m1, "ig", [128, NTT, 8], F32)
                ig2 = sbuf(m1, "ig2", [128, NTT, 8], F32)
                m1v = sbuf(m1, "m1v", [128, NTT], F32)
                m2v = sbuf(m1, "m2v", [128, NTT], F32)
                mask1 = sbuf(m1, "mask1", [128, NTT, 8], F32)
                mask2 = sbuf(m1, "mask2", [128, NTT, 8], F32)
                e2 = sbuf(m1, "e2", [128, NTT], F32)
                den = sbuf(m1, "den", [128, NTT], F32)
                OH1 = sbuf(m1, "OH1", [128, NTT, 4, 8], F32)
                OH2 = sbuf(m1, "OH2", [128, NTT, 4, 8], F32)
                OHs = sbuf(m1, "OHs", [128, NTT, 32], F32)
                cumT = sbuf(m1, "cumT", [128, NTT + 1, 32], F32)
                rank_all = sbuf(m1, "rank_all", [128, NTT, 32], F32)
                ebi = sbuf(m1, "ebi", [128, 32], I32)
                ebf = sbuf(m1, "ebf", [128, 32], F32)
                tsel = sbuf(m1, "tsel", [128, NTT, 32], F32)
                rsel = sbuf(m1, "rsel", [128, NTT], F32)
                esel = sbuf(m1, "esel", [128, NTT], F32)
                ovf = sbuf(m1, "ovf", [128, NTT], F32)

                def m1_tiles(t0, t1):
                  for i in range(t0, t1):
                    b = i % 2
                    S.dma("sp", "mxt%d" % b, lambda e, b=b, i=i: e.dma_start(out=mxt[b][:], in_=x1_d[i * 128:(i + 1) * 128, :]), writes=[("mxt", b)])
                    S.op("act", lambda e, b=b: e.activation(out=junkm[:], in_=mxt[b][:], func=AF.Square, accum_out=stm[:, b, 0:1]),
                         reads=[("mxt", b)], writes=[("mss", b)])
                    rstd_from_ss(stm[:, b, 0:1], D, stm[:, b, 1:2], stm[:, b, 2:3], ("mss", b), "mrstd%d" % b)
                    S.op("dve", lambda e, b=b: e.scalar_tensor_tensor(out=h2f[b][:], in0=mxt[b][:], scalar=stm[:, b, 2:3], in1=g_ffn_t[:], op0=ALU.mult, op1=ALU.mult),
                         reads=[("mxt", b), "mrstd%d" % b, "g_ffn_t"], writes=[("h2f", b)])
                    S.op("act", lambda e, b=b, i=i: e.activation(out=h2b[:, i, :], in_=h2f[b][:], func=AF.Copy), reads=[("h2f", b)], writes=[("h2b", i)])
                    S.op("dve", lambda e, b=b, i=i: e.tensor_tensor(out=h2lo[b][:], in0=h2f[b][:], in1=h2b[:, i, :], op=ALU.subtract),
                         reads=[("h2f", b), ("h2b", i)], writes=[("h2lo", b)])
                    pvh = bank_bf(0).rearrange("p (k t) -> p k t", k=8)
                    pvl = bank_bf(1).rearrange("p (k t) -> p k t", k=8)
                    for k in range(8):
                        S.op("pe", lambda e, k=k, i=i, pvh=pvh: e.transpose(out=pvh[:, k, :], in_=h2b[:, i, k * 128:(k + 1) * 128], identity=ident_b[:]),
                             reads=[("h2b", i), "ident_b"], writes=[PS(0)], inc=(k == 7))
                    S.op("act", lambda e, b=b, pvh=pvh: e.activation(out=hiT[b][:], in_=pvh, func=AF.Copy), reads=[PS(0)], writes=[("hiT", b)])
                    for k in range(8):
                        S.op("pe", lambda e, k=k, b=b, pvl=pvl: e.transpose(out=pvl[:, k, :], in_=h2lo[b][:, k * 128:(k + 1) * 128], identity=ident_b[:]),
                             reads=[("h2lo", b)], writes=[PS(1)], inc=(k == 7))
                    S.op("dve", lambda e, b=b, pvl=pvl: e.tensor_copy(out=loT[b][:], in_=pvl), reads=[PS(1)], writes=[("loT", b)])
                    lb = 2 + b
                    for k in range(8):
                        S.op("pe", lambda e, k=k, b=b, lb=lb: e.matmul(bank[lb][:, 0:72], lhsT=hiT[b][:, k, :], rhs=wr2[:, k, :], start=(k == 0), stop=False,
                                                                      skip_group_check=True),
                             reads=[("hiT", b), "wr2"], writes=[PS(lb)], inc=False)
                    for k in range(8):
                        S.op("pe", lambda e, k=k, b=b, lb=lb: e.matmul(bank[lb][:, 0:36], lhsT=loT[b][:, k, :], rhs=wr2[:, k, 0:36], start=False, stop=(k == 7),
                                                                      skip_group_check=True),
                             reads=[("loT", b), "wr2"], writes=[PS(lb)], inc=(k == 7))
                    S.op("dve", lambda e, b=b, lb=lb: e.tensor_tensor(out=lgt[b][:], in0=bank[lb][:, 36:72], in1=b_r_t[:], op=ALU.add),
                         reads=[PS(lb), "b_r_t"], writes=[("lgt", b)])
                    S.op("dve", lambda e, i=i, b=b, lb=lb: e.tensor_tensor(out=lg[:, i, :], in0=bank[lb][:, 0:36], in1=lgt[b][:], op=ALU.add),
                         reads=[PS(lb), ("lgt", b)], writes=["lg"])

                def m1_route(t0, t1):
                  nt = t1 - t0
                  gl = lg[:, t0:t1, 0:4]
                  el = lg[:, t0:t1, 4:36].rearrange("p t (g e) -> p t g e", g=4)

                  def bc(ap, shape, axis):
                    return ap.unsqueeze(axis).broadcast_to(shape)

                  V = lambda fn, reads, writes: S.op("dve", fn, reads=reads, writes=writes)
                  V(lambda e: e.tensor_reduce(out=gmax[:, t0:t1], in_=gl, op=ALU.max, axis=AX.X), ["lg"], ["gmax"])
                  V(lambda e: e.tensor_tensor(out=gm[:, t0:t1], in0=gl, in1=bc(gmax[:, t0:t1], [128, nt, 4], 2), op=ALU.is_equal), ["lg", "gmax"], ["gm"])
                  V(lambda e: e.tensor_tensor(out=gsh[:, t0:t1], in0=gl, in1=bc(gmax[:, t0:t1], [128, nt, 4], 2), op=ALU.subtract), ["lg", "gmax"], ["gsh"])
                  S.op("act", lambda e: e.activation(out=gsh[:, t0:t1], in_=gsh[:, t0:t1], func=AF.Exp), reads=["gsh"], writes=["gsh"])
                  V(lambda e: e.tensor_reduce(out=gsum[:, t0:t1], in_=gsh[:, t0:t1], op=ALU.add, axis=AX.X), ["gsh"], ["gsum"])
                  V(lambda e: e.reciprocal(out=ggate[:, t0:t1], in_=gsum[:, t0:t1]), ["gsum"], ["ggate"])
                  V(lambda e: e.tensor_tensor(out=t48[:, t0:t1], in0=el, in1=bc(gm[:, t0:t1], [128, nt, 4, 8], 3), op=ALU.mult), ["lg", "gm"], ["t48"])
                  V(lambda e: e.tensor_reduce(out=ig[:, t0:t1], in_=t48[:, t0:t1].rearrange("p t g e -> p t e g"), op=ALU.add, axis=AX.X), ["t48"], ["ig"])
                  V(lambda e: e.tensor_reduce(out=m1v[:, t0:t1], in_=ig[:, t0:t1], op=ALU.max, axis=AX.X), ["ig"], ["m1v"])
                  V(lambda e: e.tensor_tensor(out=mask1[:, t0:t1], in0=ig[:, t0:t1], in1=bc(m1v[:, t0:t1], [128, nt, 8], 2), op=ALU.is_equal), ["ig", "m1v"], ["mask1"])
                  V(lambda e: e.scalar_tensor_tensor(out=ig2[:, t0:t1].rearrange("p t e -> p (t e)"), in0=mask1[:, t0:t1].rearrange("p t e -> p (t e)"), scalar=-1e30,
                                                   in1=ig[:, t0:t1].rearrange("p t e -> p (t e)"), op0=ALU.mult, op1=ALU.add), ["mask1", "ig"], ["ig2"])
                  V(lambda e: e.tensor_reduce(out=m2v[:, t0:t1], in_=ig2[:, t0:t1], op=ALU.max, axis=AX.X), ["ig2"], ["m2v"])
                  V(lambda e: e.tensor_tensor(out=mask2[:, t0:t1], in0=ig2[:, t0:t1], in1=bc(m2v[:, t0:t1], [128, nt, 8], 2), op=ALU.is_equal), ["ig2", "m2v"], ["mask2"])
                  V(lambda e: e.tensor_tensor(out=e2[:, t0:t1], in0=m2v[:, t0:t1], in1=m1v[:, t0:t1], op=ALU.subtract), ["m1v", "m2v"], ["e2"])
                  S.op("act", lambda e: e.activation(out=e2[:, t0:t1], in_=e2[:, t0:t1], func=AF.Exp), reads=["e2"], writes=["e2"])
                  V(lambda e: e.tensor_scalar(out=den[:, t0:t1], in0=e2[:, t0:t1], scalar1=1.0, scalar2=None, op0=ALU.add), ["e2"], ["den"])
                  V(lambda e: e.reciprocal(out=den[:, t0:t1], in_=den[:, t0:t1]), ["den"], ["den"])
                  V(lambda e: e.tensor_tensor(out=gate1[:, t0:t1], in0=ggate[:, t0:t1], in1=den[:, t0:t1], op=ALU.mult), ["ggate", "den"], ["gate1"])
                  V(lambda e: e.tensor_tensor(out=gate2[:, t0:t1], in0=gate1[:, t0:t1], in1=e2[:, t0:t1], op=ALU.mult), ["gate1", "e2"], ["gate2"])
                  V(lambda e: e.tensor_tensor(out=OH1[:, t0:t1], in0=bc(gm[:, t0:t1], [128, nt, 4, 8], 3), in1=bc(mask1[:, t0:t1], [128, nt, 4, 8], 2), op=ALU.mult), ["gm", "mask1"], ["OH1"])
                  V(lambda e: e.tensor_tensor(out=OH2[:, t0:t1], in0=bc(gm[:, t0:t1], [128, nt, 4, 8], 3), in1=bc(mask2[:, t0:t1], [128, nt, 4, 8], 2), op=ALU.mult), ["gm", "mask2"], ["OH2"])
                  V(lambda e: e.tensor_tensor(out=OHs[:, t0:t1].rearrange("p t e -> p (t e)"), in0=OH1[:, t0:t1].rearrange("p t g e -> p (t g e)"),
                                            in1=OH2[:, t0:t1].rearrange("p t g e -> p (t g e)"), op=ALU.add), ["OH1", "OH2"], ["OHs"])
                  for i in range(t0, t1):
                    V(lambda e, i=i: e.tensor_tensor(out=cumT[:, i + 1, :], in0=cumT[:, i, :], in1=OHs[:, i, :], op=ALU.add), [("cumT", i), "OHs"], [("cumT", i + 1)])
                  for i in range(t0, t1):
                    rb = 4 + (i % 2)
                    S.op("pe", lambda e, i=i, rb=rb: e.matmul(bank[rb][:, 0:32], lhsT=ltri_f[:], rhs=OHs[:, i, :], start=True, stop=False),
                         reads=["ltri_f", "OHs"], writes=[PS(rb)], inc=False)
                    S.op("pe", lambda e, i=i, rb=rb: e.matmul(bank[rb][:, 0:32], lhsT=ones_f[:], rhs=cumT[:, i, :], start=False, stop=True),
                         reads=["ones_f", ("cumT", i)], writes=[PS(rb)], inc=True)
                    V(lambda e, i=i, rb=rb: e.tensor_copy(out=rank_all[:, i, :], in_=bank[rb][:, 0:32]), [PS(rb)], ["rank_all"])
                  for (OH, dst_i, nm) in ((OH1, d1i, "1"), (OH2, d2i, "2")):
                    ohf = OH[:, t0:t1].rearrange("p t g e -> p t (g e)")
                    V(lambda e, ohf=ohf: e.tensor_tensor(out=tsel[:, t0:t1], in0=rank_all[:, t0:t1], in1=ohf, op=ALU.mult), ["rank_all", "OH" + nm], ["tsel"])
                    V(lambda e: e.tensor_reduce(out=rsel[:, t0:t1], in_=tsel[:, t0:t1], op=ALU.add, axis=AX.X), ["tsel"], ["rsel"])
                    V(lambda e, ohf=ohf: e.tensor_tensor(out=tsel[:, t0:t1], in0=ohf, in1=bc(ebf[:], [128, nt, 32], 1), op=ALU.mult), ["ebf", "OH" + nm, "rsel"], ["tsel"])
                    V(lambda e: e.tensor_reduce(out=esel[:, t0:t1], in_=tsel[:, t0:t1], op=ALU.add, axis=AX.X), ["tsel"], ["esel"])
                    V(lambda e: e.tensor_scalar(out=ovf[:, t0:t1], in0=rsel[:, t0:t1], scalar1=float(CAPB * 128), scalar2=None, op0=ALU.is_lt), ["rsel"], ["ovf"])
                    V(lambda e: e.tensor_tensor(out=rsel[:, t0:t1], in0=rsel[:, t0:t1], in1=esel[:, t0:t1], op=ALU.add), ["rsel", "esel"], ["rsel"])
                    V(lambda e: e.scalar_tensor_tensor(out=rsel[:, t0:t1], in0=rsel[:, t0:t1], scalar=float(-NROWS), in1=ovf[:, t0:t1], op0=ALU.add, op1=ALU.mult), ["rsel", "ovf"], ["rsel"])
                    V(lambda e: e.tensor_scalar(out=rsel[:, t0:t1], in0=rsel[:, t0:t1], scalar1=float(NROWS), scalar2=None, op0=ALU.add), ["rsel"], ["rsel"])
                    V(lambda e, dst_i=dst_i: e.tensor_copy(out=dst_i[:, t0:t1], in_=rsel[:, t0:t1]), ["rsel"], ["dst" + nm])
                  for i in range(t0, t1):
                      for (dst_i, nm) in ((d1i, "1"), (d2i, "2")):
                          S.dma("pool", "scat" + nm, lambda e, i=i, dst_i=dst_i: e.indirect_dma_start(
                              out=xs_d, out_offset=bass.IndirectOffsetOnAxis(ap=dst_i[:, i:i + 1], axis=0), in_=h2b[:, i, :], in_offset=None),
                                reads=[("h2b", i), "dst" + nm, "xs_zf"], writes=[("xs_s", i, nm)])

                S.op("pool", lambda e: e.memset(cumT[:, 0, :], 0.0), writes=[("cumT", 0)])
                S.op("pool", lambda e: e.iota(ebi[:], pattern=[[CAPB * 128, 32]], base=0, channel_multiplier=0), writes=["ebi"])
                S.op("dve", lambda e: e.tensor_copy(out=ebf[:], in_=ebi[:]), reads=["ebi"], writes=["ebf"])
                NB1 = 4
                for jb in range(NB1):
                    m1_tiles(jb * (NTT // NB1), (jb + 1) * (NTT // NB1))
                    m1_route(jb * (NTT // NB1), (jb + 1) * (NTT // NB1))
                S.barrier()
            with contextlib.ExitStack() as m2:
                xblk = [sbuf(m2, "xblk%d" % i, [128, D], BF16) for i in range(8)]
                xT = [sbuf(m2, "xT%d" % i, [128, 8, 128], BF16) for i in range(3)]
                sg = [sbuf(m2, "sg%d" % i, [128, 256], F32) for i in range(2)]
                hblk = [sbuf(m2, "hblk%d" % i, [128, 256], BF16) for i in range(3)]
                hT2 = [sbuf(m2, "hT2_%d" % i, [128, 2, 128], BF16) for i in range(3)]
                yblk = [sbuf(m2, "yblk%d" % i, [128, D], BF16) for i in range(2)]
                NBLK = N_EXP * CAPB

                def P0(n):
                    xb = n % 8
                    row = n * 128
                    S.dma("sp", "xblk%d" % xb, lambda e: e.dma_start(out=xblk[xb][:], in_=xs_d[row:row + 128, :]), reads=["xs_d"], writes=[("xblk", xb)])

                def P1(n):
                    xb, pb, tb = n % 8, n % 2, n % 3
                    pv = bank_bf(pb).rearrange("p (k t) -> p k t", k=8)
                    for k in range(8):
                        S.op("pe", lambda e, k=k: e.transpose(out=pv[:, k, :], in_=xblk[xb][:, k * 128:(k + 1) * 128], identity=ident_b[:]),
                             reads=[("xblk", xb), "ident_b"], writes=[PS(pb)], inc=(k == 7))
                    S.op("act", lambda e: e.activation(out=xT[tb][:], in_=pv, func=AF.Copy), reads=[PS(pb)], writes=[("xT", tb)])

                def P2(n):
                    tb, gb, hb3, sb2 = n % 3, 2 + n % 2, n % 3, n % 2
                    wi = (n // CAPB) % NW
                    for k in range(8):
                        S.op("pe", lambda e, k=k: e.matmul(bank[gb][:], lhsT=xT[tb][:, k, :], rhs=wgu[wi][:, k, :], start=(k == 0), stop=(k == 7)),
                             reads=[("xT", tb), ("wgu", wi)], writes=[PS(gb)], inc=(k == 7))
                    S.op("act", lambda e: e.activation(out=sg[sb2][:], in_=bank[gb][:, 0:256], func=AF.Silu), reads=[PS(gb)], writes=[("sg", sb2)])
                    S.op("dve", lambda e: e.tensor_tensor(out=hblk[hb3][:], in0=sg[sb2][:], in1=bank[gb][:, 256:512], op=ALU.mult),
                         reads=[("sg", sb2), PS(gb)], writes=[("hblk", hb3)])

                def P3(n):
                    hb3, tb2 = n % 3, 4 + n % 2
                    pv2 = bank_bf(tb2).rearrange("p (k t) -> p k t", k=8)
                    for c in range(2):
                        S.op("pe", lambda e, c=c: e.transpose(out=pv2[:, c, :], in_=hblk[hb3][:, c * 128:(c + 1) * 128], identity=ident_b[:]),
                             reads=[("hblk", hb3)], writes=[PS(tb2)], inc=(c == 1))
                    S.op("dve", lambda e: e.tensor_copy(out=hT2[hb3][:], in_=pv2[:, 0:2, :]), reads=[PS(tb2)], writes=[("hT2", hb3)])

                def P4(n):
                    hb3, yb2 = n % 3, n % 2
                    wi = (n // CAPB) % NW
                    row = n * 128
                    for half in range(2):
                        yb = 6 + half
                        for c in range(2):
                            S.op("pe", lambda e, c=c, half=half, yb=yb: e.matmul(bank[yb][:], lhsT=hT2[hb3][:, c, :], rhs=wd[wi][:, c, half * 512:(half + 1) * 512],
                                                                                start=(c == 0), stop=(c == 1)),
                                 reads=[("hT2", hb3), ("wd", wi)], writes=[PS(yb)], inc=(c == 1))
                        if half == 0:
                            S.op("act", lambda e, yb=yb: e.activation(out=yblk[yb2][:, 0:512], in_=bank[yb][:], func=AF.Copy), reads=[PS(yb)], writes=[("yblk", yb2, 0)])
                        else:
                            S.op("dve", lambda e, yb=yb: e.tensor_copy(out=yblk[yb2][:, 512:1024], in_=bank[yb][:]), reads=[PS(yb)], writes=[("yblk", yb2, 1)])
                    S.dma("sp", "yblk%d" % yb2, lambda e: e.dma_start(out=ys_d[row:row + 128, :], in_=yblk[yb2][:]),
                          reads=[("yblk", yb2, 0), ("yblk", yb2, 1)], writes=[("ys_dw", yb2)])
                    if n % CAPB == CAPB - 1 and n // CAPB + NW < N_EXP:
                        load_w(n // CAPB + NW)

                for step in range(-4, NBLK + 3):
                    for stage, skew in ((P0, -4), (P1, 0), (P2, 1), (P3, 2), (P4, 3)):
                        n = step - skew
                        if 0 <= n < NBLK:
                            stage(n)
                S.barrier()
            with contextlib.ExitStack() as m3:
                y1t = [sbuf(m3, "y1t%d" % i, [128, D], BF16) for i in range(3)]
                y2t = [sbuf(m3, "y2t%d" % i, [128, D], BF16) for i in range(3)]
                fxt = [sbuf(m3, "fxt%d" % i, [128, D], F32) for i in range(3)]
                acc = [sbuf(m3, "facc%d" % i, [128, D], F32) for i in range(2)]
                ot = [sbuf(m3, "fot%d" % i, [128, D], F32) for i in range(2)]
                junkf = sbuf(m3, "junkf", [128, D], BF16)
                stf = sbuf(m3, "stf", [128, 3, 4], F32)
                for i in range(3):
                    S.op("pool", lambda e, i=i: e.memset(y1t[i][:], 0.0), writes=[("y1t", i)])
                    S.op("pool", lambda e, i=i: e.memset(y2t[i][:], 0.0), writes=[("y2t", i)])
                def m3_load(i):
                    b = i % 3
                    S.dma("pool", "y1t%d" % b, lambda e, b=b, i=i: e.indirect_dma_start(
                        out=y1t[b][:], out_offset=None, in_=ys_d, in_offset=bass.IndirectOffsetOnAxis(ap=d1i[:, i:i + 1], axis=0)),
                          reads=["ys_d"], writes=[("y1t", b)])
                    S.dma("pool", "y2t%d" % b, lambda e, b=b, i=i: e.indirect_dma_start(
                        out=y2t[b][:], out_offset=None, in_=ys_d, in_offset=bass.IndirectOffsetOnAxis(ap=d2i[:, i:i + 1], axis=0)),
                          reads=["ys_d"], writes=[("y2t", b)])
                    S.dma("sp", "fxt%d" % b, lambda e, b=b, i=i: e.dma_start(out=fxt[b][:], in_=x1_d[i * 128:(i + 1) * 128, :]), writes=[("fxt", b)])

                def m3_comp(i):
                    b = i % 3
                    c = i % 2
                    S.op("dve", lambda e, b=b, c=c, i=i: e.scalar_tensor_tensor(out=acc[c][:], in0=y1t[b][:], scalar=gate1[:, i:i + 1], in1=fxt[b][:], op0=ALU.mult, op1=ALU.add),
                         reads=[("y1t", b), ("fxt", b)], writes=[("facc", c)])
                    S.op("dve", lambda e, b=b, c=c, i=i: e.scalar_tensor_tensor(out=acc[c][:], in0=y2t[b][:], scalar=gate2[:, i:i + 1], in1=acc[c][:], op0=ALU.mult, op1=ALU.add),
                         reads=[("y2t", b), ("facc", c)], writes=[("facc", c)])
                    S.op("act", lambda e, b=b, c=c: e.activation(out=junkf[:], in_=acc[c][:], func=AF.Square, accum_out=stf[:, c, 0:1]), reads=[("facc", c)], writes=[("fss", c)])
                    rstd_from_ss(stf[:, c, 0:1], D, stf[:, c, 1:2], stf[:, c, 2:3], ("fss", c), "frstd%d" % c)
                    S.op("dve", lambda e, b=b, c=c: e.scalar_tensor_tensor(out=ot[c][:], in0=acc[c][:], scalar=stf[:, c, 2:3], in1=g_fin_t[:], op0=ALU.mult, op1=ALU.mult),
                         reads=[("facc", c), "frstd%d" % c, "g_fin_t"], writes=[("fot", c)])
                    S.dma("sp", "fot%d" % c, lambda e, b=b, c=c, i=i: e.dma_start(out=out[i * 128:(i + 1) * 128, :], in_=ot[c][:]), reads=[("fot", c)])

                PF = 2
                for i in range(PF):
                    m3_load(i)
                for i in range(NTT):
                    if i + PF < NTT:
                        m3_load(i + PF)
                    m3_comp(i)
                S.barrier()

        if stop_after is not None:
            with contextlib.ExitStack() as pz:
                z = sbuf(pz, "z", [128, D], F32)
                S.op("pool", lambda e: e.memset(z[:], 0.0), writes=["z"])
                S.dma("sp", "zout", lambda e: e.dma_start(out=out[0:128, :], in_=z[:]), reads=["z"])
                S.barrier()
        S.barrier()
        S.emit()
    return nc


def _prep_inputs(inputs):
    f = lambda a: np.ascontiguousarray(np.asarray(a, dtype=np.float32))
    rope_cs, oh = _consts()
    shared = {
        "w_in": f(inputs["w_in"][0]),
        "rel_bias": f(inputs["rel_bias"]),
        "w_q_up": f(inputs["w_q_up"][0]),
        "w_kv_up": f(inputs["w_kv_up"][0]),
        "w_out": f(inputs["w_out"][0]),
        "w_rg": f(inputs["w_router_group"][0]),
        "w_re": f(inputs["w_router_expert"][0]),
        "w_gate": f(inputs["w_gate"][0]),
        "w_up": f(inputs["w_up"][0]),
        "w_down": f(inputs["w_down"][0]),
        "g_attn": f(inputs["g_attn_norm"][0]).reshape(1, D),
        "g_q": f(inputs["g_q_latent"][0]).reshape(1, 256),
        "g_kv": f(inputs["g_kv_latent"][0]).reshape(1, 128),
        "g_out": np.concatenate([f(inputs["g_out_a"][0]), f(inputs["g_out_b"][0])]).reshape(1, D),
        "g_ffn": f(inputs["g_ffn_norm"][0]).reshape(1, D),
        "g_fin": f(inputs["g_final"]).reshape(1, D),
        "b_r": np.concatenate([f(inputs["b_router_group"][0]), f(inputs["b_router_expert"][0])]).reshape(1, 36),
        "rope_cs": rope_cs,
        "oh_bias": oh,
    }
    xs = f(inputs["x"]).reshape(N_CORES, NTOK, D)
    return [dict(shared, x=xs[c]) for c in range(N_CORES)]


def kernel(**inputs):
    in_maps = _prep_inputs(inputs)
    nc = build_nc()
    res = run_bass_kernel_spmd(nc, in_maps, core_ids=list(range(N_CORES)))
    outs = [np.asarray(r["out"], dtype=np.float32).reshape(NSEQ, SEQ, D) for r in res.results]
    return np.concatenate(outs, axis=0)
```

```python
import contextlib
import math
import numpy as np
import concourse.bass as bass
import concourse.mybir as mybir
from concourse.bass_utils import run_bass_kernel_spmd

F32 = mybir.dt.float32
BF16 = mybir.dt.bfloat16
I32 = mybir.dt.int32
AF = mybir.ActivationFunctionType
ALU = mybir.AluOpType
AX = mybir.AxisListType

N_CORES = 8
SEQ = 2048
D = 1024
NSEQ = 2
NTOK = NSEQ * SEQ
NT = SEQ // 128
EPS = 1e-6
NEG = -30000.0
PATTERNS = (16, 4, 1)
N_EXP = 32
CAPB = 4
NROWS = N_EXP * CAPB * 128
K2C = 99


class Sync:
    COMPUTE = ("pe", "act", "dve", "pool")

    def __init__(self, nc, stack):
        self.nc = nc
        self.stack = stack
        self.ops = {e: [] for e in ("pe", "act", "dve", "pool", "sp")}
        self.sem = {}
        for e in self.COMPUTE:
            self.sem[e] = stack.enter_context(nc.semaphore("s_" + e))
        self.cnt = {e: 0 for e in self.COMPUTE}
        self.seen = {e: {} for e in self.ops}
        self.keys = {}
        self.pending = {e: ([], []) for e in self.COMPUTE}
        self.slots = {}

    def _key(self, k):
        st = self.keys.get(k)
        if st is None:
            st = {"w": None, "r": []}
            self.keys[k] = st
        return st

    def _need(self, eng, reads, writes, is_dma=False):
        need = {}

        def add(p):
            if p is None:
                return
            s, v, src = p
            if eng == "pe" and src == "pe":
                return
            if need.get(id(s), (None, -1))[1] < v:
                need[id(s)] = (s, v)

        for k in reads:
            st = self._key(k)
            add(st["w"])
            if isinstance(k, tuple) and k and k[0] == "ps":
                for p in st["r"]:
                    if p[2] != eng:
                        add(p)
        for k in writes:
            st = self._key(k)
            if st["w"] is not None and (is_dma or st["w"][2] != eng):
                add(st["w"])
            for p in st["r"]:
                if is_dma or p[2] != eng:
                    add(p)
        out = []
        seen = self.seen[eng]
        for sid, (s, v) in need.items():
            if seen.get(sid, -1) >= v:
                continue
            seen[sid] = v
            out.append((s, v))
        return out

    def _commit(self, reads, writes, prod):
        for k in writes:
            st = self._key(k)
            st["w"] = prod
            st["r"] = []
        for k in reads:
            if k in writes:
                continue
            st = self._key(k)
            st["r"] = [p for p in st["r"] if p[0] is not prod[0]] + [prod]

    def op(self, eng, fn, reads=(), writes=(), inc=True):
        reads, writes = list(reads), list(writes)
        waits = self._need(eng, reads, writes)
        if inc:
            self.cnt[eng] += 1
            prod = (self.sem[eng], self.cnt[eng], eng)
            pr, pw = self.pending[eng]
            self._commit(reads + pr, writes + pw, prod)
            self.pending[eng] = ([], [])
            self.ops[eng].append((waits, fn, (self.sem[eng], 1)))
        else:
            pr, pw = self.pending[eng]
            pr.extend(reads)
            pw.extend(writes)
            self.ops[eng].append((waits, fn, None))

    def dma(self, q, slot, fn, reads=(), writes=()):
        reads, writes = list(reads), list(writes)
        if slot not in self.slots:
            s = self.stack.enter_context(self.nc.semaphore("d_" + slot))
            self.slots[slot] = [s, 0]
        sl = self.slots[slot]
        waits = self._need(q, reads, writes, is_dma=True)
        sl[1] += 16
        self._commit(reads, writes, (sl[0], sl[1], "dma"))
        self.ops[q].append((waits, fn, (sl[0], 16)))

    def barrier(self):
        targets = [(self.sem[e], self.cnt[e]) for e in self.COMPUTE if self.cnt[e] > 0]
        targets += [(s, v) for (s, v) in self.slots.values() if v > 0]
        for e in self.ops:
            waits = []
            seen = self.seen[e]
            for s, v in targets:
                if seen.get(id(s), -1) >= v:
                    continue
                seen[id(s)] = v
                waits.append((s, v))
            if waits:
                self.ops[e].append((waits, None, None))
        self.keys = {}
        self.pending = {e: ([], []) for e in self.COMPUTE}

    def emit(self):
        nc = self.nc
        ops = self.ops

        def run(e, lst):
            for waits, fn, inc in lst:
                for s, v in waits:
                    e.wait_ge(s, v)
                if fn is not None:
                    ins = fn(e)
                    if inc is not None:
                        ins.then_inc(inc[0], inc[1])

        with nc.Block() as block:
            @block.sync
            def _(e):
                run(e, ops["sp"])

            @block.tensor
            def _(e):
                run(e, ops["pe"])

            @block.scalar
            def _(e):
                run(e, ops["act"])

            @block.vector
            def _(e):
                run(e, ops["dve"])

            @block.gpsimd
            def _(e):
                run(e, ops["pool"])


def _t5_bucket(rel):
    half = 16
    max_exact = 8
    n = np.abs(rel)
    large = max_exact + (np.log(np.maximum(n, 1) / max_exact)
                         / math.log(1024 / max_exact) * (half - max_exact)).astype(np.int32)
    large = np.minimum(large, half - 1)
    return (np.where(rel > 0, half, 0) + np.where(n < max_exact, n, large)).astype(np.int32)


def _consts():
    half = 16
    inv_freq = (np.float32(10000.0) ** (-(np.arange(half, dtype=np.float32) / np.float32(half)))).astype(np.float32)
    ang = (np.arange(SEQ, dtype=np.float32)[:, None] * inv_freq[None, :]).astype(np.float32)
    cos, sin = np.cos(ang).astype(np.float32), np.sin(ang).astype(np.float32)
    rope_cs = np.concatenate([cos, cos, sin], axis=1).astype(np.float32)
    oh = np.zeros((33, 3, 512), np.float32)
    for di, d in enumerate(PATTERNS):
        m = np.arange(512)
        delta = m - 255
        valid = np.abs(delta) <= 64
        b = _t5_bucket(delta * d)
        for mm in range(512):
            if valid[mm]:
                oh[b[mm], di, mm] = 1.0
            else:
                oh[32, di, mm] = 1.0
    return rope_cs, oh.reshape(33, 1536)


def build_nc(dbg=False, stop_after=None):
    nc = bass.Bass("TRN2", target_bir_lowering=False)

    def din(name, shape, dt=F32):
        return nc.dram_tensor(name, list(shape), dt, kind="ExternalInput").ap()

    def dscr(name, shape, dt, expose=False):
        kind = "ExternalOutput" if (dbg and expose) else "Internal"
        return nc.dram_tensor(name, list(shape), dt, kind=kind).ap()

    x = din("x", [NTOK, D])
    w_in = din("w_in", [D, 1952])
    rel_bias = din("rel_bias", [32, 8])
    w_q_up = din("w_q_up", [256, 768])
    w_kv_up = din("w_kv_up", [128, 1024])
    w_out = din("w_out", [D, D])
    w_rg = din("w_rg", [D, 4])
    w_re = din("w_re", [D, 32])
    w_gate = din("w_gate", [N_EXP, D, 256])
    w_up = din("w_up", [N_EXP, D, 256])
    w_down = din("w_down", [N_EXP, 256, D])
    g_attn = din("g_attn", [1, D])
    g_q = din("g_q", [1, 256])
    g_kv = din("g_kv", [1, 128])
    g_out = din("g_out", [1, D])
    g_ffn = din("g_ffn", [1, D])
    g_fin = din("g_fin", [1, D])
    b_r = din("b_r", [1, 36])
    rope_cs = din("rope_cs", [SEQ, 48])
    oh_bias = din("oh_bias", [33, 1536])
    out = nc.dram_tensor("out", [NTOK, D], F32, kind="ExternalOutput").ap()

    va_d = dscr("va_d", [NTOK, 520], BF16)
    oa_d = dscr("oa_d", [2, NTOK, 520], F32)
    mixb_d = dscr("mixb_d", [NTOK, 512], BF16, expose=True)
    mixa_d = dscr("mixa_d", [NTOK, 512], BF16, expose=True) if dbg else None
    x1_d = dscr("x1_d", [NTOK, D], F32, expose=True)
    fvec_d = dscr("fvec_d", [8, 1536], F32)
    xs_d = dscr("xs_d", [NROWS + 128, D], BF16)
    ys_d = dscr("ys_d", [NROWS + 128, D], BF16)

    def bcast_rows(ap, n):
        return bass.AP(tensor=ap.tensor, offset=0, ap=[[0, 128], [1, n]])

    with contextlib.ExitStack() as st:
        S = Sync(nc, st)

        uniq = [0]

        def sbuf(stk, name, shape, dt):
            uniq[0] += 1
            return stk.enter_context(nc.sbuf_tensor("%s_%d" % (name, uniq[0]), list(shape), dt))

        psbig = [st.enter_context(nc.psum_tensor("psb%d" % i, [128, 1024], F32)) for i in range(4)]
        bank = [psbig[i // 2][:, (i % 2) * 512:(i % 2 + 1) * 512] for i in range(8)]

        def PS(i):
            return ("ps", i)

        def bank_bf(i):
            return bank[i][:].bitcast(BF16)

        ident_f = sbuf(st, "ident_f", [128, 128], F32)
        ident_b = sbuf(st, "ident_b", [128, 128], BF16)
        jrev_f = sbuf(st, "jrev_f", [128, 128], F32)
        jrev_b = sbuf(st, "jrev_b", [128, 128], BF16)
        ones_f = sbuf(st, "ones_f", [128, 128], F32)
        ltri_f = sbuf(st, "ltri_f", [128, 128], F32)
        eps_t = sbuf(st, "eps_t", [128, 1], F32)
        rope_t = sbuf(st, "rope_t", [128, NT, 48], F32)
        wq_b = sbuf(st, "wq_b", [128, 2, 768], BF16)
        wkv_b = sbuf(st, "wkv_b", [128, 1024], BF16)
        wr_f = sbuf(st, "wr_f", [128, 8, 36], F32)
        g_q_t = sbuf(st, "g_q_t", [128, 256], F32)
        g_kv_t = sbuf(st, "g_kv_t", [128, 128], F32)
        g_out_t = sbuf(st, "g_out_t", [128, D], F32)

        S.op("pool", lambda e: e.memset(ident_f[:], 1.0), writes=["ident_f"])
        S.op("pool", lambda e: e.affine_select(out=ident_f[:], in_=ident_f[:], pattern=[[-1, 128]],
                                               compare_op=ALU.is_equal, fill=0.0, base=0,
                                               channel_multiplier=1), reads=["ident_f"], writes=["ident_f"])
        S.op("pool", lambda e: e.memset(jrev_f[:], 1.0), writes=["jrev_f"])
        S.op("pool", lambda e: e.affine_select(out=jrev_f[:], in_=jrev_f[:], pattern=[[1, 128]],
                                               compare_op=ALU.is_equal, fill=0.0, base=-127,
                                               channel_multiplier=1), reads=["jrev_f"], writes=["jrev_f"])
        S.op("pool", lambda e: e.memset(ones_f[:], 1.0), writes=["ones_f"])
        S.op("pool", lambda e: e.memset(ltri_f[:], 1.0), writes=["ltri_f"])
        S.op("pool", lambda e: e.affine_select(out=ltri_f[:], in_=ltri_f[:], pattern=[[1, 128]],
                                               compare_op=ALU.is_ge, fill=0.0, base=-1,
                                               channel_multiplier=-1), reads=["ltri_f"], writes=["ltri_f"])
        S.op("pool", lambda e: e.memset(eps_t[:], EPS), writes=["eps_t"])
        S.op("dve", lambda e: e.tensor_copy(out=ident_b[:], in_=ident_f[:]), reads=["ident_f"], writes=["ident_b"])
        S.op("dve", lambda e: e.tensor_copy(out=jrev_b[:], in_=jrev_f[:]), reads=["jrev_f"], writes=["jrev_b"])

        S.dma("sp", "c_rope", lambda e: e.dma_start(out=rope_t[:], in_=rope_cs.rearrange("(t p) c -> p t c", p=128)),
              writes=["rope_t"])
        S.dma("pool", "c_wq", lambda e: e.dma_start(out=wq_b[:], in_=w_q_up.rearrange("(c p) n -> p c n", p=128)),
              writes=["wq_b"])
        S.dma("pool", "c_wkv", lambda e: e.dma_start(out=wkv_b[:], in_=w_kv_up), writes=["wkv_b"])
        with nc.allow_non_contiguous_dma(reason="tiny router weights"):
            S.dma("sp", "c_wr", lambda e: e.dma_start(out=wr_f[:, :, 0:4], in_=w_rg.rearrange("(k p) n -> p k n", p=128)),
                  writes=["wr_f"])
            S.dma("sp", "c_wr", lambda e: e.dma_start(out=wr_f[:, :, 4:36], in_=w_re.rearrange("(k p) n -> p k n", p=128)),
                  writes=["wr_f"])
        S.dma("sp", "c_gq", lambda e: e.dma_start(out=g_q_t[:], in_=bcast_rows(g_q, 256)), writes=["g_q_t"])
        S.dma("sp", "c_gkv", lambda e: e.dma_start(out=g_kv_t[:], in_=bcast_rows(g_kv, 128)), writes=["g_kv_t"])
        S.dma("sp", "c_gout", lambda e: e.dma_start(out=g_out_t[:], in_=bcast_rows(g_out, D)), writes=["g_out_t"])

        with contextlib.ExitStack() as s0:
            relb = sbuf(s0, "relb", [33, 8], F32)
            oh_t = sbuf(s0, "oh_t", [33, 1536], F32)
            fvec_sb = sbuf(s0, "fvec_sb", [8, 1536], F32)
            S.op("pool", lambda e: e.memset(relb[32:33, :], NEG), writes=["relb32"])
            S.dma("sp", "c_relb", lambda e: e.dma_start(out=relb[0:32, :], in_=rel_bias), writes=["relb"])
            S.dma("sp", "c_oh", lambda e: e.dma_start(out=oh_t[:], in_=oh_bias), writes=["oh_t"])
            for di in range(3):
                S.op("pe", lambda e, di=di: e.matmul(bank[di][0:8, :], lhsT=relb[:, :], rhs=oh_t[:, di * 512:(di + 1) * 512],
                                                      start=True, stop=True),
                     reads=["relb", "relb32", "oh_t"], writes=[PS(di)])
                S.op("dve", lambda e, di=di: e.tensor_copy(out=fvec_sb[:, di * 512:(di + 1) * 512], in_=bank[di][0:8, :]),
                     reads=[PS(di)], writes=["fvec_sb"])
            S.dma("sp", "c_fv", lambda e: e.dma_start(out=fvec_d, in_=fvec_sb[:]), reads=["fvec_sb"], writes=["fvec_d"])
            S.barrier()

        def rstd_from_ss(ss_ap, n, lnv_ap, rstd_ap, rk, wk):
            S.op("act", lambda e: e.activation(out=lnv_ap, in_=ss_ap, func=AF.Ln, scale=1.0 / n, bias=eps_t[:, 0:1]),
                 reads=[rk, "eps_t"], writes=[wk + "_ln"])
            S.op("act", lambda e: e.activation(out=rstd_ap, in_=lnv_ap, func=AF.Exp, scale=-0.5),
                 reads=[wk + "_ln"], writes=[wk])

        wst = contextlib.ExitStack()
        w_in_b = sbuf(wst, "w_in_b", [128, 8, 1952], BF16)
        for k in range(8):
            S.dma("pool", "w_in", lambda e, k=k: e.dma_start(out=w_in_b[:, k, :], in_=w_in[k * 128:(k + 1) * 128, :]), writes=["w_in_b"])

        def seq_body(s):
            r0 = s * SEQ
            with contextlib.ExitStack() as sq:
                qaT = sbuf(sq, "qaT", [128, 4, SEQ], BF16)
                kaT = sbuf(sq, "kaT", [128, 4, SEQ], BF16)
                sqB = contextlib.ExitStack()
                cqnT = sbuf(sqB, "cqnT", [128, 2, SEQ], BF16)
                ckvnT = sbuf(sqB, "ckvnT", [128, SEQ], BF16)
                kbT = sbuf(sqB, "kbT", [96, 8, SEQ], BF16)
                Vb = sbuf(sqB, "Vb", [128, NT, 8, 65], BF16)
                S.op("pool", lambda e: e.memset(Vb[:, :, :, 64:65], 1.0), writes=["Vb_ones"])

                with contextlib.ExitStack() as p1:
                    g_attn_t = sbuf(p1, "g_attn_t", [128, D], F32)
                    hT = sbuf(p1, "hT", [128, 8, SEQ], BF16)
                    xt = [sbuf(p1, "xt%d" % i, [128, D], F32) for i in range(3)]
                    hb = [sbuf(p1, "hb%d" % i, [128, D], BF16) for i in range(3)]
                    junk = sbuf(p1, "junk", [128, 384], BF16)
                    st1 = sbuf(p1, "st1", [128, 3, 8], F32)
                    vt = [sbuf(p1, "vt%d" % i, [128, 8, 65], BF16) for i in range(2)]
                    cqn = [sbuf(p1, "cqn%d" % i, [128, 256], BF16) for i in range(3)]
                    ckvn = [sbuf(p1, "ckvn%d" % i, [128, 128], BF16) for i in range(3)]
                    ks = [sbuf(p1, "ks%d" % i, [128, 8, 96], BF16) for i in range(3)]
                    krs = [sbuf(p1, "krs%d" % i, [128, 32], F32) for i in range(3)]
                    rtmp = [sbuf(p1, "rtmp%d" % i, [128, 64], F32) for i in range(3)]

                    S.dma("sp", "gattn", lambda e: e.dma_start(out=g_attn_t[:], in_=bcast_rows(g_attn, D)), writes=["g_attn_t"])
                    for i in range(2):
                        S.op("pool", lambda e, i=i: e.memset(vt[i][:, :, 64:65], 1.0), writes=[("vt1", i)])

                    for t in range(NT):
                        b = t % 3
                        S.dma("sp", "xt%d" % b, lambda e, b=b, t=t: e.dma_start(out=xt[b][:], in_=x[r0 + t * 128:r0 + (t + 1) * 128, :]),
                              writes=[("xt", b)])
                        S.op("act", lambda e, b=b: e.activation(out=hb[b][:], in_=xt[b][:], func=AF.Square, accum_out=st1[:, b, 0:1]),
                             reads=[("xt", b)], writes=[("ss", b), ("hb", b)])
                        rstd_from_ss(st1[:, b, 0:1], D, st1[:, b, 1:2], st1[:, b, 2:3], ("ss", b), "rstd%d" % b)
                        S.op("dve", lambda e, b=b: e.scalar_tensor_tensor(out=hb[b][:], in0=xt[b][:], scalar=st1[:, b, 2:3], in1=g_attn_t[:],
                                                                        op0=ALU.mult, op1=ALU.mult),
                             reads=[("xt", b), "rstd%d" % b, "g_attn_t"], writes=[("hb", b)])
                        pb = t % 2
                        pv = bank_bf(pb).rearrange("p (k t) -> p k t", k=8)
                        for k in range(8):
                            S.op("pe", lambda e, k=k, b=b, pv=pv: e.transpose(out=pv[:, k, :], in_=hb[b][:, k * 128:(k + 1) * 128], identity=ident_b[:]),
                                 reads=[("hb", b), "ident_b"], writes=[PS(pb)], inc=(k == 7))
                        eng = "act" if t % 2 == 0 else "dve"
                        if eng == "act":
                            S.op("act", lambda e, t=t, pv=pv: e.activation(out=hT[:, :, t * 128:(t + 1) * 128], in_=pv, func=AF.Copy),
                                 reads=[PS(pb)], writes=[("hT", t)])
                        else:
                            S.op("dve", lambda e, t=t, pv=pv: e.tensor_copy(out=hT[:, :, t * 128:(t + 1) * 128], in_=pv),
                                 reads=[PS(pb)], writes=[("hT", t)])

                    rot = [2, 3, 4, 5]
                    rc = [0]
                    sub = {"P1a": 0, "P1b": 1, "P1c": 2, "P1d": 3}.get(stop_after, 9)

                    def nextbank():
                        bk = rot[rc[0] % len(rot)]
                        rc[0] += 1
                        return bk

                    evc = [0]

                    def evac(out_ap, in_ap, reads, writes, scale=1.0):
                        evc[0] += 1
                        if evc[0] % 2 == 0:
                            S.op("act", lambda e: e.activation(out=out_ap, in_=in_ap, func=AF.Copy, scale=scale), reads=reads, writes=writes)
                        else:
                            S.op("dve", lambda e: e.tensor_scalar(out=out_ap, in0=in_ap, scalar1=scale, scalar2=None, op0=ALU.mult),
                                 reads=reads, writes=writes)

                    for c in range(8 if sub >= 1 else 0):
                        for j in range(4):
                            bk = nextbank()
                            for k in range(8):
                                S.op("pe", lambda e, c=c, j=j, k=k, bk=bk: e.matmul(bank[bk][:], lhsT=w_in_b[:, k, c * 128:(c + 1) * 128],
                                                                                    rhs=hT[:, k, j * 512:(j + 1) * 512], start=(k == 0), stop=(k == 7)),
                                     reads=["w_in_b"] + [("hT", 4 * j + i) for i in range(4)], writes=[PS(bk)], inc=(k == 7))
                            if c < 4:
                                evac(qaT[:, c, j * 512:(j + 1) * 512], bank[bk][:], [PS(bk)], [("qaT", c, j)], scale=0.125)
                            else:
                                evac(kaT[:, c - 4, j * 512:(j + 1) * 512], bank[bk][:], [PS(bk)], [("kaT", c - 4, j)])
                    for t in range(NT if sub >= 2 else 0):
                        b = t % 2
                        bk = nextbank()
                        for k in range(8):
                            S.op("pe", lambda e, t=t, k=k, bk=bk: e.matmul(bank[bk][:], lhsT=hT[:, k, t * 128:(t + 1) * 128],
                                                                            rhs=w_in_b[:, k, 1024:1536], start=(k == 0), stop=(k == 7)),
                                 reads=["w_in_b", ("hT", t)], writes=[PS(bk)], inc=(k == 7))
                        evac(vt[b][:, :, 0:64], bank[bk][:].rearrange("p (h c) -> p h c", h=8), [PS(bk), ("vt1", b)], [("vt", b)])
                        S.dma("sp", "vt%d" % b, lambda e, b=b, t=t: e.dma_start(out=va_d[r0 + t * 128:r0 + (t + 1) * 128, :],
                                                                                  in_=vt[b][:].rearrange("p h c -> p (h c)")),
                              reads=[("vt", b)], writes=[("va_d", t)])
                    for t in range((NT if K2C >= 99 else 1) if sub >= 3 else 0):
                        b = t % 3
                        bk = nextbank()
                        if K2C >= 1:
                            for k in range(8):
                                S.op("pe", lambda e, t=t, k=k, bk=bk: e.matmul(bank[bk][:, 0:416], lhsT=hT[:, k, t * 128:(t + 1) * 128],
                                                                                rhs=w_in_b[:, k, 1536:1952], start=(k == 0), stop=(k == 7)),
                                     reads=["w_in_b", ("hT", t)], writes=[PS(bk)], inc=(k == 7))
                        if K2C >= 2:
                            S.op("act", lambda e, b=b, bk=bk: e.activation(out=junk[:, 0:256], in_=bank[bk][:, 0:256], func=AF.Square,
                                                                            accum_out=st1[:, b, 3:4]), reads=[PS(bk)], writes=[("ssq", b)])
                        if K2C >= 2:
                            S.op("act", lambda e, b=b, bk=bk: e.activation(out=junk[:, 256:384], in_=bank[bk][:, 256:384], func=AF.Square,
                                                                            accum_out=st1[:, b, 5:6]), reads=[PS(bk)], writes=[("sskv", b)])
                        if K2C >= 3:
                            rstd_from_ss(st1[:, b, 3:4], 256, st1[:, b, 4:5], st1[:, b, 4:5], ("ssq", b), "rq%d" % b)
                        if K2C >= 3:
                            rstd_from_ss(st1[:, b, 5:6], 128, st1[:, b, 6:7], st1[:, b, 6:7], ("sskv", b), "rkv%d" % b)
                        if K2C >= 4:
                            S.op("dve", lambda e, b=b, bk=bk: e.scalar_tensor_tensor(out=cqn[b][:], in0=bank[bk][:, 0:256], scalar=st1[:, b, 4:5],
                                                                                      in1=g_q_t[:], op0=ALU.mult, op1=ALU.mult),
                                 reads=[PS(bk), "rq%d" % b, "g_q_t"], writes=[("cqn", b)])
                        if K2C >= 4:
                            S.op("dve", lambda e, b=b, bk=bk: e.scalar_tensor_tensor(out=ckvn[b][:], in0=bank[bk][:, 256:384], scalar=st1[:, b, 6:7],
                                                                                      in1=g_kv_t[:], op0=ALU.mult, op1=ALU.mult),
                                 reads=[PS(bk), "rkv%d" % b, "g_kv_t"], writes=[("ckvn", b)])
                        if K2C >= 5:
                            S.op("dve", lambda e, b=b, bk=bk: e.tensor_copy(out=krs[b][:], in_=bank[bk][:, 384:416]),
                                 reads=[PS(bk)], writes=[("krs", b)])
                        if K2C >= 5:
                            S.op("dve", lambda e, b=b, t=t: e.tensor_tensor(out=rtmp[b][:, 0:32], in0=krs[b][:], in1=rope_t[:, t, 0:32], op=ALU.mult),
                                 reads=[("krs", b), "rope_t"], writes=[("rtA", b)])
                        if K2C >= 5:
                            S.op("dve", lambda e, b=b, t=t: e.tensor_tensor(out=rtmp[b][:, 32:48], in0=krs[b][:, 16:32], in1=rope_t[:, t, 32:48], op=ALU.mult),
                                 reads=[("krs", b), "rope_t"], writes=[("rtB", b)])
                        if K2C >= 5:
                            S.op("dve", lambda e, b=b, t=t: e.tensor_tensor(out=rtmp[b][:, 48:64], in0=krs[b][:, 0:16], in1=rope_t[:, t, 32:48], op=ALU.mult),
                                 reads=[("krs", b), "rope_t"], writes=[("rtC", b)])
                        S.op("dve", lambda e, b=b: e.tensor_tensor(out=ks[b][:, :, 64:80], in0=rtmp[b][:, 0:16].unsqueeze(1).broadcast_to([128, 8, 16]),
                                                                    in1=rtmp[b][:, 32:48].unsqueeze(1).broadcast_to([128, 8, 16]), op=ALU.subtract),
                             reads=[("rtA", b), ("rtB", b)], writes=[("ks_r", b, 0)])
                        S.op("dve", lambda e, b=b: e.tensor_tensor(out=ks[b][:, :, 80:96], in0=rtmp[b][:, 16:32].unsqueeze(1).broadcast_to([128, 8, 16]),
                                                                    in1=rtmp[b][:, 48:64].unsqueeze(1).broadcast_to([128, 8, 16]), op=ALU.add),
                             reads=[("rtA", b), ("rtC", b)], writes=[("ks_r", b, 1)])
                        pb = t % 2
                        pv = bank_bf(pb).rearrange("p (k t) -> p k t", k=8)
                        S.op("pe", lambda e, b=b, pv=pv: e.transpose(out=pv[:, 0, :], in_=cqn[b][:, 0:128], identity=ident_b[:]),
                             reads=[("cqn", b), "ident_b"], writes=[PS(pb)], inc=False)
                        S.op("pe", lambda e, b=b, pv=pv: e.transpose(out=pv[:, 1, :], in_=cqn[b][:, 128:256], identity=ident_b[:]),
                             reads=[("cqn", b)], writes=[PS(pb)], inc=False)
                        S.op("pe", lambda e, b=b, pv=pv: e.transpose(out=pv[:, 2, :], in_=ckvn[b][:], identity=ident_b[:]),
                             reads=[("ckvn", b)], writes=[PS(pb)], inc=True)
                        S.op("act", lambda e, t=t, pv=pv: e.activation(out=cqnT[:, :, t * 128:(t + 1) * 128], in_=pv[:, 0:2, :], func=AF.Copy),
                             reads=[PS(pb)], writes=[("cqnT", t)])
                        S.op("dve", lambda e, t=t, pv=pv: e.tensor_copy(out=ckvnT[:, t * 128:(t + 1) * 128], in_=pv[:, 2, :]),
                             reads=[PS(pb)], writes=[("ckvnT", t)])
                        for half in range(2):
                            bk2 = nextbank()
                            S.op("pe", lambda e, t=t, half=half, bk2=bk2: e.matmul(bank[bk2][:], lhsT=ckvnT[:, t * 128:(t + 1) * 128],
                                                                                    rhs=wkv_b[:, half * 512:(half + 1) * 512], start=True, stop=True),
                                 reads=[("ckvnT", t), "wkv_b"], writes=[PS(bk2)])
                            kvv = bank[bk2][:].rearrange("p (h c) -> p h c", h=4)
                            S.op("act", lambda e, b=b, half=half, kvv=kvv: e.activation(out=ks[b][:, half * 4:(half + 1) * 4, 0:64], in_=kvv[:, :, 0:64], func=AF.Copy),
                                 reads=[PS(bk2)], writes=[("ks_n", b, half)])
                            S.op("dve", lambda e, t=t, half=half, kvv=kvv: e.tensor_copy(out=Vb[:, t, half * 4:(half + 1) * 4, 0:64], in_=kvv[:, :, 64:128]),
                                 reads=[PS(bk2), "Vb_ones"], writes=[("Vb", t, half)])
                        pb2 = 6 + t % 2
                        pv2 = bank_bf(pb2).rearrange("p (k t) -> p k t", k=8)
                        for h in range(8):
                            S.op("pe", lambda e, h=h, b=b, pv2=pv2: e.transpose(out=pv2[0:96, h, :], in_=ks[b][:, h, :], identity=ident_b[:]),
                                 reads=[("ks_n", b, 0), ("ks_n", b, 1), ("ks_r", b, 0), ("ks_r", b, 1), "ident_b"], writes=[PS(pb2)], inc=(h == 7))
                        S.op("act", lambda e, t=t, pv2=pv2: e.activation(out=kbT[:, :, t * 128:(t + 1) * 128], in_=pv2[0:96, :, :], func=AF.Copy),
                             reads=[PS(pb2)], writes=[("kbT", t)])
                    S.barrier()
                if stop_after in ("P1", "P1a", "P1b", "P1c", "P1d"):
                    sqB.close()
                    return

                with contextlib.ExitStack() as p3:
                    qbT = [sbuf(p3, "qbT%d" % i, [96, 8, 512], BF16) for i in range(2)]
                    qs = [sbuf(p3, "qs%d" % i, [128, 8, 96], BF16) for i in range(2)]
                    qr = [sbuf(p3, "qr%d" % i, [128, 8, 32], F32) for i in range(2)]
                    qtm = [sbuf(p3, "qtm%d" % i, [128, 8, 64], F32) for i in range(2)]
                    PT = [sbuf(p3, "PT%d" % i, [128, 1024], BF16) for i in range(3)]
                    ob = [sbuf(p3, "ob%d" % i, [128, 4, 8, 65], F32) for i in range(2)]
                    rl = sbuf(p3, "rl", [128, 8], F32)
                    onb = sbuf(p3, "onb", [128, 8, 64], F32)
                    junk3 = sbuf(p3, "junk3", [128, 512], BF16)
                    st3 = sbuf(p3, "st3", [128, 4], F32)
                    mb = [sbuf(p3, "mb%d" % i, [128, 512], BF16) for i in range(2)]

                    def mla_qproj(jq, tts=(0, 1, 2, 3), stages="ab"):
                        qb = jq % 2
                        for tt in tts:
                            t = jq * 4 + tt
                            b2 = tt % 2
                            if "a" not in stages:
                                mla_qproj_b(jq, tt)
                                continue
                            for half in range(2):
                                for c in range(2):
                                    S.op("pe", lambda e, half=half, c=c, t=t: e.matmul(bank[6 + half][:, 0:384], lhsT=cqnT[:, c, t * 128:(t + 1) * 128],
                                                                                       rhs=wq_b[:, c, half * 384:(half + 1) * 384], start=(c == 0), stop=(c == 1)),
                                         reads=["wq_b"], writes=[PS(6 + half)], inc=(c == 1))
                                pvh = bank[6 + half][:, 0:384].rearrange("p (h c) -> p h c", h=4)
                                S.op("dve", lambda e, half=half, b2=b2, pvh=pvh: e.tensor_copy(out=qs[b2][:, half * 4:(half + 1) * 4, 0:64], in_=pvh[:, :, 0:64]),
                                     reads=[PS(6 + half)], writes=[("qs_n", b2, half)])
                                S.op("dve", lambda e, half=half, b2=b2, pvh=pvh: e.tensor_copy(out=qr[b2][:, half * 4:(half + 1) * 4, :], in_=pvh[:, :, 64:96]),
                                     reads=[PS(6 + half)], writes=[("qr", b2, half)])
                            cos2 = rope_t[:, t, 0:32].unsqueeze(1).broadcast_to([128, 8, 32])
                            sin1 = rope_t[:, t, 32:48].unsqueeze(1).broadcast_to([128, 8, 16])
                            qrk = [("qr", b2, 0), ("qr", b2, 1)]
                            S.op("dve", lambda e, b2=b2, cos2=cos2: e.tensor_tensor(out=qtm[b2][:, :, 0:32], in0=qr[b2][:], in1=cos2, op=ALU.mult),
                                 reads=qrk, writes=[("qtA", b2)])
                            S.op("dve", lambda e, b2=b2, sin1=sin1: e.tensor_tensor(out=qtm[b2][:, :, 32:48], in0=qr[b2][:, :, 16:32], in1=sin1, op=ALU.mult),
                                 reads=qrk, writes=[("qtB", b2)])
                            S.op("dve", lambda e, b2=b2, sin1=sin1: e.tensor_tensor(out=qtm[b2][:, :, 48:64], in0=qr[b2][:, :, 0:16], in1=sin1, op=ALU.mult),
                                 reads=qrk, writes=[("qtC", b2)])
                            S.op("dve", lambda e, b2=b2: e.tensor_tensor(out=qs[b2][:, :, 64:80], in0=qtm[b2][:, :, 0:16], in1=qtm[b2][:, :, 32:48], op=ALU.subtract),
                                 reads=[("qtA", b2), ("qtB", b2)], writes=[("qs_r", b2, 0)])
                            S.op("dve", lambda e, b2=b2: e.tensor_tensor(out=qs[b2][:, :, 80:96], in0=qtm[b2][:, :, 16:32], in1=qtm[b2][:, :, 48:64], op=ALU.add),
                                 reads=[("qtA", b2), ("qtC", b2)], writes=[("qs_r", b2, 1)])
                            if "b" in stages:
                                mla_qproj_b(jq, tt)

                    def mla_qproj_b(jq, tt):
                        qb = jq % 2
                        b2 = tt % 2
                        pv = bank_bf(6).rearrange("p (k t) -> p k t", k=8)
                        for h in range(8):
                            S.op("pe", lambda e, h=h: e.transpose(out=pv[0:96, h, :], in_=qs[b2][:, h, :], identity=ident_b[:]),
                                 reads=[("qs_n", b2, 0), ("qs_n", b2, 1), ("qs_r", b2, 0), ("qs_r", b2, 1)], writes=[PS(6)], inc=(h == 7))
                        S.op("dve", lambda e: e.tensor_copy(out=qbT[qb][:, :, tt * 128:(tt + 1) * 128], in_=pv[0:96, :, :]),
                             reads=[PS(6)], writes=[("qbT", qb, tt)])

                    def mla_epilogue(jq, qts=(0, 1, 2, 3), stages="abc"):
                        obj = ob[jq % 2]
                        obk = [("ob", jq % 2, h) for h in range(8)]
                        for qt in qts:
                            t = jq * 4 + qt
                            m = qt % 2
                            if "a" in stages:
                                S.op("dve", lambda e, qt=qt: e.reciprocal(out=rl[:], in_=obj[:, qt, :, 64]), reads=obk, writes=["rl"])
                                S.op("dve", lambda e, qt=qt: e.tensor_tensor(out=onb[:], in0=obj[:, qt, :, 0:64], in1=rl[:].unsqueeze(2).broadcast_to([128, 8, 64]), op=ALU.mult),
                                     reads=["rl"] + obk, writes=["onb"])
                            if "b" in stages:
                                S.op("act", lambda e: e.activation(out=junk3[:], in_=onb[:].rearrange("p h c -> p (h c)"), func=AF.Square, accum_out=st3[:, 0:1]),
                                     reads=["onb"], writes=["ss3"])
                                rstd_from_ss(st3[:, 0:1], 512, st3[:, 1:2], st3[:, 2:3], "ss3", "rstd3")
                            if "c" in stages:
                                S.op("dve", lambda e, m=m: e.scalar_tensor_tensor(out=mb[m][:], in0=onb[:].rearrange("p h c -> p (h c)"), scalar=st3[:, 2:3],
                                                                                   in1=g_out_t[:, 512:1024], op0=ALU.mult, op1=ALU.mult),
                                     reads=["onb", "rstd3", "g_out_t"], writes=[("mb", m)])
                                S.dma("sp", "mb%d" % m, lambda e, m=m, t=t: e.dma_start(out=mixb_d[r0 + t * 128:r0 + (t + 1) * 128, :], in_=mb[m][:]),
                                      reads=[("mb", m)], writes=[("mixb_d", t)])

                    scale_b = 96.0 ** -0.5
                    NU = 8 * (NT // 2)
                    units = [(jq, h, ktp) for jq in range(4) for h in range(8) for ktp in range(NT // 2)]

                    def mla_S(g):
                        jq, h, ktp = units[g]
                        qb = jq % 2
                        pb = g % 2
                        for j in range(2):
                            kt = 2 * ktp + j
                            S.op("pe", lambda e, j=j, kt=kt: e.matmul(bank[2 * pb + j][:], lhsT=kbT[:, h, kt * 128:(kt + 1) * 128], rhs=qbT[qb][:, h, :],
                                                                      start=True, stop=True),
                                 reads=[("qbT", qb, i) for i in range(4)], writes=[PS(2 * pb + j)], inc=(j == 1))

                    def mla_PV(g):
                        jq, h, ktp = units[g]
                        pb = g % 2
                        pti = g % 3
                        pt = PT[pti]
                        accb = 4 + (h % 2)
                        accv = bank[accb][:, 0:260].rearrange("p (q c) -> p q c", q=4)
                        S.op("act", lambda e: e.activation(out=pt[:], in_=psbig[pb][:], func=AF.Exp, scale=scale_b),
                             reads=[PS(2 * pb), PS(2 * pb + 1)], writes=[("PT", pti)])
                        for j in range(2):
                            kt = 2 * ktp + j
                            for qt in range(4):
                                S.op("pe", lambda e, j=j, kt=kt, qt=qt: e.matmul(accv[:, qt, :], lhsT=pt[:, j * 512 + qt * 128:j * 512 + (qt + 1) * 128],
                                                                                  rhs=Vb[:, kt, h, :], start=(kt == 0 and qt == 0), stop=(kt == NT - 1),
                                                                                  skip_group_check=True),
                                     reads=[("PT", pti)], writes=[PS(accb)], inc=(j == 1 and qt == 3))
                        if ktp == NT // 2 - 1:
                            S.op("dve", lambda e: e.tensor_copy(out=ob[jq % 2][:, :, h, :], in_=accv), reads=[PS(accb)], writes=[("ob", jq % 2, h)])

                    sched = {}

                    def at(u, fn):
                        sched.setdefault(u, []).append(fn)

                    for jq in range(4):
                        base = jq * NU
                        if jq > 0:
                            for qt in range(4):
                                u = base + 4 + 6 * qt
                                at(u, lambda jq=jq, qt=qt: mla_epilogue(jq - 1, (qt,), "a"))
                                at(u + 3, lambda jq=jq, qt=qt: mla_epilogue(jq - 1, (qt,), "b"))
                                at(u + 5, lambda jq=jq, qt=qt: mla_epilogue(jq - 1, (qt,), "c"))
                        if jq < 3:
                            for tt in range(4):
                                u = base + 30 + 8 * tt
                                at(u, lambda jq=jq, tt=tt: mla_qproj(jq + 1, (tt,), "a"))
                                at(u + 5, lambda jq=jq, tt=tt: mla_qproj(jq + 1, (tt,), "b"))
                    mla_qproj(0)
                    mla_S(0)
                    for g in range(len(units)):
                        if g + 1 < len(units):
                            mla_S(g + 1)
                        mla_PV(g)
                        for fn in sched.get(g, ()):
                            fn()
                    mla_epilogue(3)
                    S.barrier()
                if stop_after == "B":
                    sqB.close()
                    return

                sqB.close()
                with contextlib.ExitStack() as p4:
                    trev = sbuf(p4, "trev", [128, 3, 8, 384], BF16)
                    for di in range(3):
                        src = bass.AP(tensor=fvec_d.tensor, offset=di * 512, ap=[[1, 128], [1536, 8], [1, 384]])
                        S.dma("pool", "trev", lambda e, di=di, src=src: e.dma_start(out=trev[:, di, :, :], in_=src), writes=["trev"])
                    Vw = [sbuf(p4, "Vw%d" % i, [128, 4, 2, 520], BF16) for i in range(2)]
                    PTa = [sbuf(p4, "PTa%d" % i, [128, 512], BF16) for i in range(4)]
                    oa = [sbuf(p4, "oa%d" % i, [128, 4, 8, 65], F32) for i in range(2)]
                    o16 = [sbuf(p4, "o16_%d" % i, [128, 520], F32) for i in range(2)]
                    o4 = [sbuf(p4, "o4_%d" % i, [128, 520], F32) for i in range(2)]
                    ma_sb = sbuf(p4, "ma_sb", [128, NT, 512], BF16)
                    rl4 = sbuf(p4, "rl4", [128, 8], F32)
                    onb4 = sbuf(p4, "onb4", [128, 8, 64], F32)
                    junk4 = sbuf(p4, "junk4", [128, 512], BF16)
                    st4 = sbuf(p4, "st4", [128, 4], F32)
                    w_out_b = sbuf(p4, "w_out_b", [128, 8, D], BF16)
                    mbt = [sbuf(p4, "mbt%d" % i, [128, 512], BF16) for i in range(2)]
                    xt5 = [sbuf(p4, "xt5_%d" % i, [128, D], F32) for i in range(2)]
                    mixT = [sbuf(p4, "mixT%d" % i, [128, 8, 128], BF16) for i in range(2)]
                    x1t = [sbuf(p4, "x1t%d" % i, [128, D], F32) for i in range(2)]

                    for k in range(8):
                        S.dma("pool", "w_out", lambda e, k=k: e.dma_start(out=w_out_b[:, k, :], in_=w_out[k * 128:(k + 1) * 128, :]), writes=["w_out_b"])

                    unitsA = [(di, d, g, h) for di, d in enumerate(PATTERNS) for g in range(4) for h in range(8)]

                    def tiles_of(d, g):
                        L = SEQ // d
                        tps = L // 128
                        npc = 2 if L >= 256 else 1
                        tl = []
                        for qt in range(4):
                            qidx = g * 4 + qt
                            r, ti = qidx // tps, qidx % tps
                            wb = min(max(ti * 128 - 64, 0), L - 128 * npc)
                            tl.append((r, ti, wb))
                        return tl, npc

                    def A_S(n):
                        di, d, g, h = unitsA[n]
                        tiles, npc = tiles_of(d, g)
                        vb = (n // 8) % 2
                        if h == 0:
                            for qt in range(4):
                                r, ti, wb = tiles[qt]
                                src = bass.AP(tensor=va_d.tensor, offset=(r0 + r + d * wb) * 520,
                                              ap=[[d * 520, 128], [128 * d * 520, npc], [1, 520]])
                                S.dma("sp", "Vw%d_%d" % (vb, qt), lambda e, qt=qt, src=src: e.dma_start(out=Vw[vb][:, qt, 0:npc, :], in_=src),
                                      writes=[("Vw", vb, qt)])
                        pair, hb_ = h // 2, (h % 2) * 64
                        set_ = n % 2
                        for qt in range(4):
                            r, ti, wb = tiles[qt]
                            qs_ = r + d * ti * 128
                            qap = qaT[hb_:hb_ + 64, pair, qs_:qs_ + d * 127 + 1:d]
                            for pc in range(npc):
                                bk = 2 * set_ + pc
                                ks_ = r + d * (wb + pc * 128)
                                kap = kaT[hb_:hb_ + 64, pair, ks_:ks_ + d * 127 + 1:d]
                                S.op("pe", lambda e, bk=bk, qt=qt, kap=kap, qap=qap: e.matmul(bank[bk][:, qt * 128:(qt + 1) * 128], lhsT=kap, rhs=qap,
                                                                                             start=(qt == 0), stop=False, skip_group_check=True),
                                     writes=[PS(bk)], inc=False)
                        for qt in range(4):
                            r, ti, wb = tiles[qt]
                            for pc in range(npc):
                                bk = 2 * set_ + pc
                                j0 = wb + pc * 128 - ti * 128 + 128
                                S.op("pe", lambda e, bk=bk, qt=qt, j0=j0: e.matmul(bank[bk][:, qt * 128:(qt + 1) * 128],
                                                                                  lhsT=trev[:, di, h, j0:j0 + 128], rhs=jrev_b[:],
                                                                                  start=False, stop=True, skip_group_check=True),
                                     reads=["trev", "jrev_b"], writes=[PS(bk)], inc=(qt == 3))

                    def A_PV(n):
                        di, d, g, h = unitsA[n]
                        tiles, npc = tiles_of(d, g)
                        vb = (n // 8) % 2
                        set_ = n % 2
                        accb = 4 + (n % 2)
                        def merge_loads(qt):
                            t = g * 4 + qt
                            m = qt % 2
                            S.dma("pool", "o16_%d" % m, lambda e: e.dma_start(out=o16[m][:], in_=oa_d[0, r0 + t * 128:r0 + (t + 1) * 128, :]),
                                  reads=[("oa_dw", 0), ("oa_dw", 1)], writes=[("o16", m)])
                            S.dma("pool", "o4_%d" % m, lambda e: e.dma_start(out=o4[m][:], in_=oa_d[1, r0 + t * 128:r0 + (t + 1) * 128, :]),
                                  reads=[("oa_dw", 0), ("oa_dw", 1)], writes=[("o4", m)])

                        if h == 0 and d == 1:
                            merge_loads(0)
                            merge_loads(1)
                        if False:
                            for qt in range(4):
                                t = g * 4 + qt
                                m = qt % 2
                                S.dma("pool", "o16_%d" % m, lambda e, m=m, t=t: e.dma_start(out=o16[m][:], in_=oa_d[0, r0 + t * 128:r0 + (t + 1) * 128, :]),
                                      reads=[("oa_dw", 0), ("oa_dw", 1)], writes=[("o16", m)])
                                S.dma("pool", "o4_%d" % m, lambda e, m=m, t=t: e.dma_start(out=o4[m][:], in_=oa_d[1, r0 + t * 128:r0 + (t + 1) * 128, :]),
                                      reads=[("oa_dw", 0), ("oa_dw", 1)], writes=[("o4", m)])
                        for pc in range(npc):
                            bk = 2 * set_ + pc
                            S.op("act", lambda e, bk=bk: e.activation(out=PTa[bk][:], in_=bank[bk][:], func=AF.Exp),
                                 reads=[PS(bk)], writes=[("PTa", bk)])
                        accv = bank[accb][:, 0:260].rearrange("p (q c) -> p q c", q=4)
                        for qt in range(4):
                            for pc in range(npc):
                                bk = 2 * set_ + pc
                                S.op("pe", lambda e, bk=bk, qt=qt, pc=pc: e.matmul(
                                    accv[:, qt, :], lhsT=PTa[bk][:, qt * 128:(qt + 1) * 128], rhs=Vw[vb][:, qt, pc, h * 65:(h + 1) * 65],
                                    start=(pc == 0), stop=(pc == npc - 1), skip_group_check=True),
                                     reads=[("PTa", bk), ("Vw", vb, qt)], writes=[PS(accb)], inc=(qt == 3 and pc == npc - 1))
                        S.op("dve", lambda e: e.tensor_copy(out=oa[vb][:, :, h, :], in_=accv), reads=[PS(accb)], writes=[("oa", vb, h)])
                        if h != 7:
                            return
                        oak = [("oa", vb, hh) for hh in range(8)]
                        if d != 1:
                            for qt in range(4):
                                r, ti, wb = tiles[qt]
                                dst = bass.AP(tensor=oa_d.tensor, offset=(di * NTOK + r0 + r + d * ti * 128) * 520, ap=[[d * 520, 128], [1, 520]])
                                S.dma("sp", "oaw%d" % vb, lambda e, qt=qt, dst=dst: e.dma_start(out=dst, in_=oa[vb][:, qt].rearrange("p h c -> p (h c)")),
                                      reads=oak, writes=[("oa_dw", vb)])
                            return
                        for qt in range(4):
                            t = g * 4 + qt
                            m = qt % 2
                            S.op("dve", lambda e, m=m: e.tensor_tensor(out=o16[m][:], in0=o16[m][:], in1=o4[m][:], op=ALU.add),
                                 reads=[("o16", m), ("o4", m)], writes=[("o16", m)])
                            S.op("dve", lambda e, m=m, qt=qt: e.tensor_tensor(out=o16[m][:], in0=o16[m][:], in1=oa[vb][:, qt].rearrange("p h c -> p (h c)"), op=ALU.add),
                                 reads=[("o16", m)] + oak, writes=[("o16", m)])
                            ov = o16[m][:].rearrange("p (h c) -> p h c", h=8)
                            S.op("dve", lambda e, ov=ov: e.reciprocal(out=rl4[:], in_=ov[:, :, 64]), reads=[("o16", m)], writes=["rl4"])
                            S.op("dve", lambda e, ov=ov: e.tensor_tensor(out=onb4[:], in0=ov[:, :, 0:64], in1=rl4[:].unsqueeze(2).broadcast_to([128, 8, 64]), op=ALU.mult),
                                 reads=["rl4", ("o16", m)], writes=["onb4"])
                            if qt + 2 < 4:
                                merge_loads(qt + 2)
                            S.op("act", lambda e: e.activation(out=junk4[:], in_=onb4[:].rearrange("p h c -> p (h c)"), func=AF.Square, accum_out=st4[:, 0:1]),
                                 reads=["onb4"], writes=["ss4"])
                            rstd_from_ss(st4[:, 0:1], 512, st4[:, 1:2], st4[:, 2:3], "ss4", "rstd4")
                            S.op("dve", lambda e, t=t: e.scalar_tensor_tensor(out=ma_sb[:, t, :], in0=onb4[:].rearrange("p h c -> p (h c)"), scalar=st4[:, 2:3],
                                                                               in1=g_out_t[:, 0:512], op0=ALU.mult, op1=ALU.mult),
                                 reads=["onb4", "rstd4", "g_out_t"], writes=[("ma", t)])
                            if dbg:
                                S.dma("sp", "dbgma", lambda e, t=t: e.dma_start(out=mixa_d[r0 + t * 128:r0 + (t + 1) * 128, :], in_=ma_sb[:, t, :]),
                                      reads=[("ma", t)])

                    A_S(0)
                    for n in range(len(unitsA)):
                        if n + 1 < len(unitsA):
                            A_S(n + 1)
                        A_PV(n)
                    for t in range(NT):
                        b = t % 2
                        S.dma("sp", "mbt%d" % b, lambda e, b=b, t=t: e.dma_start(out=mbt[b][:], in_=mixb_d[r0 + t * 128:r0 + (t + 1) * 128, :]), writes=[("mbt", b)])
                        S.dma("sp", "xt5_%d" % b, lambda e, b=b, t=t: e.dma_start(out=xt5[b][:], in_=x[r0 + t * 128:r0 + (t + 1) * 128, :]), writes=[("xt5", b)])
                        pv = bank_bf(6).rearrange("p (k t) -> p k t", k=8)
                        for k in range(4):
                            S.op("pe", lambda e, k=k, t=t, pv=pv: e.transpose(out=pv[:, k, :], in_=ma_sb[:, t, k * 128:(k + 1) * 128], identity=ident_b[:]),
                                 reads=[("ma", t), "ident_b"], writes=[PS(6)], inc=False)
                        for k in range(4):
                            S.op("pe", lambda e, k=k, b=b, pv=pv: e.transpose(out=pv[:, 4 + k, :], in_=mbt[b][:, k * 128:(k + 1) * 128], identity=ident_b[:]),
                                 reads=[("mbt", b)], writes=[PS(6)], inc=(k == 3))
                        S.op("act", lambda e, b=b, pv=pv: e.activation(out=mixT[b][:], in_=pv, func=AF.Copy), reads=[PS(6)], writes=[("mixT", b)])
                        for half in range(2):
                            for k in range(8):
                                S.op("pe", lambda e, half=half, k=k, b=b: e.matmul(bank[half][:], lhsT=mixT[b][:, k, :], rhs=w_out_b[:, k, half * 512:(half + 1) * 512],
                                                                                  start=(k == 0), stop=(k == 7)),
                                     reads=[("mixT", b), "w_out_b"], writes=[PS(half)], inc=(k == 7))
                            S.op("dve", lambda e, half=half, b=b: e.tensor_tensor(out=x1t[b][:, half * 512:(half + 1) * 512], in0=bank[half][:],
                                                                                 in1=xt5[b][:, half * 512:(half + 1) * 512], op=ALU.add),
                                 reads=[PS(half), ("xt5", b)], writes=[("x1t", b, half)])
                        S.dma("pool", "x1t%d" % b, lambda e, b=b, t=t: e.dma_start(out=x1_d[r0 + t * 128:r0 + (t + 1) * 128, :], in_=x1t[b][:]),
                              reads=[("x1t", b, 0), ("x1t", b, 1)], writes=[("x1_d", s, t)])
                    S.barrier()


        for s_ in range(NSEQ if stop_after is None else (0 if stop_after == "P0" else 1)):
            seq_body(s_)
        wst.close()

        if stop_after is None:
          with contextlib.ExitStack() as pm:
            NTT = NTOK // 128
            g_ffn_t = sbuf(pm, "g_ffn_t", [128, D], F32)
            g_fin_t = sbuf(pm, "g_fin_t", [128, D], F32)
            b_r_t = sbuf(pm, "b_r_t", [128, 36], F32)
            h2b = sbuf(pm, "h2b", [128, NTT, D], BF16)
            gate1 = sbuf(pm, "gate1", [128, NTT], F32)
            gate2 = sbuf(pm, "gate2", [128, NTT], F32)
            d1i = sbuf(pm, "d1i", [128, NTT], I32)
            d2i = sbuf(pm, "d2i", [128, NTT], I32)
            S.dma("sp", "gffn", lambda e: e.dma_start(out=g_ffn_t[:], in_=bcast_rows(g_ffn, D)), writes=["g_ffn_t"])
            S.dma("sp", "gfin", lambda e: e.dma_start(out=g_fin_t[:], in_=bcast_rows(g_fin, D)), writes=["g_fin_t"])
            S.dma("sp", "brt", lambda e: e.dma_start(out=b_r_t[:], in_=bcast_rows(b_r, 36)), writes=["b_r_t"])
            zt = sbuf(pm, "zt", [128, D], BF16)
            S.op("pool", lambda e: e.memset(zt[:], 0.0), writes=["zt"])
            S.dma("sp", "zfilly", lambda e: e.dma_start(out=ys_d[NROWS:NROWS + 128, :], in_=zt[:]), reads=["zt"], writes=["ys_d"])
            xs_v = xs_d.rearrange("(n p) d -> p n d", p=128)
            NCH = (NROWS + 128) // 128
            for c0 in range(0, NCH, 16):
                c1 = min(c0 + 16, NCH)
                S.dma("sp", "zfill", lambda e, c0=c0, c1=c1: e.dma_start(out=xs_v[:, c0:c1, :], in_=zt[:].unsqueeze(1).broadcast_to([128, c1 - c0, D])),
                      reads=["zt"], writes=["xs_zf"])
            NW = 3
            wgu = [sbuf(pm, "wgu%d" % i, [128, 8, 512], BF16) for i in range(NW)]
            wd = [sbuf(pm, "wd%d" % i, [128, 2, D], BF16) for i in range(NW)]

            def load_w(ex):
                wi = ex % NW
                S.dma("pool", "wg%d" % wi, lambda e: e.dma_start(out=wgu[wi][:, :, 0:256], in_=w_gate[ex].rearrange("(k p) f -> p k f", p=128)),
                      writes=[("wgu", wi)])
                S.dma("pool", "wg%d" % wi, lambda e: e.dma_start(out=wgu[wi][:, :, 256:512], in_=w_up[ex].rearrange("(k p) f -> p k f", p=128)),
                      writes=[("wgu", wi)])
                S.dma("pool", "wd%d" % wi, lambda e: e.dma_start(out=wd[wi][:], in_=w_down[ex].rearrange("(c p) n -> p c n", p=128)),
                      writes=[("wd", wi)])

            for ex in range(NW):
                load_w(ex)
            with contextlib.ExitStack() as m1:
                mxt = [sbuf(m1, "mxt%d" % i, [128, D], F32) for i in range(2)]
                h2f = [sbuf(m1, "h2f%d" % i, [128, D], F32) for i in range(2)]
                h2lo = [sbuf(m1, "h2lo%d" % i, [128, D], BF16) for i in range(2)]
                hiT = [sbuf(m1, "hiT%d" % i, [128, 8, 128], BF16) for i in range(2)]
                loT = [sbuf(m1, "loT%d" % i, [128, 8, 128], BF16) for i in range(2)]
                lgt = [sbuf(m1, "lgt%d" % i, [128, 36], F32) for i in range(2)]
                wr2 = sbuf(m1, "wr2", [128, 8, 72], BF16)
                wrd = sbuf(m1, "wrd", [128, 8, 36], F32)
                S.op("dve", lambda e: e.tensor_copy(out=wr2[:, :, 0:36], in_=wr_f[:]), reads=["wr_f"], writes=["wr2a"])
                S.op("dve", lambda e: e.tensor_tensor(out=wrd[:], in0=wr_f[:], in1=wr2[:, :, 0:36], op=ALU.subtract), reads=["wr_f", "wr2a"], writes=["wrd"])
                S.op("dve", lambda e: e.tensor_copy(out=wr2[:, :, 36:72], in_=wrd[:]), reads=["wrd"], writes=["wr2"])
                junkm = sbuf(m1, "junkm", [128, D], BF16)
                stm = sbuf(m1, "stm", [128, 2, 4], F32)
                lg = sbuf(m1, "lg", [128, NTT, 36], F32)
                gmax = sbuf(m1, "gmax", [128, NTT], F32)
                gm = sbuf(m1, "gm", [128, NTT, 4], F32)
                gsh = sbuf(m1, "gsh", [128, NTT, 4], F32)
                gsum = sbuf(m1, "gsum", [128, NTT], F32)
                ggate = sbuf(m1, "ggate", [128, NTT], F32)
                t48 = sbuf(m1, "t48", [128, NTT, 4, 8], F32)
                ig = sbuf(m1, "ig", [128, NTT, 8], F32)
                ig2 = sbuf(m1, "ig2", [128, NTT, 8], F32)
                m1v = sbuf(m1, "m1v", [128, NTT], F32)
                m2v = sbuf(m1, "m2v", [128, NTT], F32)
                mask1 = sbuf(m1, "mask1", [128, NTT, 8], F32)
                mask2 = sbuf(m1, "mask2", [128, NTT, 8], F32)
                e2 = sbuf(m1, "e2", [128, NTT], F32)
                den = sbuf(m1, "den", [128, NTT], F32)
                OH1 = sbuf(m1, "OH1", [128, NTT, 4, 8], F32)
                OH2 = sbuf(m1, "OH2", [128, NTT, 4, 8], F32)
                OHs = sbuf(m1, "OHs", [128, NTT, 32], F32)
                cumT = sbuf(m1, "cumT", [128, NTT + 1, 32], F32)
                rank_all = sbuf(m1, "rank_all", [128, NTT, 32], F32)
                ebi = sbuf(m1, "ebi", [128, 32], I32)
                ebf = sbuf(m1, "ebf", [128, 32], F32)
                tsel = sbuf(m1, "tsel", [128, NTT, 32], F32)
                rsel = sbuf(m1, "rsel", [128, NTT], F32)
                esel = sbuf(m1, "esel", [128, NTT], F32)
                ovf = sbuf(m1, "ovf", [128, NTT], F32)

                def m1_tiles(t0, t1):
                  for i in range(t0, t1):
                    b = i % 2
                    S.dma("sp", "mxt%d" % b, lambda e, b=b, i=i: e.dma_start(out=mxt[b][:], in_=x1_d[i * 128:(i + 1) * 128, :]), writes=[("mxt", b)])
                    S.op("act", lambda e, b=b: e.activation(out=junkm[:], in_=mxt[b][:], func=AF.Square, accum_out=stm[:, b, 0:1]),
                         reads=[("mxt", b)], writes=[("mss", b)])
                    rstd_from_ss(stm[:, b, 0:1], D, stm[:, b, 1:2], stm[:, b, 2:3], ("mss", b), "mrstd%d" % b)
                    S.op("dve", lambda e, b=b: e.scalar_tensor_tensor(out=h2f[b][:], in0=mxt[b][:], scalar=stm[:, b, 2:3], in1=g_ffn_t[:], op0=ALU.mult, op1=ALU.mult),
                         reads=[("mxt", b), "mrstd%d" % b, "g_ffn_t"], writes=[("h2f", b)])
                    S.op("act", lambda e, b=b, i=i: e.activation(out=h2b[:, i, :], in_=h2f[b][:], func=AF.Copy), reads=[("h2f", b)], writes=[("h2b", i)])
                    S.op("dve", lambda e, b=b, i=i: e.tensor_tensor(out=h2lo[b][:], in0=h2f[b][:], in1=h2b[:, i, :], op=ALU.subtract),
                         reads=[("h2f", b), ("h2b", i)], writes=[("h2lo", b)])
                    pvh = bank_bf(0).rearrange("p (k t) -> p k t", k=8)
                    pvl = bank_bf(1).rearrange("p (k t) -> p k t", k=8)
                    for k in range(8):
                        S.op("pe", lambda e, k=k, i=i, pvh=pvh: e.transpose(out=pvh[:, k, :], in_=h2b[:, i, k * 128:(k + 1) * 128], identity=ident_b[:]),
                             reads=[("h2b", i), "ident_b"], writes=[PS(0)], inc=(k == 7))
                    S.op("act", lambda e, b=b, pvh=pvh: e.activation(out=hiT[b][:], in_=pvh, func=AF.Copy), reads=[PS(0)], writes=[("hiT", b)])
                    for k in range(8):
                        S.op("pe", lambda e, k=k, b=b, pvl=pvl: e.transpose(out=pvl[:, k, :], in_=h2lo[b][:, k * 128:(k + 1) * 128], identity=ident_b[:]),
                             reads=[("h2lo", b)], writes=[PS(1)], inc=(k == 7))
                    S.op("dve", lambda e, b=b, pvl=pvl: e.tensor_copy(out=loT[b][:], in_=pvl), reads=[PS(1)], writes=[("loT", b)])
                    lb = 2 + b
                    for k in range(8):
                        S.op("pe", lambda e, k=k, b=b, lb=lb: e.matmul(bank[lb][:, 0:72], lhsT=hiT[b][:, k, :], rhs=wr2[:, k, :], start=(k == 0), stop=False,
                                                                      skip_group_check=True),
                             reads=[("hiT", b), "wr2"], writes=[PS(lb)], inc=False)
                    for k in range(8):
                        S.op("pe", lambda e, k=k, b=b, lb=lb: e.matmul(bank[lb][:, 0:36], lhsT=loT[b][:, k, :], rhs=wr2[:, k, 0:36], start=False, stop=(k == 7),
                                                                      skip_group_check=True),
                             reads=[("loT", b), "wr2"], writes=[PS(lb)], inc=(k == 7))
                    S.op("dve", lambda e, b=b, lb=lb: e.tensor_tensor(out=lgt[b][:], in0=bank[lb][:, 36:72], in1=b_r_t[:], op=ALU.add),
                         reads=[PS(lb), "b_r_t"], writes=[("lgt", b)])
                    S.op("dve", lambda e, i=i, b=b, lb=lb: e.tensor_tensor(out=lg[:, i, :], in0=bank[lb][:, 0:36], in1=lgt[b][:], op=ALU.add),
                         reads=[PS(lb), ("lgt", b)], writes=["lg"])

                def m1_route(t0, t1):
                  nt = t1 - t0
                  gl = lg[:, t0:t1, 0:4]
                  el = lg[:, t0:t1, 4:36].rearrange("p t (g e) -> p t g e", g=4)

                  def bc(ap, shape, axis):
                    return ap.unsqueeze(axis).broadcast_to(shape)

                  V = lambda fn, reads, writes: S.op("dve", fn, reads=reads, writes=writes)
                  V(lambda e: e.tensor_reduce(out=gmax[:, t0:t1], in_=gl, op=ALU.max, axis=AX.X), ["lg"], ["gmax"])
                  V(lambda e: e.tensor_tensor(out=gm[:, t0:t1], in0=gl, in1=bc(gmax[:, t0:t1], [128, nt, 4], 2), op=ALU.is_equal), ["lg", "gmax"], ["gm"])
                  V(lambda e: e.tensor_tensor(out=gsh[:, t0:t1], in0=gl, in1=bc(gmax[:, t0:t1], [128, nt, 4], 2), op=ALU.subtract), ["lg", "gmax"], ["gsh"])
                  S.op("act", lambda e: e.activation(out=gsh[:, t0:t1], in_=gsh[:, t0:t1], func=AF.Exp), reads=["gsh"], writes=["gsh"])
                  V(lambda e: e.tensor_reduce(out=gsum[:, t0:t1], in_=gsh[:, t0:t1], op=ALU.add, axis=AX.X), ["gsh"], ["gsum"])
                  V(lambda e: e.reciprocal(out=ggate[:, t0:t1], in_=gsum[:, t0:t1]), ["gsum"], ["ggate"])
                  V(lambda e: e.tensor_tensor(out=t48[:, t0:t1], in0=el, in1=bc(gm[:, t0:t1], [128, nt, 4, 8], 3), op=ALU.mult), ["lg", "gm"], ["t48"])
                  V(lambda e: e.tensor_reduce(out=ig[:, t0:t1], in_=t48[:, t0:t1].rearrange("p t g e -> p t e g"), op=ALU.add, axis=AX.X), ["t48"], ["ig"])
                  V(lambda e: e.tensor_reduce(out=m1v[:, t0:t1], in_=ig[:, t0:t1], op=ALU.max, axis=AX.X), ["ig"], ["m1v"])
                  V(lambda e: e.tensor_tensor(out=mask1[:, t0:t1], in0=ig[:, t0:t1], in1=bc(m1v[:, t0:t1], [128, nt, 8], 2), op=ALU.is_equal), ["ig", "m1v"], ["mask1"])
                  V(lambda e: e.scalar_tensor_tensor(out=ig2[:, t0:t1].rearrange("p t e -> p (t e)"), in0=mask1[:, t0:t1].rearrange("p t e -> p (t e)"), scalar=-1e30,
                                                   in1=ig[:, t0:t1].rearrange("p t e -> p (t e)"), op0=ALU.mult, op1=ALU.add), ["mask1", "ig"], ["ig2"])
                  V(lambda e: e.tensor_reduce(out=m2v[:, t0:t1], in_=ig2[:, t0:t1], op=ALU.max, axis=AX.X), ["ig2"], ["m2v"])
                  V(lambda e: e.tensor_tensor(out=mask2[:, t0:t1], in0=ig2[:, t0:t1], in1=bc(m2v[:, t0:t1], [128, nt, 8], 2), op=ALU.is_equal), ["ig2", "m2v"], ["mask2"])
                  V(lambda e: e.tensor_tensor(out=e2[:, t0:t1], in0=m2v[:, t0:t1], in1=m1v[:, t0:t1], op=ALU.subtract), ["m1v", "m2v"], ["e2"])
                  S.op("act", lambda e: e.activation(out=e2[:, t0:t1], in_=e2[:, t0:t1], func=AF.Exp), reads=["e2"], writes=["e2"])
                  V(lambda e: e.tensor_scalar(out=den[:, t0:t1], in0=e2[:, t0:t1], scalar1=1.0, scalar2=None, op0=ALU.add), ["e2"], ["den"])
                  V(lambda e: e.reciprocal(out=den[:, t0:t1], in_=den[:, t0:t1]), ["den"], ["den"])
                  V(lambda e: e.tensor_tensor(out=gate1[:, t0:t1], in0=ggate[:, t0:t1], in1=den[:, t0:t1], op=ALU.mult), ["ggate", "den"], ["gate1"])
                  V(lambda e: e.tensor_tensor(out=gate2[:, t0:t1], in0=gate1[:, t0:t1], in1=e2[:, t0:t1], op=ALU.mult), ["gate1", "e2"], ["gate2"])
                  V(lambda e: e.tensor_tensor(out=OH1[:, t0:t1], in0=bc(gm[:, t0:t1], [128, nt, 4, 8], 3), in1=bc(mask1[:, t0:t1], [128, nt, 4, 8], 2), op=ALU.mult), ["gm", "mask1"], ["OH1"])
                  V(lambda e: e.tensor_tensor(out=OH2[:, t0:t1], in0=bc(gm[:, t0:t1], [128, nt, 4, 8], 3), in1=bc(mask2[:, t0:t1], [128, nt, 4, 8], 2), op=ALU.mult), ["gm", "mask2"], ["OH2"])
                  V(lambda e: e.tensor_tensor(out=OHs[:, t0:t1].rearrange("p t e -> p (t e)"), in0=OH1[:, t0:t1].rearrange("p t g e -> p (t g e)"),
                                            in1=OH2[:, t0:t1].rearrange("p t g e -> p (t g e)"), op=ALU.add), ["OH1", "OH2"], ["OHs"])
                  for i in range(t0, t1):
                    V(lambda e, i=i: e.tensor_tensor(out=cumT[:, i + 1, :], in0=cumT[:, i, :], in1=OHs[:, i, :], op=ALU.add), [("cumT", i), "OHs"], [("cumT", i + 1)])
                  for i in range(t0, t1):
                    rb = 4 + (i % 2)
                    S.op("pe", lambda e, i=i, rb=rb: e.matmul(bank[rb][:, 0:32], lhsT=ltri_f[:], rhs=OHs[:, i, :], start=True, stop=False),
                         reads=["ltri_f", "OHs"], writes=[PS(rb)], inc=False)
                    S.op("pe", lambda e, i=i, rb=rb: e.matmul(bank[rb][:, 0:32], lhsT=ones_f[:], rhs=cumT[:, i, :], start=False, stop=True),
                         reads=["ones_f", ("cumT", i)], writes=[PS(rb)], inc=True)
                    V(lambda e, i=i, rb=rb: e.tensor_copy(out=rank_all[:, i, :], in_=bank[rb][:, 0:32]), [PS(rb)], ["rank_all"])
                  for (OH, dst_i, nm) in ((OH1, d1i, "1"), (OH2, d2i, "2")):
                    ohf = OH[:, t0:t1].rearrange("p t g e -> p t (g e)")
                    V(lambda e, ohf=ohf: e.tensor_tensor(out=tsel[:, t0:t1], in0=rank_all[:, t0:t1], in1=ohf, op=ALU.mult), ["rank_all", "OH" + nm], ["tsel"])
                    V(lambda e: e.tensor_reduce(out=rsel[:, t0:t1], in_=tsel[:, t0:t1], op=ALU.add, axis=AX.X), ["tsel"], ["rsel"])
                    V(lambda e, ohf=ohf: e.tensor_tensor(out=tsel[:, t0:t1], in0=ohf, in1=bc(ebf[:], [128, nt, 32], 1), op=ALU.mult), ["ebf", "OH" + nm, "rsel"], ["tsel"])
                    V(lambda e: e.tensor_reduce(out=esel[:, t0:t1], in_=tsel[:, t0:t1], op=ALU.add, axis=AX.X), ["tsel"], ["esel"])
                    V(lambda e: e.tensor_scalar(out=ovf[:, t0:t1], in0=rsel[:, t0:t1], scalar1=float(CAPB * 128), scalar2=None, op0=ALU.is_lt), ["rsel"], ["ovf"])
                    V(lambda e: e.tensor_tensor(out=rsel[:, t0:t1], in0=rsel[:, t0:t1], in1=esel[:, t0:t1], op=ALU.add), ["rsel", "esel"], ["rsel"])
                    V(lambda e: e.scalar_tensor_tensor(out=rsel[:, t0:t1], in0=rsel[:, t0:t1], scalar=float(-NROWS), in1=ovf[:, t0:t1], op0=ALU.add, op1=ALU.mult), ["rsel", "ovf"], ["rsel"])
                    V(lambda e: e.tensor_scalar(out=rsel[:, t0:t1], in0=rsel[:, t0:t1], scalar1=float(NROWS), scalar2=None, op0=ALU.add), ["rsel"], ["rsel"])
                    V(lambda e, dst_i=dst_i: e.tensor_copy(out=dst_i[:, t0:t1], in_=rsel[:, t0:t1]), ["rsel"], ["dst" + nm])
                  for i in range(t0, t1):
                      for (dst_i, nm) in ((d1i, "1"), (d2i, "2")):
                          S.dma("pool", "scat" + nm, lambda e, i=i, dst_i=dst_i: e.indirect_dma_start(
                              out=xs_d, out_offset=bass.IndirectOffsetOnAxis(ap=dst_i[:, i:i + 1], axis=0), in_=h2b[:, i, :], in_offset=None),
                                reads=[("h2b", i), "dst" + nm, "xs_zf"], writes=[("xs_s", i, nm)])

                S.op("pool", lambda e: e.memset(cumT[:, 0, :], 0.0), writes=[("cumT", 0)])
                S.op("pool", lambda e: e.iota(ebi[:], pattern=[[CAPB * 128, 32]], base=0, channel_multiplier=0), writes=["ebi"])
                S.op("dve", lambda e: e.tensor_copy(out=ebf[:], in_=ebi[:]), reads=["ebi"], writes=["ebf"])
                NB1 = 4
                for jb in range(NB1):
                    m1_tiles(jb * (NTT // NB1), (jb + 1) * (NTT // NB1))
                    m1_route(jb * (NTT // NB1), (jb + 1) * (NTT // NB1))
                S.barrier()
            with contextlib.ExitStack() as m2:
                xblk = [sbuf(m2, "xblk%d" % i, [128, D], BF16) for i in range(8)]
                xT = [sbuf(m2, "xT%d" % i, [128, 8, 128], BF16) for i in range(3)]
                sg = [sbuf(m2, "sg%d" % i, [128, 256], F32) for i in range(2)]
                hblk = [sbuf(m2, "hblk%d" % i, [128, 256], BF16) for i in range(3)]
                hT2 = [sbuf(m2, "hT2_%d" % i, [128, 2, 128], BF16) for i in range(3)]
                yblk = [sbuf(m2, "yblk%d" % i, [128, D], BF16) for i in range(2)]
                NBLK = N_EXP * CAPB

                def P0(n):
                    xb = n % 8
                    row = n * 128
                    S.dma("sp", "xblk%d" % xb, lambda e: e.dma_start(out=xblk[xb][:], in_=xs_d[row:row + 128, :]), reads=["xs_d"], writes=[("xblk", xb)])

                def P1(n):
                    xb, pb, tb = n % 8, n % 2, n % 3
                    pv = bank_bf(pb).rearrange("p (k t) -> p k t", k=8)
                    for k in range(8):
                        S.op("pe", lambda e, k=k: e.transpose(out=pv[:, k, :], in_=xblk[xb][:, k * 128:(k + 1) * 128], identity=ident_b[:]),
                             reads=[("xblk", xb), "ident_b"], writes=[PS(pb)], inc=(k == 7))
                    S.op("act", lambda e: e.activation(out=xT[tb][:], in_=pv, func=AF.Copy), reads=[PS(pb)], writes=[("xT", tb)])

                def P2(n):
                    tb, gb, hb3, sb2 = n % 3, 2 + n % 2, n % 3, n % 2
                    wi = (n // CAPB) % NW
                    for k in range(8):
                        S.op("pe", lambda e, k=k: e.matmul(bank[gb][:], lhsT=xT[tb][:, k, :], rhs=wgu[wi][:, k, :], start=(k == 0), stop=(k == 7)),
                             reads=[("xT", tb), ("wgu", wi)], writes=[PS(gb)], inc=(k == 7))
                    S.op("act", lambda e: e.activation(out=sg[sb2][:], in_=bank[gb][:, 0:256], func=AF.Silu), reads=[PS(gb)], writes=[("sg", sb2)])
                    S.op("dve", lambda e: e.tensor_tensor(out=hblk[hb3][:], in0=sg[sb2][:], in1=bank[gb][:, 256:512], op=ALU.mult),
                         reads=[("sg", sb2), PS(gb)], writes=[("hblk", hb3)])

                def P3(n):
                    hb3, tb2 = n % 3, 4 + n % 2
                    pv2 = bank_bf(tb2).rearrange("p (k t) -> p k t", k=8)
                    for c in range(2):
                        S.op("pe", lambda e, c=c: e.transpose(out=pv2[:, c, :], in_=hblk[hb3][:, c * 128:(c + 1) * 128], identity=ident_b[:]),
                             reads=[("hblk", hb3)], writes=[PS(tb2)], inc=(c == 1))
                    S.op("dve", lambda e: e.tensor_copy(out=hT2[hb3][:], in_=pv2[:, 0:2, :]), reads=[PS(tb2)], writes=[("hT2", hb3)])

                def P4(n):
                    hb3, yb2 = n % 3, n % 2
                    wi = (n // CAPB) % NW
                    row = n * 128
                    for half in range(2):
                        yb = 6 + half
                        for c in range(2):
                            S.op("pe", lambda e, c=c, half=half, yb=yb: e.matmul(bank[yb][:], lhsT=hT2[hb3][:, c, :], rhs=wd[wi][:, c, half * 512:(half + 1) * 512],
                                                                                start=(c == 0), stop=(c == 1)),
                                 reads=[("hT2", hb3), ("wd", wi)], writes=[PS(yb)], inc=(c == 1))
                        if half == 0:
                            S.op("act", lambda e, yb=yb: e.activation(out=yblk[yb2][:, 0:512], in_=bank[yb][:], func=AF.Copy), reads=[PS(yb)], writes=[("yblk", yb2, 0)])
                        else:
                            S.op("dve", lambda e, yb=yb: e.tensor_copy(out=yblk[yb2][:, 512:1024], in_=bank[yb][:]), reads=[PS(yb)], writes=[("yblk", yb2, 1)])
                    S.dma("sp", "yblk%d" % yb2, lambda e: e.dma_start(out=ys_d[row:row + 128, :], in_=yblk[yb2][:]),
                          reads=[("yblk", yb2, 0), ("yblk", yb2, 1)], writes=[("ys_dw", yb2)])
                    if n % CAPB == CAPB - 1 and n // CAPB + NW < N_EXP:
                        load_w(n // CAPB + NW)

                for step in range(-4, NBLK + 3):
                    for stage, skew in ((P0, -4), (P1, 0), (P2, 1), (P3, 2), (P4, 3)):
                        n = step - skew
                        if 0 <= n < NBLK:
                            stage(n)
                S.barrier()
            with contextlib.ExitStack() as m3:
                y1t = [sbuf(m3, "y1t%d" % i, [128, D], BF16) for i in range(3)]
                y2t = [sbuf(m3, "y2t%d" % i, [128, D], BF16) for i in range(3)]
                fxt = [sbuf(m3, "fxt%d" % i, [128, D], F32) for i in range(3)]
                acc = [sbuf(m3, "facc%d" % i, [128, D], F32) for i in range(2)]
                ot = [sbuf(m3, "fot%d" % i, [128, D], F32) for i in range(2)]
                junkf = sbuf(m3, "junkf", [128, D], BF16)
                stf = sbuf(m3, "stf", [128, 3, 4], F32)
                for i in range(3):
                    S.op("pool", lambda e, i=i: e.memset(y1t[i][:], 0.0), writes=[("y1t", i)])
                    S.op("pool", lambda e, i=i: e.memset(y2t[i][:], 0.0), writes=[("y2t", i)])
                def m3_load(i):
                    b = i % 3
                    S.dma("pool", "y1t%d" % b, lambda e, b=b, i=i: e.indirect_dma_start(
                        out=y1t[b][:], out_offset=None, in_=ys_d, in_offset=bass.IndirectOffsetOnAxis(ap=d1i[:, i:i + 1], axis=0)),
                          reads=["ys_d"], writes=[("y1t", b)])
                    S.dma("pool", "y2t%d" % b, lambda e, b=b, i=i: e.indirect_dma_start(
                        out=y2t[b][:], out_offset=None, in_=ys_d, in_offset=bass.IndirectOffsetOnAxis(ap=d2i[:, i:i + 1], axis=0)),
                          reads=["ys_d"], writes=[("y2t", b)])
                    S.dma("sp", "fxt%d" % b, lambda e, b=b, i=i: e.dma_start(out=fxt[b][:], in_=x1_d[i * 128:(i + 1) * 128, :]), writes=[("fxt", b)])

                def m3_comp(i):
                    b = i % 3
                    c = i % 2
                    S.op("dve", lambda e, b=b, c=c, i=i: e.scalar_tensor_tensor(out=acc[c][:], in0=y1t[b][:], scalar=gate1[:, i:i + 1], in1=fxt[b][:], op0=ALU.mult, op1=ALU.add),
                         reads=[("y1t", b), ("fxt", b)], writes=[("facc", c)])
                    S.op("dve", lambda e, b=b, c=c, i=i: e.scalar_tensor_tensor(out=acc[c][:], in0=y2t[b][:], scalar=gate2[:, i:i + 1], in1=acc[c][:], op0=ALU.mult, op1=ALU.add),
                         reads=[("y2t", b), ("facc", c)], writes=[("facc", c)])
                    S.op("act", lambda e, b=b, c=c: e.activation(out=junkf[:], in_=acc[c][:], func=AF.Square, accum_out=stf[:, c, 0:1]), reads=[("facc", c)], writes=[("fss", c)])
                    rstd_from_ss(stf[:, c, 0:1], D, stf[:, c, 1:2], stf[:, c, 2:3], ("fss", c), "frstd%d" % c)
                    S.op("dve", lambda e, b=b, c=c: e.scalar_tensor_tensor(out=ot[c][:], in0=acc[c][:], scalar=stf[:, c, 2:3], in1=g_fin_t[:], op0=ALU.mult, op1=ALU.mult),
                         reads=[("facc", c), "frstd%d" % c, "g_fin_t"], writes=[("fot", c)])
                    S.dma("sp", "fot%d" % c, lambda e, b=b, c=c, i=i: e.dma_start(out=out[i * 128:(i + 1) * 128, :], in_=ot[c][:]), reads=[("fot", c)])

                PF = 2
                for i in range(PF):
                    m3_load(i)
                for i in range(NTT):
                    if i + PF < NTT:
                        m3_load(i + PF)
                    m3_comp(i)
                S.barrier()

        if stop_after is not None:
            with contextlib.ExitStack() as pz:
                z = sbuf(pz, "z", [128, D], F32)
                S.op("pool", lambda e: e.memset(z[:], 0.0), writes=["z"])
                S.dma("sp", "zout", lambda e: e.dma_start(out=out[0:128, :], in_=z[:]), reads=["z"])
                S.barrier()
        S.barrier()
        S.emit()
    return nc


def _prep_inputs(inputs):
    f = lambda a: np.ascontiguousarray(np.asarray(a, dtype=np.float32))
    rope_cs, oh = _consts()
    shared = {
        "w_in": f(inputs["w_in"][0]),
        "rel_bias": f(inputs["rel_bias"]),
        "w_q_up": f(inputs["w_q_up"][0]),
        "w_kv_up": f(inputs["w_kv_up"][0]),
        "w_out": f(inputs["w_out"][0]),
        "w_rg": f(inputs["w_router_group"][0]),
        "w_re": f(inputs["w_router_expert"][0]),
        "w_gate": f(inputs["w_gate"][0]),
        "w_up": f(inputs["w_up"][0]),
        "w_down": f(inputs["w_down"][0]),
        "g_attn": f(inputs["g_attn_norm"][0]).reshape(1, D),
        "g_q": f(inputs["g_q_latent"][0]).reshape(1, 256),
        "g_kv": f(inputs["g_kv_latent"][0]).reshape(1, 128),
        "g_out": np.concatenate([f(inputs["g_out_a"][0]), f(inputs["g_out_b"][0])]).reshape(1, D),
        "g_ffn": f(inputs["g_ffn_norm"][0]).reshape(1, D),
        "g_fin": f(inputs["g_final"]).reshape(1, D),
        "b_r": np.concatenate([f(inputs["b_router_group"][0]), f(inputs["b_router_expert"][0])]).reshape(1, 36),
        "rope_cs": rope_cs,
        "oh_bias": oh,
    }
    xs = f(inputs["x"]).reshape(N_CORES, NTOK, D)
    return [dict(shared, x=xs[c]) for c in range(N_CORES)]


def kernel(**inputs):
    in_maps = _prep_inputs(inputs)
    nc = build_nc()
    res = run_bass_kernel_spmd(nc, in_maps, core_ids=list(range(N_CORES)))
    outs = [np.asarray(r["out"], dtype=np.float32).reshape(NSEQ, SEQ, D) for r in res.results]
    return np.concatenate(outs, axis=0)
```

```python
import contextlib
import math
import numpy as np
import concourse.bass as bass
import concourse.mybir as mybir
from concourse.bass_utils import run_bass_kernel_spmd

F32 = mybir.dt.float32
BF16 = mybir.dt.bfloat16
I32 = mybir.dt.int32
AF = mybir.ActivationFunctionType
ALU = mybir.AluOpType
AX = mybir.AxisListType

N_CORES = 8
SEQ = 2048
D = 1024
NSEQ = 2
NTOK = NSEQ * SEQ
NT = SEQ // 128
EPS = 1e-6
NEG = -30000.0
PATTERNS = (16, 4, 1)
N_EXP = 32
CAPB = 4
NROWS = N_EXP * CAPB * 128
K2C = 99


class Sync:
    COMPUTE = ("pe", "act", "dve", "pool")

    def __init__(self, nc, stack):
        self.nc = nc
        self.stack = stack
        self.ops = {e: [] for e in ("pe", "act", "dve", "pool", "sp")}
        self.sem = {}
        for e in self.COMPUTE:
            self.sem[e] = stack.enter_context(nc.semaphore("s_" + e))
        self.cnt = {e: 0 for e in self.COMPUTE}
        self.seen = {e: {} for e in self.ops}
        self.keys = {}
        self.pending = {e: ([], []) for e in self.COMPUTE}
        self.slots = {}

    def _key(self, k):
        st = self.keys.get(k)
        if st is None:
            st = {"w": None, "r": []}
            self.keys[k] = st
        return st

    def _need(self, eng, reads, writes, is_dma=False):
        need = {}

        def add(p):
            if p is None:
                return
            s, v, src = p
            if eng == "pe" and src == "pe":
                return
            if need.get(id(s), (None, -1))[1] < v:
                need[id(s)] = (s, v)

        for k in reads:
            st = self._key(k)
            add(st["w"])
            if isinstance(k, tuple) and k and k[0] == "ps":
                for p in st["r"]:
                    if p[2] != eng:
                        add(p)
        for k in writes:
            st = self._key(k)
            if st["w"] is not None and (is_dma or st["w"][2] != eng):
                add(st["w"])
            for p in st["r"]:
                if is_dma or p[2] != eng:
                    add(p)
        out = []
        seen = self.seen[eng]
        for sid, (s, v) in need.items():
            if seen.get(sid, -1) >= v:
                continue
            seen[sid] = v
            out.append((s, v))
        return out

    def _commit(self, reads, writes, prod):
        for k in writes:
            st = self._key(k)
            st["w"] = prod
            st["r"] = []
        for k in reads:
            if k in writes:
                continue
            st = self._key(k)
            st["r"] = [p for p in st["r"] if p[0] is not prod[0]] + [prod]

    def op(self, eng, fn, reads=(), writes=(), inc=True):
        reads, writes = list(reads), list(writes)
        waits = self._need(eng, reads, writes)
        if inc:
            self.cnt[eng] += 1
            prod = (self.sem[eng], self.cnt[eng], eng)
            pr, pw = self.pending[eng]
            self._commit(reads + pr, writes + pw, prod)
            self.pending[eng] = ([], [])
            self.ops[eng].append((waits, fn, (self.sem[eng], 1)))
        else:
            pr, pw = self.pending[eng]
            pr.extend(reads)
            pw.extend(writes)
            self.ops[eng].append((waits, fn, None))

    def dma(self, q, slot, fn, reads=(), writes=()):
        reads, writes = list(reads), list(writes)
        if slot not in self.slots:
            s = self.stack.enter_context(self.nc.semaphore("d_" + slot))
            self.slots[slot] = [s, 0]
        sl = self.slots[slot]
        waits = self._need(q, reads, writes, is_dma=True)
        sl[1] += 16
        self._commit(reads, writes, (sl[0], sl[1], "dma"))
        self.ops[q].append((waits, fn, (sl[0], 16)))

    def barrier(self):
        targets = [(self.sem[e], self.cnt[e]) for e in self.COMPUTE if self.cnt[e] > 0]
        targets += [(s, v) for (s, v) in self.slots.values() if v > 0]
        for e in self.ops:
            waits = []
            seen = self.seen[e]
            for s, v in targets:
                if seen.get(id(s), -1) >= v:
                    continue
                seen[id(s)] = v
                waits.append((s, v))
            if waits:
                self.ops[e].append((waits, None, None))
        self.keys = {}
        self.pending = {e: ([], []) for e in self.COMPUTE}

    def emit(self):
        nc = self.nc
        ops = self.ops

        def run(e, lst):
            for waits, fn, inc in lst:
                for s, v in waits:
                    e.wait_ge(s, v)
                if fn is not None:
                    ins = fn(e)
                    if inc is not None:
                        ins.then_inc(inc[0], inc[1])

        with nc.Block() as block:
            @block.sync
            def _(e):
                run(e, ops["sp"])

            @block.tensor
            def _(e):
                run(e, ops["pe"])

            @block.scalar
            def _(e):
                run(e, ops["act"])

            @block.vector
            def _(e):
                run(e, ops["dve"])

            @block.gpsimd
            def _(e):
                run(e, ops["pool"])


def _t5_bucket(rel):
    half = 16
    max_exact = 8
    n = np.abs(rel)
    large = max_exact + (np.log(np.maximum(n, 1) / max_exact)
                         / math.log(1024 / max_exact) * (half - max_exact)).astype(np.int32)
    large = np.minimum(large, half - 1)
    return (np.where(rel > 0, half, 0) + np.where(n < max_exact, n, large)).astype(np.int32)


def _consts():
    half = 16
    inv_freq = (np.float32(10000.0) ** (-(np.arange(half, dtype=np.float32) / np.float32(half)))).astype(np.float32)
    ang = (np.arange(SEQ, dtype=np.float32)[:, None] * inv_freq[None, :]).astype(np.float32)
    cos, sin = np.cos(ang).astype(np.float32), np.sin(ang).astype(np.float32)
    rope_cs = np.concatenate([cos, cos, sin], axis=1).astype(np.float32)
    oh = np.zeros((33, 3, 512), np.float32)
    for di, d in enumerate(PATTERNS):
        m = np.arange(512)
        delta = m - 255
        valid = np.abs(delta) <= 64
        b = _t5_bucket(delta * d)
        for mm in range(512):
            if valid[mm]:
                oh[b[mm], di, mm] = 1.0
            else:
                oh[32, di, mm] = 1.0
    return rope_cs, oh.reshape(33, 1536)


def build_nc(dbg=False, stop_after=None):
    nc = bass.Bass("TRN2", target_bir_lowering=False)

    def din(name, shape, dt=F32):
        return nc.dram_tensor(name, list(shape), dt, kind="ExternalInput").ap()

    def dscr(name, shape, dt, expose=False):
        kind = "ExternalOutput" if (dbg and expose) else "Internal"
        return nc.dram_tensor(name, list(shape), dt, kind=kind).ap()

    x = din("x", [NTOK, D])
    w_in = din("w_in", [D, 1952])
    rel_bias = din("rel_bias", [32, 8])
    w_q_up = din("w_q_up", [256, 768])
    w_kv_up = din("w_kv_up", [128, 1024])
    w_out = din("w_out", [D, D])
    w_rg = din("w_rg", [D, 4])
    w_re = din("w_re", [D, 32])
    w_gate = din("w_gate", [N_EXP, D, 256])
    w_up = din("w_up", [N_EXP, D, 256])
    w_down = din("w_down", [N_EXP, 256, D])
    g_attn = din("g_attn", [1, D])
    g_q = din("g_q", [1, 256])
    g_kv = din("g_kv", [1, 128])
    g_out = din("g_out", [1, D])
    g_ffn = din("g_ffn", [1, D])
    g_fin = din("g_fin", [1, D])
    b_r = din("b_r", [1, 36])
    rope_cs = din("rope_cs", [SEQ, 48])
    oh_bias = din("oh_bias", [33, 1536])
    out = nc.dram_tensor("out", [NTOK, D], F32, kind="ExternalOutput").ap()

    va_d = dscr("va_d", [NTOK, 520], BF16)
    oa_d = dscr("oa_d", [2, NTOK, 520], F32)
    mixb_d = dscr("mixb_d", [NTOK, 512], BF16, expose=True)
    mixa_d = dscr("mixa_d", [NTOK, 512], BF16, expose=True) if dbg else None
    x1_d = dscr("x1_d", [NTOK, D], F32, expose=True)
    fvec_d = dscr("fvec_d", [8, 1536], F32)
    xs_d = dscr("xs_d", [NROWS + 128, D], BF16)
    ys_d = dscr("ys_d", [NROWS + 128, D], BF16)

    def bcast_rows(ap, n):
        return bass.AP(tensor=ap.tensor, offset=0, ap=[[0, 128], [1, n]])

    with contextlib.ExitStack() as st:
        S = Sync(nc, st)

        uniq = [0]

        def sbuf(stk, name, shape, dt):
            uniq[0] += 1
            return stk.enter_context(nc.sbuf_tensor("%s_%d" % (name, uniq[0]), list(shape), dt))

        psbig = [st.enter_context(nc.psum_tensor("psb%d" % i, [128, 1024], F32)) for i in range(4)]
        bank = [psbig[i // 2][:, (i % 2) * 512:(i % 2 + 1) * 512] for i in range(8)]

        def PS(i):
            return ("ps", i)

        def bank_bf(i):
            return bank[i][:].bitcast(BF16)

        ident_f = sbuf(st, "ident_f", [128, 128], F32)
        ident_b = sbuf(st, "ident_b", [128, 128], BF16)
        jrev_f = sbuf(st, "jrev_f", [128, 128], F32)
        jrev_b = sbuf(st, "jrev_b", [128, 128], BF16)
        ones_f = sbuf(st, "ones_f", [128, 128], F32)
        ltri_f = sbuf(st, "ltri_f", [128, 128], F32)
        eps_t = sbuf(st, "eps_t", [128, 1], F32)
        rope_t = sbuf(st, "rope_t", [128, NT, 48], F32)
        wq_b = sbuf(st, "wq_b", [128, 2, 768], BF16)
        wkv_b = sbuf(st, "wkv_b", [128, 1024], BF16)
        wr_f = sbuf(st, "wr_f", [128, 8, 36], F32)
        g_q_t = sbuf(st, "g_q_t", [128, 256], F32)
        g_kv_t = sbuf(st, "g_kv_t", [128, 128], F32)
        g_out_t = sbuf(st, "g_out_t", [128, D], F32)

        S.op("pool", lambda e: e.memset(ident_f[:], 1.0), writes=["ident_f"])
        S.op("pool", lambda e: e.affine_select(out=ident_f[:], in_=ident_f[:], pattern=[[-1, 128]],
                                               compare_op=ALU.is_equal, fill=0.0, base=0,
                                               channel_multiplier=1), reads=["ident_f"], writes=["ident_f"])
        S.op("pool", lambda e: e.memset(jrev_f[:], 1.0), writes=["jrev_f"])
        S.op("pool", lambda e: e.affine_select(out=jrev_f[:], in_=jrev_f[:], pattern=[[1, 128]],
                                               compare_op=ALU.is_equal, fill=0.0, base=-127,
                                               channel_multiplier=1), reads=["jrev_f"], writes=["jrev_f"])
        S.op("pool", lambda e: e.memset(ones_f[:], 1.0), writes=["ones_f"])
        S.op("pool", lambda e: e.memset(ltri_f[:], 1.0), writes=["ltri_f"])
        S.op("pool", lambda e: e.affine_select(out=ltri_f[:], in_=ltri_f[:], pattern=[[1, 128]],
                                               compare_op=ALU.is_ge, fill=0.0, base=-1,
                                               channel_multiplier=-1), reads=["ltri_f"], writes=["ltri_f"])
        S.op("pool", lambda e: e.memset(eps_t[:], EPS), writes=["eps_t"])
        S.op("dve", lambda e: e.tensor_copy(out=ident_b[:], in_=ident_f[:]), reads=["ident_f"], writes=["ident_b"])
        S.op("dve", lambda e: e.tensor_copy(out=jrev_b[:], in_=jrev_f[:]), reads=["jrev_f"], writes=["jrev_b"])

        S.dma("sp", "c_rope", lambda e: e.dma_start(out=rope_t[:], in_=rope_cs.rearrange("(t p) c -> p t c", p=128)),
              writes=["rope_t"])
        S.dma("pool", "c_wq", lambda e: e.dma_start(out=wq_b[:], in_=w_q_up.rearrange("(c p) n -> p c n", p=128)),
              writes=["wq_b"])
        S.dma("pool", "c_wkv", lambda e: e.dma_start(out=wkv_b[:], in_=w_kv_up), writes=["wkv_b"])
        with nc.allow_non_contiguous_dma(reason="tiny router weights"):
            S.dma("sp", "c_wr", lambda e: e.dma_start(out=wr_f[:, :, 0:4], in_=w_rg.rearrange("(k p) n -> p k n", p=128)),
                  writes=["wr_f"])
            S.dma("sp", "c_wr", lambda e: e.dma_start(out=wr_f[:, :, 4:36], in_=w_re.rearrange("(k p) n -> p k n", p=128)),
                  writes=["wr_f"])
        S.dma("sp", "c_gq", lambda e: e.dma_start(out=g_q_t[:], in_=bcast_rows(g_q, 256)), writes=["g_q_t"])
        S.dma("sp", "c_gkv", lambda e: e.dma_start(out=g_kv_t[:], in_=bcast_rows(g_kv, 128)), writes=["g_kv_t"])
        S.dma("sp", "c_gout", lambda e: e.dma_start(out=g_out_t[:], in_=bcast_rows(g_out, D)), writes=["g_out_t"])

        with contextlib.ExitStack() as s0:
            relb = sbuf(s0, "relb", [33, 8], F32)
            oh_t = sbuf(s0, "oh_t", [33, 1536], F32)
            fvec_sb = sbuf(s0, "fvec_sb", [8, 1536], F32)
            S.op("pool", lambda e: e.memset(relb[32:33, :], NEG), writes=["relb32"])
            S.dma("sp", "c_relb", lambda e: e.dma_start(out=relb[0:32, :], in_=rel_bias), writes=["relb"])
            S.dma("sp", "c_oh", lambda e: e.dma_start(out=oh_t[:], in_=oh_bias), writes=["oh_t"])
            for di in range(3):
                S.op("pe", lambda e, di=di: e.matmul(bank[di][0:8, :], lhsT=relb[:, :], rhs=oh_t[:, di * 512:(di + 1) * 512],
                                                      start=True, stop=True),
                     reads=["relb", "relb32", "oh_t"], writes=[PS(di)])
                S.op("dve", lambda e, di=di: e.tensor_copy(out=fvec_sb[:, di * 512:(di + 1) * 512], in_=bank[di][0:8, :]),
                     reads=[PS(di)], writes=["fvec_sb"])
            S.dma("sp", "c_fv", lambda e: e.dma_start(out=fvec_d, in_=fvec_sb[:]), reads=["fvec_sb"], writes=["fvec_d"])
            S.barrier()

        def rstd_from_ss(ss_ap, n, lnv_ap, rstd_ap, rk, wk):
            S.op("act", lambda e: e.activation(out=lnv_ap, in_=ss_ap, func=AF.Ln, scale=1.0 / n, bias=eps_t[:, 0:1]),
                 reads=[rk, "eps_t"], writes=[wk + "_ln"])
            S.op("act", lambda e: e.activation(out=rstd_ap, in_=lnv_ap, func=AF.Exp, scale=-0.5),
                 reads=[wk + "_ln"], writes=[wk])

        wst = contextlib.ExitStack()
        w_in_b = sbuf(wst, "w_in_b", [128, 8, 1952], BF16)
        for k in range(8):
            S.dma("pool", "w_in", lambda e, k=k: e.dma_start(out=w_in_b[:, k, :], in_=w_in[k * 128:(k + 1) * 128, :]), writes=["w_in_b"])

        def seq_body(s):
            r0 = s * SEQ
            with contextlib.ExitStack() as sq:
                qaT = sbuf(sq, "qaT", [128, 4, SEQ], BF16)
                kaT = sbuf(sq, "kaT", [128, 4, SEQ], BF16)
                sqB = contextlib.ExitStack()
                cqnT = sbuf(sqB, "cqnT", [128, 2, SEQ], BF16)
                ckvnT = sbuf(sqB, "ckvnT", [128, SEQ], BF16)
                kbT = sbuf(sqB, "kbT", [96, 8, SEQ], BF16)
                Vb = sbuf(sqB, "Vb", [128, NT, 8, 65], BF16)
                S.op("pool", lambda e: e.memset(Vb[:, :, :, 64:65], 1.0), writes=["Vb_ones"])

                with contextlib.ExitStack() as p1:
                    g_attn_t = sbuf(p1, "g_attn_t", [128, D], F32)
                    hT = sbuf(p1, "hT", [128, 8, SEQ], BF16)
                    xt = [sbuf(p1, "xt%d" % i, [128, D], F32) for i in range(3)]
                    hb = [sbuf(p1, "hb%d" % i, [128, D], BF16) for i in range(3)]
                    junk = sbuf(p1, "junk", [128, 384], BF16)
                    st1 = sbuf(p1, "st1", [128, 3, 8], F32)
                    vt = [sbuf(p1, "vt%d" % i, [128, 8, 65], BF16) for i in range(2)]
                    cqn = [sbuf(p1, "cqn%d" % i, [128, 256], BF16) for i in range(3)]
                    ckvn = [sbuf(p1, "ckvn%d" % i, [128, 128], BF16) for i in range(3)]
                    ks = [sbuf(p1, "ks%d" % i, [128, 8, 96], BF16) for i in range(3)]
                    krs = [sbuf(p1, "krs%d" % i, [128, 32], F32) for i in range(3)]
                    rtmp = [sbuf(p1, "rtmp%d" % i, [128, 64], F32) for i in range(3)]

                    S.dma("sp", "gattn", lambda e: e.dma_start(out=g_attn_t[:], in_=bcast_rows(g_attn, D)), writes=["g_attn_t"])
                    for i in range(2):
                        S.op("pool", lambda e, i=i: e.memset(vt[i][:, :, 64:65], 1.0), writes=[("vt1", i)])

                    for t in range(NT):
                        b = t % 3
                        S.dma("sp", "xt%d" % b, lambda e, b=b, t=t: e.dma_start(out=xt[b][:], in_=x[r0 + t * 128:r0 + (t + 1) * 128, :]),
                              writes=[("xt", b)])
                        S.op("act", lambda e, b=b: e.activation(out=hb[b][:], in_=xt[b][:], func=AF.Square, accum_out=st1[:, b, 0:1]),
                             reads=[("xt", b)], writes=[("ss", b), ("hb", b)])
                        rstd_from_ss(st1[:, b, 0:1], D, st1[:, b, 1:2], st1[:, b, 2:3], ("ss", b), "rstd%d" % b)
                        S.op("dve", lambda e, b=b: e.scalar_tensor_tensor(out=hb[b][:], in0=xt[b][:], scalar=st1[:, b, 2:3], in1=g_attn_t[:],
                                                                        op0=ALU.mult, op1=ALU.mult),
                             reads=[("xt", b), "rstd%d" % b, "g_attn_t"], writes=[("hb", b)])
                        pb = t % 2
                        pv = bank_bf(pb).rearrange("p (k t) -> p k t", k=8)
                        for k in range(8):
                            S.op("pe", lambda e, k=k, b=b, pv=pv: e.transpose(out=pv[:, k, :], in_=hb[b][:, k * 128:(k + 1) * 128], identity=ident_b[:]),
                                 reads=[("hb", b), "ident_b"], writes=[PS(pb)], inc=(k == 7))
                        eng = "act" if t % 2 == 0 else "dve"
                        if eng == "act":
                            S.op("act", lambda e, t=t, pv=pv: e.activation(out=hT[:, :, t * 128:(t + 1) * 128], in_=pv, func=AF.Copy),
                                 reads=[PS(pb)], writes=[("hT", t)])
                        else:
                            S.op("dve", lambda e, t=t, pv=pv: e.tensor_copy(out=hT[:, :, t * 128:(t + 1) * 128], in_=pv),
                                 reads=[PS(pb)], writes=[("hT", t)])

                    rot = [2, 3, 4, 5]
                    rc = [0]
                    sub = {"P1a": 0, "P1b": 1, "P1c": 2, "P1d": 3}.get(stop_after, 9)

                    def nextbank():
                        bk = rot[rc[0] % len(rot)]
                        rc[0] += 1
                        return bk

                    evc = [0]

                    def evac(out_ap, in_ap, reads, writes, scale=1.0):
                        evc[0] += 1
                        if evc[0] % 2 == 0:
                            S.op("act", lambda e: e.activation(out=out_ap, in_=in_ap, func=AF.Copy, scale=scale), reads=reads, writes=writes)
                        else:
                            S.op("dve", lambda e: e.tensor_scalar(out=out_ap, in0=in_ap, scalar1=scale, scalar2=None, op0=ALU.mult),
                                 reads=reads, writes=writes)

                    for c in range(8 if sub >= 1 else 0):
                        for j in range(4):
                            bk = nextbank()
                            for k in range(8):
                                S.op("pe", lambda e, c=c, j=j, k=k, bk=bk: e.matmul(bank[bk][:], lhsT=w_in_b[:, k, c * 128:(c + 1) * 128],
                                                                                    rhs=hT[:, k, j * 512:(j + 1) * 512], start=(k == 0), stop=(k == 7)),
                                     reads=["w_in_b"] + [("hT", 4 * j + i) for i in range(4)], writes=[PS(bk)], inc=(k == 7))
                            if c < 4:
                                evac(qaT[:, c, j * 512:(j + 1) * 512], bank[bk][:], [PS(bk)], [("qaT", c, j)], scale=0.125)
                            else:
                                evac(kaT[:, c - 4, j * 512:(j + 1) * 512], bank[bk][:], [PS(bk)], [("kaT", c - 4, j)])
                    for t in range(NT if sub >= 2 else 0):
                        b = t % 2
                        bk = nextbank()
                        for k in range(8):
                            S.op("pe", lambda e, t=t, k=k, bk=bk: e.matmul(bank[bk][:], lhsT=hT[:, k, t * 128:(t + 1) * 128],
                                                                            rhs=w_in_b[:, k, 1024:1536], start=(k == 0), stop=(k == 7)),
                                 reads=["w_in_b", ("hT", t)], writes=[PS(bk)], inc=(k == 7))
                        evac(vt[b][:, :, 0:64], bank[bk][:].rearrange("p (h c) -> p h c", h=8), [PS(bk), ("vt1", b)], [("vt", b)])
                        S.dma("sp", "vt%d" % b, lambda e, b=b, t=t: e.dma_start(out=va_d[r0 + t * 128:r0 + (t + 1) * 128, :],
                                                                                  in_=vt[b][:].rearrange("p h c -> p (h c)")),
                              reads=[("vt", b)], writes=[("va_d", t)])
                    for t in range((NT if K2C >= 99 else 1) if sub >= 3 else 0):
                        b = t % 3
                        bk = nextbank()
                        if K2C >= 1:
                            for k in range(8):
                                S.op("pe", lambda e, t=t, k=k, bk=bk: e.matmul(bank[bk][:, 0:416], lhsT=hT[:, k, t * 128:(t + 1) * 128],
                                                                                rhs=w_in_b[:, k, 1536:1952], start=(k == 0), stop=(k == 7)),
                                     reads=["w_in_b", ("hT", t)], writes=[PS(bk)], inc=(k == 7))
                        if K2C >= 2:
                            S.op("act", lambda e, b=b, bk=bk: e.activation(out=junk[:, 0:256], in_=bank[bk][:, 0:256], func=AF.Square,
                                                                            accum_out=st1[:, b, 3:4]), reads=[PS(bk)], writes=[("ssq", b)])
                        if K2C >= 2:
                            S.op("act", lambda e, b=b, bk=bk: e.activation(out=junk[:, 256:384], in_=bank[bk][:, 256:384], func=AF.Square,
                                                                            accum_out=st1[:, b, 5:6]), reads=[PS(bk)], writes=[("sskv", b)])
                        if K2C >= 3:
                            rstd_from_ss(st1[:, b, 3:4], 256, st1[:, b, 4:5], st1[:, b, 4:5], ("ssq", b), "rq%d" % b)
                        if K2C >= 3:
                            rstd_from_ss(st1[:, b, 5:6], 128, st1[:, b, 6:7], st1[:, b, 6:7], ("sskv", b), "rkv%d" % b)
                        if K2C >= 4:
                            S.op("dve", lambda e, b=b, bk=bk: e.scalar_tensor_tensor(out=cqn[b][:], in0=bank[bk][:, 0:256], scalar=st1[:, b, 4:5],
                                                                                      in1=g_q_t[:], op0=ALU.mult, op1=ALU.mult),
                                 reads=[PS(bk), "rq%d" % b, "g_q_t"], writes=[("cqn", b)])
                        if K2C >= 4:
                            S.op("dve", lambda e, b=b, bk=bk: e.scalar_tensor_tensor(out=ckvn[b][:], in0=bank[bk][:, 256:384], scalar=st1[:, b, 6:7],
                                                                                      in1=g_kv_t[:], op0=ALU.mult, op1=ALU.mult),
                                 reads=[PS(bk), "rkv%d" % b, "g_kv_t"], writes=[("ckvn", b)])
                        if K2C >= 5:
                            S.op("dve", lambda e, b=b, bk=bk: e.tensor_copy(out=krs[b][:], in_=bank[bk][:, 384:416]),
                                 reads=[PS(bk)], writes=[("krs", b)])
                        if K2C >= 5:
                            S.op("dve", lambda e, b=b, t=t: e.tensor_tensor(out=rtmp[b][:, 0:32], in0=krs[b][:], in1=rope_t[:, t, 0:32], op=ALU.mult),
                                 reads=[("krs", b), "rope_t"], writes=[("rtA", b)])
                        if K2C >= 5:
                            S.op("dve", lambda e, b=b, t=t: e.tensor_tensor(out=rtmp[b][:, 32:48], in0=krs[b][:, 16:32], in1=rope_t[:, t, 32:48], op=ALU.mult),
                                 reads=[("krs", b), "rope_t"], writes=[("rtB", b)])
                        if K2C >= 5:
                            S.op("dve", lambda e, b=b, t=t: e.tensor_tensor(out=rtmp[b][:, 48:64], in0=krs[b][:, 0:16], in1=rope_t[:, t, 32:48], op=ALU.mult),
                                 reads=[("krs", b), "rope_t"], writes=[("rtC", b)])
                        S.op("dve", lambda e, b=b: e.tensor_tensor(out=ks[b][:, :, 64:80], in0=rtmp[b][:, 0:16].unsqueeze(1).broadcast_to([128, 8, 16]),
                                                                    in1=rtmp[b][:, 32:48].unsqueeze(1).broadcast_to([128, 8, 16]), op=ALU.subtract),
                             reads=[("rtA", b), ("rtB", b)], writes=[("ks_r", b, 0)])
                        S.op("dve", lambda e, b=b: e.tensor_tensor(out=ks[b][:, :, 80:96], in0=rtmp[b][:, 16:32].unsqueeze(1).broadcast_to([128, 8, 16]),
                                                                    in1=rtmp[b][:, 48:64].unsqueeze(1).broadcast_to([128, 8, 16]), op=ALU.add),
                             reads=[("rtA", b), ("rtC", b)], writes=[("ks_r", b, 1)])
                        pb = t % 2
                        pv = bank_bf(pb).rearrange("p (k t) -> p k t", k=8)
                        S.op("pe", lambda e, b=b, pv=pv: e.transpose(out=pv[:, 0, :], in_=cqn[b][:, 0:128], identity=ident_b[:]),
                             reads=[("cqn", b), "ident_b"], writes=[PS(pb)], inc=False)
                        S.op("pe", lambda e, b=b, pv=pv: e.transpose(out=pv[:, 1, :], in_=cqn[b][:, 128:256], identity=ident_b[:]),
                             reads=[("cqn", b)], writes=[PS(pb)], inc=False)
                        S.op("pe", lambda e, b=b, pv=pv: e.transpose(out=pv[:, 2, :], in_=ckvn[b][:], identity=ident_b[:]),
                             reads=[("ckvn", b)], writes=[PS(pb)], inc=True)
                        S.op("act", lambda e, t=t, pv=pv: e.activation(out=cqnT[:, :, t * 128:(t + 1) * 128], in_=pv[:, 0:2, :], func=AF.Copy),
                             reads=[PS(pb)], writes=[("cqnT", t)])
                        S.op("dve", lambda e, t=t, pv=pv: e.tensor_copy(out=ckvnT[:, t * 128:(t + 1) * 128], in_=pv[:, 2, :]),
                             reads=[PS(pb)], writes=[("ckvnT", t)])
                        for half in range(2):
                            bk2 = nextbank()
                            S.op("pe", lambda e, t=t, half=half, bk2=bk2: e.matmul(bank[bk2][:], lhsT=ckvnT[:, t * 128:(t + 1) * 128],
                                                                                    rhs=wkv_b[:, half * 512:(half + 1) * 512], start=True, stop=True),
                                 reads=[("ckvnT", t), "wkv_b"], writes=[PS(bk2)])
                            kvv = bank[bk2][:].rearrange("p (h c) -> p h c", h=4)
                            S.op("act", lambda e, b=b, half=half, kvv=kvv: e.activation(out=ks[b][:, half * 4:(half + 1) * 4, 0:64], in_=kvv[:, :, 0:64], func=AF.Copy),
                                 reads=[PS(bk2)], writes=[("ks_n", b, half)])
                            S.op("dve", lambda e, t=t, half=half, kvv=kvv: e.tensor_copy(out=Vb[:, t, half * 4:(half + 1) * 4, 0:64], in_=kvv[:, :, 64:128]),
                                 reads=[PS(bk2), "Vb_ones"], writes=[("Vb", t, half)])
                        pb2 = 6 + t % 2
                        pv2 = bank_bf(pb2).rearrange("p (k t) -> p k t", k=8)
                        for h in range(8):
                            S.op("pe", lambda e, h=h, b=b, pv2=pv2: e.transpose(out=pv2[0:96, h, :], in_=ks[b][:, h, :], identity=ident_b[:]),
                                 reads=[("ks_n", b, 0), ("ks_n", b, 1), ("ks_r", b, 0), ("ks_r", b, 1), "ident_b"], writes=[PS(pb2)], inc=(h == 7))
                        S.op("act", lambda e, t=t, pv2=pv2: e.activation(out=kbT[:, :, t * 128:(t + 1) * 128], in_=pv2[0:96, :, :], func=AF.Copy),
                             reads=[PS(pb2)], writes=[("kbT", t)])
                    S.barrier()
                if stop_after in ("P1", "P1a", "P1b", "P1c", "P1d"):
                    sqB.close()
                    return

                with contextlib.ExitStack() as p3:
                    qbT = [sbuf(p3, "qbT%d" % i, [96, 8, 512], BF16) for i in range(2)]
                    qs = [sbuf(p3, "qs%d" % i, [128, 8, 96], BF16) for i in range(2)]
                    qr = [sbuf(p3, "qr%d" % i, [128, 8, 32], F32) for i in range(2)]
                    qtm = [sbuf(p3, "qtm%d" % i, [128, 8, 64], F32) for i in range(2)]
                    PT = [sbuf(p3, "PT%d" % i, [128, 1024], BF16) for i in range(3)]
                    ob = [sbuf(p3, "ob%d" % i, [128, 4, 8, 65], F32) for i in range(2)]
                    rl = sbuf(p3, "rl", [128, 8], F32)
                    onb = sbuf(p3, "onb", [128, 8, 64], F32)
                    junk3 = sbuf(p3, "junk3", [128, 512], BF16)
                    st3 = sbuf(p3, "st3", [128, 4], F32)
                    mb = [sbuf(p3, "mb%d" % i, [128, 512], BF16) for i in range(2)]

                    def mla_qproj(jq, tts=(0, 1, 2, 3), stages="ab"):
                        qb = jq % 2
                        for tt in tts:
                            t = jq * 4 + tt
                            b2 = tt % 2
                            if "a" not in stages:
                                mla_qproj_b(jq, tt)
                                continue
                            for half in range(2):
                                for c in range(2):
                                    S.op("pe", lambda e, half=half, c=c, t=t: e.matmul(bank[6 + half][:, 0:384], lhsT=cqnT[:, c, t * 128:(t + 1) * 128],
                                                                                       rhs=wq_b[:, c, half * 384:(half + 1) * 384], start=(c == 0), stop=(c == 1)),
                                         reads=["wq_b"], writes=[PS(6 + half)], inc=(c == 1))
                                pvh = bank[6 + half][:, 0:384].rearrange("p (h c) -> p h c", h=4)
                                S.op("dve", lambda e, half=half, b2=b2, pvh=pvh: e.tensor_copy(out=qs[b2][:, half * 4:(half + 1) * 4, 0:64], in_=pvh[:, :, 0:64]),
                                     reads=[PS(6 + half)], writes=[("qs_n", b2, half)])
                                S.op("dve", lambda e, half=half, b2=b2, pvh=pvh: e.tensor_copy(out=qr[b2][:, half * 4:(half + 1) * 4, :], in_=pvh[:, :, 64:96]),
                                     reads=[PS(6 + half)], writes=[("qr", b2, half)])
                            cos2 = rope_t[:, t, 0:32].unsqueeze(1).broadcast_to([128, 8, 32])
                            sin1 = rope_t[:, t, 32:48].unsqueeze(1).broadcast_to([128, 8, 16])
                            qrk = [("qr", b2, 0), ("qr", b2, 1)]
                            S.op("dve", lambda e, b2=b2, cos2=cos2: e.tensor_tensor(out=qtm[b2][:, :, 0:32], in0=qr[b2][:], in1=cos2, op=ALU.mult),
                                 reads=qrk, writes=[("qtA", b2)])
                            S.op("dve", lambda e, b2=b2, sin1=sin1: e.tensor_tensor(out=qtm[b2][:, :, 32:48], in0=qr[b2][:, :, 16:32], in1=sin1, op=ALU.mult),
                                 reads=qrk, writes=[("qtB", b2)])
                            S.op("dve", lambda e, b2=b2, sin1=sin1: e.tensor_tensor(out=qtm[b2][:, :, 48:64], in0=qr[b2][:, :, 0:16], in1=sin1, op=ALU.mult),
                                 reads=qrk, writes=[("qtC", b2)])
                            S.op("dve", lambda e, b2=b2: e.tensor_tensor(out=qs[b2][:, :, 64:80], in0=qtm[b2][:, :, 0:16], in1=qtm[b2][:, :, 32:48], op=ALU.subtract),
                                 reads=[("qtA", b2), ("qtB", b2)], writes=[("qs_r", b2, 0)])
                            S.op("dve", lambda e, b2=b2: e.tensor_tensor(out=qs[b2][:, :, 80:96], in0=qtm[b2][:, :, 16:32], in1=qtm[b2][:, :, 48:64], op=ALU.add),
                                 reads=[("qtA", b2), ("qtC", b2)], writes=[("qs_r", b2, 1)])
                            if "b" in stages:
                                mla_qproj_b(jq, tt)

                    def mla_qproj_b(jq, tt):
                        qb = jq % 2
                        b2 = tt % 2
                        pv = bank_bf(6).rearrange("p (k t) -> p k t", k=8)
                        for h in range(8):
                            S.op("pe", lambda e, h=h: e.transpose(out=pv[0:96, h, :], in_=qs[b2][:, h, :], identity=ident_b[:]),
                                 reads=[("qs_n", b2, 0), ("qs_n", b2, 1), ("qs_r", b2, 0), ("qs_r", b2, 1)], writes=[PS(6)], inc=(h == 7))
                        S.op("dve", lambda e: e.tensor_copy(out=qbT[qb][:, :, tt * 128:(tt + 1) * 128], in_=pv[0:96, :, :]),
                             reads=[PS(6)], writes=[("qbT", qb, tt)])

                    def mla_epilogue(jq, qts=(0, 1, 2, 3), stages="abc"):
                        obj = ob[jq % 2]
                        obk = [("ob", jq % 2, h) for h in range(8)]
                        for qt in qts:
                            t = jq * 4 + qt
                            m = qt % 2
                            if "a" in stages:
                                S.op("dve", lambda e, qt=qt: e.reciprocal(out=rl[:], in_=obj[:, qt, :, 64]), reads=obk, writes=["rl"])
                                S.op("dve", lambda e, qt=qt: e.tensor_tensor(out=onb[:], in0=obj[:, qt, :, 0:64], in1=rl[:].unsqueeze(2).broadcast_to([128, 8, 64]), op=ALU.mult),
                                     reads=["rl"] + obk, writes=["onb"])
                            if "b" in stages:
                                S.op("act", lambda e: e.activation(out=junk3[:], in_=onb[:].rearrange("p h c -> p (h c)"), func=AF.Square, accum_out=st3[:, 0:1]),
                                     reads=["onb"], writes=["ss3"])
                                rstd_from_ss(st3[:, 0:1], 512, st3[:, 1:2], st3[:, 2:3], "ss3", "rstd3")
                            if "c" in stages:
                                S.op("dve", lambda e, m=m: e.scalar_tensor_tensor(out=mb[m][:], in0=onb[:].rearrange("p h c -> p (h c)"), scalar=st3[:, 2:3],
                                                                                   in1=g_out_t[:, 512:1024], op0=ALU.mult, op1=ALU.mult),
                                     reads=["onb", "rstd3", "g_out_t"], writes=[("mb", m)])
                                S.dma("sp", "mb%d" % m, lambda e, m=m, t=t: e.dma_start(out=mixb_d[r0 + t * 128:r0 + (t + 1) * 128, :], in_=mb[m][:]),
                                      reads=[("mb", m)], writes=[("mixb_d", t)])

                    scale_b = 96.0 ** -0.5
                    NU = 8 * (NT // 2)
                    units = [(jq, h, ktp) for jq in range(4) for h in range(8) for ktp in range(NT // 2)]

                    def mla_S(g):
                        jq, h, ktp = units[g]
                        qb = jq % 2
                        pb = g % 2
                        for j in range(2):
                            kt = 2 * ktp + j
                            S.op("pe", lambda e, j=j, kt=kt: e.matmul(bank[2 * pb + j][:], lhsT=kbT[:, h, kt * 128:(kt + 1) * 128], rhs=qbT[qb][:, h, :],
                                                                      start=True, stop=True),
                                 reads=[("qbT", qb, i) for i in range(4)], writes=[PS(2 * pb + j)], inc=(j == 1))

                    def mla_PV(g):
                        jq, h, ktp = units[g]
                        pb = g % 2
                        pti = g % 3
                        pt = PT[pti]
                        accb = 4 + (h % 2)
                        accv = bank[accb][:, 0:260].rearrange("p (q c) -> p q c", q=4)
                        S.op("act", lambda e: e.activation(out=pt[:], in_=psbig[pb][:], func=AF.Exp, scale=scale_b),
                             reads=[PS(2 * pb), PS(2 * pb + 1)], writes=[("PT", pti)])
                        for j in range(2):
                            kt = 2 * ktp + j
                            for qt in range(4):
                                S.op("pe", lambda e, j=j, kt=kt, qt=qt: e.matmul(accv[:, qt, :], lhsT=pt[:, j * 512 + qt * 128:j * 512 + (qt + 1) * 128],
                                                                                  rhs=Vb[:, kt, h, :], start=(kt == 0 and qt == 0), stop=(kt == NT - 1),
                                                                                  skip_group_check=True),
                                     reads=[("PT", pti)], writes=[PS(accb)], inc=(j == 1 and qt == 3))
                        if ktp == NT // 2 - 1:
                            S.op("dve", lambda e: e.tensor_copy(out=ob[jq % 2][:, :, h, :], in_=accv), reads=[PS(accb)], writes=[("ob", jq % 2, h)])

                    sched = {}

                    def at(u, fn):
                        sched.setdefault(u, []).append(fn)

                    for jq in range(4):
                        base = jq * NU
                        if jq > 0:
                            for qt in range(4):
                                u = base + 4 + 6 * qt
                                at(u, lambda jq=jq, qt=qt: mla_epilogue(jq - 1, (qt,), "a"))
                                at(u + 3, lambda jq=jq, qt=qt: mla_epilogue(jq - 1, (qt,), "b"))
                                at(u + 5, lambda jq=jq, qt=qt: mla_epilogue(jq - 1, (qt,), "c"))
                        if jq < 3:
                            for tt in range(4):
                                u = base + 30 + 8 * tt
                                at(u, lambda jq=jq, tt=tt: mla_qproj(jq + 1, (tt,), "a"))
                                at(u + 5, lambda jq=jq, tt=tt: mla_qproj(jq + 1, (tt,), "b"))
                    mla_qproj(0)
                    mla_S(0)
                    for g in range(len(units)):
                        if g + 1 < len(units):
                            mla_S(g + 1)
                        mla_PV(g)
                        for fn in sched.get(g, ()):
                            fn()
                    mla_epilogue(3)
                    S.barrier()
                if stop_after == "B":
                    sqB.close()
                    return

                sqB.close()
                with contextlib.ExitStack() as p4:
                    trev = sbuf(p4, "trev", [128, 3, 8, 384], BF16)
                    for di in range(3):
                        src = bass.AP(tensor=fvec_d.tensor, offset=di * 512, ap=[[1, 128], [1536, 8], [1, 384]])
                        S.dma("pool", "trev", lambda e, di=di, src=src: e.dma_start(out=trev[:, di, :, :], in_=src), writes=["trev"])
                    Vw = [sbuf(p4, "Vw%d" % i, [128, 4, 2, 520], BF16) for i in range(2)]
                    PTa = [sbuf(p4, "PTa%d" % i, [128, 512], BF16) for i in range(4)]
                    oa = [sbuf(p4, "oa%d" % i, [128, 4, 8, 65], F32) for i in range(2)]
                    o16 = [sbuf(p4, "o16_%d" % i, [128, 520], F32) for i in range(2)]
                    o4 = [sbuf(p4, "o4_%d" % i, [128, 520], F32) for i in range(2)]
                    ma_sb = sbuf(p4, "ma_sb", [128, NT, 512], BF16)
                    rl4 = sbuf(p4, "rl4", [128, 8], F32)
                    onb4 = sbuf(p4, "onb4", [128, 8, 64], F32)
                    junk4 = sbuf(p4, "junk4", [128, 512], BF16)
                    st4 = sbuf(p4, "st4", [128, 4], F32)
                    w_out_b = sbuf(p4, "w_out_b", [128, 8, D], BF16)
                    mbt = [sbuf(p4, "mbt%d" % i, [128, 512], BF16) for i in range(2)]
                    xt5 = [sbuf(p4, "xt5_%d" % i, [128, D], F32) for i in range(2)]
                    mixT = [sbuf(p4, "mixT%d" % i, [128, 8, 128], BF16) for i in range(2)]
                    x1t = [sbuf(p4, "x1t%d" % i, [128, D], F32) for i in range(2)]

                    for k in range(8):
                        S.dma("pool", "w_out", lambda e, k=k: e.dma_start(out=w_out_b[:, k, :], in_=w_out[k * 128:(k + 1) * 128, :]), writes=["w_out_b"])

                    unitsA = [(di, d, g, h) for di, d in enumerate(PATTERNS) for g in range(4) for h in range(8)]

                    def tiles_of(d, g):
                        L = SEQ // d
                        tps = L // 128
                        npc = 2 if L >= 256 else 1
                        tl = []
                        for qt in range(4):
                            qidx = g * 4 + qt
                            r, ti = qidx // tps, qidx % tps
                            wb = min(max(ti * 128 - 64, 0), L - 128 * npc)
                            tl.append((r, ti, wb))
                        return tl, npc

                    def A_S(n):
                        di, d, g, h = unitsA[n]
                        tiles, npc = tiles_of(d, g)
                        vb = (n // 8) % 2
                        if h == 0:
                            for qt in range(4):
                                r, ti, wb = tiles[qt]
                                src = bass.AP(tensor=va_d.tensor, offset=(r0 + r + d * wb) * 520,
                                              ap=[[d * 520, 128], [128 * d * 520, npc], [1, 520]])
                                S.dma("sp", "Vw%d_%d" % (vb, qt), lambda e, qt=qt, src=src: e.dma_start(out=Vw[vb][:, qt, 0:npc, :], in_=src),
                                      writes=[("Vw", vb, qt)])
                        pair, hb_ = h // 2, (h % 2) * 64
                        set_ = n % 2
                        for qt in range(4):
                            r, ti, wb = tiles[qt]
                            qs_ = r + d * ti * 128
                            qap = qaT[hb_:hb_ + 64, pair, qs_:qs_ + d * 127 + 1:d]
                            for pc in range(npc):
                                bk = 2 * set_ + pc
                                ks_ = r + d * (wb + pc * 128)
                                kap = kaT[hb_:hb_ + 64, pair, ks_:ks_ + d * 127 + 1:d]
                                S.op("pe", lambda e, bk=bk, qt=qt, kap=kap, qap=qap: e.matmul(bank[bk][:, qt * 128:(qt + 1) * 128], lhsT=kap, rhs=qap,
                                                                                             start=(qt == 0), stop=False, skip_group_check=True),
                                     writes=[PS(bk)], inc=False)
                        for qt in range(4):
                            r, ti, wb = tiles[qt]
                            for pc in range(npc):
                                bk = 2 * set_ + pc
                                j0 = wb + pc * 128 - ti * 128 + 128
                                S.op("pe", lambda e, bk=bk, qt=qt, j0=j0: e.matmul(bank[bk][:, qt * 128:(qt + 1) * 128],
                                                                                  lhsT=trev[:, di, h, j0:j0 + 128], rhs=jrev_b[:],
                                                                                  start=False, stop=True, skip_group_check=True),
                                     reads=["trev", "jrev_b"], writes=[PS(bk)], inc=(qt == 3))

                    def A_PV(n):
                        di, d, g, h = unitsA[n]
                        tiles, npc = tiles_of(d, g)
                        vb = (n // 8) % 2
                        set_ = n % 2
                        accb = 4 + (n % 2)
                        for pc in range(npc):
                            bk = 2 * set_ + pc
                            S.op("act", lambda e, bk=bk: e.activation(out=PTa[bk][:], in_=bank[bk][:], func=AF.Exp),
                                 reads=[PS(bk)], writes=[("PTa", bk)])
                        accv = bank[accb][:, 0:260].rearrange("p (q c) -> p q c", q=4)
                        for qt in range(4):
                            for pc in range(npc):
                                bk = 2 * set_ + pc
                                S.op("pe", lambda e, bk=bk, qt=qt, pc=pc: e.matmul(
                                    accv[:, qt, :], lhsT=PTa[bk][:, qt * 128:(qt + 1) * 128], rhs=Vw[vb][:, qt, pc, h * 65:(h + 1) * 65],
                                    start=(pc == 0), stop=(pc == npc - 1), skip_group_check=True),
                                     reads=[("PTa", bk), ("Vw", vb, qt)], writes=[PS(accb)], inc=(qt == 3 and pc == npc - 1))
                        S.op("dve", lambda e: e.tensor_copy(out=oa[vb][:, :, h, :], in_=accv), reads=[PS(accb)], writes=[("oa", vb, h)])
                        if h != 7:
                            return
                        oak = [("oa", vb, hh) for hh in range(8)]
                        if d != 1:
                            for qt in range(4):
                                r, ti, wb = tiles[qt]
                                dst = bass.AP(tensor=oa_d.tensor, offset=(di * NTOK + r0 + r + d * ti * 128) * 520, ap=[[d * 520, 128], [1, 520]])
                                S.dma("sp", "oaw%d" % vb, lambda e, qt=qt, dst=dst: e.dma_start(out=dst, in_=oa[vb][:, qt].rearrange("p h c -> p (h c)")),
                                      reads=oak, writes=[("oa_dw", vb)])
                            return
                        if Gd1(n) == 0:
                            merge_loads(0)
                            merge_loads(1)

                    def Gd1(n):
                        return (n - 64) // 8

                    def merge_loads(K):
                        t = K
                        m = K % 2
                        S.dma("pool", "o16_%d" % m, lambda e: e.dma_start(out=o16[m][:], in_=oa_d[0, r0 + t * 128:r0 + (t + 1) * 128, :]),
                              reads=[("oa_dw", 0), ("oa_dw", 1)], writes=[("o16", m)])
                        S.dma("pool", "o4_%d" % m, lambda e: e.dma_start(out=o4[m][:], in_=oa_d[1, r0 + t * 128:r0 + (t + 1) * 128, :]),
                              reads=[("oa_dw", 0), ("oa_dw", 1)], writes=[("o4", m)])

                    def epiA(K):
                        Gd, qt = K // 4, K % 4
                        vb = Gd % 2
                        m = K % 2
                        oak = [("oa", vb, hh) for hh in range(8)]
                        S.op("dve", lambda e: e.tensor_tensor(out=o16[m][:], in0=o16[m][:], in1=o4[m][:], op=ALU.add),
                             reads=[("o16", m), ("o4", m)], writes=[("o16", m)])
                        S.op("dve", lambda e: e.tensor_tensor(out=o16[m][:], in0=o16[m][:], in1=oa[vb][:, qt].rearrange("p h c -> p (h c)"), op=ALU.add),
                             reads=[("o16", m)] + oak, writes=[("o16", m)])
                        ov = o16[m][:].rearrange("p (h c) -> p h c", h=8)
                        S.op("dve", lambda e: e.reciprocal(out=rl4[:], in_=ov[:, :, 64]), reads=[("o16", m)], writes=["rl4"])
                        S.op("dve", lambda e: e.tensor_tensor(out=onb4[:], in0=ov[:, :, 0:64], in1=rl4[:].unsqueeze(2).broadcast_to([128, 8, 64]), op=ALU.mult),
                             reads=["rl4", ("o16", m)], writes=["onb4"])
                        if K + 2 < 16:
                            merge_loads(K + 2)

                    def epiB(K):
                        S.op("act", lambda e: e.activation(out=junk4[:], in_=onb4[:].rearrange("p h c -> p (h c)"), func=AF.Square, accum_out=st4[:, 0:1]),
                             reads=["onb4"], writes=["ss4"])
                        rstd_from_ss(st4[:, 0:1], 512, st4[:, 1:2], st4[:, 2:3], "ss4", "rstd4")

                    def epiC(K):
                        t = K
                        S.op("dve", lambda e: e.scalar_tensor_tensor(out=ma_sb[:, t, :], in0=onb4[:].rearrange("p h c -> p (h c)"), scalar=st4[:, 2:3],
                                                                      in1=g_out_t[:, 0:512], op0=ALU.mult, op1=ALU.mult),
                             reads=["onb4", "rstd4", "g_out_t"], writes=[("ma", t)])
                        if dbg:
                            S.dma("sp", "dbgma", lambda e: e.dma_start(out=mixa_d[r0 + t * 128:r0 + (t + 1) * 128, :], in_=ma_sb[:, t, :]),
                                  reads=[("ma", t)])

                    schedA = {}
                    for K in range(16):
                        Gd, qt = K // 4, K % 4
                        u = 64 + 8 * (Gd + 1) + 2 * qt
                        schedA.setdefault(u, []).append(lambda K=K: epiA(K))
                        schedA.setdefault(u + 1, []).append(lambda K=K: epiB(K))
                        schedA.setdefault(u + 2, []).append(lambda K=K: epiC(K))

                    A_S(0)
                    for n in range(len(unitsA)):
                        if n + 1 < len(unitsA):
                            A_S(n + 1)
                        A_PV(n)
                        for fn in schedA.pop(n, ()):
                            fn()
                    for u in sorted(schedA):
                        for fn in schedA[u]:
                            fn()
                    for t in range(NT):
                        b = t % 2
                        S.dma("sp", "mbt%d" % b, lambda e, b=b, t=t: e.dma_start(out=mbt[b][:], in_=mixb_d[r0 + t * 128:r0 + (t + 1) * 128, :]), writes=[("mbt", b)])
                        S.dma("sp", "xt5_%d" % b, lambda e, b=b, t=t: e.dma_start(out=xt5[b][:], in_=x[r0 + t * 128:r0 + (t + 1) * 128, :]), writes=[("xt5", b)])
                        pv = bank_bf(6).rearrange("p (k t) -> p k t", k=8)
                        for k in range(4):
                            S.op("pe", lambda e, k=k, t=t, pv=pv: e.transpose(out=pv[:, k, :], in_=ma_sb[:, t, k * 128:(k + 1) * 128], identity=ident_b[:]),
                                 reads=[("ma", t), "ident_b"], writes=[PS(6)], inc=False)
                        for k in range(4):
                            S.op("pe", lambda e, k=k, b=b, pv=pv: e.transpose(out=pv[:, 4 + k, :], in_=mbt[b][:, k * 128:(k + 1) * 128], identity=ident_b[:]),
                                 reads=[("mbt", b)], writes=[PS(6)], inc=(k == 3))
                        S.op("act", lambda e, b=b, pv=pv: e.activation(out=mixT[b][:], in_=pv, func=AF.Copy), reads=[PS(6)], writes=[("mixT", b)])
                        for half in range(2):
                            for k in range(8):
                                S.op("pe", lambda e, half=half, k=k, b=b: e.matmul(bank[half][:], lhsT=mixT[b][:, k, :], rhs=w_out_b[:, k, half * 512:(half + 1) * 512],
                                                                                  start=(k == 0), stop=(k == 7)),
                                     reads=[("mixT", b), "w_out_b"], writes=[PS(half)], inc=(k == 7))
                            S.op("dve", lambda e, half=half, b=b: e.tensor_tensor(out=x1t[b][:, half * 512:(half + 1) * 512], in0=bank[half][:],
                                                                                 in1=xt5[b][:, half * 512:(half + 1) * 512], op=ALU.add),
                                 reads=[PS(half), ("xt5", b)], writes=[("x1t", b, half)])
                        S.dma("pool", "x1t%d" % b, lambda e, b=b, t=t: e.dma_start(out=x1_d[r0 + t * 128:r0 + (t + 1) * 128, :], in_=x1t[b][:]),
                              reads=[("x1t", b, 0), ("x1t", b, 1)], writes=[("x1_d", s, t)])
                    S.barrier()


        for s_ in range(NSEQ if stop_after is None else (0 if stop_after == "P0" else 1)):
            seq_body(s_)
        wst.close()

        if stop_after is None:
          with contextlib.ExitStack() as pm:
            NTT = NTOK // 128
            g_ffn_t = sbuf(pm, "g_ffn_t", [128, D], F32)
            g_fin_t = sbuf(pm, "g_fin_t", [128, D], F32)
            b_r_t = sbuf(pm, "b_r_t", [128, 36], F32)
            h2b = sbuf(pm, "h2b", [128, NTT, D], BF16)
            gate1 = sbuf(pm, "gate1", [128, NTT], F32)
            gate2 = sbuf(pm, "gate2", [128, NTT], F32)
            d1i = sbuf(pm, "d1i", [128, NTT], I32)
            d2i = sbuf(pm, "d2i", [128, NTT], I32)
            S.dma("sp", "gffn", lambda e: e.dma_start(out=g_ffn_t[:], in_=bcast_rows(g_ffn, D)), writes=["g_ffn_t"])
            S.dma("sp", "gfin", lambda e: e.dma_start(out=g_fin_t[:], in_=bcast_rows(g_fin, D)), writes=["g_fin_t"])
            S.dma("sp", "brt", lambda e: e.dma_start(out=b_r_t[:], in_=bcast_rows(b_r, 36)), writes=["b_r_t"])
            zt = sbuf(pm, "zt", [128, D], BF16)
            S.op("pool", lambda e: e.memset(zt[:], 0.0), writes=["zt"])
            S.dma("sp", "zfilly", lambda e: e.dma_start(out=ys_d[NROWS:NROWS + 128, :], in_=zt[:]), reads=["zt"], writes=["ys_d"])
            xs_v = xs_d.rearrange("(n p) d -> p n d", p=128)
            NCH = (NROWS + 128) // 128
            for c0 in range(0, NCH, 16):
                c1 = min(c0 + 16, NCH)
                S.dma("sp", "zfill", lambda e, c0=c0, c1=c1: e.dma_start(out=xs_v[:, c0:c1, :], in_=zt[:].unsqueeze(1).broadcast_to([128, c1 - c0, D])),
                      reads=["zt"], writes=["xs_zf"])
            NW = 3
            wgu = [sbuf(pm, "wgu%d" % i, [128, 8, 512], BF16) for i in range(NW)]
            wd = [sbuf(pm, "wd%d" % i, [128, 2, D], BF16) for i in range(NW)]

            def load_w(ex):
                wi = ex % NW
                S.dma("pool", "wg%d" % wi, lambda e: e.dma_start(out=wgu[wi][:, :, 0:256], in_=w_gate[ex].rearrange("(k p) f -> p k f", p=128)),
                      writes=[("wgu", wi)])
                S.dma("pool", "wg%d" % wi, lambda e: e.dma_start(out=wgu[wi][:, :, 256:512], in_=w_up[ex].rearrange("(k p) f -> p k f", p=128)),
                      writes=[("wgu", wi)])
                S.dma("pool", "wd%d" % wi, lambda e: e.dma_start(out=wd[wi][:], in_=w_down[ex].rearrange("(c p) n -> p c n", p=128)),
                      writes=[("wd", wi)])

            for ex in range(NW):
                load_w(ex)
            with contextlib.ExitStack() as m1:
                mxt = [sbuf(m1, "mxt%d" % i, [128, D], F32) for i in range(2)]
                h2f = [sbuf(m1, "h2f%d" % i, [128, D], F32) for i in range(2)]
                h2lo = [sbuf(m1, "h2lo%d" % i, [128, D], BF16) for i in range(2)]
                hiT = [sbuf(m1, "hiT%d" % i, [128, 8, 128], BF16) for i in range(2)]
                loT = [sbuf(m1, "loT%d" % i, [128, 8, 128], BF16) for i in range(2)]
                lgt = [sbuf(m1, "lgt%d" % i, [128, 36], F32) for i in range(2)]
                wr2 = sbuf(m1, "wr2", [128, 8, 72], BF16)
                wrd = sbuf(m1, "wrd", [128, 8, 36], F32)
                S.op("dve", lambda e: e.tensor_copy(out=wr2[:, :, 0:36], in_=wr_f[:]), reads=["wr_f"], writes=["wr2a"])
                S.op("dve", lambda e: e.tensor_tensor(out=wrd[:], in0=wr_f[:], in1=wr2[:, :, 0:36], op=ALU.subtract), reads=["wr_f", "wr2a"], writes=["wrd"])
                S.op("dve", lambda e: e.tensor_copy(out=wr2[:, :, 36:72], in_=wrd[:]), reads=["wrd"], writes=["wr2"])
                junkm = sbuf(m1, "junkm", [128, D], BF16)
                stm = sbuf(m1, "stm", [128, 2, 4], F32)
                lg = sbuf(m1, "lg", [128, NTT, 36], F32)
                gmax = sbuf(m1, "gmax", [128, NTT], F32)
                gm = sbuf(m1, "gm", [128, NTT, 4], F32)
                gsh = sbuf(m1, "gsh", [128, NTT, 4], F32)
                gsum = sbuf(m1, "gsum", [128, NTT], F32)
                ggate = sbuf(m1, "ggate", [128, NTT], F32)
                t48 = sbuf(m1, "t48", [128, NTT, 4, 8], F32)
                ig = sbuf(m1, "ig", [128, NTT, 8], F32)
                ig2 = sbuf(m1, "ig2", [128, NTT, 8], F32)
                m1v = sbuf(m1, "m1v", [128, NTT], F32)
                m2v = sbuf(m1, "m2v", [128, NTT], F32)
                mask1 = sbuf(m1, "mask1", [128, NTT, 8], F32)
                mask2 = sbuf(m1, "mask2", [128, NTT, 8], F32)
                e2 = sbuf(m1, "e2", [128, NTT], F32)
                den = sbuf(m1, "den", [128, NTT], F32)
                OH1 = sbuf(m1, "OH1", [128, NTT, 4, 8], F32)
                OH2 = sbuf(m1, "OH2", [128, NTT, 4, 8], F32)
                OHs = sbuf(m1, "OHs", [128, NTT, 32], F32)
                cumT = sbuf(m1, "cumT", [128, NTT + 1, 32], F32)
                rank_all = sbuf(m1, "rank_all", [128, NTT, 32], F32)
                ebi = sbuf(m1, "ebi", [128, 32], I32)
                ebf = sbuf(m1, "ebf", [128, 32], F32)
                tsel = sbuf(m1, "tsel", [128, NTT, 32], F32)
                rsel = sbuf(m1, "rsel", [128, NTT], F32)
                esel = sbuf(m1, "esel", [128, NTT], F32)
                ovf = sbuf(m1, "ovf", [128, NTT], F32)

                def m1_tiles(t0, t1):
                  for i in range(t0, t1):
                    b = i % 2
                    S.dma("sp", "mxt%d" % b, lambda e, b=b, i=i: e.dma_start(out=mxt[b][:], in_=x1_d[i * 128:(i + 1) * 128, :]), writes=[("mxt", b)])
                    S.op("act", lambda e, b=b: e.activation(out=junkm[:], in_=mxt[b][:], func=AF.Square, accum_out=stm[:, b, 0:1]),
                         reads=[("mxt", b)], writes=[("mss", b)])
                    rstd_from_ss(stm[:, b, 0:1], D, stm[:, b, 1:2], stm[:, b, 2:3], ("mss", b), "mrstd%d" % b)
                    S.op("dve", lambda e, b=b: e.scalar_tensor_tensor(out=h2f[b][:], in0=mxt[b][:], scalar=stm[:, b, 2:3], in1=g_ffn_t[:], op0=ALU.mult, op1=ALU.mult),
                         reads=[("mxt", b), "mrstd%d" % b, "g_ffn_t"], writes=[("h2f", b)])
                    S.op("act", lambda e, b=b, i=i: e.activation(out=h2b[:, i, :], in_=h2f[b][:], func=AF.Copy), reads=[("h2f", b)], writes=[("h2b", i)])
                    S.op("dve", lambda e, b=b, i=i: e.tensor_tensor(out=h2lo[b][:], in0=h2f[b][:], in1=h2b[:, i, :], op=ALU.subtract),
                         reads=[("h2f", b), ("h2b", i)], writes=[("h2lo", b)])
                    pvh = bank_bf(0).rearrange("p (k t) -> p k t", k=8)
                    pvl = bank_bf(1).rearrange("p (k t) -> p k t", k=8)
                    for k in range(8):
                        S.op("pe", lambda e, k=k, i=i, pvh=pvh: e.transpose(out=pvh[:, k, :], in_=h2b[:, i, k * 128:(k + 1) * 128], identity=ident_b[:]),
                             reads=[("h2b", i), "ident_b"], writes=[PS(0)], inc=(k == 7))
                    S.op("act", lambda e, b=b, pvh=pvh: e.activation(out=hiT[b][:], in_=pvh, func=AF.Copy), reads=[PS(0)], writes=[("hiT", b)])
                    for k in range(8):
                        S.op("pe", lambda e, k=k, b=b, pvl=pvl: e.transpose(out=pvl[:, k, :], in_=h2lo[b][:, k * 128:(k + 1) * 128], identity=ident_b[:]),
                             reads=[("h2lo", b)], writes=[PS(1)], inc=(k == 7))
                    S.op("dve", lambda e, b=b, pvl=pvl: e.tensor_copy(out=loT[b][:], in_=pvl), reads=[PS(1)], writes=[("loT", b)])
                    lb = 2 + b
                    for k in range(8):
                        S.op("pe", lambda e, k=k, b=b, lb=lb: e.matmul(bank[lb][:, 0:72], lhsT=hiT[b][:, k, :], rhs=wr2[:, k, :], start=(k == 0), stop=False,
                                                                      skip_group_check=True),
                             reads=[("hiT", b), "wr2"], writes=[PS(lb)], inc=False)
                    for k in range(8):
                        S.op("pe", lambda e, k=k, b=b, lb=lb: e.matmul(bank[lb][:, 0:36], lhsT=loT[b][:, k, :], rhs=wr2[:, k, 0:36], start=False, stop=(k == 7),
                                                                      skip_group_check=True),
                             reads=[("loT", b), "wr2"], writes=[PS(lb)], inc=(k == 7))
                    S.op("dve", lambda e, b=b, lb=lb: e.tensor_tensor(out=lgt[b][:], in0=bank[lb][:, 36:72], in1=b_r_t[:], op=ALU.add),
                         reads=[PS(lb), "b_r_t"], writes=[("lgt", b)])
                    S.op("dve", lambda e, i=i, b=b, lb=lb: e.tensor_tensor(out=lg[:, i, :], in0=bank[lb][:, 0:36], in1=lgt[b][:], op=ALU.add),
                         reads=[PS(lb), ("lgt", b)], writes=["lg"])

                def m1_route(t0, t1):
                  nt = t1 - t0
                  gl = lg[:, t0:t1, 0:4]
                  el = lg[:, t0:t1, 4:36].rearrange("p t (g e) -> p t g e", g=4)

                  def bc(ap, shape, axis):
                    return ap.unsqueeze(axis).broadcast_to(shape)

                  V = lambda fn, reads, writes: S.op("dve", fn, reads=reads, writes=writes)
                  V(lambda e: e.tensor_reduce(out=gmax[:, t0:t1], in_=gl, op=ALU.max, axis=AX.X), ["lg"], ["gmax"])
                  V(lambda e: e.tensor_tensor(out=gm[:, t0:t1], in0=gl, in1=bc(gmax[:, t0:t1], [128, nt, 4], 2), op=ALU.is_equal), ["lg", "gmax"], ["gm"])
                  V(lambda e: e.tensor_tensor(out=gsh[:, t0:t1], in0=gl, in1=bc(gmax[:, t0:t1], [128, nt, 4], 2), op=ALU.subtract), ["lg", "gmax"], ["gsh"])
                  S.op("act", lambda e: e.activation(out=gsh[:, t0:t1], in_=gsh[:, t0:t1], func=AF.Exp), reads=["gsh"], writes=["gsh"])
                  V(lambda e: e.tensor_reduce(out=gsum[:, t0:t1], in_=gsh[:, t0:t1], op=ALU.add, axis=AX.X), ["gsh"], ["gsum"])
                  V(lambda e: e.reciprocal(out=ggate[:, t0:t1], in_=gsum[:, t0:t1]), ["gsum"], ["ggate"])
                  V(lambda e: e.tensor_tensor(out=t48[:, t0:t1], in0=el, in1=bc(gm[:, t0:t1], [128, nt, 4, 8], 3), op=ALU.mult), ["lg", "gm"], ["t48"])
                  V(lambda e: e.tensor_reduce(out=ig[:, t0:t1], in_=t48[:, t0:t1].rearrange("p t g e -> p t e g"), op=ALU.add, axis=AX.X), ["t48"], ["ig"])
                  V(lambda e: e.tensor_reduce(out=m1v[:, t0:t1], in_=ig[:, t0:t1], op=ALU.max, axis=AX.X), ["ig"], ["m1v"])
                  V(lambda e: e.tensor_tensor(out=mask1[:, t0:t1], in0=ig[:, t0:t1], in1=bc(m1v[:, t0:t1], [128, nt, 8], 2), op=ALU.is_equal), ["ig", "m1v"], ["mask1"])
                  V(lambda e: e.scalar_tensor_tensor(out=ig2[:, t0:t1].rearrange("p t e -> p (t e)"), in0=mask1[:, t0:t1].rearrange("p t e -> p (t e)"), scalar=-1e30,
                                                   in1=ig[:, t0:t1].rearrange("p t e -> p (t e)"), op0=ALU.mult, op1=ALU.add), ["mask1", "ig"], ["ig2"])
                  V(lambda e: e.tensor_reduce(out=m2v[:, t0:t1], in_=ig2[:, t0:t1], op=ALU.max, axis=AX.X), ["ig2"], ["m2v"])
                  V(lambda e: e.tensor_tensor(out=mask2[:, t0:t1], in0=ig2[:, t0:t1], in1=bc(m2v[:, t0:t1], [128, nt, 8], 2), op=ALU.is_equal), ["ig2", "m2v"], ["mask2"])
                  V(lambda e: e.tensor_tensor(out=e2[:, t0:t1], in0=m2v[:, t0:t1], in1=m1v[:, t0:t1], op=ALU.subtract), ["m1v", "m2v"], ["e2"])
                  S.op("act", lambda e: e.activation(out=e2[:, t0:t1], in_=e2[:, t0:t1], func=AF.Exp), reads=["e2"], writes=["e2"])
                  V(lambda e: e.tensor_scalar(out=den[:, t0:t1], in0=e2[:, t0:t1], scalar1=1.0, scalar2=None, op0=ALU.add), ["e2"], ["den"])
                  V(lambda e: e.reciprocal(out=den[:, t0:t1], in_=den[:, t0:t1]), ["den"], ["den"])
                  V(lambda e: e.tensor_tensor(out=gate1[:, t0:t1], in0=ggate[:, t0:t1], in1=den[:, t0:t1], op=ALU.mult), ["ggate", "den"], ["gate1"])
                  V(lambda e: e.tensor_tensor(out=gate2[:, t0:t1], in0=gate1[:, t0:t1], in1=e2[:, t0:t1], op=ALU.mult), ["gate1", "e2"], ["gate2"])
                  V(lambda e: e.tensor_tensor(out=OH1[:, t0:t1], in0=bc(gm[:, t0:t1], [128, nt, 4, 8], 3), in1=bc(mask1[:, t0:t1], [128, nt, 4, 8], 2), op=ALU.mult), ["gm", "mask1"], ["OH1"])
                  V(lambda e: e.tensor_tensor(out=OH2[:, t0:t1], in0=bc(gm[:, t0:t1], [128, nt, 4, 8], 3), in1=bc(mask2[:, t0:t1], [128, nt, 4, 8], 2), op=ALU.mult), ["gm", "mask2"], ["OH2"])
                  V(lambda e: e.tensor_tensor(out=OHs[:, t0:t1].rearrange("p t e -> p (t e)"), in0=OH1[:, t0:t1].rearrange("p t g e -> p (t g e)"),
                                            in1=OH2[:, t0:t1].rearrange("p t g e -> p (t g e)"), op=ALU.add), ["OH1", "OH2"], ["OHs"])
                  for i in range(t0, t1):
                    V(lambda e, i=i: e.tensor_tensor(out=cumT[:, i + 1, :], in0=cumT[:, i, :], in1=OHs[:, i, :], op=ALU.add), [("cumT", i), "OHs"], [("cumT", i + 1)])
                  for i in range(t0, t1):
                    rb = 4 + (i % 2)
                    S.op("pe", lambda e, i=i, rb=rb: e.matmul(bank[rb][:, 0:32], lhsT=ltri_f[:], rhs=OHs[:, i, :], start=True, stop=False),
                         reads=["ltri_f", "OHs"], writes=[PS(rb)], inc=False)
                    S.op("pe", lambda e, i=i, rb=rb: e.matmul(bank[rb][:, 0:32], lhsT=ones_f[:], rhs=cumT[:, i, :], start=False, stop=True),
                         reads=["ones_f", ("cumT", i)], writes=[PS(rb)], inc=True)
                    V(lambda e, i=i, rb=rb: e.tensor_copy(out=rank_all[:, i, :], in_=bank[rb][:, 0:32]), [PS(rb)], ["rank_all"])
                  for (OH, dst_i, nm) in ((OH1, d1i, "1"), (OH2, d2i, "2")):
                    ohf = OH[:, t0:t1].rearrange("p t g e -> p t (g e)")
                    V(lambda e, ohf=ohf: e.tensor_tensor(out=tsel[:, t0:t1], in0=rank_all[:, t0:t1], in1=ohf, op=ALU.mult), ["rank_all", "OH" + nm], ["tsel"])
                    V(lambda e: e.tensor_reduce(out=rsel[:, t0:t1], in_=tsel[:, t0:t1], op=ALU.add, axis=AX.X), ["tsel"], ["rsel"])
                    V(lambda e, ohf=ohf: e.tensor_tensor(out=tsel[:, t0:t1], in0=ohf, in1=bc(ebf[:], [128, nt, 32], 1), op=ALU.mult), ["ebf", "OH" + nm, "rsel"], ["tsel"])
                    V(lambda e: e.tensor_reduce(out=esel[:, t0:t1], in_=tsel[:, t0:t1], op=ALU.add, axis=AX.X), ["tsel"], ["esel"])
                    V(lambda e: e.tensor_scalar(out=ovf[:, t0:t1], in0=rsel[:, t0:t1], scalar1=float(CAPB * 128), scalar2=None, op0=ALU.is_lt), ["rsel"], ["ovf"])
                    V(lambda e: e.tensor_tensor(out=rsel[:, t0:t1], in0=rsel[:, t0:t1], in1=esel[:, t0:t1], op=ALU.add), ["rsel", "esel"], ["rsel"])
                    V(lambda e: e.scalar_tensor_tensor(out=rsel[:, t0:t1], in0=rsel[:, t0:t1], scalar=float(-NROWS), in1=ovf[:, t0:t1], op0=ALU.add, op1=ALU.mult), ["rsel", "ovf"], ["rsel"])
                    V(lambda e: e.tensor_scalar(out=rsel[:, t0:t1], in0=rsel[:, t0:t1], scalar1=float(NROWS), scalar2=None, op0=ALU.add), ["rsel"], ["rsel"])
                    V(lambda e, dst_i=dst_i: e.tensor_copy(out=dst_i[:, t0:t1], in_=rsel[:, t0:t1]), ["rsel"], ["dst" + nm])
                  for i in range(t0, t1):
                      for (dst_i, nm) in ((d1i, "1"), (d2i, "2")):
                          S.dma("pool", "scat" + nm, lambda e, i=i, dst_i=dst_i: e.indirect_dma_start(
                              out=xs_d, out_offset=bass.IndirectOffsetOnAxis(ap=dst_i[:, i:i + 1], axis=0), in_=h2b[:, i, :], in_offset=None),
                                reads=[("h2b", i), "dst" + nm, "xs_zf"], writes=[("xs_s", i, nm)])

                S.op("pool", lambda e: e.memset(cumT[:, 0, :], 0.0), writes=[("cumT", 0)])
                S.op("pool", lambda e: e.iota(ebi[:], pattern=[[CAPB * 128, 32]], base=0, channel_multiplier=0), writes=["ebi"])
                S.op("dve", lambda e: e.tensor_copy(out=ebf[:], in_=ebi[:]), reads=["ebi"], writes=["ebf"])
                NB1 = 4
                for jb in range(NB1):
                    m1_tiles(jb * (NTT // NB1), (jb + 1) * (NTT // NB1))
                    m1_route(jb * (NTT // NB1), (jb + 1) * (NTT // NB1))
                S.barrier()
            with contextlib.ExitStack() as m2:
                xblk = [sbuf(m2, "xblk%d" % i, [128, D], BF16) for i in range(8)]
                xT = [sbuf(m2, "xT%d" % i, [128, 8, 128], BF16) for i in range(3)]
                sg = [sbuf(m2, "sg%d" % i, [128, 256], F32) for i in range(2)]
                hblk = [sbuf(m2, "hblk%d" % i, [128, 256], BF16) for i in range(3)]
                hT2 = [sbuf(m2, "hT2_%d" % i, [128, 2, 128], BF16) for i in range(3)]
                yblk = [sbuf(m2, "yblk%d" % i, [128, D], BF16) for i in range(2)]
                NBLK = N_EXP * CAPB

                def P0(n):
                    xb = n % 8
                    row = n * 128
                    S.dma("sp", "xblk%d" % xb, lambda e: e.dma_start(out=xblk[xb][:], in_=xs_d[row:row + 128, :]), reads=["xs_d"], writes=[("xblk", xb)])

                def P1(n):
                    xb, pb, tb = n % 8, n % 2, n % 3
                    pv = bank_bf(pb).rearrange("p (k t) -> p k t", k=8)
                    for k in range(8):
                        S.op("pe", lambda e, k=k: e.transpose(out=pv[:, k, :], in_=xblk[xb][:, k * 128:(k + 1) * 128], identity=ident_b[:]),
                             reads=[("xblk", xb), "ident_b"], writes=[PS(pb)], inc=(k == 7))
                    S.op("act", lambda e: e.activation(out=xT[tb][:], in_=pv, func=AF.Copy), reads=[PS(pb)], writes=[("xT", tb)])

                def P2(n):
                    tb, gb, hb3, sb2 = n % 3, 2 + n % 2, n % 3, n % 2
                    wi = (n // CAPB) % NW
                    for k in range(8):
                        S.op("pe", lambda e, k=k: e.matmul(bank[gb][:], lhsT=xT[tb][:, k, :], rhs=wgu[wi][:, k, :], start=(k == 0), stop=(k == 7)),
                             reads=[("xT", tb), ("wgu", wi)], writes=[PS(gb)], inc=(k == 7))
                    S.op("act", lambda e: e.activation(out=sg[sb2][:], in_=bank[gb][:, 0:256], func=AF.Silu), reads=[PS(gb)], writes=[("sg", sb2)])
                    S.op("dve", lambda e: e.tensor_tensor(out=hblk[hb3][:], in0=sg[sb2][:], in1=bank[gb][:, 256:512], op=ALU.mult),
                         reads=[("sg", sb2), PS(gb)], writes=[("hblk", hb3)])

                def P3(n):
                    hb3, tb2 = n % 3, 4 + n % 2
                    pv2 = bank_bf(tb2).rearrange("p (k t) -> p k t", k=8)
                    for c in range(2):
                        S.op("pe", lambda e, c=c: e.transpose(out=pv2[:, c, :], in_=hblk[hb3][:, c * 128:(c + 1) * 128], identity=ident_b[:]),
                             reads=[("hblk", hb3)], writes=[PS(tb2)], inc=(c == 1))
                    S.op("dve", lambda e: e.tensor_copy(out=hT2[hb3][:], in_=pv2[:, 0:2, :]), reads=[PS(tb2)], writes=[("hT2", hb3)])

                def P4(n):
                    hb3, yb2 = n % 3, n % 2
                    wi = (n // CAPB) % NW
                    row = n * 128
                    for half in range(2):
                        yb = 6 + half
                        for c in range(2):
                            S.op("pe", lambda e, c=c, half=half, yb=yb: e.matmul(bank[yb][:], lhsT=hT2[hb3][:, c, :], rhs=wd[wi][:, c, half * 512:(half + 1) * 512],
                                                                                start=(c == 0), stop=(c == 1)),
                                 reads=[("hT2", hb3), ("wd", wi)], writes=[PS(yb)], inc=(c == 1))
                        if half == 0:
                            S.op("act", lambda e, yb=yb: e.activation(out=yblk[yb2][:, 0:512], in_=bank[yb][:], func=AF.Copy), reads=[PS(yb)], writes=[("yblk", yb2, 0)])
                        else:
                            S.op("dve", lambda e, yb=yb: e.tensor_copy(out=yblk[yb2][:, 512:1024], in_=bank[yb][:]), reads=[PS(yb)], writes=[("yblk", yb2, 1)])
                    S.dma("sp", "yblk%d" % yb2, lambda e: e.dma_start(out=ys_d[row:row + 128, :], in_=yblk[yb2][:]),
                          reads=[("yblk", yb2, 0), ("yblk", yb2, 1)], writes=[("ys_dw", yb2)])
                    if n % CAPB == CAPB - 1 and n // CAPB + NW < N_EXP:
                        load_w(n // CAPB + NW)

                for step in range(-4, NBLK + 3):
                    for stage, skew in ((P0, -4), (P1, 0), (P2, 1), (P3, 2), (P4, 3)):
                        n = step - skew
                        if 0 <= n < NBLK:
                            stage(n)
                S.barrier()
            with contextlib.ExitStack() as m3:
                y1t = [sbuf(m3, "y1t%d" % i, [128, D], BF16) for i in range(3)]
                y2t = [sbuf(m3, "y2t%d" % i, [128, D], BF16) for i in range(3)]
                fxt = [sbuf(m3, "fxt%d" % i, [128, D], F32) for i in range(3)]
                acc = [sbuf(m3, "facc%d" % i, [128, D], F32) for i in range(2)]
                ot = [sbuf(m3, "fot%d" % i, [128, D], F32) for i in range(2)]
                junkf = sbuf(m3, "junkf", [128, D], BF16)
                stf = sbuf(m3, "stf", [128, 3, 4], F32)
                for i in range(3):
                    S.op("pool", lambda e, i=i: e.memset(y1t[i][:], 0.0), writes=[("y1t", i)])
                    S.op("pool", lambda e, i=i: e.memset(y2t[i][:], 0.0), writes=[("y2t", i)])
                def m3_load(i):
                    b = i % 3
                    S.dma("pool", "y1t%d" % b, lambda e, b=b, i=i: e.indirect_dma_start(
                        out=y1t[b][:], out_offset=None, in_=ys_d, in_offset=bass.IndirectOffsetOnAxis(ap=d1i[:, i:i + 1], axis=0)),
                          reads=["ys_d"], writes=[("y1t", b)])
                    S.dma("pool", "y2t%d" % b, lambda e, b=b, i=i: e.indirect_dma_start(
                        out=y2t[b][:], out_offset=None, in_=ys_d, in_offset=bass.IndirectOffsetOnAxis(ap=d2i[:, i:i + 1], axis=0)),
                          reads=["ys_d"], writes=[("y2t", b)])
                    S.dma("sp", "fxt%d" % b, lambda e, b=b, i=i: e.dma_start(out=fxt[b][:], in_=x1_d[i * 128:(i + 1) * 128, :]), writes=[("fxt", b)])

                def m3_comp(i):
                    b = i % 3
                    c = i % 2
                    S.op("dve", lambda e, b=b, c=c, i=i: e.scalar_tensor_tensor(out=acc[c][:], in0=y1t[b][:], scalar=gate1[:, i:i + 1], in1=fxt[b][:], op0=ALU.mult, op1=ALU.add),
                         reads=[("y1t", b), ("fxt", b)], writes=[("facc", c)])
                    S.op("dve", lambda e, b=b, c=c, i=i: e.scalar_tensor_tensor(out=acc[c][:], in0=y2t[b][:], scalar=gate2[:, i:i + 1], in1=acc[c][:], op0=ALU.mult, op1=ALU.add),
                         reads=[("y2t", b), ("facc", c)], writes=[("facc", c)])
                    S.op("act", lambda e, b=b, c=c: e.activation(out=junkf[:], in_=acc[c][:], func=AF.Square, accum_out=stf[:, c, 0:1]), reads=[("facc", c)], writes=[("fss", c)])
                    rstd_from_ss(stf[:, c, 0:1], D, stf[:, c, 1:2], stf[:, c, 2:3], ("fss", c), "frstd%d" % c)
                    S.op("dve", lambda e, b=b, c=c: e.scalar_tensor_tensor(out=ot[c][:], in0=acc[c][:], scalar=stf[:, c, 2:3], in1=g_fin_t[:], op0=ALU.mult, op1=ALU.mult),
                         reads=[("facc", c), "frstd%d" % c, "g_fin_t"], writes=[("fot", c)])
                    S.dma("sp", "fot%d" % c, lambda e, b=b, c=c, i=i: e.dma_start(out=out[i * 128:(i + 1) * 128, :], in_=ot[c][:]), reads=[("fot", c)])

                PF = 2
                for i in range(PF):
                    m3_load(i)
                for i in range(NTT):
                    if i + PF < NTT:
                        m3_load(i + PF)
                    m3_comp(i)
                S.barrier()

        if stop_after is not None:
            with contextlib.ExitStack() as pz:
                z = sbuf(pz, "z", [128, D], F32)
                S.op("pool", lambda e: e.memset(z[:], 0.0), writes=["z"])
                S.dma("sp", "zout", lambda e: e.dma_start(out=out[0:128, :], in_=z[:]), reads=["z"])
                S.barrier()
        S.barrier()
        S.emit()
    return nc


def _prep_inputs(inputs):
    f = lambda a: np.ascontiguousarray(np.asarray(a, dtype=np.float32))
    rope_cs, oh = _consts()
    shared = {
        "w_in": f(inputs["w_in"][0]),
        "rel_bias": f(inputs["rel_bias"]),
        "w_q_up": f(inputs["w_q_up"][0]),
        "w_kv_up": f(inputs["w_kv_up"][0]),
        "w_out": f(inputs["w_out"][0]),
        "w_rg": f(inputs["w_router_group"][0]),
        "w_re": f(inputs["w_router_expert"][0]),
        "w_gate": f(inputs["w_gate"][0]),
        "w_up": f(inputs["w_up"][0]),
        "w_down": f(inputs["w_down"][0]),
        "g_attn": f(inputs["g_attn_norm"][0]).reshape(1, D),
        "g_q": f(inputs["g_q_latent"][0]).reshape(1, 256),
        "g_kv": f(inputs["g_kv_latent"][0]).reshape(1, 128),
        "g_out": np.concatenate([f(inputs["g_out_a"][0]), f(inputs["g_out_b"][0])]).reshape(1, D),
        "g_ffn": f(inputs["g_ffn_norm"][0]).reshape(1, D),
        "g_fin": f(inputs["g_final"]).reshape(1, D),
        "b_r": np.concatenate([f(inputs["b_router_group"][0]), f(inputs["b_router_expert"][0])]).reshape(1, 36),
        "rope_cs": rope_cs,
        "oh_bias": oh,
    }
    xs = f(inputs["x"]).reshape(N_CORES, NTOK, D)
    return [dict(shared, x=xs[c]) for c in range(N_CORES)]


def kernel(**inputs):
    in_maps = _prep_inputs(inputs)
    nc = build_nc()
    res = run_bass_kernel_spmd(nc, in_maps, core_ids=list(range(N_CORES)))
    outs = [np.asarray(r["out"], dtype=np.float32).reshape(NSEQ, SEQ, D) for r in res.results]
    return np.concatenate(outs, axis=0)
```

```python
import contextlib
import math
import numpy as np
import concourse.bass as bass
import concourse.mybir as mybir
from concourse.bass_utils import run_bass_kernel_spmd

F32 = mybir.dt.float32
BF16 = mybir.dt.bfloat16
I32 = mybir.dt.int32
AF = mybir.ActivationFunctionType
ALU = mybir.AluOpType
AX = mybir.AxisListType

N_CORES = 8
SEQ = 2048
D = 1024
NSEQ = 2
NTOK = NSEQ * SEQ
NT = SEQ // 128
EPS = 1e-6
NEG = -30000.0
PATTERNS = (16, 4, 1)
N_EXP = 32
CAPB = 4
NROWS = N_EXP * CAPB * 128
K2C = 99


class Sync:
    COMPUTE = ("pe", "act", "dve", "pool")

    def __init__(self, nc, stack):
        self.nc = nc
        self.stack = stack
        self.ops = {e: [] for e in ("pe", "act", "dve", "pool", "sp")}
        self.sem = {}
        for e in self.COMPUTE:
            self.sem[e] = stack.enter_context(nc.semaphore("s_" + e))
        self.cnt = {e: 0 for e in self.COMPUTE}
        self.seen = {e: {} for e in self.ops}
        self.keys = {}
        self.pending = {e: ([], []) for e in self.COMPUTE}
        self.slots = {}

    def _key(self, k):
        st = self.keys.get(k)
        if st is None:
            st = {"w": None, "r": []}
            self.keys[k] = st
        return st

    def _need(self, eng, reads, writes, is_dma=False):
        need = {}

        def add(p):
            if p is None:
                return
            s, v, src = p
            if eng == "pe" and src == "pe":
                return
            if need.get(id(s), (None, -1))[1] < v:
                need[id(s)] = (s, v)

        for k in reads:
            st = self._key(k)
            add(st["w"])
            if isinstance(k, tuple) and k and k[0] == "ps":
                for p in st["r"]:
                    if p[2] != eng:
                        add(p)
        for k in writes:
            st = self._key(k)
            if st["w"] is not None and (is_dma or st["w"][2] != eng):
                add(st["w"])
            for p in st["r"]:
                if is_dma or p[2] != eng:
                    add(p)
        out = []
        seen = self.seen[eng]
        for sid, (s, v) in need.items():
            if seen.get(sid, -1) >= v:
                continue
            seen[sid] = v
            out.append((s, v))
        return out

    def _commit(self, reads, writes, prod):
        for k in writes:
            st = self._key(k)
            st["w"] = prod
            st["r"] = []
        for k in reads:
            if k in writes:
                continue
            st = self._key(k)
            st["r"] = [p for p in st["r"] if p[0] is not prod[0]] + [prod]

    def op(self, eng, fn, reads=(), writes=(), inc=True):
        reads, writes = list(reads), list(writes)
        waits = self._need(eng, reads, writes)
        if inc:
            self.cnt[eng] += 1
            prod = (self.sem[eng], self.cnt[eng], eng)
            pr, pw = self.pending[eng]
            self._commit(reads + pr, writes + pw, prod)
            self.pending[eng] = ([], [])
            self.ops[eng].append((waits, fn, (self.sem[eng], 1)))
        else:
            pr, pw = self.pending[eng]
            pr.extend(reads)
            pw.extend(writes)
            self.ops[eng].append((waits, fn, None))

    def dma(self, q, slot, fn, reads=(), writes=()):
        reads, writes = list(reads), list(writes)
        if slot not in self.slots:
            s = self.stack.enter_context(self.nc.semaphore("d_" + slot))
            self.slots[slot] = [s, 0]
        sl = self.slots[slot]
        waits = self._need(q, reads, writes, is_dma=True)
        sl[1] += 16
        self._commit(reads, writes, (sl[0], sl[1], "dma"))
        self.ops[q].append((waits, fn, (sl[0], 16)))

    def barrier(self):
        targets = [(self.sem[e], self.cnt[e]) for e in self.COMPUTE if self.cnt[e] > 0]
        targets += [(s, v) for (s, v) in self.slots.values() if v > 0]
        for e in self.ops:
            waits = []
            seen = self.seen[e]
            for s, v in targets:
                if seen.get(id(s), -1) >= v:
                    continue
                seen[id(s)] = v
                waits.append((s, v))
            if waits:
                self.ops[e].append((waits, None, None))
        self.keys = {}
        self.pending = {e: ([], []) for e in self.COMPUTE}

    def emit(self):
        nc = self.nc
        ops = self.ops

        def run(e, lst):
            for waits, fn, inc in lst:
                for s, v in waits:
                    e.wait_ge(s, v)
                if fn is not None:
                    ins = fn(e)
                    if inc is not None:
                        ins.then_inc(inc[0], inc[1])

        with nc.Block() as block:
            @block.sync
            def _(e):
                run(e, ops["sp"])

            @block.tensor
            def _(e):
                run(e, ops["pe"])

            @block.scalar
            def _(e):
                run(e, ops["act"])

            @block.vector
            def _(e):
                run(e, ops["dve"])

            @block.gpsimd
            def _(e):
                run(e, ops["pool"])


def _t5_bucket(rel):
    half = 16
    max_exact = 8
    n = np.abs(rel)
    large = max_exact + (np.log(np.maximum(n, 1) / max_exact)
                         / math.log(1024 / max_exact) * (half - max_exact)).astype(np.int32)
    large = np.minimum(large, half - 1)
    return (np.where(rel > 0, half, 0) + np.where(n < max_exact, n, large)).astype(np.int32)


def _consts():
    half = 16
    inv_freq = (np.float32(10000.0) ** (-(np.arange(half, dtype=np.float32) / np.float32(half)))).astype(np.float32)
    ang = (np.arange(SEQ, dtype=np.float32)[:, None] * inv_freq[None, :]).astype(np.float32)
    cos, sin = np.cos(ang).astype(np.float32), np.sin(ang).astype(np.float32)
    rope_cs = np.concatenate([cos, cos, sin], axis=1).astype(np.float32)
    oh = np.zeros((33, 3, 512), np.float32)
    for di, d in enumerate(PATTERNS):
        m = np.arange(512)
        delta = m - 255
        valid = np.abs(delta) <= 64
        b = _t5_bucket(delta * d)
        for mm in range(512):
            if valid[mm]:
                oh[b[mm], di, mm] = 1.0
            else:
                oh[32, di, mm] = 1.0
    return rope_cs, oh.reshape(33, 1536)


def build_nc(dbg=False, stop_after=None):
    nc = bass.Bass("TRN2", target_bir_lowering=False)

    def din(name, shape, dt=F32):
        return nc.dram_tensor(name, list(shape), dt, kind="ExternalInput").ap()

    def dscr(name, shape, dt, expose=False):
        kind = "ExternalOutput" if (dbg and expose) else "Internal"
        return nc.dram_tensor(name, list(shape), dt, kind=kind).ap()

    x = din("x", [NTOK, D])
    w_in = din("w_in", [D, 1952])
    rel_bias = din("rel_bias", [32, 8])
    w_q_up = din("w_q_up", [256, 768])
    w_kv_up = din("w_kv_up", [128, 1024])
    w_out = din("w_out", [D, D])
    w_rg = din("w_rg", [D, 4])
    w_re = din("w_re", [D, 32])
    w_gate = din("w_gate", [N_EXP, D, 256])
    w_up = din("w_up", [N_EXP, D, 256])
    w_down = din("w_down", [N_EXP, 256, D])
    g_attn = din("g_attn", [1, D])
    g_q = din("g_q", [1, 256])
    g_kv = din("g_kv", [1, 128])
    g_out = din("g_out", [1, D])
    g_ffn = din("g_ffn", [1, D])
    g_fin = din("g_fin", [1, D])
    b_r = din("b_r", [1, 36])
    rope_cs = din("rope_cs", [SEQ, 48])
    oh_bias = din("oh_bias", [33, 1536])
    out = nc.dram_tensor("out", [NTOK, D], F32, kind="ExternalOutput").ap()

    va_d = dscr("va_d", [NTOK, 520], BF16)
    oa_d = dscr("oa_d", [2, NTOK, 520], F32)
    mixb_d = dscr("mixb_d", [NTOK, 512], BF16, expose=True)
    mixa_d = dscr("mixa_d", [NTOK, 512], BF16, expose=True) if dbg else None
    x1_d = dscr("x1_d", [NTOK, D], F32, expose=True)
    fvec_d = dscr("fvec_d", [8, 1536], F32)
    xs_d = dscr("xs_d", [NROWS + 128, D], BF16)
    ys_d = dscr("ys_d", [NROWS + 128, D], BF16)

    def bcast_rows(ap, n):
        return bass.AP(tensor=ap.tensor, offset=0, ap=[[0, 128], [1, n]])

    with contextlib.ExitStack() as st:
        S = Sync(nc, st)

        uniq = [0]

        def sbuf(stk, name, shape, dt):
            uniq[0] += 1
            return stk.enter_context(nc.sbuf_tensor("%s_%d" % (name, uniq[0]), list(shape), dt))

        psbig = [st.enter_context(nc.psum_tensor("psb%d" % i, [128, 1024], F32)) for i in range(4)]
        bank = [psbig[i // 2][:, (i % 2) * 512:(i % 2 + 1) * 512] for i in range(8)]

        def PS(i):
            return ("ps", i)

        def bank_bf(i):
            return bank[i][:].bitcast(BF16)

        ident_f = sbuf(st, "ident_f", [128, 128], F32)
        ident_b = sbuf(st, "ident_b", [128, 128], BF16)
        jrev_f = sbuf(st, "jrev_f", [128, 128], F32)
        jrev_b = sbuf(st, "jrev_b", [128, 128], BF16)
        ones_f = sbuf(st, "ones_f", [128, 128], F32)
        ltri_f = sbuf(st, "ltri_f", [128, 128], F32)
        eps_t = sbuf(st, "eps_t", [128, 1], F32)
        rope_t = sbuf(st, "rope_t", [128, NT, 48], F32)
        wq_b = sbuf(st, "wq_b", [128, 2, 768], BF16)
        wkv_b = sbuf(st, "wkv_b", [128, 1024], BF16)
        wr_f = sbuf(st, "wr_f", [128, 8, 36], F32)
        g_q_t = sbuf(st, "g_q_t", [128, 256], F32)
        g_kv_t = sbuf(st, "g_kv_t", [128, 128], F32)
        g_out_t = sbuf(st, "g_out_t", [128, D], F32)

        S.op("pool", lambda e: e.memset(ident_f[:], 1.0), writes=["ident_f"])
        S.op("pool", lambda e: e.affine_select(out=ident_f[:], in_=ident_f[:], pattern=[[-1, 128]],
                                               compare_op=ALU.is_equal, fill=0.0, base=0,
                                               channel_multiplier=1), reads=["ident_f"], writes=["ident_f"])
        S.op("pool", lambda e: e.memset(jrev_f[:], 1.0), writes=["jrev_f"])
        S.op("pool", lambda e: e.affine_select(out=jrev_f[:], in_=jrev_f[:], pattern=[[1, 128]],
                                               compare_op=ALU.is_equal, fill=0.0, base=-127,
                                               channel_multiplier=1), reads=["jrev_f"], writes=["jrev_f"])
        S.op("pool", lambda e: e.memset(ones_f[:], 1.0), writes=["ones_f"])
        S.op("pool", lambda e: e.memset(ltri_f[:], 1.0), writes=["ltri_f"])
        S.op("pool", lambda e: e.affine_select(out=ltri_f[:], in_=ltri_f[:], pattern=[[1, 128]],
                                               compare_op=ALU.is_ge, fill=0.0, base=-1,
                                               channel_multiplier=-1), reads=["ltri_f"], writes=["ltri_f"])
        S.op("pool", lambda e: e.memset(eps_t[:], EPS), writes=["eps_t"])
        S.op("dve", lambda e: e.tensor_copy(out=ident_b[:], in_=ident_f[:]), reads=["ident_f"], writes=["ident_b"])
        S.op("dve", lambda e: e.tensor_copy(out=jrev_b[:], in_=jrev_f[:]), reads=["jrev_f"], writes=["jrev_b"])

        S.dma("sp", "c_rope", lambda e: e.dma_start(out=rope_t[:], in_=rope_cs.rearrange("(t p) c -> p t c", p=128)),
              writes=["rope_t"])
        S.dma("pool", "c_wq", lambda e: e.dma_start(out=wq_b[:], in_=w_q_up.rearrange("(c p) n -> p c n", p=128)),
              writes=["wq_b"])
        S.dma("pool", "c_wkv", lambda e: e.dma_start(out=wkv_b[:], in_=w_kv_up), writes=["wkv_b"])
        with nc.allow_non_contiguous_dma(reason="tiny router weights"):
            S.dma("sp", "c_wr", lambda e: e.dma_start(out=wr_f[:, :, 0:4], in_=w_rg.rearrange("(k p) n -> p k n", p=128)),
                  writes=["wr_f"])
            S.dma("sp", "c_wr", lambda e: e.dma_start(out=wr_f[:, :, 4:36], in_=w_re.rearrange("(k p) n -> p k n", p=128)),
                  writes=["wr_f"])
        S.dma("sp", "c_gq", lambda e: e.dma_start(out=g_q_t[:], in_=bcast_rows(g_q, 256)), writes=["g_q_t"])
        S.dma("sp", "c_gkv", lambda e: e.dma_start(out=g_kv_t[:], in_=bcast_rows(g_kv, 128)), writes=["g_kv_t"])
        S.dma("sp", "c_gout", lambda e: e.dma_start(out=g_out_t[:], in_=bcast_rows(g_out, D)), writes=["g_out_t"])

        with contextlib.ExitStack() as s0:
            relb = sbuf(s0, "relb", [33, 8], F32)
            oh_t = sbuf(s0, "oh_t", [33, 1536], F32)
            fvec_sb = sbuf(s0, "fvec_sb", [8, 1536], F32)
            S.op("pool", lambda e: e.memset(relb[32:33, :], NEG), writes=["relb32"])
            S.dma("sp", "c_relb", lambda e: e.dma_start(out=relb[0:32, :], in_=rel_bias), writes=["relb"])
            S.dma("sp", "c_oh", lambda e: e.dma_start(out=oh_t[:], in_=oh_bias), writes=["oh_t"])
            for di in range(3):
                S.op("pe", lambda e, di=di: e.matmul(bank[di][0:8, :], lhsT=relb[:, :], rhs=oh_t[:, di * 512:(di + 1) * 512],
                                                      start=True, stop=True),
                     reads=["relb", "relb32", "oh_t"], writes=[PS(di)])
                S.op("dve", lambda e, di=di: e.tensor_copy(out=fvec_sb[:, di * 512:(di + 1) * 512], in_=bank[di][0:8, :]),
                     reads=[PS(di)], writes=["fvec_sb"])
            S.dma("sp", "c_fv", lambda e: e.dma_start(out=fvec_d, in_=fvec_sb[:]), reads=["fvec_sb"], writes=["fvec_d"])
            S.barrier()

        def rstd_from_ss(ss_ap, n, lnv_ap, rstd_ap, rk, wk):
            S.op("act", lambda e: e.activation(out=lnv_ap, in_=ss_ap, func=AF.Ln, scale=1.0 / n, bias=eps_t[:, 0:1]),
                 reads=[rk, "eps_t"], writes=[wk + "_ln"])
            S.op("act", lambda e: e.activation(out=rstd_ap, in_=lnv_ap, func=AF.Exp, scale=-0.5),
                 reads=[wk + "_ln"], writes=[wk])

        wst = contextlib.ExitStack()
        w_in_b = sbuf(wst, "w_in_b", [128, 8, 1952], BF16)
        for k in range(8):
            S.dma("pool", "w_in", lambda e, k=k: e.dma_start(out=w_in_b[:, k, :], in_=w_in[k * 128:(k + 1) * 128, :]), writes=["w_in_b"])

        def seq_body(s):
            r0 = s * SEQ
            with contextlib.ExitStack() as sq:
                qaT = sbuf(sq, "qaT", [128, 4, SEQ], BF16)
                kaT = sbuf(sq, "kaT", [128, 4, SEQ], BF16)
                sqB = contextlib.ExitStack()
                cqnT = sbuf(sqB, "cqnT", [128, 2, SEQ], BF16)
                ckvnT = sbuf(sqB, "ckvnT", [128, SEQ], BF16)
                kbT = sbuf(sqB, "kbT", [96, 8, SEQ], BF16)
                Vb = sbuf(sqB, "Vb", [128, NT, 8, 65], BF16)
                S.op("pool", lambda e: e.memset(Vb[:, :, :, 64:65], 1.0), writes=["Vb_ones"])

                with contextlib.ExitStack() as p1:
                    g_attn_t = sbuf(p1, "g_attn_t", [128, D], F32)
                    hT = sbuf(p1, "hT", [128, 8, SEQ], BF16)
                    xt = [sbuf(p1, "xt%d" % i, [128, D], F32) for i in range(3)]
                    hb = [sbuf(p1, "hb%d" % i, [128, D], BF16) for i in range(3)]
                    junk = sbuf(p1, "junk", [128, 384], BF16)
                    st1 = sbuf(p1, "st1", [128, 3, 8], F32)
                    vt = [sbuf(p1, "vt%d" % i, [128, 8, 65], BF16) for i in range(2)]
                    cqn = [sbuf(p1, "cqn%d" % i, [128, 256], BF16) for i in range(3)]
                    ckvn = [sbuf(p1, "ckvn%d" % i, [128, 128], BF16) for i in range(3)]
                    ks = [sbuf(p1, "ks%d" % i, [128, 8, 96], BF16) for i in range(3)]
                    krs = [sbuf(p1, "krs%d" % i, [128, 32], F32) for i in range(3)]
                    rtmp = [sbuf(p1, "rtmp%d" % i, [128, 64], F32) for i in range(3)]

                    S.dma("sp", "gattn", lambda e: e.dma_start(out=g_attn_t[:], in_=bcast_rows(g_attn, D)), writes=["g_attn_t"])
                    for i in range(2):
                        S.op("pool", lambda e, i=i: e.memset(vt[i][:, :, 64:65], 1.0), writes=[("vt1", i)])

                    for t in range(NT):
                        b = t % 3
                        S.dma("sp", "xt%d" % b, lambda e, b=b, t=t: e.dma_start(out=xt[b][:], in_=x[r0 + t * 128:r0 + (t + 1) * 128, :]),
                              writes=[("xt", b)])
                        S.op("act", lambda e, b=b: e.activation(out=hb[b][:], in_=xt[b][:], func=AF.Square, accum_out=st1[:, b, 0:1]),
                             reads=[("xt", b)], writes=[("ss", b), ("hb", b)])
                        rstd_from_ss(st1[:, b, 0:1], D, st1[:, b, 1:2], st1[:, b, 2:3], ("ss", b), "rstd%d" % b)
                        S.op("dve", lambda e, b=b: e.scalar_tensor_tensor(out=hb[b][:], in0=xt[b][:], scalar=st1[:, b, 2:3], in1=g_attn_t[:],
                                                                        op0=ALU.mult, op1=ALU.mult),
                             reads=[("xt", b), "rstd%d" % b, "g_attn_t"], writes=[("hb", b)])
                        pb = t % 2
                        pv = bank_bf(pb).rearrange("p (k t) -> p k t", k=8)
                        for k in range(8):
                            S.op("pe", lambda e, k=k, b=b, pv=pv: e.transpose(out=pv[:, k, :], in_=hb[b][:, k * 128:(k + 1) * 128], identity=ident_b[:]),
                                 reads=[("hb", b), "ident_b"], writes=[PS(pb)], inc=(k == 7))
                        eng = "act" if t % 2 == 0 else "dve"
                        if eng == "act":
                            S.op("act", lambda e, t=t, pv=pv: e.activation(out=hT[:, :, t * 128:(t + 1) * 128], in_=pv, func=AF.Copy),
                                 reads=[PS(pb)], writes=[("hT", t)])
                        else:
                            S.op("dve", lambda e, t=t, pv=pv: e.tensor_copy(out=hT[:, :, t * 128:(t + 1) * 128], in_=pv),
                                 reads=[PS(pb)], writes=[("hT", t)])

                    rot = [2, 3, 4, 5]
                    rc = [0]
                    sub = {"P1a": 0, "P1b": 1, "P1c": 2, "P1d": 3}.get(stop_after, 9)

                    def nextbank():
                        bk = rot[rc[0] % len(rot)]
                        rc[0] += 1
                        return bk

                    evc = [0]

                    def evac(out_ap, in_ap, reads, writes, scale=1.0):
                        evc[0] += 1
                        if evc[0] % 2 == 0:
                            S.op("act", lambda e: e.activation(out=out_ap, in_=in_ap, func=AF.Copy, scale=scale), reads=reads, writes=writes)
                        else:
                            S.op("dve", lambda e: e.tensor_scalar(out=out_ap, in0=in_ap, scalar1=scale, scalar2=None, op0=ALU.mult),
                                 reads=reads, writes=writes)

                    for c in range(8 if sub >= 1 else 0):
                        for j in range(4):
                            bk = nextbank()
                            for k in range(8):
                                S.op("pe", lambda e, c=c, j=j, k=k, bk=bk: e.matmul(bank[bk][:], lhsT=w_in_b[:, k, c * 128:(c + 1) * 128],
                                                                                    rhs=hT[:, k, j * 512:(j + 1) * 512], start=(k == 0), stop=(k == 7)),
                                     reads=["w_in_b"] + [("hT", 4 * j + i) for i in range(4)], writes=[PS(bk)], inc=(k == 7))
                            if c < 4:
                                evac(qaT[:, c, j * 512:(j + 1) * 512], bank[bk][:], [PS(bk)], [("qaT", c, j)], scale=0.125)
                            else:
                                evac(kaT[:, c - 4, j * 512:(j + 1) * 512], bank[bk][:], [PS(bk)], [("kaT", c - 4, j)])
                    for t in range(NT if sub >= 2 else 0):
                        b = t % 2
                        bk = nextbank()
                        for k in range(8):
                            S.op("pe", lambda e, t=t, k=k, bk=bk: e.matmul(bank[bk][:], lhsT=hT[:, k, t * 128:(t + 1) * 128],
                                                                            rhs=w_in_b[:, k, 1024:1536], start=(k == 0), stop=(k == 7)),
                                 reads=["w_in_b", ("hT", t)], writes=[PS(bk)], inc=(k == 7))
                        evac(vt[b][:, :, 0:64], bank[bk][:].rearrange("p (h c) -> p h c", h=8), [PS(bk), ("vt1", b)], [("vt", b)])
                        S.dma("sp", "vt%d" % b, lambda e, b=b, t=t: e.dma_start(out=va_d[r0 + t * 128:r0 + (t + 1) * 128, :],
                                                                                  in_=vt[b][:].rearrange("p h c -> p (h c)")),
                              reads=[("vt", b)], writes=[("va_d", t)])
                    for t in range((NT if K2C >= 99 else 1) if sub >= 3 else 0):
                        b = t % 3
                        bk = nextbank()
                        if K2C >= 1:
                            for k in range(8):
                                S.op("pe", lambda e, t=t, k=k, bk=bk: e.matmul(bank[bk][:, 0:416], lhsT=hT[:, k, t * 128:(t + 1) * 128],
                                                                                rhs=w_in_b[:, k, 1536:1952], start=(k == 0), stop=(k == 7)),
                                     reads=["w_in_b", ("hT", t)], writes=[PS(bk)], inc=(k == 7))
                        if K2C >= 2:
                            S.op("act", lambda e, b=b, bk=bk: e.activation(out=junk[:, 0:256], in_=bank[bk][:, 0:256], func=AF.Square,
                                                                            accum_out=st1[:, b, 3:4]), reads=[PS(bk)], writes=[("ssq", b)])
                        if K2C >= 2:
                            S.op("act", lambda e, b=b, bk=bk: e.activation(out=junk[:, 256:384], in_=bank[bk][:, 256:384], func=AF.Square,
                                                                            accum_out=st1[:, b, 5:6]), reads=[PS(bk)], writes=[("sskv", b)])
                        if K2C >= 3:
                            rstd_from_ss(st1[:, b, 3:4], 256, st1[:, b, 4:5], st1[:, b, 4:5], ("ssq", b), "rq%d" % b)
                        if K2C >= 3:
                            rstd_from_ss(st1[:, b, 5:6], 128, st1[:, b, 6:7], st1[:, b, 6:7], ("sskv", b), "rkv%d" % b)
                        if K2C >= 4:
                            S.op("dve", lambda e, b=b, bk=bk: e.scalar_tensor_tensor(out=cqn[b][:], in0=bank[bk][:, 0:256], scalar=st1[:, b, 4:5],
                                                                                      in1=g_q_t[:], op0=ALU.mult, op1=ALU.mult),
                                 reads=[PS(bk), "rq%d" % b, "g_q_t"], writes=[("cqn", b)])
                        if K2C >= 4:
                            S.op("dve", lambda e, b=b, bk=bk: e.scalar_tensor_tensor(out=ckvn[b][:], in0=bank[bk][:, 256:384], scalar=st1[:, b, 6:7],
                                                                                      in1=g_kv_t[:], op0=ALU.mult, op1=ALU.mult),
                                 reads=[PS(bk), "rkv%d" % b, "g_kv_t"], writes=[("ckvn", b)])
                        if K2C >= 5:
                            S.op("dve", lambda e, b=b, bk=bk: e.tensor_copy(out=krs[b][:], in_=bank[bk][:, 384:416]),
                                 reads=[PS(bk)], writes=[("krs", b)])
                        if K2C >= 5:
                            S.op("dve", lambda e, b=b, t=t: e.tensor_tensor(out=rtmp[b][:, 0:32], in0=krs[b][:], in1=rope_t[:, t, 0:32], op=ALU.mult),
                                 reads=[("krs", b), "rope_t"], writes=[("rtA", b)])
                        if K2C >= 5:
                            S.op("dve", lambda e, b=b, t=t: e.tensor_tensor(out=rtmp[b][:, 32:48], in0=krs[b][:, 16:32], in1=rope_t[:, t, 32:48], op=ALU.mult),
                                 reads=[("krs", b), "rope_t"], writes=[("rtB", b)])
                        if K2C >= 5:
                            S.op("dve", lambda e, b=b, t=t: e.tensor_tensor(out=rtmp[b][:, 48:64], in0=krs[b][:, 0:16], in1=rope_t[:, t, 32:48], op=ALU.mult),
                                 reads=[("krs", b), "rope_t"], writes=[("rtC", b)])
                        S.op("dve", lambda e, b=b: e.tensor_tensor(out=ks[b][:, :, 64:80], in0=rtmp[b][:, 0:16].unsqueeze(1).broadcast_to([128, 8, 16]),
                                                                    in1=rtmp[b][:, 32:48].unsqueeze(1).broadcast_to([128, 8, 16]), op=ALU.subtract),
                             reads=[("rtA", b), ("rtB", b)], writes=[("ks_r", b, 0)])
                        S.op("dve", lambda e, b=b: e.tensor_tensor(out=ks[b][:, :, 80:96], in0=rtmp[b][:, 16:32].unsqueeze(1).broadcast_to([128, 8, 16]),
                                                                    in1=rtmp[b][:, 48:64].unsqueeze(1).broadcast_to([128, 8, 16]), op=ALU.add),
                             reads=[("rtA", b), ("rtC", b)], writes=[("ks_r", b, 1)])
                        pb = t % 2
                        pv = bank_bf(pb).rearrange("p (k t) -> p k t", k=8)
                        S.op("pe", lambda e, b=b, pv=pv: e.transpose(out=pv[:, 0, :], in_=cqn[b][:, 0:128], identity=ident_b[:]),
                             reads=[("cqn", b), "ident_b"], writes=[PS(pb)], inc=False)
                        S.op("pe", lambda e, b=b, pv=pv: e.transpose(out=pv[:, 1, :], in_=cqn[b][:, 128:256], identity=ident_b[:]),
                             reads=[("cqn", b)], writes=[PS(pb)], inc=False)
                        S.op("pe", lambda e, b=b, pv=pv: e.transpose(out=pv[:, 2, :], in_=ckvn[b][:], identity=ident_b[:]),
                             reads=[("ckvn", b)], writes=[PS(pb)], inc=True)
                        S.op("act", lambda e, t=t, pv=pv: e.activation(out=cqnT[:, :, t * 128:(t + 1) * 128], in_=pv[:, 0:2, :], func=AF.Copy),
                             reads=[PS(pb)], writes=[("cqnT", t)])
                        S.op("dve", lambda e, t=t, pv=pv: e.tensor_copy(out=ckvnT[:, t * 128:(t + 1) * 128], in_=pv[:, 2, :]),
                             reads=[PS(pb)], writes=[("ckvnT", t)])
                        for half in range(2):
                            bk2 = nextbank()
                            S.op("pe", lambda e, t=t, half=half, bk2=bk2: e.matmul(bank[bk2][:], lhsT=ckvnT[:, t * 128:(t + 1) * 128],
                                                                                    rhs=wkv_b[:, half * 512:(half + 1) * 512], start=True, stop=True),
                                 reads=[("ckvnT", t), "wkv_b"], writes=[PS(bk2)])
                            kvv = bank[bk2][:].rearrange("p (h c) -> p h c", h=4)
                            S.op("act", lambda e, b=b, half=half, kvv=kvv: e.activation(out=ks[b][:, half * 4:(half + 1) * 4, 0:64], in_=kvv[:, :, 0:64], func=AF.Copy),
                                 reads=[PS(bk2)], writes=[("ks_n", b, half)])
                            S.op("dve", lambda e, t=t, half=half, kvv=kvv: e.tensor_copy(out=Vb[:, t, half * 4:(half + 1) * 4, 0:64], in_=kvv[:, :, 64:128]),
                                 reads=[PS(bk2), "Vb_ones"], writes=[("Vb", t, half)])
                        pb2 = 6 + t % 2
                        pv2 = bank_bf(pb2).rearrange("p (k t) -> p k t", k=8)
                        for h in range(8):
                            S.op("pe", lambda e, h=h, b=b, pv2=pv2: e.transpose(out=pv2[0:96, h, :], in_=ks[b][:, h, :], identity=ident_b[:]),
                                 reads=[("ks_n", b, 0), ("ks_n", b, 1), ("ks_r", b, 0), ("ks_r", b, 1), "ident_b"], writes=[PS(pb2)], inc=(h == 7))
                        S.op("act", lambda e, t=t, pv2=pv2: e.activation(out=kbT[:, :, t * 128:(t + 1) * 128], in_=pv2[0:96, :, :], func=AF.Copy),
                             reads=[PS(pb2)], writes=[("kbT", t)])
                    S.barrier()
                if stop_after in ("P1", "P1a", "P1b", "P1c", "P1d"):
                    sqB.close()
                    return

                with contextlib.ExitStack() as p3:
                    qbT = [sbuf(p3, "qbT%d" % i, [96, 8, 512], BF16) for i in range(2)]
                    qs = [sbuf(p3, "qs%d" % i, [128, 8, 96], BF16) for i in range(2)]
                    qr = [sbuf(p3, "qr%d" % i, [128, 8, 32], F32) for i in range(2)]
                    qtm = [sbuf(p3, "qtm%d" % i, [128, 8, 64], F32) for i in range(2)]
                    PT = [sbuf(p3, "PT%d" % i, [128, 1024], BF16) for i in range(3)]
                    ob = [sbuf(p3, "ob%d" % i, [128, 4, 8, 65], F32) for i in range(2)]
                    rl = sbuf(p3, "rl", [128, 8], F32)
                    onb = sbuf(p3, "onb", [128, 8, 64], F32)
                    junk3 = sbuf(p3, "junk3", [128, 512], BF16)
                    st3 = sbuf(p3, "st3", [128, 4], F32)
                    mb = [sbuf(p3, "mb%d" % i, [128, 512], BF16) for i in range(2)]

                    def mla_qproj(jq, tts=(0, 1, 2, 3), stages="ab"):
                        qb = jq % 2
                        for tt in tts:
                            t = jq * 4 + tt
                            b2 = tt % 2
                            if "a" not in stages:
                                mla_qproj_b(jq, tt)
                                continue
                            for half in range(2):
                                for c in range(2):
                                    S.op("pe", lambda e, half=half, c=c, t=t: e.matmul(bank[6 + half][:, 0:384], lhsT=cqnT[:, c, t * 128:(t + 1) * 128],
                                                                                       rhs=wq_b[:, c, half * 384:(half + 1) * 384], start=(c == 0), stop=(c == 1)),
                                         reads=["wq_b"], writes=[PS(6 + half)], inc=(c == 1))
                                pvh = bank[6 + half][:, 0:384].rearrange("p (h c) -> p h c", h=4)
                                S.op("dve", lambda e, half=half, b2=b2, pvh=pvh: e.tensor_copy(out=qs[b2][:, half * 4:(half + 1) * 4, 0:64], in_=pvh[:, :, 0:64]),
                                     reads=[PS(6 + half)], writes=[("qs_n", b2, half)])
                                S.op("dve", lambda e, half=half, b2=b2, pvh=pvh: e.tensor_copy(out=qr[b2][:, half * 4:(half + 1) * 4, :], in_=pvh[:, :, 64:96]),
                                     reads=[PS(6 + half)], writes=[("qr", b2, half)])
                            cos2 = rope_t[:, t, 0:32].unsqueeze(1).broadcast_to([128, 8, 32])
                            sin1 = rope_t[:, t, 32:48].unsqueeze(1).broadcast_to([128, 8, 16])
                            qrk = [("qr", b2, 0), ("qr", b2, 1)]
                            S.op("dve", lambda e, b2=b2, cos2=cos2: e.tensor_tensor(out=qtm[b2][:, :, 0:32], in0=qr[b2][:], in1=cos2, op=ALU.mult),
                                 reads=qrk, writes=[("qtA", b2)])
                            S.op("dve", lambda e, b2=b2, sin1=sin1: e.tensor_tensor(out=qtm[b2][:, :, 32:48], in0=qr[b2][:, :, 16:32], in1=sin1, op=ALU.mult),
                                 reads=qrk, writes=[("qtB", b2)])
                            S.op("dve", lambda e, b2=b2, sin1=sin1: e.tensor_tensor(out=qtm[b2][:, :, 48:64], in0=qr[b2][:, :, 0:16], in1=sin1, op=ALU.mult),
                                 reads=qrk, writes=[("qtC", b2)])
                            S.op("dve", lambda e, b2=b2: e.tensor_tensor(out=qs[b2][:, :, 64:80], in0=qtm[b2][:, :, 0:16], in1=qtm[b2][:, :, 32:48], op=ALU.subtract),
                                 reads=[("qtA", b2), ("qtB", b2)], writes=[("qs_r", b2, 0)])
                            S.op("dve", lambda e, b2=b2: e.tensor_tensor(out=qs[b2][:, :, 80:96], in0=qtm[b2][:, :, 16:32], in1=qtm[b2][:, :, 48:64], op=ALU.add),
                                 reads=[("qtA", b2), ("qtC", b2)], writes=[("qs_r", b2, 1)])
                            if "b" in stages:
                                mla_qproj_b(jq, tt)

                    def mla_qproj_b(jq, tt):
                        qb = jq % 2
                        b2 = tt % 2
                        pv = bank_bf(6).rearrange("p (k t) -> p k t", k=8)
                        for h in range(8):
                            S.op("pe", lambda e, h=h: e.transpose(out=pv[0:96, h, :], in_=qs[b2][:, h, :], identity=ident_b[:]),
                                 reads=[("qs_n", b2, 0), ("qs_n", b2, 1), ("qs_r", b2, 0), ("qs_r", b2, 1)], writes=[PS(6)], inc=(h == 7))
                        S.op("dve", lambda e: e.tensor_copy(out=qbT[qb][:, :, tt * 128:(tt + 1) * 128], in_=pv[0:96, :, :]),
                             reads=[PS(6)], writes=[("qbT", qb, tt)])

                    def mla_epilogue(jq, qts=(0, 1, 2, 3), stages="abc"):
                        obj = ob[jq % 2]
                        obk = [("ob", jq % 2, h) for h in range(8)]
                        for qt in qts:
                            t = jq * 4 + qt
                            m = qt % 2
                            if "a" in stages:
                                S.op("dve", lambda e, qt=qt: e.reciprocal(out=rl[:], in_=obj[:, qt, :, 64]), reads=obk, writes=["rl"])
                                S.op("dve", lambda e, qt=qt: e.tensor_tensor(out=onb[:], in0=obj[:, qt, :, 0:64], in1=rl[:].unsqueeze(2).broadcast_to([128, 8, 64]), op=ALU.mult),
                                     reads=["rl"] + obk, writes=["onb"])
                            if "b" in stages:
                                S.op("act", lambda e: e.activation(out=junk3[:], in_=onb[:].rearrange("p h c -> p (h c)"), func=AF.Square, accum_out=st3[:, 0:1]),
                                     reads=["onb"], writes=["ss3"])
                                rstd_from_ss(st3[:, 0:1], 512, st3[:, 1:2], st3[:, 2:3], "ss3", "rstd3")
                            if "c" in stages:
                                S.op("dve", lambda e, m=m: e.scalar_tensor_tensor(out=mb[m][:], in0=onb[:].rearrange("p h c -> p (h c)"), scalar=st3[:, 2:3],
                                                                                   in1=g_out_t[:, 512:1024], op0=ALU.mult, op1=ALU.mult),
                                     reads=["onb", "rstd3", "g_out_t"], writes=[("mb", m)])
                                S.dma("sp", "mb%d" % m, lambda e, m=m, t=t: e.dma_start(out=mixb_d[r0 + t * 128:r0 + (t + 1) * 128, :], in_=mb[m][:]),
                                      reads=[("mb", m)], writes=[("mixb_d", t)])

                    scale_b = 96.0 ** -0.5
                    NU = 8 * (NT // 2)
                    units = [(jq, h, ktp) for jq in range(4) for h in range(8) for ktp in range(NT // 2)]

                    def mla_S(g):
                        jq, h, ktp = units[g]
                        qb = jq % 2
                        pb = g % 2
                        for j in range(2):
                            kt = 2 * ktp + j
                            S.op("pe", lambda e, j=j, kt=kt: e.matmul(bank[2 * pb + j][:], lhsT=kbT[:, h, kt * 128:(kt + 1) * 128], rhs=qbT[qb][:, h, :],
                                                                      start=True, stop=True),
                                 reads=[("qbT", qb, i) for i in range(4)], writes=[PS(2 * pb + j)], inc=(j == 1))

                    def mla_PV(g):
                        jq, h, ktp = units[g]
                        pb = g % 2
                        pti = g % 3
                        pt = PT[pti]
                        accb = 4 + (h % 2)
                        accv = bank[accb][:, 0:260].rearrange("p (q c) -> p q c", q=4)
                        S.op("act", lambda e: e.activation(out=pt[:], in_=psbig[pb][:], func=AF.Exp, scale=scale_b),
                             reads=[PS(2 * pb), PS(2 * pb + 1)], writes=[("PT", pti)])
                        for j in range(2):
                            kt = 2 * ktp + j
                            for qt in range(4):
                                S.op("pe", lambda e, j=j, kt=kt, qt=qt: e.matmul(accv[:, qt, :], lhsT=pt[:, j * 512 + qt * 128:j * 512 + (qt + 1) * 128],
                                                                                  rhs=Vb[:, kt, h, :], start=(kt == 0 and qt == 0), stop=(kt == NT - 1),
                                                                                  skip_group_check=True),
                                     reads=[("PT", pti)], writes=[PS(accb)], inc=(j == 1 and qt == 3))
                        if ktp == NT // 2 - 1:
                            S.op("dve", lambda e: e.tensor_copy(out=ob[jq % 2][:, :, h, :], in_=accv), reads=[PS(accb)], writes=[("ob", jq % 2, h)])

                    sched = {}

                    def at(u, fn):
                        sched.setdefault(u, []).append(fn)

                    for jq in range(4):
                        base = jq * NU
                        if jq > 0:
                            for qt in range(4):
                                u = base + 4 + 6 * qt
                                at(u, lambda jq=jq, qt=qt: mla_epilogue(jq - 1, (qt,), "a"))
                                at(u + 3, lambda jq=jq, qt=qt: mla_epilogue(jq - 1, (qt,), "b"))
                                at(u + 5, lambda jq=jq, qt=qt: mla_epilogue(jq - 1, (qt,), "c"))
                        if jq < 3:
                            for tt in range(4):
                                u = base + 30 + 8 * tt
                                at(u, lambda jq=jq, tt=tt: mla_qproj(jq + 1, (tt,), "a"))
                                at(u + 5, lambda jq=jq, tt=tt: mla_qproj(jq + 1, (tt,), "b"))
                    mla_qproj(0)
                    mla_S(0)
                    for g in range(len(units)):
                        if g + 1 < len(units):
                            mla_S(g + 1)
                        mla_PV(g)
                        for fn in sched.get(g, ()):
                            fn()
                    mla_epilogue(3)
                    S.barrier()
                if stop_after == "B":
                    sqB.close()
                    return

                sqB.close()
                with contextlib.ExitStack() as p4:
                    trev = sbuf(p4, "trev", [128, 3, 8, 384], BF16)
                    for di in range(3):
                        src = bass.AP(tensor=fvec_d.tensor, offset=di * 512, ap=[[1, 128], [1536, 8], [1, 384]])
                        S.dma("pool", "trev", lambda e, di=di, src=src: e.dma_start(out=trev[:, di, :, :], in_=src), writes=["trev"])
                    Vw = [sbuf(p4, "Vw%d" % i, [128, 4, 2, 520], BF16) for i in range(2)]
                    PTa = [sbuf(p4, "PTa%d" % i, [128, 512], BF16) for i in range(4)]
                    oa = [sbuf(p4, "oa%d" % i, [128, 4, 8, 65], F32) for i in range(2)]
                    o16 = [sbuf(p4, "o16_%d" % i, [128, 520], F32) for i in range(2)]
                    o4 = [sbuf(p4, "o4_%d" % i, [128, 520], F32) for i in range(2)]
                    ma_sb = sbuf(p4, "ma_sb", [128, NT, 512], BF16)
                    rl4 = sbuf(p4, "rl4", [128, 8], F32)
                    onb4 = sbuf(p4, "onb4", [128, 8, 64], F32)
                    junk4 = sbuf(p4, "junk4", [128, 512], BF16)
                    st4 = sbuf(p4, "st4", [128, 4], F32)
                    w_out_b = sbuf(p4, "w_out_b", [128, 8, D], BF16)
                    mbt = [sbuf(p4, "mbt%d" % i, [128, 512], BF16) for i in range(2)]
                    xt5 = [sbuf(p4, "xt5_%d" % i, [128, D], F32) for i in range(2)]
                    mixT = [sbuf(p4, "mixT%d" % i, [128, 8, 128], BF16) for i in range(2)]
                    x1t = [sbuf(p4, "x1t%d" % i, [128, D], F32) for i in range(2)]

                    for k in range(8):
                        S.dma("pool", "w_out", lambda e, k=k: e.dma_start(out=w_out_b[:, k, :], in_=w_out[k * 128:(k + 1) * 128, :]), writes=["w_out_b"])

                    unitsA = [(di, d, g, h) for di, d in enumerate(PATTERNS) for g in range(4) for h in range(8)]

                    def tiles_of(d, g):
                        L = SEQ // d
                        tps = L // 128
                        npc = 2 if L >= 256 else 1
                        tl = []
                        for qt in range(4):
                            qidx = g * 4 + qt
                            r, ti = qidx // tps, qidx % tps
                            wb = min(max(ti * 128 - 64, 0), L - 128 * npc)
                            tl.append((r, ti, wb))
                        return tl, npc

                    def A_S(n):
                        di, d, g, h = unitsA[n]
                        tiles, npc = tiles_of(d, g)
                        vb = (n // 8) % 2
                        if h == 0:
                            for qt in range(4):
                                r, ti, wb = tiles[qt]
                                src = bass.AP(tensor=va_d.tensor, offset=(r0 + r + d * wb) * 520,
                                              ap=[[d * 520, 128], [128 * d * 520, npc], [1, 520]])
                                S.dma("sp", "Vw%d_%d" % (vb, qt), lambda e, qt=qt, src=src: e.dma_start(out=Vw[vb][:, qt, 0:npc, :], in_=src),
                                      writes=[("Vw", vb, qt)])
                        pair, hb_ = h // 2, (h % 2) * 64
                        set_ = n % 2
                        for qt in range(4):
                            r, ti, wb = tiles[qt]
                            qs_ = r + d * ti * 128
                            qap = qaT[hb_:hb_ + 64, pair, qs_:qs_ + d * 127 + 1:d]
                            for pc in range(npc):
                                bk = 2 * set_ + pc
                                ks_ = r + d * (wb + pc * 128)
                                kap = kaT[hb_:hb_ + 64, pair, ks_:ks_ + d * 127 + 1:d]
                                S.op("pe", lambda e, bk=bk, qt=qt, kap=kap, qap=qap: e.matmul(bank[bk][:, qt * 128:(qt + 1) * 128], lhsT=kap, rhs=qap,
                                                                                             start=(qt == 0), stop=False, skip_group_check=True),
                                     writes=[PS(bk)], inc=False)
                        for qt in range(4):
                            r, ti, wb = tiles[qt]
                            for pc in range(npc):
                                bk = 2 * set_ + pc
                                j0 = wb + pc * 128 - ti * 128 + 128
                                S.op("pe", lambda e, bk=bk, qt=qt, j0=j0: e.matmul(bank[bk][:, qt * 128:(qt + 1) * 128],
                                                                                  lhsT=trev[:, di, h, j0:j0 + 128], rhs=jrev_b[:],
                                                                                  start=False, stop=True, skip_group_check=True),
                                     reads=["trev", "jrev_b"], writes=[PS(bk)], inc=(qt == 3))

                    def A_PV(n):
                        di, d, g, h = unitsA[n]
                        tiles, npc = tiles_of(d, g)
                        vb = (n // 8) % 2
                        set_ = n % 2
                        accb = 4 + (n % 2)
                        for pc in range(npc):
                            bk = 2 * set_ + pc
                            S.op("act", lambda e, bk=bk: e.activation(out=PTa[bk][:], in_=bank[bk][:], func=AF.Exp),
                                 reads=[PS(bk)], writes=[("PTa", bk)])
                        accv = bank[accb][:, 0:260].rearrange("p (q c) -> p q c", q=4)
                        for qt in range(4):
                            for pc in range(npc):
                                bk = 2 * set_ + pc
                                S.op("pe", lambda e, bk=bk, qt=qt, pc=pc: e.matmul(
                                    accv[:, qt, :], lhsT=PTa[bk][:, qt * 128:(qt + 1) * 128], rhs=Vw[vb][:, qt, pc, h * 65:(h + 1) * 65],
                                    start=(pc == 0), stop=(pc == npc - 1), skip_group_check=True),
                                     reads=[("PTa", bk), ("Vw", vb, qt)], writes=[PS(accb)], inc=(qt == 3 and pc == npc - 1))
                        S.op("dve", lambda e: e.tensor_copy(out=oa[vb][:, :, h, :], in_=accv), reads=[PS(accb)], writes=[("oa", vb, h)])
                        if h != 7:
                            return
                        oak = [("oa", vb, hh) for hh in range(8)]
                        if d != 1:
                            for qt in range(4):
                                r, ti, wb = tiles[qt]
                                dst = bass.AP(tensor=oa_d.tensor, offset=(di * NTOK + r0 + r + d * ti * 128) * 520, ap=[[d * 520, 128], [1, 520]])
                                S.dma("sp", "oaw%d" % vb, lambda e, qt=qt, dst=dst: e.dma_start(out=dst, in_=oa[vb][:, qt].rearrange("p h c -> p (h c)")),
                                      reads=oak, writes=[("oa_dw", vb)])
                            return
                        if Gd1(n) == 0:
                            merge_loads(0)
                            merge_loads(1)

                    def Gd1(n):
                        return (n - 64) // 8

                    def merge_loads(K):
                        t = K
                        m = K % 2
                        S.dma("pool", "o16_%d" % m, lambda e: e.dma_start(out=o16[m][:], in_=oa_d[0, r0 + t * 128:r0 + (t + 1) * 128, :]),
                              reads=[("oa_dw", 0), ("oa_dw", 1)], writes=[("o16", m)])
                        S.dma("pool", "o4_%d" % m, lambda e: e.dma_start(out=o4[m][:], in_=oa_d[1, r0 + t * 128:r0 + (t + 1) * 128, :]),
                              reads=[("oa_dw", 0), ("oa_dw", 1)], writes=[("o4", m)])

                    def epiA(K):
                        Gd, qt = K // 4, K % 4
                        vb = Gd % 2
                        m = K % 2
                        oak = [("oa", vb, hh) for hh in range(8)]
                        S.op("dve", lambda e: e.tensor_tensor(out=o16[m][:], in0=o16[m][:], in1=o4[m][:], op=ALU.add),
                             reads=[("o16", m), ("o4", m)], writes=[("o16", m)])
                        S.op("dve", lambda e: e.tensor_tensor(out=o16[m][:], in0=o16[m][:], in1=oa[vb][:, qt].rearrange("p h c -> p (h c)"), op=ALU.add),
                             reads=[("o16", m)] + oak, writes=[("o16", m)])
                        ov = o16[m][:].rearrange("p (h c) -> p h c", h=8)
                        S.op("dve", lambda e: e.reciprocal(out=rl4[:], in_=ov[:, :, 64]), reads=[("o16", m)], writes=["rl4"])
                        S.op("dve", lambda e: e.tensor_tensor(out=onb4[:], in0=ov[:, :, 0:64], in1=rl4[:].unsqueeze(2).broadcast_to([128, 8, 64]), op=ALU.mult),
                             reads=["rl4", ("o16", m)], writes=["onb4"])
                        if K + 2 < 16:
                            merge_loads(K + 2)

                    def epiB(K):
                        S.op("act", lambda e: e.activation(out=junk4[:], in_=onb4[:].rearrange("p h c -> p (h c)"), func=AF.Square, accum_out=st4[:, 0:1]),
                             reads=["onb4"], writes=["ss4"])
                        rstd_from_ss(st4[:, 0:1], 512, st4[:, 1:2], st4[:, 2:3], "ss4", "rstd4")

                    def epiC(K):
                        t = K
                        S.op("dve", lambda e: e.scalar_tensor_tensor(out=ma_sb[:, t, :], in0=onb4[:].rearrange("p h c -> p (h c)"), scalar=st4[:, 2:3],
                                                                      in1=g_out_t[:, 0:512], op0=ALU.mult, op1=ALU.mult),
                             reads=["onb4", "rstd4", "g_out_t"], writes=[("ma", t)])
                        if dbg:
                            S.dma("sp", "dbgma", lambda e: e.dma_start(out=mixa_d[r0 + t * 128:r0 + (t + 1) * 128, :], in_=ma_sb[:, t, :]),
                                  reads=[("ma", t)])

                    schedA = {}
                    for K in range(16):
                        Gd, qt = K // 4, K % 4
                        u = 64 + 8 * (Gd + 1) + 2 * qt
                        schedA.setdefault(u, []).append(lambda K=K: epiA(K))
                        schedA.setdefault(u + 1, []).append(lambda K=K: epiB(K))
                        schedA.setdefault(u + 2, []).append(lambda K=K: epiC(K))

                    A_S(0)
                    for n in range(len(unitsA)):
                        if n + 1 < len(unitsA):
                            A_S(n + 1)
                        A_PV(n)
                        for fn in schedA.pop(n, ()):
                            fn()
                    for u in sorted(schedA):
                        for fn in schedA[u]:
                            fn()
                    for t in range(NT):
                        b = t % 2
                        S.dma("sp", "mbt%d" % b, lambda e, b=b, t=t: e.dma_start(out=mbt[b][:], in_=mixb_d[r0 + t * 128:r0 + (t + 1) * 128, :]), writes=[("mbt", b)])
                        S.dma("sp", "xt5_%d" % b, lambda e, b=b, t=t: e.dma_start(out=xt5[b][:], in_=x[r0 + t * 128:r0 + (t + 1) * 128, :]), writes=[("xt5", b)])
                        pv = bank_bf(6).rearrange("p (k t) -> p k t", k=8)
                        for k in range(4):
                            S.op("pe", lambda e, k=k, t=t, pv=pv: e.transpose(out=pv[:, k, :], in_=ma_sb[:, t, k * 128:(k + 1) * 128], identity=ident_b[:]),
                                 reads=[("ma", t), "ident_b"], writes=[PS(6)], inc=False)
                        for k in range(4):
                            S.op("pe", lambda e, k=k, b=b, pv=pv: e.transpose(out=pv[:, 4 + k, :], in_=mbt[b][:, k * 128:(k + 1) * 128], identity=ident_b[:]),
                                 reads=[("mbt", b)], writes=[PS(6)], inc=(k == 3))
                        S.op("act", lambda e, b=b, pv=pv: e.activation(out=mixT[b][:], in_=pv, func=AF.Copy), reads=[PS(6)], writes=[("mixT", b)])
                        for half in range(2):
                            for k in range(8):
                                S.op("pe", lambda e, half=half, k=k, b=b: e.matmul(bank[half][:], lhsT=mixT[b][:, k, :], rhs=w_out_b[:, k, half * 512:(half + 1) * 512],
                                                                                  start=(k == 0), stop=(k == 7)),
                                     reads=[("mixT", b), "w_out_b"], writes=[PS(half)], inc=(k == 7))
                            S.op("dve", lambda e, half=half, b=b: e.tensor_tensor(out=x1t[b][:, half * 512:(half + 1) * 512], in0=bank[half][:],
                                                                                 in1=xt5[b][:, half * 512:(half + 1) * 512], op=ALU.add),
                                 reads=[PS(half), ("xt5", b)], writes=[("x1t", b, half)])
                        S.dma("pool", "x1t%d" % b, lambda e, b=b, t=t: e.dma_start(out=x1_d[r0 + t * 128:r0 + (t + 1) * 128, :], in_=x1t[b][:]),
                              reads=[("x1t", b, 0), ("x1t", b, 1)], writes=[("x1_d", s, t)])
                    S.barrier()


        for s_ in range(NSEQ if stop_after is None else (0 if stop_after == "P0" else 1)):
            seq_body(s_)
        wst.close()

        if stop_after is None:
          with contextlib.ExitStack() as pm:
            NTT = NTOK // 128
            g_ffn_t = sbuf(pm, "g_ffn_t", [128, D], F32)
            g_fin_t = sbuf(pm, "g_fin_t", [128, D], F32)
            b_r_t = sbuf(pm, "b_r_t", [128, 36], F32)
            h2b = sbuf(pm, "h2b", [128, NTT, D], BF16)
            gate1 = sbuf(pm, "gate1", [128, NTT], F32)
            gate2 = sbuf(pm, "gate2", [128, NTT], F32)
            d1i = sbuf(pm, "d1i", [128, NTT], I32)
            d2i = sbuf(pm, "d2i", [128, NTT], I32)
            S.dma("sp", "gffn", lambda e: e.dma_start(out=g_ffn_t[:], in_=bcast_rows(g_ffn, D)), writes=["g_ffn_t"])
            S.dma("sp", "gfin", lambda e: e.dma_start(out=g_fin_t[:], in_=bcast_rows(g_fin, D)), writes=["g_fin_t"])
            S.dma("sp", "brt", lambda e: e.dma_start(out=b_r_t[:], in_=bcast_rows(b_r, 36)), writes=["b_r_t"])
            zt = sbuf(pm, "zt", [128, D], BF16)
            S.op("pool", lambda e: e.memset(zt[:], 0.0), writes=["zt"])
            S.dma("sp", "zfilly", lambda e: e.dma_start(out=ys_d[NROWS:NROWS + 128, :], in_=zt[:]), reads=["zt"], writes=["ys_d"])
            xs_v = xs_d.rearrange("(n p) d -> p n d", p=128)
            NCH = (NROWS + 128) // 128
            for c0 in range(0, NCH, 16):
                c1 = min(c0 + 16, NCH)
                S.dma("sp", "zfill", lambda e, c0=c0, c1=c1: e.dma_start(out=xs_v[:, c0:c1, :], in_=zt[:].unsqueeze(1).broadcast_to([128, c1 - c0, D])),
                      reads=["zt"], writes=["xs_zf"])
            NW = 3
            wgu = [sbuf(pm, "wgu%d" % i, [128, 8, 512], BF16) for i in range(NW)]
            wd = [sbuf(pm, "wd%d" % i, [128, 2, D], BF16) for i in range(NW)]

            def load_w(ex):
                wi = ex % NW
                S.dma("pool", "wg%d" % wi, lambda e: e.dma_start(out=wgu[wi][:, :, 0:256], in_=w_gate[ex].rearrange("(k p) f -> p k f", p=128)),
                      writes=[("wgu", wi)])
                S.dma("pool", "wg%d" % wi, lambda e: e.dma_start(out=wgu[wi][:, :, 256:512], in_=w_up[ex].rearrange("(k p) f -> p k f", p=128)),
                      writes=[("wgu", wi)])
                S.dma("pool", "wd%d" % wi, lambda e: e.dma_start(out=wd[wi][:], in_=w_down[ex].rearrange("(c p) n -> p c n", p=128)),
                      writes=[("wd", wi)])

            for ex in range(NW):
                load_w(ex)
            with contextlib.ExitStack() as m1:
                mxt = [sbuf(m1, "mxt%d" % i, [128, D], F32) for i in range(2)]
                h2f = [sbuf(m1, "h2f%d" % i, [128, D], F32) for i in range(2)]
                h2lo = [sbuf(m1, "h2lo%d" % i, [128, D], BF16) for i in range(2)]
                hiT = [sbuf(m1, "hiT%d" % i, [128, 8, 128], BF16) for i in range(2)]
                loT = [sbuf(m1, "loT%d" % i, [128, 8, 128], BF16) for i in range(2)]
                lgt = [sbuf(m1, "lgt%d" % i, [128, 36], F32) for i in range(2)]
                wr2 = sbuf(m1, "wr2", [128, 8, 72], BF16)
                wrd = sbuf(m1, "wrd", [128, 8, 36], F32)
                S.op("dve", lambda e: e.tensor_copy(out=wr2[:, :, 0:36], in_=wr_f[:]), reads=["wr_f"], writes=["wr2a"])
                S.op("dve", lambda e: e.tensor_tensor(out=wrd[:], in0=wr_f[:], in1=wr2[:, :, 0:36], op=ALU.subtract), reads=["wr_f", "wr2a"], writes=["wrd"])
                S.op("dve", lambda e: e.tensor_copy(out=wr2[:, :, 36:72], in_=wrd[:]), reads=["wrd"], writes=["wr2"])
                junkm = sbuf(m1, "junkm", [128, D], BF16)
                stm = sbuf(m1, "stm", [128, 2, 4], F32)
                lg = sbuf(m1, "lg", [128, NTT, 36], F32)
                gmax = sbuf(m1, "gmax", [128, NTT], F32)
                gm = sbuf(m1, "gm", [128, NTT, 4], F32)
                gsh = sbuf(m1, "gsh", [128, NTT, 4], F32)
                gsum = sbuf(m1, "gsum", [128, NTT], F32)
                ggate = sbuf(m1, "ggate", [128, NTT], F32)
                t48 = sbuf(m1, "t48", [128, NTT, 4, 8], F32)
                ig = sbuf(m1, "ig", [128, NTT, 8], F32)
                ig2 = sbuf(m1, "ig2", [128, NTT, 8], F32)
                m1v = sbuf(m1, "m1v", [128, NTT], F32)
                m2v = sbuf(m1, "m2v", [128, NTT], F32)
                mask1 = sbuf(m1, "mask1", [128, NTT, 8], F32)
                mask2 = sbuf(m1, "mask2", [128, NTT, 8], F32)
                e2 = sbuf(m1, "e2", [128, NTT], F32)
                den = sbuf(m1, "den", [128, NTT], F32)
                OH1 = sbuf(m1, "OH1", [128, NTT, 4, 8], F32)
                OH2 = sbuf(m1, "OH2", [128, NTT, 4, 8], F32)
                OHs = sbuf(m1, "OHs", [128, NTT, 32], F32)
                cumT = sbuf(m1, "cumT", [128, NTT + 1, 32], F32)
                rank_all = sbuf(m1, "rank_all", [128, NTT, 32], F32)
                ebi = sbuf(m1, "ebi", [128, 32], I32)
                ebf = sbuf(m1, "ebf", [128, 32], F32)
                tsel = sbuf(m1, "tsel", [128, NTT, 32], F32)
                rsel = sbuf(m1, "rsel", [128, NTT], F32)
                esel = sbuf(m1, "esel", [128, NTT], F32)
                ovf = sbuf(m1, "ovf", [128, NTT], F32)

                def m1_A(i):
                    b = i % 2
                    S.dma("sp", "mxt%d" % b, lambda e, b=b, i=i: e.dma_start(out=mxt[b][:], in_=x1_d[i * 128:(i + 1) * 128, :]), writes=[("mxt", b)])
                    S.op("act", lambda e, b=b: e.activation(out=junkm[:], in_=mxt[b][:], func=AF.Square, accum_out=stm[:, b, 0:1]),
                         reads=[("mxt", b)], writes=[("mss", b)])
                    rstd_from_ss(stm[:, b, 0:1], D, stm[:, b, 1:2], stm[:, b, 2:3], ("mss", b), "mrstd%d" % b)
                    S.op("dve", lambda e, b=b: e.scalar_tensor_tensor(out=h2f[b][:], in0=mxt[b][:], scalar=stm[:, b, 2:3], in1=g_ffn_t[:], op0=ALU.mult, op1=ALU.mult),
                         reads=[("mxt", b), "mrstd%d" % b, "g_ffn_t"], writes=[("h2f", b)])
                    S.op("act", lambda e, b=b, i=i: e.activation(out=h2b[:, i, :], in_=h2f[b][:], func=AF.Copy), reads=[("h2f", b)], writes=[("h2b", i)])
                    S.op("dve", lambda e, b=b, i=i: e.tensor_tensor(out=h2lo[b][:], in0=h2f[b][:], in1=h2b[:, i, :], op=ALU.subtract),
                         reads=[("h2f", b), ("h2b", i)], writes=[("h2lo", b)])

                def m1_B(i):
                    b = i % 2
                    pvh = bank_bf(0).rearrange("p (k t) -> p k t", k=8)
                    pvl = bank_bf(1).rearrange("p (k t) -> p k t", k=8)
                    for k in range(8):
                        S.op("pe", lambda e, k=k, i=i, pvh=pvh: e.transpose(out=pvh[:, k, :], in_=h2b[:, i, k * 128:(k + 1) * 128], identity=ident_b[:]),
                             reads=[("h2b", i), "ident_b"], writes=[PS(0)], inc=(k == 7))
                    S.op("act", lambda e, b=b, pvh=pvh: e.activation(out=hiT[b][:], in_=pvh, func=AF.Copy), reads=[PS(0)], writes=[("hiT", b)])
                    for k in range(8):
                        S.op("pe", lambda e, k=k, b=b, pvl=pvl: e.transpose(out=pvl[:, k, :], in_=h2lo[b][:, k * 128:(k + 1) * 128], identity=ident_b[:]),
                             reads=[("h2lo", b)], writes=[PS(1)], inc=(k == 7))
                    S.op("dve", lambda e, b=b, pvl=pvl: e.tensor_copy(out=loT[b][:], in_=pvl), reads=[PS(1)], writes=[("loT", b)])
                    lb = 2 + b
                    for k in range(8):
                        S.op("pe", lambda e, k=k, b=b, lb=lb: e.matmul(bank[lb][:, 0:72], lhsT=hiT[b][:, k, :], rhs=wr2[:, k, :], start=(k == 0), stop=False,
                                                                      skip_group_check=True),
                             reads=[("hiT", b), "wr2"], writes=[PS(lb)], inc=False)
                    for k in range(8):
                        S.op("pe", lambda e, k=k, b=b, lb=lb: e.matmul(bank[lb][:, 0:36], lhsT=loT[b][:, k, :], rhs=wr2[:, k, 0:36], start=False, stop=(k == 7),
                                                                      skip_group_check=True),
                             reads=[("loT", b), "wr2"], writes=[PS(lb)], inc=(k == 7))
                    S.op("dve", lambda e, b=b, lb=lb: e.tensor_tensor(out=lgt[b][:], in0=bank[lb][:, 36:72], in1=b_r_t[:], op=ALU.add),
                         reads=[PS(lb), "b_r_t"], writes=[("lgt", b)])
                    S.op("dve", lambda e, i=i, b=b, lb=lb: e.tensor_tensor(out=lg[:, i, :], in0=bank[lb][:, 0:36], in1=lgt[b][:], op=ALU.add),
                         reads=[PS(lb), ("lgt", b)], writes=["lg"])


                def m1_tiles(t0, t1):
                    m1_A(t0)
                    for i in range(t0, t1):
                        if i + 1 < t1:
                            m1_A(i + 1)
                        m1_B(i)

                def m1_route(t0, t1):
                  nt = t1 - t0
                  gl = lg[:, t0:t1, 0:4]
                  el = lg[:, t0:t1, 4:36].rearrange("p t (g e) -> p t g e", g=4)

                  def bc(ap, shape, axis):
                    return ap.unsqueeze(axis).broadcast_to(shape)

                  V = lambda fn, reads, writes: S.op("dve", fn, reads=reads, writes=writes)
                  V(lambda e: e.tensor_reduce(out=gmax[:, t0:t1], in_=gl, op=ALU.max, axis=AX.X), ["lg"], ["gmax"])
                  V(lambda e: e.tensor_tensor(out=gm[:, t0:t1], in0=gl, in1=bc(gmax[:, t0:t1], [128, nt, 4], 2), op=ALU.is_equal), ["lg", "gmax"], ["gm"])
                  V(lambda e: e.tensor_tensor(out=gsh[:, t0:t1], in0=gl, in1=bc(gmax[:, t0:t1], [128, nt, 4], 2), op=ALU.subtract), ["lg", "gmax"], ["gsh"])
                  S.op("act", lambda e: e.activation(out=gsh[:, t0:t1], in_=gsh[:, t0:t1], func=AF.Exp), reads=["gsh"], writes=["gsh"])
                  V(lambda e: e.tensor_reduce(out=gsum[:, t0:t1], in_=gsh[:, t0:t1], op=ALU.add, axis=AX.X), ["gsh"], ["gsum"])
                  V(lambda e: e.reciprocal(out=ggate[:, t0:t1], in_=gsum[:, t0:t1]), ["gsum"], ["ggate"])
                  V(lambda e: e.tensor_tensor(out=t48[:, t0:t1], in0=el, in1=bc(gm[:, t0:t1], [128, nt, 4, 8], 3), op=ALU.mult), ["lg", "gm"], ["t48"])
                  V(lambda e: e.tensor_reduce(out=ig[:, t0:t1], in_=t48[:, t0:t1].rearrange("p t g e -> p t e g"), op=ALU.add, axis=AX.X), ["t48"], ["ig"])
                  V(lambda e: e.tensor_reduce(out=m1v[:, t0:t1], in_=ig[:, t0:t1], op=ALU.max, axis=AX.X), ["ig"], ["m1v"])
                  V(lambda e: e.tensor_tensor(out=mask1[:, t0:t1], in0=ig[:, t0:t1], in1=bc(m1v[:, t0:t1], [128, nt, 8], 2), op=ALU.is_equal), ["ig", "m1v"], ["mask1"])
                  V(lambda e: e.scalar_tensor_tensor(out=ig2[:, t0:t1].rearrange("p t e -> p (t e)"), in0=mask1[:, t0:t1].rearrange("p t e -> p (t e)"), scalar=-1e30,
                                                   in1=ig[:, t0:t1].rearrange("p t e -> p (t e)"), op0=ALU.mult, op1=ALU.add), ["mask1", "ig"], ["ig2"])
                  V(lambda e: e.tensor_reduce(out=m2v[:, t0:t1], in_=ig2[:, t0:t1], op=ALU.max, axis=AX.X), ["ig2"], ["m2v"])
                  V(lambda e: e.tensor_tensor(out=mask2[:, t0:t1], in0=ig2[:, t0:t1], in1=bc(m2v[:, t0:t1], [128, nt, 8], 2), op=ALU.is_equal), ["ig2", "m2v"], ["mask2"])
                  V(lambda e: e.tensor_tensor(out=e2[:, t0:t1], in0=m2v[:, t0:t1], in1=m1v[:, t0:t1], op=ALU.subtract), ["m1v", "m2v"], ["e2"])
                  S.op("act", lambda e: e.activation(out=e2[:, t0:t1], in_=e2[:, t0:t1], func=AF.Exp), reads=["e2"], writes=["e2"])
                  V(lambda e: e.tensor_scalar(out=den[:, t0:t1], in0=e2[:, t0:t1], scalar1=1.0, scalar2=None, op0=ALU.add), ["e2"], ["den"])
                  V(lambda e: e.reciprocal(out=den[:, t0:t1], in_=den[:, t0:t1]), ["den"], ["den"])
                  V(lambda e: e.tensor_tensor(out=gate1[:, t0:t1], in0=ggate[:, t0:t1], in1=den[:, t0:t1], op=ALU.mult), ["ggate", "den"], ["gate1"])
                  V(lambda e: e.tensor_tensor(out=gate2[:, t0:t1], in0=gate1[:, t0:t1], in1=e2[:, t0:t1], op=ALU.mult), ["gate1", "e2"], ["gate2"])
                  V(lambda e: e.tensor_tensor(out=OH1[:, t0:t1], in0=bc(gm[:, t0:t1], [128, nt, 4, 8], 3), in1=bc(mask1[:, t0:t1], [128, nt, 4, 8], 2), op=ALU.mult), ["gm", "mask1"], ["OH1"])
                  V(lambda e: e.tensor_tensor(out=OH2[:, t0:t1], in0=bc(gm[:, t0:t1], [128, nt, 4, 8], 3), in1=bc(mask2[:, t0:t1], [128, nt, 4, 8], 2), op=ALU.mult), ["gm", "mask2"], ["OH2"])
                  V(lambda e: e.tensor_tensor(out=OHs[:, t0:t1].rearrange("p t e -> p (t e)"), in0=OH1[:, t0:t1].rearrange("p t g e -> p (t g e)"),
                                            in1=OH2[:, t0:t1].rearrange("p t g e -> p (t g e)"), op=ALU.add), ["OH1", "OH2"], ["OHs"])
                  for i in range(t0, t1):
                    V(lambda e, i=i: e.tensor_tensor(out=cumT[:, i + 1, :], in0=cumT[:, i, :], in1=OHs[:, i, :], op=ALU.add), [("cumT", i), "OHs"], [("cumT", i + 1)])
                  for i in range(t0, t1):
                    rb = 4 + (i % 2)
                    S.op("pe", lambda e, i=i, rb=rb: e.matmul(bank[rb][:, 0:32], lhsT=ltri_f[:], rhs=OHs[:, i, :], start=True, stop=False),
                         reads=["ltri_f", "OHs"], writes=[PS(rb)], inc=False)
                    S.op("pe", lambda e, i=i, rb=rb: e.matmul(bank[rb][:, 0:32], lhsT=ones_f[:], rhs=cumT[:, i, :], start=False, stop=True),
                         reads=["ones_f", ("cumT", i)], writes=[PS(rb)], inc=True)
                    V(lambda e, i=i, rb=rb: e.tensor_copy(out=rank_all[:, i, :], in_=bank[rb][:, 0:32]), [PS(rb)], ["rank_all"])
                  for (OH, dst_i, nm) in ((OH1, d1i, "1"), (OH2, d2i, "2")):
                    ohf = OH[:, t0:t1].rearrange("p t g e -> p t (g e)")
                    V(lambda e, ohf=ohf: e.tensor_tensor(out=tsel[:, t0:t1], in0=rank_all[:, t0:t1], in1=ohf, op=ALU.mult), ["rank_all", "OH" + nm], ["tsel"])
                    V(lambda e: e.tensor_reduce(out=rsel[:, t0:t1], in_=tsel[:, t0:t1], op=ALU.add, axis=AX.X), ["tsel"], ["rsel"])
                    V(lambda e, ohf=ohf: e.tensor_tensor(out=tsel[:, t0:t1], in0=ohf, in1=bc(ebf[:], [128, nt, 32], 1), op=ALU.mult), ["ebf", "OH" + nm, "rsel"], ["tsel"])
                    V(lambda e: e.tensor_reduce(out=esel[:, t0:t1], in_=tsel[:, t0:t1], op=ALU.add, axis=AX.X), ["tsel"], ["esel"])
                    V(lambda e: e.tensor_scalar(out=ovf[:, t0:t1], in0=rsel[:, t0:t1], scalar1=float(CAPB * 128), scalar2=None, op0=ALU.is_lt), ["rsel"], ["ovf"])
                    V(lambda e: e.tensor_tensor(out=rsel[:, t0:t1], in0=rsel[:, t0:t1], in1=esel[:, t0:t1], op=ALU.add), ["rsel", "esel"], ["rsel"])
                    V(lambda e: e.scalar_tensor_tensor(out=rsel[:, t0:t1], in0=rsel[:, t0:t1], scalar=float(-NROWS), in1=ovf[:, t0:t1], op0=ALU.add, op1=ALU.mult), ["rsel", "ovf"], ["rsel"])
                    V(lambda e: e.tensor_scalar(out=rsel[:, t0:t1], in0=rsel[:, t0:t1], scalar1=float(NROWS), scalar2=None, op0=ALU.add), ["rsel"], ["rsel"])
                    V(lambda e, dst_i=dst_i: e.tensor_copy(out=dst_i[:, t0:t1], in_=rsel[:, t0:t1]), ["rsel"], ["dst" + nm])
                  for i in range(t0, t1):
                      for (dst_i, nm) in ((d1i, "1"), (d2i, "2")):
                          S.dma("pool", "scat" + nm, lambda e, i=i, dst_i=dst_i: e.indirect_dma_start(
                              out=xs_d, out_offset=bass.IndirectOffsetOnAxis(ap=dst_i[:, i:i + 1], axis=0), in_=h2b[:, i, :], in_offset=None),
                                reads=[("h2b", i), "dst" + nm, "xs_zf"], writes=[("xs_s", i, nm)])

                S.op("pool", lambda e: e.memset(cumT[:, 0, :], 0.0), writes=[("cumT", 0)])
                S.op("pool", lambda e: e.iota(ebi[:], pattern=[[CAPB * 128, 32]], base=0, channel_multiplier=0), writes=["ebi"])
                S.op("dve", lambda e: e.tensor_copy(out=ebf[:], in_=ebi[:]), reads=["ebi"], writes=["ebf"])
                NB1 = 4
                for jb in range(NB1):
                    m1_tiles(jb * (NTT // NB1), (jb + 1) * (NTT // NB1))
                    m1_route(jb * (NTT // NB1), (jb + 1) * (NTT // NB1))
                S.barrier()
            with contextlib.ExitStack() as m2:
                xblk = [sbuf(m2, "xblk%d" % i, [128, D], BF16) for i in range(8)]
                xT = [sbuf(m2, "xT%d" % i, [128, 8, 128], BF16) for i in range(3)]
                sg = [sbuf(m2, "sg%d" % i, [128, 256], F32) for i in range(2)]
                hblk = [sbuf(m2, "hblk%d" % i, [128, 256], BF16) for i in range(3)]
                hT2 = [sbuf(m2, "hT2_%d" % i, [128, 2, 128], BF16) for i in range(3)]
                yblk = [sbuf(m2, "yblk%d" % i, [128, D], BF16) for i in range(2)]
                NBLK = N_EXP * CAPB

                def P0(n):
                    xb = n % 8
                    row = n * 128
                    S.dma("sp", "xblk%d" % xb, lambda e: e.dma_start(out=xblk[xb][:], in_=xs_d[row:row + 128, :]), reads=["xs_d"], writes=[("xblk", xb)])

                def P1(n):
                    xb, pb, tb = n % 8, n % 2, n % 3
                    pv = bank_bf(pb).rearrange("p (k t) -> p k t", k=8)
                    for k in range(8):
                        S.op("pe", lambda e, k=k: e.transpose(out=pv[:, k, :], in_=xblk[xb][:, k * 128:(k + 1) * 128], identity=ident_b[:]),
                             reads=[("xblk", xb), "ident_b"], writes=[PS(pb)], inc=(k == 7))
                    S.op("act", lambda e: e.activation(out=xT[tb][:], in_=pv, func=AF.Copy), reads=[PS(pb)], writes=[("xT", tb)])

                def P2(n):
                    tb, gb, hb3, sb2 = n % 3, 2 + n % 2, n % 3, n % 2
                    wi = (n // CAPB) % NW
                    for k in range(8):
                        S.op("pe", lambda e, k=k: e.matmul(bank[gb][:], lhsT=xT[tb][:, k, :], rhs=wgu[wi][:, k, :], start=(k == 0), stop=(k == 7)),
                             reads=[("xT", tb), ("wgu", wi)], writes=[PS(gb)], inc=(k == 7))
                    S.op("act", lambda e: e.activation(out=sg[sb2][:], in_=bank[gb][:, 0:256], func=AF.Silu), reads=[PS(gb)], writes=[("sg", sb2)])
                    S.op("dve", lambda e: e.tensor_tensor(out=hblk[hb3][:], in0=sg[sb2][:], in1=bank[gb][:, 256:512], op=ALU.mult),
                         reads=[("sg", sb2), PS(gb)], writes=[("hblk", hb3)])

                def P3(n):
                    hb3, tb2 = n % 3, 4 + n % 2
                    pv2 = bank_bf(tb2).rearrange("p (k t) -> p k t", k=8)
                    for c in range(2):
                        S.op("pe", lambda e, c=c: e.transpose(out=pv2[:, c, :], in_=hblk[hb3][:, c * 128:(c + 1) * 128], identity=ident_b[:]),
                             reads=[("hblk", hb3)], writes=[PS(tb2)], inc=(c == 1))
                    S.op("dve", lambda e: e.tensor_copy(out=hT2[hb3][:], in_=pv2[:, 0:2, :]), reads=[PS(tb2)], writes=[("hT2", hb3)])

                def P4(n):
                    hb3, yb2 = n % 3, n % 2
                    wi = (n // CAPB) % NW
                    row = n * 128
                    for half in range(2):
                        yb = 6 + half
                        for c in range(2):
                            S.op("pe", lambda e, c=c, half=half, yb=yb: e.matmul(bank[yb][:], lhsT=hT2[hb3][:, c, :], rhs=wd[wi][:, c, half * 512:(half + 1) * 512],
                                                                                start=(c == 0), stop=(c == 1)),
                                 reads=[("hT2", hb3), ("wd", wi)], writes=[PS(yb)], inc=(c == 1))
                        if half == 0:
                            S.op("act", lambda e, yb=yb: e.activation(out=yblk[yb2][:, 0:512], in_=bank[yb][:], func=AF.Copy), reads=[PS(yb)], writes=[("yblk", yb2, 0)])
                        else:
                            S.op("dve", lambda e, yb=yb: e.tensor_copy(out=yblk[yb2][:, 512:1024], in_=bank[yb][:]), reads=[PS(yb)], writes=[("yblk", yb2, 1)])
                    S.dma("sp", "yblk%d" % yb2, lambda e: e.dma_start(out=ys_d[row:row + 128, :], in_=yblk[yb2][:]),
                          reads=[("yblk", yb2, 0), ("yblk", yb2, 1)], writes=[("ys_dw", yb2)])
                    if n % CAPB == CAPB - 1 and n // CAPB + NW < N_EXP:
                        load_w(n // CAPB + NW)

                for step in range(-4, NBLK + 3):
                    for stage, skew in ((P0, -4), (P1, 0), (P2, 1), (P3, 2), (P4, 3)):
                        n = step - skew
                        if 0 <= n < NBLK:
                            stage(n)
                S.barrier()
            with contextlib.ExitStack() as m3:
                y1t = [sbuf(m3, "y1t%d" % i, [128, D], BF16) for i in range(3)]
                y2t = [sbuf(m3, "y2t%d" % i, [128, D], BF16) for i in range(3)]
                fxt = [sbuf(m3, "fxt%d" % i, [128, D], F32) for i in range(3)]
                acc = [sbuf(m3, "facc%d" % i, [128, D], F32) for i in range(2)]
                ot = [sbuf(m3, "fot%d" % i, [128, D], F32) for i in range(2)]
                junkf = sbuf(m3, "junkf", [128, D], BF16)
                stf = sbuf(m3, "stf", [128, 3, 4], F32)
                for i in range(3):
                    S.op("pool", lambda e, i=i: e.memset(y1t[i][:], 0.0), writes=[("y1t", i)])
                    S.op("pool", lambda e, i=i: e.memset(y2t[i][:], 0.0), writes=[("y2t", i)])
                def m3_load(i):
                    b = i % 3
                    S.dma("pool", "y1t%d" % b, lambda e, b=b, i=i: e.indirect_dma_start(
                        out=y1t[b][:], out_offset=None, in_=ys_d, in_offset=bass.IndirectOffsetOnAxis(ap=d1i[:, i:i + 1], axis=0)),
                          reads=["ys_d"], writes=[("y1t", b)])
                    S.dma("pool", "y2t%d" % b, lambda e, b=b, i=i: e.indirect_dma_start(
                        out=y2t[b][:], out_offset=None, in_=ys_d, in_offset=bass.IndirectOffsetOnAxis(ap=d2i[:, i:i + 1], axis=0)),
                          reads=["ys_d"], writes=[("y2t", b)])
                    S.dma("sp", "fxt%d" % b, lambda e, b=b, i=i: e.dma_start(out=fxt[b][:], in_=x1_d[i * 128:(i + 1) * 128, :]), writes=[("fxt", b)])

                def m3_comp(i):
                    b = i % 3
                    c = i % 2
                    S.op("dve", lambda e, b=b, c=c, i=i: e.scalar_tensor_tensor(out=acc[c][:], in0=y1t[b][:], scalar=gate1[:, i:i + 1], in1=fxt[b][:], op0=ALU.mult, op1=ALU.add),
                         reads=[("y1t", b), ("fxt", b)], writes=[("facc", c)])
                    S.op("dve", lambda e, b=b, c=c, i=i: e.scalar_tensor_tensor(out=acc[c][:], in0=y2t[b][:], scalar=gate2[:, i:i + 1], in1=acc[c][:], op0=ALU.mult, op1=ALU.add),
                         reads=[("y2t", b), ("facc", c)], writes=[("facc", c)])
                    S.op("act", lambda e, b=b, c=c: e.activation(out=junkf[:], in_=acc[c][:], func=AF.Square, accum_out=stf[:, c, 0:1]), reads=[("facc", c)], writes=[("fss", c)])
                    rstd_from_ss(stf[:, c, 0:1], D, stf[:, c, 1:2], stf[:, c, 2:3], ("fss", c), "frstd%d" % c)
                    S.op("dve", lambda e, b=b, c=c: e.scalar_tensor_tensor(out=ot[c][:], in0=acc[c][:], scalar=stf[:, c, 2:3], in1=g_fin_t[:], op0=ALU.mult, op1=ALU.mult),
                         reads=[("facc", c), "frstd%d" % c, "g_fin_t"], writes=[("fot", c)])
                    S.dma("sp", "fot%d" % c, lambda e, b=b, c=c, i=i: e.dma_start(out=out[i * 128:(i + 1) * 128, :], in_=ot[c][:]), reads=[("fot", c)])

                PF = 2
                for i in range(PF):
                    m3_load(i)
                for i in range(NTT):
                    if i + PF < NTT:
                        m3_load(i + PF)
                    m3_comp(i)
                S.barrier()

        if stop_after is not None:
            with contextlib.ExitStack() as pz:
                z = sbuf(pz, "z", [128, D], F32)
                S.op("pool", lambda e: e.memset(z[:], 0.0), writes=["z"])
                S.dma("sp", "zout", lambda e: e.dma_start(out=out[0:128, :], in_=z[:]), reads=["z"])
                S.barrier()
        S.barrier()
        S.emit()
    return nc


def _prep_inputs(inputs):
    f = lambda a: np.ascontiguousarray(np.asarray(a, dtype=np.float32))
    rope_cs, oh = _consts()
    shared = {
        "w_in": f(inputs["w_in"][0]),
        "rel_bias": f(inputs["rel_bias"]),
        "w_q_up": f(inputs["w_q_up"][0]),
        "w_kv_up": f(inputs["w_kv_up"][0]),
        "w_out": f(inputs["w_out"][0]),
        "w_rg": f(inputs["w_router_group"][0]),
        "w_re": f(inputs["w_router_expert"][0]),
        "w_gate": f(inputs["w_gate"][0]),
        "w_up": f(inputs["w_up"][0]),
        "w_down": f(inputs["w_down"][0]),
        "g_attn": f(inputs["g_attn_norm"][0]).reshape(1, D),
        "g_q": f(inputs["g_q_latent"][0]).reshape(1, 256),
        "g_kv": f(inputs["g_kv_latent"][0]).reshape(1, 128),
        "g_out": np.concatenate([f(inputs["g_out_a"][0]), f(inputs["g_out_b"][0])]).reshape(1, D),
        "g_ffn": f(inputs["g_ffn_norm"][0]).reshape(1, D),
        "g_fin": f(inputs["g_final"]).reshape(1, D),
        "b_r": np.concatenate([f(inputs["b_router_group"][0]), f(inputs["b_router_expert"][0])]).reshape(1, 36),
        "rope_cs": rope_cs,
        "oh_bias": oh,
    }
    xs = f(inputs["x"]).reshape(N_CORES, NTOK, D)
    return [dict(shared, x=xs[c]) for c in range(N_CORES)]


def kernel(**inputs):
    in_maps = _prep_inputs(inputs)
    nc = build_nc()
    res = run_bass_kernel_spmd(nc, in_maps, core_ids=list(range(N_CORES)))
    outs = [np.asarray(r["out"], dtype=np.float32).reshape(NSEQ, SEQ, D) for r in res.results]
    return np.concatenate(outs, axis=0)
```

```python
import contextlib
import math
import numpy as np
import concourse.bass as bass
import concourse.mybir as mybir
from concourse.bass_utils import run_bass_kernel_spmd

F32 = mybir.dt.float32
BF16 = mybir.dt.bfloat16
I32 = mybir.dt.int32
AF = mybir.ActivationFunctionType
ALU = mybir.AluOpType
AX = mybir.AxisListType

N_CORES = 8
SEQ = 2048
D = 1024
NSEQ = 2
NTOK = NSEQ * SEQ
NT = SEQ // 128
EPS = 1e-6
NEG = -30000.0
PATTERNS = (16, 4, 1)
N_EXP = 32
CAPB = 4
NROWS = N_EXP * CAPB * 128
K2C = 99


class Sync:
    COMPUTE = ("pe", "act", "dve", "pool")

    def __init__(self, nc, stack):
        self.nc = nc
        self.stack = stack
        self.ops = {e: [] for e in ("pe", "act", "dve", "pool", "sp")}
        self.sem = {}
        for e in self.COMPUTE:
            self.sem[e] = stack.enter_context(nc.semaphore("s_" + e))
        self.cnt = {e: 0 for e in self.COMPUTE}
        self.seen = {e: {} for e in self.ops}
        self.keys = {}
        self.pending = {e: ([], []) for e in self.COMPUTE}
        self.slots = {}

    def _key(self, k):
        st = self.keys.get(k)
        if st is None:
            st = {"w": None, "r": []}
            self.keys[k] = st
        return st

    def _need(self, eng, reads, writes, is_dma=False):
        need = {}

        def add(p):
            if p is None:
                return
            s, v, src = p
            if eng == "pe" and src == "pe":
                return
            if need.get(id(s), (None, -1))[1] < v:
                need[id(s)] = (s, v)

        for k in reads:
            st = self._key(k)
            add(st["w"])
            if isinstance(k, tuple) and k and k[0] == "ps":
                for p in st["r"]:
                    if p[2] != eng:
                        add(p)
        for k in writes:
            st = self._key(k)
            if st["w"] is not None and (is_dma or st["w"][2] != eng):
                add(st["w"])
            for p in st["r"]:
                if is_dma or p[2] != eng:
                    add(p)
        out = []
        seen = self.seen[eng]
        for sid, (s, v) in need.items():
            if seen.get(sid, -1) >= v:
                continue
            seen[sid] = v
            out.append((s, v))
        return out

    def _commit(self, reads, writes, prod):
        for k in writes:
            st = self._key(k)
            st["w"] = prod
            st["r"] = []
        for k in reads:
            if k in writes:
                continue
            st = self._key(k)
            st["r"] = [p for p in st["r"] if p[0] is not prod[0]] + [prod]

    def op(self, eng, fn, reads=(), writes=(), inc=True):
        reads, writes = list(reads), list(writes)
        waits = self._need(eng, reads, writes)
        if inc:
            self.cnt[eng] += 1
            prod = (self.sem[eng], self.cnt[eng], eng)
            pr, pw = self.pending[eng]
            self._commit(reads + pr, writes + pw, prod)
            self.pending[eng] = ([], [])
            self.ops[eng].append((waits, fn, (self.sem[eng], 1)))
        else:
            pr, pw = self.pending[eng]
            pr.extend(reads)
            pw.extend(writes)
            self.ops[eng].append((waits, fn, None))

    def dma(self, q, slot, fn, reads=(), writes=()):
        reads, writes = list(reads), list(writes)
        if slot not in self.slots:
            s = self.stack.enter_context(self.nc.semaphore("d_" + slot))
            self.slots[slot] = [s, 0]
        sl = self.slots[slot]
        waits = self._need(q, reads, writes, is_dma=True)
        sl[1] += 16
        self._commit(reads, writes, (sl[0], sl[1], "dma"))
        self.ops[q].append((waits, fn, (sl[0], 16)))

    def barrier(self):
        targets = [(self.sem[e], self.cnt[e]) for e in self.COMPUTE if self.cnt[e] > 0]
        targets += [(s, v) for (s, v) in self.slots.values() if v > 0]
        for e in self.ops:
            waits = []
            seen = self.seen[e]
            for s, v in targets:
                if seen.get(id(s), -1) >= v:
                    continue
                seen[id(s)] = v
                waits.append((s, v))
            if waits:
                self.ops[e].append((waits, None, None))
        self.keys = {}
        self.pending = {e: ([], []) for e in self.COMPUTE}

    def emit(self):
        nc = self.nc
        ops = self.ops

        def run(e, lst):
            for waits, fn, inc in lst:
                for s, v in waits:
                    e.wait_ge(s, v)
                if fn is not None:
                    ins = fn(e)
                    if inc is not None:
                        ins.then_inc(inc[0], inc[1])

        with nc.Block() as block:
            @block.sync
            def _(e):
                run(e, ops["sp"])

            @block.tensor
            def _(e):
                run(e, ops["pe"])

            @block.scalar
            def _(e):
                run(e, ops["act"])

            @block.vector
            def _(e):
                run(e, ops["dve"])

            @block.gpsimd
            def _(e):
                run(e, ops["pool"])


def _t5_bucket(rel):
    half = 16
    max_exact = 8
    n = np.abs(rel)
    large = max_exact + (np.log(np.maximum(n, 1) / max_exact)
                         / math.log(1024 / max_exact) * (half - max_exact)).astype(np.int32)
    large = np.minimum(large, half - 1)
    return (np.where(rel > 0, half, 0) + np.where(n < max_exact, n, large)).astype(np.int32)


def _consts():
    half = 16
    inv_freq = (np.float32(10000.0) ** (-(np.arange(half, dtype=np.float32) / np.float32(half)))).astype(np.float32)
    ang = (np.arange(SEQ, dtype=np.float32)[:, None] * inv_freq[None, :]).astype(np.float32)
    cos, sin = np.cos(ang).astype(np.float32), np.sin(ang).astype(np.float32)
    rope_cs = np.concatenate([cos, cos, sin], axis=1).astype(np.float32)
    oh = np.zeros((33, 3, 512), np.float32)
    for di, d in enumerate(PATTERNS):
        m = np.arange(512)
        delta = m - 255
        valid = np.abs(delta) <= 64
        b = _t5_bucket(delta * d)
        for mm in range(512):
            if valid[mm]:
                oh[b[mm], di, mm] = 1.0
            else:
                oh[32, di, mm] = 1.0
    return rope_cs, oh.reshape(33, 1536)


def build_nc(dbg=False, stop_after=None):
    nc = bass.Bass("TRN2", target_bir_lowering=False)

    def din(name, shape, dt=F32):
        return nc.dram_tensor(name, list(shape), dt, kind="ExternalInput").ap()

    def dscr(name, shape, dt, expose=False):
        kind = "ExternalOutput" if (dbg and expose) else "Internal"
        return nc.dram_tensor(name, list(shape), dt, kind=kind).ap()

    x = din("x", [NTOK, D])
    w_in = din("w_in", [D, 1952])
    rel_bias = din("rel_bias", [32, 8])
    w_q_up = din("w_q_up", [256, 768])
    w_kv_up = din("w_kv_up", [128, 1024])
    w_out = din("w_out", [D, D])
    w_rg = din("w_rg", [D, 4])
    w_re = din("w_re", [D, 32])
    w_gate = din("w_gate", [N_EXP, D, 256])
    w_up = din("w_up", [N_EXP, D, 256])
    w_down = din("w_down", [N_EXP, 256, D])
    g_attn = din("g_attn", [1, D])
    g_q = din("g_q", [1, 256])
    g_kv = din("g_kv", [1, 128])
    g_out = din("g_out", [1, D])
    g_ffn = din("g_ffn", [1, D])
    g_fin = din("g_fin", [1, D])
    b_r = din("b_r", [1, 36])
    rope_cs = din("rope_cs", [SEQ, 48])
    oh_bias = din("oh_bias", [33, 1536])
    out = nc.dram_tensor("out", [NTOK, D], F32, kind="ExternalOutput").ap()

    va_d = dscr("va_d", [NTOK, 520], BF16)
    oa_d = dscr("oa_d", [2, NTOK, 520], F32)
    mixb_d = dscr("mixb_d", [NTOK, 512], BF16, expose=True)
    mixa_d = dscr("mixa_d", [NTOK, 512], BF16, expose=True) if dbg else None
    x1_d = dscr("x1_d", [NTOK, D], F32, expose=True)
    fvec_d = dscr("fvec_d", [8, 1536], F32)
    xs_d = dscr("xs_d", [NROWS + 128, D], BF16)
    ys_d = dscr("ys_d", [NROWS + 128, D], BF16)

    def bcast_rows(ap, n):
        return bass.AP(tensor=ap.tensor, offset=0, ap=[[0, 128], [1, n]])

    with contextlib.ExitStack() as st:
        S = Sync(nc, st)

        uniq = [0]

        def sbuf(stk, name, shape, dt):
            uniq[0] += 1
            return stk.enter_context(nc.sbuf_tensor("%s_%d" % (name, uniq[0]), list(shape), dt))

        psbig = [st.enter_context(nc.psum_tensor("psb%d" % i, [128, 1024], F32)) for i in range(4)]
        bank = [psbig[i // 2][:, (i % 2) * 512:(i % 2 + 1) * 512] for i in range(8)]

        def PS(i):
            return ("ps", i)

        def bank_bf(i):
            return bank[i][:].bitcast(BF16)

        ident_f = sbuf(st, "ident_f", [128, 128], F32)
        ident_b = sbuf(st, "ident_b", [128, 128], BF16)
        jrev_f = sbuf(st, "jrev_f", [128, 128], F32)
        jrev_b = sbuf(st, "jrev_b", [128, 128], BF16)
        ones_f = sbuf(st, "ones_f", [128, 128], F32)
        ltri_f = sbuf(st, "ltri_f", [128, 128], F32)
        eps_t = sbuf(st, "eps_t", [128, 1], F32)
        rope_t = sbuf(st, "rope_t", [128, NT, 48], F32)
        wq_b = sbuf(st, "wq_b", [128, 2, 768], BF16)
        wkv_b = sbuf(st, "wkv_b", [128, 1024], BF16)
        wr_f = sbuf(st, "wr_f", [128, 8, 36], F32)
        g_q_t = sbuf(st, "g_q_t", [128, 256], F32)
        g_kv_t = sbuf(st, "g_kv_t", [128, 128], F32)
        g_out_t = sbuf(st, "g_out_t", [128, D], F32)

        S.op("pool", lambda e: e.memset(ident_f[:], 1.0), writes=["ident_f"])
        S.op("pool", lambda e: e.affine_select(out=ident_f[:], in_=ident_f[:], pattern=[[-1, 128]],
                                               compare_op=ALU.is_equal, fill=0.0, base=0,
                                               channel_multiplier=1), reads=["ident_f"], writes=["ident_f"])
        S.op("pool", lambda e: e.memset(jrev_f[:], 1.0), writes=["jrev_f"])
        S.op("pool", lambda e: e.affine_select(out=jrev_f[:], in_=jrev_f[:], pattern=[[1, 128]],
                                               compare_op=ALU.is_equal, fill=0.0, base=-127,
                                               channel_multiplier=1), reads=["jrev_f"], writes=["jrev_f"])
        S.op("pool", lambda e: e.memset(ones_f[:], 1.0), writes=["ones_f"])
        S.op("pool", lambda e: e.memset(ltri_f[:], 1.0), writes=["ltri_f"])
        S.op("pool", lambda e: e.affine_select(out=ltri_f[:], in_=ltri_f[:], pattern=[[1, 128]],
                                               compare_op=ALU.is_ge, fill=0.0, base=-1,
                                               channel_multiplier=-1), reads=["ltri_f"], writes=["ltri_f"])
        S.op("pool", lambda e: e.memset(eps_t[:], EPS), writes=["eps_t"])
        S.op("dve", lambda e: e.tensor_copy(out=ident_b[:], in_=ident_f[:]), reads=["ident_f"], writes=["ident_b"])
        S.op("dve", lambda e: e.tensor_copy(out=jrev_b[:], in_=jrev_f[:]), reads=["jrev_f"], writes=["jrev_b"])

        S.dma("sp", "c_rope", lambda e: e.dma_start(out=rope_t[:], in_=rope_cs.rearrange("(t p) c -> p t c", p=128)),
              writes=["rope_t"])
        S.dma("pool", "c_wq", lambda e: e.dma_start(out=wq_b[:], in_=w_q_up.rearrange("(c p) n -> p c n", p=128)),
              writes=["wq_b"])
        S.dma("pool", "c_wkv", lambda e: e.dma_start(out=wkv_b[:], in_=w_kv_up), writes=["wkv_b"])
        with nc.allow_non_contiguous_dma(reason="tiny router weights"):
            S.dma("sp", "c_wr", lambda e: e.dma_start(out=wr_f[:, :, 0:4], in_=w_rg.rearrange("(k p) n -> p k n", p=128)),
                  writes=["wr_f"])
            S.dma("sp", "c_wr", lambda e: e.dma_start(out=wr_f[:, :, 4:36], in_=w_re.rearrange("(k p) n -> p k n", p=128)),
                  writes=["wr_f"])
        S.dma("sp", "c_gq", lambda e: e.dma_start(out=g_q_t[:], in_=bcast_rows(g_q, 256)), writes=["g_q_t"])
        S.dma("sp", "c_gkv", lambda e: e.dma_start(out=g_kv_t[:], in_=bcast_rows(g_kv, 128)), writes=["g_kv_t"])
        S.dma("sp", "c_gout", lambda e: e.dma_start(out=g_out_t[:], in_=bcast_rows(g_out, D)), writes=["g_out_t"])

        with contextlib.ExitStack() as s0:
            relb = sbuf(s0, "relb", [33, 8], F32)
            oh_t = sbuf(s0, "oh_t", [33, 1536], F32)
            fvec_sb = sbuf(s0, "fvec_sb", [8, 1536], F32)
            S.op("pool", lambda e: e.memset(relb[32:33, :], NEG), writes=["relb32"])
            S.dma("sp", "c_relb", lambda e: e.dma_start(out=relb[0:32, :], in_=rel_bias), writes=["relb"])
            S.dma("sp", "c_oh", lambda e: e.dma_start(out=oh_t[:], in_=oh_bias), writes=["oh_t"])
            for di in range(3):
                S.op("pe", lambda e, di=di: e.matmul(bank[di][0:8, :], lhsT=relb[:, :], rhs=oh_t[:, di * 512:(di + 1) * 512],
                                                      start=True, stop=True),
                     reads=["relb", "relb32", "oh_t"], writes=[PS(di)])
                S.op("dve", lambda e, di=di: e.tensor_copy(out=fvec_sb[:, di * 512:(di + 1) * 512], in_=bank[di][0:8, :]),
                     reads=[PS(di)], writes=["fvec_sb"])
            S.dma("sp", "c_fv", lambda e: e.dma_start(out=fvec_d, in_=fvec_sb[:]), reads=["fvec_sb"], writes=["fvec_d"])
            S.barrier()

        def rstd_from_ss(ss_ap, n, lnv_ap, rstd_ap, rk, wk):
            S.op("act", lambda e: e.activation(out=lnv_ap, in_=ss_ap, func=AF.Ln, scale=1.0 / n, bias=eps_t[:, 0:1]),
                 reads=[rk, "eps_t"], writes=[wk + "_ln"])
            S.op("act", lambda e: e.activation(out=rstd_ap, in_=lnv_ap, func=AF.Exp, scale=-0.5),
                 reads=[wk + "_ln"], writes=[wk])

        wst = contextlib.ExitStack()
        w_in_b = sbuf(wst, "w_in_b", [128, 8, 1952], BF16)
        for k in range(8):
            S.dma("pool", "w_in", lambda e, k=k: e.dma_start(out=w_in_b[:, k, :], in_=w_in[k * 128:(k + 1) * 128, :]), writes=["w_in_b"])

        def seq_body(s):
            r0 = s * SEQ
            with contextlib.ExitStack() as sq:
                qaT = sbuf(sq, "qaT", [128, 4, SEQ], BF16)
                kaT = sbuf(sq, "kaT", [128, 4, SEQ], BF16)
                sqB = contextlib.ExitStack()
                cqnT = sbuf(sqB, "cqnT", [128, 2, SEQ], BF16)
                ckvnT = sbuf(sqB, "ckvnT", [128, SEQ], BF16)
                kbT = sbuf(sqB, "kbT", [96, 8, SEQ], BF16)
                Vb = sbuf(sqB, "Vb", [128, NT, 8, 65], BF16)
                S.op("pool", lambda e: e.memset(Vb[:, :, :, 64:65], 1.0), writes=["Vb_ones"])

                with contextlib.ExitStack() as p1:
                    g_attn_t = sbuf(p1, "g_attn_t", [128, D], F32)
                    hT = sbuf(p1, "hT", [128, 8, SEQ], BF16)
                    xt = [sbuf(p1, "xt%d" % i, [128, D], F32) for i in range(3)]
                    hb = [sbuf(p1, "hb%d" % i, [128, D], BF16) for i in range(3)]
                    junk = sbuf(p1, "junk", [128, 384], BF16)
                    st1 = sbuf(p1, "st1", [128, 3, 8], F32)
                    vt = [sbuf(p1, "vt%d" % i, [128, 8, 65], BF16) for i in range(2)]
                    cqn = [sbuf(p1, "cqn%d" % i, [128, 256], BF16) for i in range(3)]
                    ckvn = [sbuf(p1, "ckvn%d" % i, [128, 128], BF16) for i in range(3)]
                    ks = [sbuf(p1, "ks%d" % i, [128, 8, 96], BF16) for i in range(3)]
                    krs = [sbuf(p1, "krs%d" % i, [128, 32], F32) for i in range(3)]
                    rtmp = [sbuf(p1, "rtmp%d" % i, [128, 64], F32) for i in range(3)]

                    S.dma("sp", "gattn", lambda e: e.dma_start(out=g_attn_t[:], in_=bcast_rows(g_attn, D)), writes=["g_attn_t"])
                    for i in range(2):
                        S.op("pool", lambda e, i=i: e.memset(vt[i][:, :, 64:65], 1.0), writes=[("vt1", i)])

                    for t in range(NT):
                        b = t % 3
                        S.dma("sp", "xt%d" % b, lambda e, b=b, t=t: e.dma_start(out=xt[b][:], in_=x[r0 + t * 128:r0 + (t + 1) * 128, :]),
                              writes=[("xt", b)])
                        S.op("act", lambda e, b=b: e.activation(out=hb[b][:], in_=xt[b][:], func=AF.Square, accum_out=st1[:, b, 0:1]),
                             reads=[("xt", b)], writes=[("ss", b), ("hb", b)])
                        rstd_from_ss(st1[:, b, 0:1], D, st1[:, b, 1:2], st1[:, b, 2:3], ("ss", b), "rstd%d" % b)
                        S.op("dve", lambda e, b=b: e.scalar_tensor_tensor(out=hb[b][:], in0=xt[b][:], scalar=st1[:, b, 2:3], in1=g_attn_t[:],
                                                                        op0=ALU.mult, op1=ALU.mult),
                             reads=[("xt", b), "rstd%d" % b, "g_attn_t"], writes=[("hb", b)])
                        pb = t % 2
                        pv = bank_bf(pb).rearrange("p (k t) -> p k t", k=8)
                        for k in range(8):
                            S.op("pe", lambda e, k=k, b=b, pv=pv: e.transpose(out=pv[:, k, :], in_=hb[b][:, k * 128:(k + 1) * 128], identity=ident_b[:]),
                                 reads=[("hb", b), "ident_b"], writes=[PS(pb)], inc=(k == 7))
                        eng = "act" if t % 2 == 0 else "dve"
                        if eng == "act":
                            S.op("act", lambda e, t=t, pv=pv: e.activation(out=hT[:, :, t * 128:(t + 1) * 128], in_=pv, func=AF.Copy),
                                 reads=[PS(pb)], writes=[("hT", t)])
                        else:
                            S.op("dve", lambda e, t=t, pv=pv: e.tensor_copy(out=hT[:, :, t * 128:(t + 1) * 128], in_=pv),
                                 reads=[PS(pb)], writes=[("hT", t)])

                    rot = [2, 3, 4, 5]
                    rc = [0]
                    sub = {"P1a": 0, "P1b": 1, "P1c": 2, "P1d": 3}.get(stop_after, 9)

                    def nextbank():
                        bk = rot[rc[0] % len(rot)]
                        rc[0] += 1
                        return bk

                    evc = [0]

                    def evac(out_ap, in_ap, reads, writes, scale=1.0):
                        evc[0] += 1
                        if evc[0] % 2 == 0:
                            S.op("act", lambda e: e.activation(out=out_ap, in_=in_ap, func=AF.Copy, scale=scale), reads=reads, writes=writes)
                        else:
                            S.op("dve", lambda e: e.tensor_scalar(out=out_ap, in0=in_ap, scalar1=scale, scalar2=None, op0=ALU.mult),
                                 reads=reads, writes=writes)

                    for c in range(8 if sub >= 1 else 0):
                        for j in range(4):
                            bk = nextbank()
                            for k in range(8):
                                S.op("pe", lambda e, c=c, j=j, k=k, bk=bk: e.matmul(bank[bk][:], lhsT=w_in_b[:, k, c * 128:(c + 1) * 128],
                                                                                    rhs=hT[:, k, j * 512:(j + 1) * 512], start=(k == 0), stop=(k == 7)),
                                     reads=["w_in_b"] + [("hT", 4 * j + i) for i in range(4)], writes=[PS(bk)], inc=(k == 7))
                            if c < 4:
                                evac(qaT[:, c, j * 512:(j + 1) * 512], bank[bk][:], [PS(bk)], [("qaT", c, j)], scale=0.125)
                            else:
                                evac(kaT[:, c - 4, j * 512:(j + 1) * 512], bank[bk][:], [PS(bk)], [("kaT", c - 4, j)])
                    for t in range(NT if sub >= 2 else 0):
                        b = t % 2
                        bk = nextbank()
                        for k in range(8):
                            S.op("pe", lambda e, t=t, k=k, bk=bk: e.matmul(bank[bk][:], lhsT=hT[:, k, t * 128:(t + 1) * 128],
                                                                            rhs=w_in_b[:, k, 1024:1536], start=(k == 0), stop=(k == 7)),
                                 reads=["w_in_b", ("hT", t)], writes=[PS(bk)], inc=(k == 7))
                        evac(vt[b][:, :, 0:64], bank[bk][:].rearrange("p (h c) -> p h c", h=8), [PS(bk), ("vt1", b)], [("vt", b)])
                        S.dma("sp", "vt%d" % b, lambda e, b=b, t=t: e.dma_start(out=va_d[r0 + t * 128:r0 + (t + 1) * 128, :],
                                                                                  in_=vt[b][:].rearrange("p h c -> p (h c)")),
                              reads=[("vt", b)], writes=[("va_d", t)])
                    def c2_A(t):
                        b = t % 3
                        bk = nextbank()
                        if K2C >= 1:
                            for k in range(8):
                                S.op("pe", lambda e, t=t, k=k, bk=bk: e.matmul(bank[bk][:, 0:416], lhsT=hT[:, k, t * 128:(t + 1) * 128],
                                                                                rhs=w_in_b[:, k, 1536:1952], start=(k == 0), stop=(k == 7)),
                                     reads=["w_in_b", ("hT", t)], writes=[PS(bk)], inc=(k == 7))
                        if K2C >= 2:
                            S.op("act", lambda e, b=b, bk=bk: e.activation(out=junk[:, 0:256], in_=bank[bk][:, 0:256], func=AF.Square,
                                                                            accum_out=st1[:, b, 3:4]), reads=[PS(bk)], writes=[("ssq", b)])
                        if K2C >= 2:
                            S.op("act", lambda e, b=b, bk=bk: e.activation(out=junk[:, 256:384], in_=bank[bk][:, 256:384], func=AF.Square,
                                                                            accum_out=st1[:, b, 5:6]), reads=[PS(bk)], writes=[("sskv", b)])
                        if K2C >= 3:
                            rstd_from_ss(st1[:, b, 3:4], 256, st1[:, b, 4:5], st1[:, b, 4:5], ("ssq", b), "rq%d" % b)
                        if K2C >= 3:
                            rstd_from_ss(st1[:, b, 5:6], 128, st1[:, b, 6:7], st1[:, b, 6:7], ("sskv", b), "rkv%d" % b)
                        if K2C >= 4:
                            S.op("dve", lambda e, b=b, bk=bk: e.scalar_tensor_tensor(out=cqn[b][:], in0=bank[bk][:, 0:256], scalar=st1[:, b, 4:5],
                                                                                      in1=g_q_t[:], op0=ALU.mult, op1=ALU.mult),
                                 reads=[PS(bk), "rq%d" % b, "g_q_t"], writes=[("cqn", b)])
                        if K2C >= 4:
                            S.op("dve", lambda e, b=b, bk=bk: e.scalar_tensor_tensor(out=ckvn[b][:], in0=bank[bk][:, 256:384], scalar=st1[:, b, 6:7],
                                                                                      in1=g_kv_t[:], op0=ALU.mult, op1=ALU.mult),
                                 reads=[PS(bk), "rkv%d" % b, "g_kv_t"], writes=[("ckvn", b)])
                        if K2C >= 5:
                            S.op("dve", lambda e, b=b, bk=bk: e.tensor_copy(out=krs[b][:], in_=bank[bk][:, 384:416]),
                                 reads=[PS(bk)], writes=[("krs", b)])
                        if K2C >= 5:
                            S.op("dve", lambda e, b=b, t=t: e.tensor_tensor(out=rtmp[b][:, 0:32], in0=krs[b][:], in1=rope_t[:, t, 0:32], op=ALU.mult),
                                 reads=[("krs", b), "rope_t"], writes=[("rtA", b)])
                        if K2C >= 5:
                            S.op("dve", lambda e, b=b, t=t: e.tensor_tensor(out=rtmp[b][:, 32:48], in0=krs[b][:, 16:32], in1=rope_t[:, t, 32:48], op=ALU.mult),
                                 reads=[("krs", b), "rope_t"], writes=[("rtB", b)])
                        if K2C >= 5:
                            S.op("dve", lambda e, b=b, t=t: e.tensor_tensor(out=rtmp[b][:, 48:64], in0=krs[b][:, 0:16], in1=rope_t[:, t, 32:48], op=ALU.mult),
                                 reads=[("krs", b), "rope_t"], writes=[("rtC", b)])
                        S.op("dve", lambda e, b=b: e.tensor_tensor(out=ks[b][:, :, 64:80], in0=rtmp[b][:, 0:16].unsqueeze(1).broadcast_to([128, 8, 16]),
                                                                    in1=rtmp[b][:, 32:48].unsqueeze(1).broadcast_to([128, 8, 16]), op=ALU.subtract),
                             reads=[("rtA", b), ("rtB", b)], writes=[("ks_r", b, 0)])
                        S.op("dve", lambda e, b=b: e.tensor_tensor(out=ks[b][:, :, 80:96], in0=rtmp[b][:, 16:32].unsqueeze(1).broadcast_to([128, 8, 16]),
                                                                    in1=rtmp[b][:, 48:64].unsqueeze(1).broadcast_to([128, 8, 16]), op=ALU.add),
                             reads=[("rtA", b), ("rtC", b)], writes=[("ks_r", b, 1)])

                    def c2_B(t):
                        b = t % 3
                        pb = t % 2
                        pv = bank_bf(pb).rearrange("p (k t) -> p k t", k=8)
                        S.op("pe", lambda e, b=b, pv=pv: e.transpose(out=pv[:, 0, :], in_=cqn[b][:, 0:128], identity=ident_b[:]),
                             reads=[("cqn", b), "ident_b"], writes=[PS(pb)], inc=False)
                        S.op("pe", lambda e, b=b, pv=pv: e.transpose(out=pv[:, 1, :], in_=cqn[b][:, 128:256], identity=ident_b[:]),
                             reads=[("cqn", b)], writes=[PS(pb)], inc=False)
                        S.op("pe", lambda e, b=b, pv=pv: e.transpose(out=pv[:, 2, :], in_=ckvn[b][:], identity=ident_b[:]),
                             reads=[("ckvn", b)], writes=[PS(pb)], inc=True)
                        S.op("act", lambda e, t=t, pv=pv: e.activation(out=cqnT[:, :, t * 128:(t + 1) * 128], in_=pv[:, 0:2, :], func=AF.Copy),
                             reads=[PS(pb)], writes=[("cqnT", t)])
                        S.op("dve", lambda e, t=t, pv=pv: e.tensor_copy(out=ckvnT[:, t * 128:(t + 1) * 128], in_=pv[:, 2, :]),
                             reads=[PS(pb)], writes=[("ckvnT", t)])
                        for half in range(2):
                            bk2 = nextbank()
                            S.op("pe", lambda e, t=t, half=half, bk2=bk2: e.matmul(bank[bk2][:], lhsT=ckvnT[:, t * 128:(t + 1) * 128],
                                                                                    rhs=wkv_b[:, half * 512:(half + 1) * 512], start=True, stop=True),
                                 reads=[("ckvnT", t), "wkv_b"], writes=[PS(bk2)])
                            kvv = bank[bk2][:].rearrange("p (h c) -> p h c", h=4)
                            S.op("act", lambda e, b=b, half=half, kvv=kvv: e.activation(out=ks[b][:, half * 4:(half + 1) * 4, 0:64], in_=kvv[:, :, 0:64], func=AF.Copy),
                                 reads=[PS(bk2)], writes=[("ks_n", b, half)])
                            S.op("dve", lambda e, t=t, half=half, kvv=kvv: e.tensor_copy(out=Vb[:, t, half * 4:(half + 1) * 4, 0:64], in_=kvv[:, :, 64:128]),
                                 reads=[PS(bk2), "Vb_ones"], writes=[("Vb", t, half)])
                        pb2 = 6 + t % 2
                        pv2 = bank_bf(pb2).rearrange("p (k t) -> p k t", k=8)
                        for h in range(8):
                            S.op("pe", lambda e, h=h, b=b, pv2=pv2: e.transpose(out=pv2[0:96, h, :], in_=ks[b][:, h, :], identity=ident_b[:]),
                                 reads=[("ks_n", b, 0), ("ks_n", b, 1), ("ks_r", b, 0), ("ks_r", b, 1), "ident_b"], writes=[PS(pb2)], inc=(h == 7))
                        S.op("act", lambda e, t=t, pv2=pv2: e.activation(out=kbT[:, :, t * 128:(t + 1) * 128], in_=pv2[0:96, :, :], func=AF.Copy),
                             reads=[PS(pb2)], writes=[("kbT", t)])

                    c2_n = (NT if K2C >= 99 else 1) if sub >= 3 else 0
                    if c2_n:
                        c2_A(0)
                    for t in range(c2_n):
                        if t + 1 < c2_n:
                            c2_A(t + 1)
                        c2_B(t)
                    S.barrier()
                if stop_after in ("P1", "P1a", "P1b", "P1c", "P1d"):
                    sqB.close()
                    return

                with contextlib.ExitStack() as p3:
                    qbT = [sbuf(p3, "qbT%d" % i, [96, 8, 512], BF16) for i in range(2)]
                    qs = [sbuf(p3, "qs%d" % i, [128, 8, 96], BF16) for i in range(2)]
                    qr = [sbuf(p3, "qr%d" % i, [128, 8, 32], F32) for i in range(2)]
                    qtm = [sbuf(p3, "qtm%d" % i, [128, 8, 64], F32) for i in range(2)]
                    PT = [sbuf(p3, "PT%d" % i, [128, 1024], BF16) for i in range(3)]
                    ob = [sbuf(p3, "ob%d" % i, [128, 4, 8, 65], F32) for i in range(2)]
                    rl = sbuf(p3, "rl", [128, 8], F32)
                    onb = sbuf(p3, "onb", [128, 8, 64], F32)
                    junk3 = sbuf(p3, "junk3", [128, 512], BF16)
                    st3 = sbuf(p3, "st3", [128, 4], F32)
                    mb = [sbuf(p3, "mb%d" % i, [128, 512], BF16) for i in range(2)]

                    def mla_qproj(jq, tts=(0, 1, 2, 3), stages="ab"):
                        qb = jq % 2
                        for tt in tts:
                            t = jq * 4 + tt
                            b2 = tt % 2
                            if "a" not in stages:
                                mla_qproj_b(jq, tt)
                                continue
                            for half in range(2):
                                for c in range(2):
                                    S.op("pe", lambda e, half=half, c=c, t=t: e.matmul(bank[6 + half][:, 0:384], lhsT=cqnT[:, c, t * 128:(t + 1) * 128],
                                                                                       rhs=wq_b[:, c, half * 384:(half + 1) * 384], start=(c == 0), stop=(c == 1)),
                                         reads=["wq_b"], writes=[PS(6 + half)], inc=(c == 1))
                                pvh = bank[6 + half][:, 0:384].rearrange("p (h c) -> p h c", h=4)
                                S.op("dve", lambda e, half=half, b2=b2, pvh=pvh: e.tensor_copy(out=qs[b2][:, half * 4:(half + 1) * 4, 0:64], in_=pvh[:, :, 0:64]),
                                     reads=[PS(6 + half)], writes=[("qs_n", b2, half)])
                                S.op("dve", lambda e, half=half, b2=b2, pvh=pvh: e.tensor_copy(out=qr[b2][:, half * 4:(half + 1) * 4, :], in_=pvh[:, :, 64:96]),
                                     reads=[PS(6 + half)], writes=[("qr", b2, half)])
                            cos2 = rope_t[:, t, 0:32].unsqueeze(1).broadcast_to([128, 8, 32])
                            sin1 = rope_t[:, t, 32:48].unsqueeze(1).broadcast_to([128, 8, 16])
                            qrk = [("qr", b2, 0), ("qr", b2, 1)]
                            S.op("dve", lambda e, b2=b2, cos2=cos2: e.tensor_tensor(out=qtm[b2][:, :, 0:32], in0=qr[b2][:], in1=cos2, op=ALU.mult),
                                 reads=qrk, writes=[("qtA", b2)])
                            S.op("dve", lambda e, b2=b2, sin1=sin1: e.tensor_tensor(out=qtm[b2][:, :, 32:48], in0=qr[b2][:, :, 16:32], in1=sin1, op=ALU.mult),
                                 reads=qrk, writes=[("qtB", b2)])
                            S.op("dve", lambda e, b2=b2, sin1=sin1: e.tensor_tensor(out=qtm[b2][:, :, 48:64], in0=qr[b2][:, :, 0:16], in1=sin1, op=ALU.mult),
                                 reads=qrk, writes=[("qtC", b2)])
                            S.op("dve", lambda e, b2=b2: e.tensor_tensor(out=qs[b2][:, :, 64:80], in0=qtm[b2][:, :, 0:16], in1=qtm[b2][:, :, 32:48], op=ALU.subtract),
                                 reads=[("qtA", b2), ("qtB", b2)], writes=[("qs_r", b2, 0)])
                            S.op("dve", lambda e, b2=b2: e.tensor_tensor(out=qs[b2][:, :, 80:96], in0=qtm[b2][:, :, 16:32], in1=qtm[b2][:, :, 48:64], op=ALU.add),
                                 reads=[("qtA", b2), ("qtC", b2)], writes=[("qs_r", b2, 1)])
                            if "b" in stages:
                                mla_qproj_b(jq, tt)

                    def mla_qproj_b(jq, tt):
                        qb = jq % 2
                        b2 = tt % 2
                        pv = bank_bf(6).rearrange("p (k t) -> p k t", k=8)
                        for h in range(8):
                            S.op("pe", lambda e, h=h: e.transpose(out=pv[0:96, h, :], in_=qs[b2][:, h, :], identity=ident_b[:]),
                                 reads=[("qs_n", b2, 0), ("qs_n", b2, 1), ("qs_r", b2, 0), ("qs_r", b2, 1)], writes=[PS(6)], inc=(h == 7))
                        S.op("dve", lambda e: e.tensor_copy(out=qbT[qb][:, :, tt * 128:(tt + 1) * 128], in_=pv[0:96, :, :]),
                             reads=[PS(6)], writes=[("qbT", qb, tt)])

                    def mla_epilogue(jq, qts=(0, 1, 2, 3), stages="abc"):
                        obj = ob[jq % 2]
                        obk = [("ob", jq % 2, h) for h in range(8)]
                        for qt in qts:
                            t = jq * 4 + qt
                            m = qt % 2
                            if "a" in stages:
                                S.op("dve", lambda e, qt=qt: e.reciprocal(out=rl[:], in_=obj[:, qt, :, 64]), reads=obk, writes=["rl"])
                                S.op("dve", lambda e, qt=qt: e.tensor_tensor(out=onb[:], in0=obj[:, qt, :, 0:64], in1=rl[:].unsqueeze(2).broadcast_to([128, 8, 64]), op=ALU.mult),
                                     reads=["rl"] + obk, writes=["onb"])
                            if "b" in stages:
                                S.op("act", lambda e: e.activation(out=junk3[:], in_=onb[:].rearrange("p h c -> p (h c)"), func=AF.Square, accum_out=st3[:, 0:1]),
                                     reads=["onb"], writes=["ss3"])
                                rstd_from_ss(st3[:, 0:1], 512, st3[:, 1:2], st3[:, 2:3], "ss3", "rstd3")
                            if "c" in stages:
                                S.op("dve", lambda e, m=m: e.scalar_tensor_tensor(out=mb[m][:], in0=onb[:].rearrange("p h c -> p (h c)"), scalar=st3[:, 2:3],
                                                                                   in1=g_out_t[:, 512:1024], op0=ALU.mult, op1=ALU.mult),
                                     reads=["onb", "rstd3", "g_out_t"], writes=[("mb", m)])
                                S.dma("sp", "mb%d" % m, lambda e, m=m, t=t: e.dma_start(out=mixb_d[r0 + t * 128:r0 + (t + 1) * 128, :], in_=mb[m][:]),
                                      reads=[("mb", m)], writes=[("mixb_d", t)])

                    scale_b = 96.0 ** -0.5
                    NU = 8 * (NT // 2)
                    units = [(jq, h, ktp) for jq in range(4) for h in range(8) for ktp in range(NT // 2)]

                    def mla_S(g):
                        jq, h, ktp = units[g]
                        qb = jq % 2
                        pb = g % 2
                        for j in range(2):
                            kt = 2 * ktp + j
                            S.op("pe", lambda e, j=j, kt=kt: e.matmul(bank[2 * pb + j][:], lhsT=kbT[:, h, kt * 128:(kt + 1) * 128], rhs=qbT[qb][:, h, :],
                                                                      start=True, stop=True),
                                 reads=[("qbT", qb, i) for i in range(4)], writes=[PS(2 * pb + j)], inc=(j == 1))

                    def mla_PV(g):
                        jq, h, ktp = units[g]
                        pb = g % 2
                        pti = g % 3
                        pt = PT[pti]
                        accb = 4 + (h % 2)
                        accv = bank[accb][:, 0:260].rearrange("p (q c) -> p q c", q=4)
                        S.op("act", lambda e: e.activation(out=pt[:], in_=psbig[pb][:], func=AF.Exp, scale=scale_b),
                             reads=[PS(2 * pb), PS(2 * pb + 1)], writes=[("PT", pti)])
                        for j in range(2):
                            kt = 2 * ktp + j
                            for qt in range(4):
                                S.op("pe", lambda e, j=j, kt=kt, qt=qt: e.matmul(accv[:, qt, :], lhsT=pt[:, j * 512 + qt * 128:j * 512 + (qt + 1) * 128],
                                                                                  rhs=Vb[:, kt, h, :], start=(kt == 0 and qt == 0), stop=(kt == NT - 1),
                                                                                  skip_group_check=True),
                                     reads=[("PT", pti)], writes=[PS(accb)], inc=(j == 1 and qt == 3))
                        if ktp == NT // 2 - 1:
                            S.op("dve", lambda e: e.tensor_copy(out=ob[jq % 2][:, :, h, :], in_=accv), reads=[PS(accb)], writes=[("ob", jq % 2, h)])

                    sched = {}

                    def at(u, fn):
                        sched.setdefault(u, []).append(fn)

                    for jq in range(4):
                        base = jq * NU
                        if jq > 0:
                            for qt in range(4):
                                u = base + 4 + 6 * qt
                                at(u, lambda jq=jq, qt=qt: mla_epilogue(jq - 1, (qt,), "a"))
                                at(u + 3, lambda jq=jq, qt=qt: mla_epilogue(jq - 1, (qt,), "b"))
                                at(u + 5, lambda jq=jq, qt=qt: mla_epilogue(jq - 1, (qt,), "c"))
                        if jq < 3:
                            for tt in range(4):
                                u = base + 30 + 8 * tt
                                at(u, lambda jq=jq, tt=tt: mla_qproj(jq + 1, (tt,), "a"))
                                at(u + 5, lambda jq=jq, tt=tt: mla_qproj(jq + 1, (tt,), "b"))
                    mla_qproj(0)
                    mla_S(0)
                    for g in range(len(units)):
                        if g + 1 < len(units):
                            mla_S(g + 1)
                        mla_PV(g)
                        for fn in sched.get(g, ()):
                            fn()
                    mla_epilogue(3)
                    S.barrier()
                if stop_after == "B":
                    sqB.close()
                    return

                sqB.close()
                with contextlib.ExitStack() as p4:
                    trev = sbuf(p4, "trev", [128, 3, 8, 384], BF16)
                    for di in range(3):
                        src = bass.AP(tensor=fvec_d.tensor, offset=di * 512, ap=[[1, 128], [1536, 8], [1, 384]])
                        S.dma("pool", "trev", lambda e, di=di, src=src: e.dma_start(out=trev[:, di, :, :], in_=src), writes=["trev"])
                    Vw = [sbuf(p4, "Vw%d" % i, [128, 4, 2, 520], BF16) for i in range(2)]
                    PTa = [sbuf(p4, "PTa%d" % i, [128, 512], BF16) for i in range(4)]
                    oa = [sbuf(p4, "oa%d" % i, [128, 4, 8, 65], F32) for i in range(2)]
                    o16 = [sbuf(p4, "o16_%d" % i, [128, 520], F32) for i in range(2)]
                    o4 = [sbuf(p4, "o4_%d" % i, [128, 520], F32) for i in range(2)]
                    ma_sb = sbuf(p4, "ma_sb", [128, NT, 512], BF16)
                    rl4 = sbuf(p4, "rl4", [128, 8], F32)
                    onb4 = sbuf(p4, "onb4", [128, 8, 64], F32)
                    junk4 = sbuf(p4, "junk4", [128, 512], BF16)
                    st4 = sbuf(p4, "st4", [128, 4], F32)
                    w_out_b = sbuf(p4, "w_out_b", [128, 8, D], BF16)
                    mbt = [sbuf(p4, "mbt%d" % i, [128, 512], BF16) for i in range(2)]
                    xt5 = [sbuf(p4, "xt5_%d" % i, [128, D], F32) for i in range(2)]
                    mixT = [sbuf(p4, "mixT%d" % i, [128, 8, 128], BF16) for i in range(2)]
                    x1t = [sbuf(p4, "x1t%d" % i, [128, D], F32) for i in range(2)]

                    for k in range(8):
                        S.dma("pool", "w_out", lambda e, k=k: e.dma_start(out=w_out_b[:, k, :], in_=w_out[k * 128:(k + 1) * 128, :]), writes=["w_out_b"])

                    unitsA = [(di, d, g, h) for di, d in enumerate(PATTERNS) for g in range(4) for h in range(8)]

                    def tiles_of(d, g):
                        L = SEQ // d
                        tps = L // 128
                        npc = 2 if L >= 256 else 1
                        tl = []
                        for qt in range(4):
                            qidx = g * 4 + qt
                            r, ti = qidx // tps, qidx % tps
                            wb = min(max(ti * 128 - 64, 0), L - 128 * npc)
                            tl.append((r, ti, wb))
                        return tl, npc

                    def A_S(n):
                        di, d, g, h = unitsA[n]
                        tiles, npc = tiles_of(d, g)
                        vb = (n // 8) % 2
                        if h == 0:
                            for qt in range(4):
                                r, ti, wb = tiles[qt]
                                src = bass.AP(tensor=va_d.tensor, offset=(r0 + r + d * wb) * 520,
                                              ap=[[d * 520, 128], [128 * d * 520, npc], [1, 520]])
                                S.dma("sp", "Vw%d_%d" % (vb, qt), lambda e, qt=qt, src=src: e.dma_start(out=Vw[vb][:, qt, 0:npc, :], in_=src),
                                      writes=[("Vw", vb, qt)])
                        pair, hb_ = h // 2, (h % 2) * 64
                        set_ = n % 2
                        for qt in range(4):
                            r, ti, wb = tiles[qt]
                            qs_ = r + d * ti * 128
                            qap = qaT[hb_:hb_ + 64, pair, qs_:qs_ + d * 127 + 1:d]
                            for pc in range(npc):
                                bk = 2 * set_ + pc
                                ks_ = r + d * (wb + pc * 128)
                                kap = kaT[hb_:hb_ + 64, pair, ks_:ks_ + d * 127 + 1:d]
                                S.op("pe", lambda e, bk=bk, qt=qt, kap=kap, qap=qap: e.matmul(bank[bk][:, qt * 128:(qt + 1) * 128], lhsT=kap, rhs=qap,
                                                                                             start=(qt == 0), stop=False, skip_group_check=True),
                                     writes=[PS(bk)], inc=False)
                        for qt in range(4):
                            r, ti, wb = tiles[qt]
                            for pc in range(npc):
                                bk = 2 * set_ + pc
                                j0 = wb + pc * 128 - ti * 128 + 128
                                S.op("pe", lambda e, bk=bk, qt=qt, j0=j0: e.matmul(bank[bk][:, qt * 128:(qt + 1) * 128],
                                                                                  lhsT=trev[:, di, h, j0:j0 + 128], rhs=jrev_b[:],
                                                                                  start=False, stop=True, skip_group_check=True),
                                     reads=["trev", "jrev_b"], writes=[PS(bk)], inc=(qt == 3))

                    def A_PV(n):
                        di, d, g, h = unitsA[n]
                        tiles, npc = tiles_of(d, g)
                        vb = (n // 8) % 2
                        set_ = n % 2
                        accb = 4 + (n % 2)
                        for pc in range(npc):
                            bk = 2 * set_ + pc
                            S.op("act", lambda e, bk=bk: e.activation(out=PTa[bk][:], in_=bank[bk][:], func=AF.Exp),
                                 reads=[PS(bk)], writes=[("PTa", bk)])
                        accv = bank[accb][:, 0:260].rearrange("p (q c) -> p q c", q=4)
                        for qt in range(4):
                            for pc in range(npc):
                                bk = 2 * set_ + pc
                                S.op("pe", lambda e, bk=bk, qt=qt, pc=pc: e.matmul(
                                    accv[:, qt, :], lhsT=PTa[bk][:, qt * 128:(qt + 1) * 128], rhs=Vw[vb][:, qt, pc, h * 65:(h + 1) * 65],
                                    start=(pc == 0), stop=(pc == npc - 1), skip_group_check=True),
                                     reads=[("PTa", bk), ("Vw", vb, qt)], writes=[PS(accb)], inc=(qt == 3 and pc == npc - 1))
                        S.op("dve", lambda e: e.tensor_copy(out=oa[vb][:, :, h, :], in_=accv), reads=[PS(accb)], writes=[("oa", vb, h)])
                        if h != 7:
                            return
                        oak = [("oa", vb, hh) for hh in range(8)]
                        if d != 1:
                            for qt in range(4):
                                r, ti, wb = tiles[qt]
                                dst = bass.AP(tensor=oa_d.tensor, offset=(di * NTOK + r0 + r + d * ti * 128) * 520, ap=[[d * 520, 128], [1, 520]])
                                S.dma("sp", "oaw%d" % vb, lambda e, qt=qt, dst=dst: e.dma_start(out=dst, in_=oa[vb][:, qt].rearrange("p h c -> p (h c)")),
                                      reads=oak, writes=[("oa_dw", vb)])
                            return
                        if Gd1(n) == 0:
                            merge_loads(0)
                            merge_loads(1)

                    def Gd1(n):
                        return (n - 64) // 8

                    def merge_loads(K):
                        t = K
                        m = K % 2
                        S.dma("pool", "o16_%d" % m, lambda e: e.dma_start(out=o16[m][:], in_=oa_d[0, r0 + t * 128:r0 + (t + 1) * 128, :]),
                              reads=[("oa_dw", 0), ("oa_dw", 1)], writes=[("o16", m)])
                        S.dma("pool", "o4_%d" % m, lambda e: e.dma_start(out=o4[m][:], in_=oa_d[1, r0 + t * 128:r0 + (t + 1) * 128, :]),
                              reads=[("oa_dw", 0), ("oa_dw", 1)], writes=[("o4", m)])

                    def epiA(K):
                        Gd, qt = K // 4, K % 4
                        vb = Gd % 2
                        m = K % 2
                        oak = [("oa", vb, hh) for hh in range(8)]
                        S.op("dve", lambda e: e.tensor_tensor(out=o16[m][:], in0=o16[m][:], in1=o4[m][:], op=ALU.add),
                             reads=[("o16", m), ("o4", m)], writes=[("o16", m)])
                        S.op("dve", lambda e: e.tensor_tensor(out=o16[m][:], in0=o16[m][:], in1=oa[vb][:, qt].rearrange("p h c -> p (h c)"), op=ALU.add),
                             reads=[("o16", m)] + oak, writes=[("o16", m)])
                        ov = o16[m][:].rearrange("p (h c) -> p h c", h=8)
                        S.op("dve", lambda e: e.reciprocal(out=rl4[:], in_=ov[:, :, 64]), reads=[("o16", m)], writes=["rl4"])
                        S.op("dve", lambda e: e.tensor_tensor(out=onb4[:], in0=ov[:, :, 0:64], in1=rl4[:].unsqueeze(2).broadcast_to([128, 8, 64]), op=ALU.mult),
                             reads=["rl4", ("o16", m)], writes=["onb4"])
                        if K + 2 < 16:
                            merge_loads(K + 2)

                    def epiB(K):
                        S.op("act", lambda e: e.activation(out=junk4[:], in_=onb4[:].rearrange("p h c -> p (h c)"), func=AF.Square, accum_out=st4[:, 0:1]),
                             reads=["onb4"], writes=["ss4"])
                        rstd_from_ss(st4[:, 0:1], 512, st4[:, 1:2], st4[:, 2:3], "ss4", "rstd4")

                    def epiC(K):
                        t = K
                        S.op("dve", lambda e: e.scalar_tensor_tensor(out=ma_sb[:, t, :], in0=onb4[:].rearrange("p h c -> p (h c)"), scalar=st4[:, 2:3],
                                                                      in1=g_out_t[:, 0:512], op0=ALU.mult, op1=ALU.mult),
                             reads=["onb4", "rstd4", "g_out_t"], writes=[("ma", t)])
                        if dbg:
                            S.dma("sp", "dbgma", lambda e: e.dma_start(out=mixa_d[r0 + t * 128:r0 + (t + 1) * 128, :], in_=ma_sb[:, t, :]),
                                  reads=[("ma", t)])

                    schedA = {}
                    for K in range(16):
                        Gd, qt = K // 4, K % 4
                        u = 64 + 8 * (Gd + 1) + 2 * qt
                        schedA.setdefault(u, []).append(lambda K=K: epiA(K))
                        schedA.setdefault(u + 1, []).append(lambda K=K: epiB(K))
                        schedA.setdefault(u + 2, []).append(lambda K=K: epiC(K))

                    A_S(0)
                    for n in range(len(unitsA)):
                        if n + 1 < len(unitsA):
                            A_S(n + 1)
                        A_PV(n)
                        for fn in schedA.pop(n, ()):
                            fn()
                    for u in sorted(schedA):
                        for fn in schedA[u]:
                            fn()
                    for t in range(NT):
                        b = t % 2
                        S.dma("sp", "mbt%d" % b, lambda e, b=b, t=t: e.dma_start(out=mbt[b][:], in_=mixb_d[r0 + t * 128:r0 + (t + 1) * 128, :]), writes=[("mbt", b)])
                        S.dma("sp", "xt5_%d" % b, lambda e, b=b, t=t: e.dma_start(out=xt5[b][:], in_=x[r0 + t * 128:r0 + (t + 1) * 128, :]), writes=[("xt5", b)])
                        pv = bank_bf(6).rearrange("p (k t) -> p k t", k=8)
                        for k in range(4):
                            S.op("pe", lambda e, k=k, t=t, pv=pv: e.transpose(out=pv[:, k, :], in_=ma_sb[:, t, k * 128:(k + 1) * 128], identity=ident_b[:]),
                                 reads=[("ma", t), "ident_b"], writes=[PS(6)], inc=False)
                        for k in range(4):
                            S.op("pe", lambda e, k=k, b=b, pv=pv: e.transpose(out=pv[:, 4 + k, :], in_=mbt[b][:, k * 128:(k + 1) * 128], identity=ident_b[:]),
                                 reads=[("mbt", b)], writes=[PS(6)], inc=(k == 3))
                        S.op("act", lambda e, b=b, pv=pv: e.activation(out=mixT[b][:], in_=pv, func=AF.Copy), reads=[PS(6)], writes=[("mixT", b)])
                        for half in range(2):
                            for k in range(8):
                                S.op("pe", lambda e, half=half, k=k, b=b: e.matmul(bank[half][:], lhsT=mixT[b][:, k, :], rhs=w_out_b[:, k, half * 512:(half + 1) * 512],
                                                                                  start=(k == 0), stop=(k == 7)),
                                     reads=[("mixT", b), "w_out_b"], writes=[PS(half)], inc=(k == 7))
                            S.op("dve", lambda e, half=half, b=b: e.tensor_tensor(out=x1t[b][:, half * 512:(half + 1) * 512], in0=bank[half][:],
                                                                                 in1=xt5[b][:, half * 512:(half + 1) * 512], op=ALU.add),
                                 reads=[PS(half), ("xt5", b)], writes=[("x1t", b, half)])
                        S.dma("pool", "x1t%d" % b, lambda e, b=b, t=t: e.dma_start(out=x1_d[r0 + t * 128:r0 + (t + 1) * 128, :], in_=x1t[b][:]),
                              reads=[("x1t", b, 0), ("x1t", b, 1)], writes=[("x1_d", s, t)])
                    S.barrier()


        for s_ in range(NSEQ if stop_after is None else (0 if stop_after == "P0" else 1)):
            seq_body(s_)
        wst.close()

        if stop_after is None:
          with contextlib.ExitStack() as pm:
            NTT = NTOK // 128
            g_ffn_t = sbuf(pm, "g_ffn_t", [128, D], F32)
            g_fin_t = sbuf(pm, "g_fin_t", [128, D], F32)
            b_r_t = sbuf(pm, "b_r_t", [128, 36], F32)
            h2b = sbuf(pm, "h2b", [128, NTT, D], BF16)
            gate1 = sbuf(pm, "gate1", [128, NTT], F32)
            gate2 = sbuf(pm, "gate2", [128, NTT], F32)
            d1i = sbuf(pm, "d1i", [128, NTT], I32)
            d2i = sbuf(pm, "d2i", [128, NTT], I32)
            S.dma("sp", "gffn", lambda e: e.dma_start(out=g_ffn_t[:], in_=bcast_rows(g_ffn, D)), writes=["g_ffn_t"])
            S.dma("sp", "gfin", lambda e: e.dma_start(out=g_fin_t[:], in_=bcast_rows(g_fin, D)), writes=["g_fin_t"])
            S.dma("sp", "brt", lambda e: e.dma_start(out=b_r_t[:], in_=bcast_rows(b_r, 36)), writes=["b_r_t"])
            zt = sbuf(pm, "zt", [128, D], BF16)
            S.op("pool", lambda e: e.memset(zt[:], 0.0), writes=["zt"])
            S.dma("sp", "zfilly", lambda e: e.dma_start(out=ys_d[NROWS:NROWS + 128, :], in_=zt[:]), reads=["zt"], writes=["ys_d"])
            xs_v = xs_d.rearrange("(n p) d -> p n d", p=128)
            NCH = (NROWS + 128) // 128
            for c0 in range(0, NCH, 16):
                c1 = min(c0 + 16, NCH)
                S.dma("sp", "zfill", lambda e, c0=c0, c1=c1: e.dma_start(out=xs_v[:, c0:c1, :], in_=zt[:].unsqueeze(1).broadcast_to([128, c1 - c0, D])),
                      reads=["zt"], writes=["xs_zf"])
            NW = 3
            wgu = [sbuf(pm, "wgu%d" % i, [128, 8, 512], BF16) for i in range(NW)]
            wd = [sbuf(pm, "wd%d" % i, [128, 2, D], BF16) for i in range(NW)]

            def load_w(ex):
                wi = ex % NW
                S.dma("pool", "wg%d" % wi, lambda e: e.dma_start(out=wgu[wi][:, :, 0:256], in_=w_gate[ex].rearrange("(k p) f -> p k f", p=128)),
                      writes=[("wgu", wi)])
                S.dma("pool", "wg%d" % wi, lambda e: e.dma_start(out=wgu[wi][:, :, 256:512], in_=w_up[ex].rearrange("(k p) f -> p k f", p=128)),
                      writes=[("wgu", wi)])
                S.dma("pool", "wd%d" % wi, lambda e: e.dma_start(out=wd[wi][:], in_=w_down[ex].rearrange("(c p) n -> p c n", p=128)),
                      writes=[("wd", wi)])

            for ex in range(NW):
                load_w(ex)
            with contextlib.ExitStack() as m1:
                mxt = [sbuf(m1, "mxt%d" % i, [128, D], F32) for i in range(2)]
                h2f = [sbuf(m1, "h2f%d" % i, [128, D], F32) for i in range(2)]
                h2lo = [sbuf(m1, "h2lo%d" % i, [128, D], BF16) for i in range(2)]
                hiT = [sbuf(m1, "hiT%d" % i, [128, 8, 128], BF16) for i in range(2)]
                loT = [sbuf(m1, "loT%d" % i, [128, 8, 128], BF16) for i in range(2)]
                lgt = [sbuf(m1, "lgt%d" % i, [128, 36], F32) for i in range(2)]
                wr2 = sbuf(m1, "wr2", [128, 8, 72], BF16)
                wrd = sbuf(m1, "wrd", [128, 8, 36], F32)
                S.op("dve", lambda e: e.tensor_copy(out=wr2[:, :, 0:36], in_=wr_f[:]), reads=["wr_f"], writes=["wr2a"])
                S.op("dve", lambda e: e.tensor_tensor(out=wrd[:], in0=wr_f[:], in1=wr2[:, :, 0:36], op=ALU.subtract), reads=["wr_f", "wr2a"], writes=["wrd"])
                S.op("dve", lambda e: e.tensor_copy(out=wr2[:, :, 36:72], in_=wrd[:]), reads=["wrd"], writes=["wr2"])
                junkm = sbuf(m1, "junkm", [128, D], BF16)
                stm = sbuf(m1, "stm", [128, 2, 4], F32)
                lg = sbuf(m1, "lg", [128, NTT, 36], F32)
                gmax = sbuf(m1, "gmax", [128, NTT], F32)
                gm = sbuf(m1, "gm", [128, NTT, 4], F32)
                gsh = sbuf(m1, "gsh", [128, NTT, 4], F32)
                gsum = sbuf(m1, "gsum", [128, NTT], F32)
                ggate = sbuf(m1, "ggate", [128, NTT], F32)
                t48 = sbuf(m1, "t48", [128, NTT, 4, 8], F32)
                ig = sbuf(m1, "ig", [128, NTT, 8], F32)
                ig2 = sbuf(m1, "ig2", [128, NTT, 8], F32)
                m1v = sbuf(m1, "m1v", [128, NTT], F32)
                m2v = sbuf(m1, "m2v", [128, NTT], F32)
                mask1 = sbuf(m1, "mask1", [128, NTT, 8], F32)
                mask2 = sbuf(m1, "mask2", [128, NTT, 8], F32)
                e2 = sbuf(m1, "e2", [128, NTT], F32)
                den = sbuf(m1, "den", [128, NTT], F32)
                OH1 = sbuf(m1, "OH1", [128, NTT, 4, 8], F32)
                OH2 = sbuf(m1, "OH2", [128, NTT, 4, 8], F32)
                OHs = sbuf(m1, "OHs", [128, NTT, 32], F32)
                cumT = sbuf(m1, "cumT", [128, NTT + 1, 32], F32)
                rank_all = sbuf(m1, "rank_all", [128, NTT, 32], F32)
                ebi = sbuf(m1, "ebi", [128, 32], I32)
                ebf = sbuf(m1, "ebf", [128, 32], F32)
                tsel = sbuf(m1, "tsel", [128, NTT, 32], F32)
                rsel = sbuf(m1, "rsel", [128, NTT], F32)
                esel = sbuf(m1, "esel", [128, NTT], F32)
                ovf = sbuf(m1, "ovf", [128, NTT], F32)

                def m1_A(i):
                    b = i % 2
                    S.dma("sp", "mxt%d" % b, lambda e, b=b, i=i: e.dma_start(out=mxt[b][:], in_=x1_d[i * 128:(i + 1) * 128, :]), writes=[("mxt", b)])
                    S.op("act", lambda e, b=b: e.activation(out=junkm[:], in_=mxt[b][:], func=AF.Square, accum_out=stm[:, b, 0:1]),
                         reads=[("mxt", b)], writes=[("mss", b)])
                    rstd_from_ss(stm[:, b, 0:1], D, stm[:, b, 1:2], stm[:, b, 2:3], ("mss", b), "mrstd%d" % b)
                    S.op("dve", lambda e, b=b: e.scalar_tensor_tensor(out=h2f[b][:], in0=mxt[b][:], scalar=stm[:, b, 2:3], in1=g_ffn_t[:], op0=ALU.mult, op1=ALU.mult),
                         reads=[("mxt", b), "mrstd%d" % b, "g_ffn_t"], writes=[("h2f", b)])
                    S.op("act", lambda e, b=b, i=i: e.activation(out=h2b[:, i, :], in_=h2f[b][:], func=AF.Copy), reads=[("h2f", b)], writes=[("h2b", i)])
                    S.op("dve", lambda e, b=b, i=i: e.tensor_tensor(out=h2lo[b][:], in0=h2f[b][:], in1=h2b[:, i, :], op=ALU.subtract),
                         reads=[("h2f", b), ("h2b", i)], writes=[("h2lo", b)])

                def m1_B(i):
                    b = i % 2
                    pvh = bank_bf(0).rearrange("p (k t) -> p k t", k=8)
                    pvl = bank_bf(1).rearrange("p (k t) -> p k t", k=8)
                    for k in range(8):
                        S.op("pe", lambda e, k=k, i=i, pvh=pvh: e.transpose(out=pvh[:, k, :], in_=h2b[:, i, k * 128:(k + 1) * 128], identity=ident_b[:]),
                             reads=[("h2b", i), "ident_b"], writes=[PS(0)], inc=(k == 7))
                    S.op("act", lambda e, b=b, pvh=pvh: e.activation(out=hiT[b][:], in_=pvh, func=AF.Copy), reads=[PS(0)], writes=[("hiT", b)])
                    for k in range(8):
                        S.op("pe", lambda e, k=k, b=b, pvl=pvl: e.transpose(out=pvl[:, k, :], in_=h2lo[b][:, k * 128:(k + 1) * 128], identity=ident_b[:]),
                             reads=[("h2lo", b)], writes=[PS(1)], inc=(k == 7))
                    S.op("dve", lambda e, b=b, pvl=pvl: e.tensor_copy(out=loT[b][:], in_=pvl), reads=[PS(1)], writes=[("loT", b)])
                    lb = 2 + b
                    for k in range(8):
                        S.op("pe", lambda e, k=k, b=b, lb=lb: e.matmul(bank[lb][:, 0:72], lhsT=hiT[b][:, k, :], rhs=wr2[:, k, :], start=(k == 0), stop=False,
                                                                      skip_group_check=True),
                             reads=[("hiT", b), "wr2"], writes=[PS(lb)], inc=False)
                    for k in range(8):
                        S.op("pe", lambda e, k=k, b=b, lb=lb: e.matmul(bank[lb][:, 0:36], lhsT=loT[b][:, k, :], rhs=wr2[:, k, 0:36], start=False, stop=(k == 7),
                                                                      skip_group_check=True),
                             reads=[("loT", b), "wr2"], writes=[PS(lb)], inc=(k == 7))
                    S.op("dve", lambda e, b=b, lb=lb: e.tensor_tensor(out=lgt[b][:], in0=bank[lb][:, 36:72], in1=b_r_t[:], op=ALU.add),
                         reads=[PS(lb), "b_r_t"], writes=[("lgt", b)])
                    S.op("dve", lambda e, i=i, b=b, lb=lb: e.tensor_tensor(out=lg[:, i, :], in0=bank[lb][:, 0:36], in1=lgt[b][:], op=ALU.add),
                         reads=[PS(lb), ("lgt", b)], writes=["lg"])


                def m1_tiles(t0, t1):
                    m1_A(t0)
                    for i in range(t0, t1):
                        if i + 1 < t1:
                            m1_A(i + 1)
                        m1_B(i)

                def m1_route(t0, t1):
                  nt = t1 - t0
                  gl = lg[:, t0:t1, 0:4]
                  el = lg[:, t0:t1, 4:36].rearrange("p t (g e) -> p t g e", g=4)

                  def bc(ap, shape, axis):
                    return ap.unsqueeze(axis).broadcast_to(shape)

                  V = lambda fn, reads, writes: S.op("dve", fn, reads=reads, writes=writes)
                  V(lambda e: e.tensor_reduce(out=gmax[:, t0:t1], in_=gl, op=ALU.max, axis=AX.X), ["lg"], ["gmax"])
                  V(lambda e: e.tensor_tensor(out=gm[:, t0:t1], in0=gl, in1=bc(gmax[:, t0:t1], [128, nt, 4], 2), op=ALU.is_equal), ["lg", "gmax"], ["gm"])
                  V(lambda e: e.tensor_tensor(out=gsh[:, t0:t1], in0=gl, in1=bc(gmax[:, t0:t1], [128, nt, 4], 2), op=ALU.subtract), ["lg", "gmax"], ["gsh"])
                  S.op("act", lambda e: e.activation(out=gsh[:, t0:t1], in_=gsh[:, t0:t1], func=AF.Exp), reads=["gsh"], writes=["gsh"])
                  V(lambda e: e.tensor_reduce(out=gsum[:, t0:t1], in_=gsh[:, t0:t1], op=ALU.add, axis=AX.X), ["gsh"], ["gsum"])
                  V(lambda e: e.reciprocal(out=ggate[:, t0:t1], in_=gsum[:, t0:t1]), ["gsum"], ["ggate"])
                  V(lambda e: e.tensor_tensor(out=t48[:, t0:t1], in0=el, in1=bc(gm[:, t0:t1], [128, nt, 4, 8], 3), op=ALU.mult), ["lg", "gm"], ["t48"])
                  V(lambda e: e.tensor_reduce(out=ig[:, t0:t1], in_=t48[:, t0:t1].rearrange("p t g e -> p t e g"), op=ALU.add, axis=AX.X), ["t48"], ["ig"])
                  V(lambda e: e.tensor_reduce(out=m1v[:, t0:t1], in_=ig[:, t0:t1], op=ALU.max, axis=AX.X), ["ig"], ["m1v"])
                  V(lambda e: e.tensor_tensor(out=mask1[:, t0:t1], in0=ig[:, t0:t1], in1=bc(m1v[:, t0:t1], [128, nt, 8], 2), op=ALU.is_equal), ["ig", "m1v"], ["mask1"])
                  V(lambda e: e.scalar_tensor_tensor(out=ig2[:, t0:t1].rearrange("p t e -> p (t e)"), in0=mask1[:, t0:t1].rearrange("p t e -> p (t e)"), scalar=-1e30,
                                                   in1=ig[:, t0:t1].rearrange("p t e -> p (t e)"), op0=ALU.mult, op1=ALU.add), ["mask1", "ig"], ["ig2"])
                  V(lambda e: e.tensor_reduce(out=m2v[:, t0:t1], in_=ig2[:, t0:t1], op=ALU.max, axis=AX.X), ["ig2"], ["m2v"])
                  V(lambda e: e.tensor_tensor(out=mask2[:, t0:t1], in0=ig2[:, t0:t1], in1=bc(m2v[:, t0:t1], [128, nt, 8], 2), op=ALU.is_equal), ["ig2", "m2v"], ["mask2"])
                  V(lambda e: e.tensor_tensor(out=e2[:, t0:t1], in0=m2v[:, t0:t1], in1=m1v[:, t0:t1], op=ALU.subtract), ["m1v", "m2v"], ["e2"])
                  S.op("act", lambda e: e.activation(out=e2[:, t0:t1], in_=e2[:, t0:t1], func=AF.Exp), reads=["e2"], writes=["e2"])
                  V(lambda e: e.tensor_scalar(out=den[:, t0:t1], in0=e2[:, t0:t1], scalar1=1.0, scalar2=None, op0=ALU.add), ["e2"], ["den"])
                  V(lambda e: e.reciprocal(out=den[:, t0:t1], in_=den[:, t0:t1]), ["den"], ["den"])
                  V(lambda e: e.tensor_tensor(out=gate1[:, t0:t1], in0=ggate[:, t0:t1], in1=den[:, t0:t1], op=ALU.mult), ["ggate", "den"], ["gate1"])
                  V(lambda e: e.tensor_tensor(out=gate2[:, t0:t1], in0=gate1[:, t0:t1], in1=e2[:, t0:t1], op=ALU.mult), ["gate1", "e2"], ["gate2"])
                  V(lambda e: e.tensor_tensor(out=OH1[:, t0:t1], in0=bc(gm[:, t0:t1], [128, nt, 4, 8], 3), in1=bc(mask1[:, t0:t1], [128, nt, 4, 8], 2), op=ALU.mult), ["gm", "mask1"], ["OH1"])
                  V(lambda e: e.tensor_tensor(out=OH2[:, t0:t1], in0=bc(gm[:, t0:t1], [128, nt, 4, 8], 3), in1=bc(mask2[:, t0:t1], [128, nt, 4, 8], 2), op=ALU.mult), ["gm", "mask2"], ["OH2"])
                  V(lambda e: e.tensor_tensor(out=OHs[:, t0:t1].rearrange("p t e -> p (t e)"), in0=OH1[:, t0:t1].rearrange("p t g e -> p (t g e)"),
                                            in1=OH2[:, t0:t1].rearrange("p t g e -> p (t g e)"), op=ALU.add), ["OH1", "OH2"], ["OHs"])
                  for i in range(t0, t1):
                    V(lambda e, i=i: e.tensor_tensor(out=cumT[:, i + 1, :], in0=cumT[:, i, :], in1=OHs[:, i, :], op=ALU.add), [("cumT", i), "OHs"], [("cumT", i + 1)])
                  for i in range(t0, t1):
                    rb = 4 + (i % 2)
                    S.op("pe", lambda e, i=i, rb=rb: e.matmul(bank[rb][:, 0:32], lhsT=ltri_f[:], rhs=OHs[:, i, :], start=True, stop=False),
                         reads=["ltri_f", "OHs"], writes=[PS(rb)], inc=False)
                    S.op("pe", lambda e, i=i, rb=rb: e.matmul(bank[rb][:, 0:32], lhsT=ones_f[:], rhs=cumT[:, i, :], start=False, stop=True),
                         reads=["ones_f", ("cumT", i)], writes=[PS(rb)], inc=True)
                    V(lambda e, i=i, rb=rb: e.tensor_copy(out=rank_all[:, i, :], in_=bank[rb][:, 0:32]), [PS(rb)], ["rank_all"])
                  for (OH, dst_i, nm) in ((OH1, d1i, "1"), (OH2, d2i, "2")):
                    ohf = OH[:, t0:t1].rearrange("p t g e -> p t (g e)")
                    V(lambda e, ohf=ohf: e.tensor_tensor(out=tsel[:, t0:t1], in0=rank_all[:, t0:t1], in1=ohf, op=ALU.mult), ["rank_all", "OH" + nm], ["tsel"])
                    V(lambda e: e.tensor_reduce(out=rsel[:, t0:t1], in_=tsel[:, t0:t1], op=ALU.add, axis=AX.X), ["tsel"], ["rsel"])
                    V(lambda e, ohf=ohf: e.tensor_tensor(out=tsel[:, t0:t1], in0=ohf, in1=bc(ebf[:], [128, nt, 32], 1), op=ALU.mult), ["ebf", "OH" + nm, "rsel"], ["tsel"])
                    V(lambda e: e.tensor_reduce(out=esel[:, t0:t1], in_=tsel[:, t0:t1], op=ALU.add, axis=AX.X), ["tsel"], ["esel"])
                    V(lambda e: e.tensor_scalar(out=ovf[:, t0:t1], in0=rsel[:, t0:t1], scalar1=float(CAPB * 128), scalar2=None, op0=ALU.is_lt), ["rsel"], ["ovf"])
                    V(lambda e: e.tensor_tensor(out=rsel[:, t0:t1], in0=rsel[:, t0:t1], in1=esel[:, t0:t1], op=ALU.add), ["rsel", "esel"], ["rsel"])
                    V(lambda e: e.scalar_tensor_tensor(out=rsel[:, t0:t1], in0=rsel[:, t0:t1], scalar=float(-NROWS), in1=ovf[:, t0:t1], op0=ALU.add, op1=ALU.mult), ["rsel", "ovf"], ["rsel"])
                    V(lambda e: e.tensor_scalar(out=rsel[:, t0:t1], in0=rsel[:, t0:t1], scalar1=float(NROWS), scalar2=None, op0=ALU.add), ["rsel"], ["rsel"])
                    V(lambda e, dst_i=dst_i: e.tensor_copy(out=dst_i[:, t0:t1], in_=rsel[:, t0:t1]), ["rsel"], ["dst" + nm])
                  for i in range(t0, t1):
                      for (dst_i, nm) in ((d1i, "1"), (d2i, "2")):
                          S.dma("pool", "scat" + nm, lambda e, i=i, dst_i=dst_i: e.indirect_dma_start(
                              out=xs_d, out_offset=bass.IndirectOffsetOnAxis(ap=dst_i[:, i:i + 1], axis=0), in_=h2b[:, i, :], in_offset=None),
                                reads=[("h2b", i), "dst" + nm, "xs_zf"], writes=[("xs_s", i, nm)])

                S.op("pool", lambda e: e.memset(cumT[:, 0, :], 0.0), writes=[("cumT", 0)])
                S.op("pool", lambda e: e.iota(ebi[:], pattern=[[CAPB * 128, 32]], base=0, channel_multiplier=0), writes=["ebi"])
                S.op("dve", lambda e: e.tensor_copy(out=ebf[:], in_=ebi[:]), reads=["ebi"], writes=["ebf"])
                NB1 = 4
                for jb in range(NB1):
                    m1_tiles(jb * (NTT // NB1), (jb + 1) * (NTT // NB1))
                    m1_route(jb * (NTT // NB1), (jb + 1) * (NTT // NB1))
                S.barrier()
            with contextlib.ExitStack() as m2:
                xblk = [sbuf(m2, "xblk%d" % i, [128, D], BF16) for i in range(8)]
                xT = [sbuf(m2, "xT%d" % i, [128, 8, 128], BF16) for i in range(3)]
                sg = [sbuf(m2, "sg%d" % i, [128, 256], F32) for i in range(2)]
                hblk = [sbuf(m2, "hblk%d" % i, [128, 256], BF16) for i in range(3)]
                hT2 = [sbuf(m2, "hT2_%d" % i, [128, 2, 128], BF16) for i in range(3)]
                yblk = [sbuf(m2, "yblk%d" % i, [128, D], BF16) for i in range(2)]
                NBLK = N_EXP * CAPB

                def P0(n):
                    xb = n % 8
                    row = n * 128
                    S.dma("sp", "xblk%d" % xb, lambda e: e.dma_start(out=xblk[xb][:], in_=xs_d[row:row + 128, :]), reads=["xs_d"], writes=[("xblk", xb)])

                def P1(n):
                    xb, pb, tb = n % 8, n % 2, n % 3
                    pv = bank_bf(pb).rearrange("p (k t) -> p k t", k=8)
                    for k in range(8):
                        S.op("pe", lambda e, k=k: e.transpose(out=pv[:, k, :], in_=xblk[xb][:, k * 128:(k + 1) * 128], identity=ident_b[:]),
                             reads=[("xblk", xb), "ident_b"], writes=[PS(pb)], inc=(k == 7))
                    S.op("act", lambda e: e.activation(out=xT[tb][:], in_=pv, func=AF.Copy), reads=[PS(pb)], writes=[("xT", tb)])

                def P2(n):
                    tb, gb, hb3, sb2 = n % 3, 2 + n % 2, n % 3, n % 2
                    wi = (n // CAPB) % NW
                    for k in range(8):
                        S.op("pe", lambda e, k=k: e.matmul(bank[gb][:], lhsT=xT[tb][:, k, :], rhs=wgu[wi][:, k, :], start=(k == 0), stop=(k == 7)),
                             reads=[("xT", tb), ("wgu", wi)], writes=[PS(gb)], inc=(k == 7))
                    S.op("act", lambda e: e.activation(out=sg[sb2][:], in_=bank[gb][:, 0:256], func=AF.Silu), reads=[PS(gb)], writes=[("sg", sb2)])
                    S.op("dve", lambda e: e.tensor_tensor(out=hblk[hb3][:], in0=sg[sb2][:], in1=bank[gb][:, 256:512], op=ALU.mult),
                         reads=[("sg", sb2), PS(gb)], writes=[("hblk", hb3)])

                def P3(n):
                    hb3, tb2 = n % 3, 4 + n % 2
                    pv2 = bank_bf(tb2).rearrange("p (k t) -> p k t", k=8)
                    for c in range(2):
                        S.op("pe", lambda e, c=c: e.transpose(out=pv2[:, c, :], in_=hblk[hb3][:, c * 128:(c + 1) * 128], identity=ident_b[:]),
                             reads=[("hblk", hb3)], writes=[PS(tb2)], inc=(c == 1))
                    S.op("dve", lambda e: e.tensor_copy(out=hT2[hb3][:], in_=pv2[:, 0:2, :]), reads=[PS(tb2)], writes=[("hT2", hb3)])

                def P4(n):
                    hb3, yb2 = n % 3, n % 2
                    wi = (n // CAPB) % NW
                    row = n * 128
                    for half in range(2):
                        yb = 6 + half
                        for c in range(2):
                            S.op("pe", lambda e, c=c, half=half, yb=yb: e.matmul(bank[yb][:], lhsT=hT2[hb3][:, c, :], rhs=wd[wi][:, c, half * 512:(half + 1) * 512],
                                                                                start=(c == 0), stop=(c == 1)),
                                 reads=[("hT2", hb3), ("wd", wi)], writes=[PS(yb)], inc=(c == 1))
                        if half == 0:
                            S.op("act", lambda e, yb=yb: e.activation(out=yblk[yb2][:, 0:512], in_=bank[yb][:], func=AF.Copy), reads=[PS(yb)], writes=[("yblk", yb2, 0)])
                        else:
                            S.op("dve", lambda e, yb=yb: e.tensor_copy(out=yblk[yb2][:, 512:1024], in_=bank[yb][:]), reads=[PS(yb)], writes=[("yblk", yb2, 1)])
                    S.dma("sp", "yblk%d" % yb2, lambda e: e.dma_start(out=ys_d[row:row + 128, :], in_=yblk[yb2][:]),
                          reads=[("yblk", yb2, 0), ("yblk", yb2, 1)], writes=[("ys_dw", yb2)])
                    if n % CAPB == CAPB - 1 and n // CAPB + NW < N_EXP:
                        load_w(n // CAPB + NW)

                for step in range(-4, NBLK + 3):
                    for stage, skew in ((P0, -4), (P1, 0), (P2, 1), (P3, 2), (P4, 3)):
                        n = step - skew
                        if 0 <= n < NBLK:
                            stage(n)
                S.barrier()
            with contextlib.ExitStack() as m3:
                y1t = [sbuf(m3, "y1t%d" % i, [128, D], BF16) for i in range(3)]
                y2t = [sbuf(m3, "y2t%d" % i, [128, D], BF16) for i in range(3)]
                fxt = [sbuf(m3, "fxt%d" % i, [128, D], F32) for i in range(3)]
                acc = [sbuf(m3, "facc%d" % i, [128, D], F32) for i in range(2)]
                ot = [sbuf(m3, "fot%d" % i, [128, D], F32) for i in range(2)]
                junkf = sbuf(m3, "junkf", [128, D], BF16)
                stf = sbuf(m3, "stf", [128, 3, 4], F32)
                for i in range(3):
                    S.op("pool", lambda e, i=i: e.memset(y1t[i][:], 0.0), writes=[("y1t", i)])
                    S.op("pool", lambda e, i=i: e.memset(y2t[i][:], 0.0), writes=[("y2t", i)])
                def m3_load(i):
                    b = i % 3
                    S.dma("pool", "y1t%d" % b, lambda e, b=b, i=i: e.indirect_dma_start(
                        out=y1t[b][:], out_offset=None, in_=ys_d, in_offset=bass.IndirectOffsetOnAxis(ap=d1i[:, i:i + 1], axis=0)),
                          reads=["ys_d"], writes=[("y1t", b)])
                    S.dma("pool", "y2t%d" % b, lambda e, b=b, i=i: e.indirect_dma_start(
                        out=y2t[b][:], out_offset=None, in_=ys_d, in_offset=bass.IndirectOffsetOnAxis(ap=d2i[:, i:i + 1], axis=0)),
                          reads=["ys_d"], writes=[("y2t", b)])
                    S.dma("sp", "fxt%d" % b, lambda e, b=b, i=i: e.dma_start(out=fxt[b][:], in_=x1_d[i * 128:(i + 1) * 128, :]), writes=[("fxt", b)])

                def m3_comp(i):
                    b = i % 3
                    c = i % 2
                    S.op("dve", lambda e, b=b, c=c, i=i: e.scalar_tensor_tensor(out=acc[c][:], in0=y1t[b][:], scalar=gate1[:, i:i + 1], in1=fxt[b][:], op0=ALU.mult, op1=ALU.add),
                         reads=[("y1t", b), ("fxt", b)], writes=[("facc", c)])
                    S.op("dve", lambda e, b=b, c=c, i=i: e.scalar_tensor_tensor(out=acc[c][:], in0=y2t[b][:], scalar=gate2[:, i:i + 1], in1=acc[c][:], op0=ALU.mult, op1=ALU.add),
                         reads=[("y2t", b), ("facc", c)], writes=[("facc", c)])
                    S.op("act", lambda e, b=b, c=c: e.activation(out=junkf[:], in_=acc[c][:], func=AF.Square, accum_out=stf[:, c, 0:1]), reads=[("facc", c)], writes=[("fss", c)])
                    rstd_from_ss(stf[:, c, 0:1], D, stf[:, c, 1:2], stf[:, c, 2:3], ("fss", c), "frstd%d" % c)
                    S.op("dve", lambda e, b=b, c=c: e.scalar_tensor_tensor(out=ot[c][:], in0=acc[c][:], scalar=stf[:, c, 2:3], in1=g_fin_t[:], op0=ALU.mult, op1=ALU.mult),
                         reads=[("facc", c), "frstd%d" % c, "g_fin_t"], writes=[("fot", c)])
                    S.dma("sp", "fot%d" % c, lambda e, b=b, c=c, i=i: e.dma_start(out=out[i * 128:(i + 1) * 128, :], in_=ot[c][:]), reads=[("fot", c)])

                PF = 2
                for i in range(PF):
                    m3_load(i)
                for i in range(NTT):
                    if i + PF < NTT:
                        m3_load(i + PF)
                    m3_comp(i)
                S.barrier()

        if stop_after is not None:
            with contextlib.ExitStack() as pz:
                z = sbuf(pz, "z", [128, D], F32)
                S.op("pool", lambda e: e.memset(z[:], 0.0), writes=["z"])
                S.dma("sp", "zout", lambda e: e.dma_start(out=out[0:128, :], in_=z[:]), reads=["z"])
                S.barrier()
        S.barrier()
        S.emit()
    return nc


def _prep_inputs(inputs):
    f = lambda a: np.ascontiguousarray(np.asarray(a, dtype=np.float32))
    rope_cs, oh = _consts()
    shared = {
        "w_in": f(inputs["w_in"][0]),
        "rel_bias": f(inputs["rel_bias"]),
        "w_q_up": f(inputs["w_q_up"][0]),
        "w_kv_up": f(inputs["w_kv_up"][0]),
        "w_out": f(inputs["w_out"][0]),
        "w_rg": f(inputs["w_router_group"][0]),
        "w_re": f(inputs["w_router_expert"][0]),
        "w_gate": f(inputs["w_gate"][0]),
        "w_up": f(inputs["w_up"][0]),
        "w_down": f(inputs["w_down"][0]),
        "g_attn": f(inputs["g_attn_norm"][0]).reshape(1, D),
        "g_q": f(inputs["g_q_latent"][0]).reshape(1, 256),
        "g_kv": f(inputs["g_kv_latent"][0]).reshape(1, 128),
        "g_out": np.concatenate([f(inputs["g_out_a"][0]), f(inputs["g_out_b"][0])]).reshape(1, D),
        "g_ffn": f(inputs["g_ffn_norm"][0]).reshape(1, D),
        "g_fin": f(inputs["g_final"]).reshape(1, D),
        "b_r": np.concatenate([f(inputs["b_router_group"][0]), f(inputs["b_router_expert"][0])]).reshape(1, 36),
        "rope_cs": rope_cs,
        "oh_bias": oh,
    }
    xs = f(inputs["x"]).reshape(N_CORES, NTOK, D)
    return [dict(shared, x=xs[c]) for c in range(N_CORES)]


def kernel(**inputs):
    in_maps = _prep_inputs(inputs)
    nc = build_nc()
    res = run_bass_kernel_spmd(nc, in_maps, core_ids=list(range(N_CORES)))
    outs = [np.asarray(r["out"], dtype=np.float32).reshape(NSEQ, SEQ, D) for r in res.results]
    return np.concatenate(outs, axis=0)
```
